# Optimizing a Trainium2 kernel written in Bass

```python
import math
import numpy as np
import jax
import jax.numpy as jnp
from jax import lax

D_MODEL = 1024
BATCH = 2
SEQ = 8192
DEPTH = 1

GRID_W = 64
CTX_LEN = 256

RWKV_HEADS = 8
RWKV_HEAD_DIM = 64
RWKV_WIDTH = RWKV_HEADS * RWKV_HEAD_DIM
DECAY_LORA = 64
ICLR_LORA = 64
GATE_LORA = 128
GN_EPS = 64e-5
RWKV_SPLIT = [RWKV_WIDTH, RWKV_WIDTH, RWKV_WIDTH, DECAY_LORA, ICLR_LORA, GATE_LORA]
RWKV_COLS = sum(RWKV_SPLIT)

MLA_HEADS = 4
QK_NOPE_DIM = 128
QK_ROPE_DIM = 64
QK_HEAD_DIM = QK_NOPE_DIM + QK_ROPE_DIM
V_HEAD_DIM = 128
Q_LORA_RANK = 256
KV_LORA_RANK = 128
MLA_WIDTH = MLA_HEADS * V_HEAD_DIM
MLA_SPLIT = [Q_LORA_RANK, KV_LORA_RANK, QK_ROPE_DIM]
MLA_COLS = sum(MLA_SPLIT)
ROPE_THETA = 10000.0
ROPE_FREQS = QK_ROPE_DIM // 4
Q_BLOCK = 128

IN_COLS = RWKV_COLS + MLA_COLS
MIX_WIDTH = RWKV_WIDTH + MLA_WIDTH

N_EXPERTS = 32
TOP_K = 4
D_EXPERT = D_MODEL
SWIGLU_ALPHA = 1.702
SWIGLU_LIMIT = 7.0
EXPERT_BLOCK = 128

RMS_EPS = 1e-6
N_MOD = 6

kernel_name = "hybrid_rwkv7_mla_moe_dit_block"


def rmsnorm(x, g):
    xf = x.astype(jnp.float32)
    y = xf * lax.rsqrt(jnp.mean(xf * xf, axis=-1, keepdims=True) + RMS_EPS)
    return (y * g.astype(jnp.float32)).astype(x.dtype)


def adaln(cvec, w_mod, b_mod, n):
    m = jax.nn.silu(cvec) @ w_mod[:, : n * D_MODEL] + b_mod[: n * D_MODEL]
    return jnp.split(m, n, axis=-1)


def modulate(h, shift, scale):
    return h * (1.0 + scale) + shift


def split_cols(p, sizes):
    return jnp.split(p, np.cumsum(sizes)[:-1].tolist(), axis=-1)


def centred_shift(p, mu):
    pad = jnp.pad(p, ((0, 0), (1, 1), (0, 0)))
    return p + mu * (0.5 * (pad[:, :-2] + pad[:, 2:]) - p)


def to_heads(t):
    return t.reshape(t.shape[:-1] + (RWKV_HEADS, RWKV_HEAD_DIM))


def rwkv_prepare(p, mu, w0, w2, a0, a2, k_k, k_a):
    r, k, v, xw, xa, xg = split_cols(centred_shift(p, mu), RWKV_SPLIT)
    wl = (w0[:, None, None, :] + jnp.einsum("btl,dlc->dbtc", jnp.tanh(xw), w2)).astype(jnp.float32)
    decay = jnp.exp(-jnp.exp(-jax.nn.softplus(-wl) - 0.5))
    a = jax.nn.sigmoid((a0[:, None, None, :] + jnp.einsum("btl,dlc->dbtc", xa, a2)).astype(jnp.float32))
    kk = to_heads((k * k_k).astype(jnp.float32))
    kk = kk * lax.rsqrt(jnp.maximum(jnp.sum(kk * kk, axis=-1, keepdims=True), 1e-24))
    k_dir = k.astype(jnp.float32)[None] * (1.0 + (a - 1.0) * k_a.astype(jnp.float32))
    return {"r": to_heads(r.astype(jnp.float32)), "k": to_heads(k_dir), "v": to_heads(v.astype(jnp.float32)),
            "kk": kk, "a": to_heads(a), "decay": to_heads(decay), "xg": xg}


def both_dirs(t):
    return jnp.broadcast_to(t[None], (2,) + t.shape)


def time_major(t):
    t = jnp.stack([t[0], jnp.flip(t[1], axis=1)])
    return jnp.moveaxis(t, 2, 0)


def from_time_major(o):
    o = jnp.moveaxis(o, 0, 2)
    return o[0] + jnp.flip(o[1], axis=1)


def scan_operands(prep):
    return (time_major(prep["decay"]), time_major(prep["k"]), time_major(both_dirs(prep["v"])),
            time_major(both_dirs(prep["kk"])), time_major(prep["a"]))


def wkv_update(S, w_t, k_t, v_t, kk_t, a_t):
    sa = jnp.einsum("dbhvk,dbhk->dbhv", S, kk_t)
    return S * w_t[..., None, :] - sa[..., :, None] * (kk_t * a_t)[..., None, :] + v_t[..., :, None] * k_t[..., None, :]


def wkv_final_state(S0, ops):
    def step(S, inp):
        return wkv_update(S, *inp), None
    S, _ = lax.scan(step, S0, ops)
    return S


def wkv_outputs(S0, r, ops):
    def step(S, inp):
        r_t, rest = inp
        S = wkv_update(S, *rest)
        return S, jnp.einsum("dbhvk,dbhk->dbhv", S, r_t)
    S, out = lax.scan(step, S0, (r, ops))
    return S, from_time_major(out)


def rwkv_readout(o, prep, r_k, g2, ln_w, ln_b):
    B, T = o.shape[:2]
    mean = jnp.mean(o, axis=-1, keepdims=True)
    var = jnp.mean(jnp.square(o - mean), axis=-1, keepdims=True)
    y = ((o - mean) * lax.rsqrt(var + GN_EPS)).reshape(B, T, RWKV_WIDTH) * ln_w + ln_b
    bonus = jnp.sum(prep["r"][None] * prep["k"] * r_k, axis=(0, -1))[..., None] * prep["v"]
    g = jax.nn.sigmoid(prep["xg"]) @ g2
    return (y + bonus.reshape(B, T, RWKV_WIDTH)) * g


def rwkv_mixer(p_lat, p_ctx, mu, w0, w2, a0, a2, k_k, k_a, r_k, g2, ln_w, ln_b, ctx_out):
    lat = rwkv_prepare(p_lat, mu, w0, w2, a0, a2, k_k, k_a)
    ctx = rwkv_prepare(p_ctx, mu, w0, w2, a0, a2, k_k, k_a)
    s0 = jnp.zeros((2, p_lat.shape[0], RWKV_HEADS, RWKV_HEAD_DIM, RWKV_HEAD_DIM), jnp.float32)
    y_ctx = None
    if ctx_out:
        s_ctx, o_ctx = wkv_outputs(s0, time_major(both_dirs(ctx["r"])), scan_operands(ctx))
        y_ctx = rwkv_readout(o_ctx, ctx, r_k, g2, ln_w, ln_b).astype(p_ctx.dtype)
    else:
        s_ctx = wkv_final_state(s0, scan_operands(ctx))
    _, o_lat = wkv_outputs(s_ctx, time_major(both_dirs(lat["r"])), scan_operands(lat))
    y_lat = rwkv_readout(o_lat, lat, r_k, g2, ln_w, ln_b).astype(p_lat.dtype)
    return y_lat, y_ctx


def rope_angles(n_tok):
    n_rows = n_tok // GRID_W
    row = jnp.repeat(jnp.arange(n_rows, dtype=jnp.float32), GRID_W)
    col = jnp.tile(jnp.arange(GRID_W, dtype=jnp.float32), n_rows)
    freqs = ROPE_THETA ** (-jnp.arange(ROPE_FREQS, dtype=jnp.float32) / ROPE_FREQS)
    return jnp.stack([row[:, None] * freqs, col[:, None] * freqs], axis=1)


def axial_rope(x, ang):
    if ang is None:
        return x
    shape = x.shape
    xa = x.astype(jnp.float32).reshape(shape[:-1] + (2, 2, ROPE_FREQS))
    x1, x2 = xa[..., 0, :], xa[..., 1, :]
    bshape = (shape[1],) + (1,) * (x.ndim - 3) + (2, ROPE_FREQS)
    cos, sin = jnp.cos(ang).reshape(bshape), jnp.sin(ang).reshape(bshape)
    out = jnp.stack([x1 * cos - x2 * sin, x2 * cos + x1 * sin], axis=-2)
    return out.reshape(shape).astype(x.dtype)


def mla_keys_values(p, kv_norm, w_ukv, ang):
    B, T, _ = p.shape
    _, c_kv, k_rope = split_cols(p, MLA_SPLIT)
    kv = (rmsnorm(c_kv, kv_norm) @ w_ukv).reshape(B, T, MLA_HEADS, QK_NOPE_DIM + V_HEAD_DIM)
    k_rope = jnp.broadcast_to(axial_rope(k_rope, ang)[:, :, None, :], (B, T, MLA_HEADS, QK_ROPE_DIM))
    return jnp.concatenate([kv[..., :QK_NOPE_DIM], k_rope], axis=-1), kv[..., QK_NOPE_DIM:]


def mla_queries(p, q_norm, w_uq, ang):
    B, T, _ = p.shape
    q = (rmsnorm(p[..., :Q_LORA_RANK], q_norm) @ w_uq).reshape(B, T, MLA_HEADS, QK_HEAD_DIM)
    return jnp.concatenate([q[..., :QK_NOPE_DIM], axial_rope(q[..., QK_NOPE_DIM:], ang)], axis=-1)


def attend(q, k, v):
    s = jnp.einsum("bqhd,bkhd->bhqk", q, k, preferred_element_type=jnp.float32) / math.sqrt(QK_HEAD_DIM)
    p = jax.nn.softmax(s, axis=-1).astype(v.dtype)
    return jnp.einsum("bhqk,bkhd->bqhd", p, v)


def attend_blocked(q, k, v):
    B, T = q.shape[:2]
    qb = jnp.moveaxis(q.reshape((B, T // Q_BLOCK, Q_BLOCK) + q.shape[2:]), 1, 0)
    o = lax.map(lambda blk: attend(blk, k, v), qb)
    return jnp.moveaxis(o, 0, 1).reshape(B, T, MLA_WIDTH)


def mla_mixer(p_lat, p_ctx, q_norm, w_uq, kv_norm, w_ukv, ang, ctx_out):
    k_ctx, v_ctx = mla_keys_values(p_ctx, kv_norm, w_ukv, None)
    k_lat, v_lat = mla_keys_values(p_lat, kv_norm, w_ukv, ang)
    q_lat = mla_queries(p_lat, q_norm, w_uq, ang)
    y_lat = attend_blocked(q_lat, jnp.concatenate([k_ctx, k_lat], axis=1), jnp.concatenate([v_ctx, v_lat], axis=1))
    y_ctx = None
    if ctx_out:
        B, L, _ = p_ctx.shape
        y_ctx = attend(mla_queries(p_ctx, q_norm, w_uq, None), k_ctx, v_ctx).reshape(B, L, MLA_WIDTH)
    return y_lat, y_ctx


def moe_ffn(h, router_w, router_b, w1, b1, w2, b2):
    n_tok, d = h.shape
    n_slot = n_tok * TOP_K
    logits = (h @ router_w + router_b).astype(jnp.float32)
    top_logits, top_e = lax.top_k(logits, TOP_K)
    gates = jax.nn.softmax(top_logits, axis=-1).astype(h.dtype)
    flat_e = top_e.reshape(-1)
    order = jnp.argsort(flat_e)
    sorted_e = flat_e[order]
    counts = jnp.bincount(flat_e, length=N_EXPERTS)
    padded = (counts + EXPERT_BLOCK - 1) // EXPERT_BLOCK * EXPERT_BLOCK
    pend = jnp.cumsum(padded)
    rank = jnp.arange(n_slot) - (jnp.cumsum(counts) - counts)[sorted_e]
    dest_sorted = ((pend - padded)[sorted_e] + rank).astype(jnp.int32)
    slot_dest = jnp.zeros((n_slot,), jnp.int32).at[order].set(dest_sorted)
    n_blocks = -(-n_slot // EXPERT_BLOCK) + N_EXPERTS
    slot_tok = jnp.full((n_blocks * EXPERT_BLOCK,), n_tok, jnp.int32).at[slot_dest].set(
        jnp.arange(n_slot, dtype=jnp.int32) // TOP_K)
    block_e = jnp.minimum(jnp.searchsorted(pend, jnp.arange(n_blocks) * EXPERT_BLOCK, side="right"), N_EXPERTS - 1)
    h_pad = jnp.concatenate([h, jnp.zeros((1, d), h.dtype)], axis=0)
    xb = h_pad[slot_tok].reshape(n_blocks, EXPERT_BLOCK, d)

    def expert_block(args):
        xblk, e = args
        u = xblk @ w1[e] + b1[e]
        glu = jnp.minimum(u[:, 0::2], SWIGLU_LIMIT)
        lin = jnp.clip(u[:, 1::2], -SWIGLU_LIMIT, SWIGLU_LIMIT)
        return (glu * jax.nn.sigmoid(SWIGLU_ALPHA * glu) * (lin + 1.0)) @ w2[e] + b2[e]

    yb = lax.map(expert_block, (xb, block_e)).reshape(n_blocks * EXPERT_BLOCK, d)
    y = yb[slot_dest].reshape(n_tok, TOP_K, d)
    return jnp.einsum("nk,nkd->nd", gates, y)


def setup_inputs(seed: int = 0) -> dict:
    key = jax.random.key(seed)
    ks = iter(list(jax.random.split(key, 40)))

    def nrm(shape, s):
        return s * jax.random.normal(next(ks), shape, jnp.float32)

    D, C, L = D_MODEL, RWKV_WIDTH, DEPTH
    return {
        "x": nrm((BATCH, SEQ, D), 1.0),
        "c": nrm((BATCH, D), 1.0),
        "ctx": nrm((BATCH, CTX_LEN, D), 1.0),
        "c_ctx": nrm((D,), 1.0),
        "mod_w": nrm((L, D, N_MOD * D), 0.5 * D ** -0.5),
        "mod_b": nrm((L, N_MOD * D), 0.02),
        "norm1_g": 1.0 + nrm((L, D), 0.02),
        "w_in": nrm((L, D, IN_COLS), D ** -0.5),
        "rwkv_mu": jax.random.uniform(next(ks), (L, RWKV_COLS), jnp.float32),
        "rwkv_w0": jax.random.uniform(next(ks), (L, 2, C), jnp.float32, -6.5, -1.5),
        "rwkv_w2": nrm((L, 2, DECAY_LORA, C), 0.1),
        "rwkv_a0": nrm((L, 2, C), 0.1),
        "rwkv_a2": nrm((L, 2, ICLR_LORA, C), 0.1),
        "rwkv_k_k": 0.85 + nrm((L, C), 0.02),
        "rwkv_k_a": 1.0 + nrm((L, C), 0.02),
        "rwkv_r_k": nrm((L, RWKV_HEADS, RWKV_HEAD_DIM), 0.1),
        "rwkv_g2": nrm((L, GATE_LORA, C), GATE_LORA ** -0.5),
        "rwkv_ln_w": 1.0 + nrm((L, C), 0.02),
        "rwkv_ln_b": nrm((L, C), 0.02),
        "mla_q_norm": 1.0 + nrm((L, Q_LORA_RANK), 0.02),
        "mla_w_uq": nrm((L, Q_LORA_RANK, MLA_HEADS * QK_HEAD_DIM), Q_LORA_RANK ** -0.5),
        "mla_kv_norm": 1.0 + nrm((L, KV_LORA_RANK), 0.02),
        "mla_w_ukv": nrm((L, KV_LORA_RANK, MLA_HEADS * (QK_NOPE_DIM + V_HEAD_DIM)), KV_LORA_RANK ** -0.5),
        "w_out": nrm((L, MIX_WIDTH, D), MIX_WIDTH ** -0.5),
        "norm2_g": 1.0 + nrm((L, D), 0.02),
        "router_w": nrm((L, D, N_EXPERTS), D ** -0.5),
        "router_b": nrm((L, N_EXPERTS), 0.01),
        "exp_w1": nrm((L, N_EXPERTS, D, 2 * D_EXPERT), D ** -0.5),
        "exp_b1": nrm((L, N_EXPERTS, 2 * D_EXPERT), 0.02),
        "exp_w2": nrm((L, N_EXPERTS, D_EXPERT, D), D_EXPERT ** -0.5),
        "exp_b2": nrm((L, N_EXPERTS, D), 0.02),
        "final_norm_g": 1.0 + nrm((D,), 0.02),
    }


def reference(x, c, ctx, c_ctx, mod_w, mod_b, norm1_g, w_in, rwkv_mu, rwkv_w0, rwkv_w2, rwkv_a0, rwkv_a2,
              rwkv_k_k, rwkv_k_a, rwkv_r_k, rwkv_g2, rwkv_ln_w, rwkv_ln_b, mla_q_norm, mla_w_uq, mla_kv_norm,
              mla_w_ukv, w_out, norm2_g, router_w, router_b, exp_w1, exp_b1, exp_w2, exp_b2, final_norm_g):
    B, T, D = x.shape
    ang = rope_angles(T)
    for l in range(DEPTH):
        ctx_out = l < DEPTH - 1
        sh1, sc1, gt1, sh2, sc2, gt2 = [m[:, None, :] for m in adaln(c, mod_w[l], mod_b[l], N_MOD)]
        mc = adaln(c_ctx, mod_w[l], mod_b[l], N_MOD if ctx_out else 2)
        p_lat = modulate(rmsnorm(x, norm1_g[l]), sh1, sc1) @ w_in[l]
        p_ctx = modulate(rmsnorm(ctx, norm1_g[l]), mc[0], mc[1]) @ w_in[l]
        y_rwkv, yc_rwkv = rwkv_mixer(p_lat[..., :RWKV_COLS], p_ctx[..., :RWKV_COLS], rwkv_mu[l], rwkv_w0[l],
                                     rwkv_w2[l], rwkv_a0[l], rwkv_a2[l], rwkv_k_k[l], rwkv_k_a[l], rwkv_r_k[l],
                                     rwkv_g2[l], rwkv_ln_w[l], rwkv_ln_b[l], ctx_out)
        y_mla, yc_mla = mla_mixer(p_lat[..., RWKV_COLS:], p_ctx[..., RWKV_COLS:], mla_q_norm[l], mla_w_uq[l],
                                  mla_kv_norm[l], mla_w_ukv[l], ang, ctx_out)
        x = x + gt1 * (jnp.concatenate([y_rwkv, y_mla], axis=-1) @ w_out[l])
        h2 = modulate(rmsnorm(x, norm2_g[l]), sh2, sc2).reshape(B * T, D)
        if ctx_out:
            ctx = ctx + mc[2] * (jnp.concatenate([yc_rwkv, yc_mla], axis=-1) @ w_out[l])
            hc2 = modulate(rmsnorm(ctx, norm2_g[l]), mc[3], mc[4]).reshape(-1, D)
            f = moe_ffn(jnp.concatenate([h2, hc2], axis=0), router_w[l], router_b[l],
                        exp_w1[l], exp_b1[l], exp_w2[l], exp_b2[l])
            ctx = ctx + mc[5] * f[B * T:].reshape(ctx.shape)
            f = f[: B * T]
        else:
            f = moe_ffn(h2, router_w[l], router_b[l], exp_w1[l], exp_b1[l], exp_w2[l], exp_b2[l])
        x = x + gt2 * f.reshape(B, T, D)
    return rmsnorm(x, final_norm_g)
```

```python
import os
import numpy as np
from contextlib import ExitStack
import concourse.bass as bass
import concourse.mybir as mybir
from concourse.bass_utils import run_bass_kernel_spmd

F32 = mybir.dt.float32
AF = mybir.ActivationFunctionType
ALU = mybir.AluOpType
AX = mybir.AxisListType

D = 1024
T = 8192
CTX = 256
TS = CTX + T
QT = 2048
NBLK_FULL = [(0, 256)] + [(256 + i * 512, 512) for i in range(16)]
WCOLS = 2304
DEBUG = os.environ.get("KDEBUG", "")


class KB:
    NRING = 12

    def __init__(self, nc, stack, same_engine_sync=True):
        self.nc = nc
        self.stack = stack
        self.E = {'pe': nc.tensor, 'dve': nc.vector, 'act': nc.scalar, 'pool': nc.gpsimd, 'sp': nc.sync}
        self.sem = {e: stack.enter_context(nc.semaphore("s_" + e)) for e in ('pe', 'dve', 'act', 'pool')}
        self.cnt = {e: 0 for e in self.sem}
        self.ring = [stack.enter_context(nc.semaphore("d%d" % i)) for i in range(self.NRING)]
        self.ndma = 0
        self.seen = {e: {} for e in self.E}
        self.res = {}
        self.pend = {e: ([], []) for e in self.E}
        self.ses = same_engine_sync
        self.nins = 0
        for s_ in list(self.sem.values()) + self.ring:
            nc.gpsimd.sem_clear(s_)
        nc.all_engine_barrier()

    def _wait(self, eng, sem, val):
        k = sem.name
        if self.seen[eng].get(k, 0) >= val:
            return
        self.E[eng].wait_ge(sem, val)
        self.seen[eng][k] = val

    def _deps(self, eng, reads, writes):
        deps = []
        for r in reads:
            st = self.res.get(r)
            if st and st[0]:
                deps.append(st[0])
        for w in writes:
            st = self.res.get(w)
            if st:
                if st[0]:
                    deps.append(st[0])
                deps.extend(st[1].values())
        own = self.sem.get(eng)
        for (sem, val) in deps:
            if own is not None and sem.name == own.name and (eng == 'pe' or not self.ses):
                continue
            self._wait(eng, sem, val)

    def _record(self, tok, reads, writes):
        for r in reads:
            st = self.res.setdefault(r, [None, {}])
            old = st[1].get(tok[0].name)
            if old is None or old[1] < tok[1]:
                st[1][tok[0].name] = tok
        for w in writes:
            self.res[w] = [tok, {}]

    def op(self, eng, fn, reads=(), writes=(), inc=True):
        self._deps(eng, reads, writes)
        ins = fn(self.E[eng])
        self.nins += 1
        pr, pw = self.pend[eng]
        pr.extend(reads)
        pw.extend(writes)
        if inc:
            self.cnt[eng] += 1
            ins.then_inc(self.sem[eng], 1)
            self._record((self.sem[eng], self.cnt[eng]), pr, pw)
            self.pend[eng] = ([], [])
        return ins

    def dma(self, q, out, in_, reads=(), writes=(), **kw):
        i = self.ndma
        self.ndma += 1
        sem = self.ring[i % self.NRING]
        val = 16 * (i // self.NRING + 1)
        if val > 16:
            self._wait(q, sem, val - 16)
        self._deps(q, reads, writes)
        ins = self.E[q].dma_start(out=out, in_=in_, **kw).then_inc(sem, 16)
        self.nins += 1
        self._record((sem, val), list(reads), list(writes))
        return ins

    def barrier(self, engines=('pe', 'dve', 'act', 'pool', 'sp')):
        for e in engines:
            for o, s in self.sem.items():
                if o != e and self.cnt[o] > 0:
                    self._wait(e, s, self.cnt[o])
            for j, s in enumerate(self.ring):
                n = (self.ndma - 1 - j) // self.NRING + 1 if self.ndma > j else 0
                if n > 0:
                    self._wait(e, s, 16 * n)


class Rot:
    def __init__(self, nc, st, name, shape, n, dtype=F32, psum=False):
        mk = nc.psum_tensor if psum else nc.sbuf_tensor
        self.t = [st.enter_context(mk("%s%d" % (name, i), shape, dtype)) for i in range(n)]
        self.name = name
        self.i = 0

    def next(self):
        j = self.i % len(self.t)
        self.i += 1
        return self.t[j], (self.name, j)


KRW_BLKS = int(os.environ.get('KRW_BLKS', '99'))
KRW_BLK0 = int(os.environ.get('KRW_BLK0', '0'))
KRW_BSTOP = int(os.environ.get('KRW_BSTOP', '99'))
KRW_NOB = int(os.environ.get('KRW_NOB', '0'))
KRW_NOCHAIN = int(os.environ.get('KRW_NOCHAIN', '0'))
KRW_MARK = int(os.environ.get('KRW_MARK', '99'))


class StopRegion(Exception):
    pass


def mark(i):
    if i >= KRW_MARK:
        raise StopRegion()


CDEC = 0.6065306597126334
GN_EPS = 64e-5
FULL_BLKS = [(0, 256, True, True)] + [(256 + i * 512, 512, i == 0, i == 15) for i in range(16)]
OWN_RBLKS = [(i * 512, 512, i == 0, i == 3) for i in range(4)]
NCH_FULL = TS // 64
NCH_OWN = QT // 64


def rwkv_stage(nc, kb, st, pT, poT, yT, C, dscr):
    idt = C["idt"]
    GTs = dscr("GTs", [2, 4, 128, NCH_FULL, 128])
    Nsc = dscr("Nsc", [2, 4, 128, NCH_FULL, 128])
    GTo = dscr("GTo", [2, 4, 128, NCH_OWN, 128])
    Nso = dscr("Nso", [2, 4, 128, NCH_OWN, 128])
    RhTo = dscr("RhTo", [2, 4, 128, NCH_OWN, 128])
    Oho = dscr("Oho", [2, 4, 128, NCH_OWN, 128])
    BONs = dscr("BONs", [4, 128, QT])
    Gsc = dscr("Gsc", [4, 128, QT])
    if os.environ.get('KRW_ALLOC_ONLY'):
        return
    with ExitStack() as ph:
        sbp = lambda name, shape: ph.enter_context(nc.sbuf_tensor(name, shape, F32))
        def ld(name, shape, src, **kw):
            t = sbp(name, shape)
            kb.dma('sp', t[:], src, writes=[name], **kw)
            return t
        mu_t = ld("mu_t", [128, 14], C["mu"].rearrange("(c p) -> p c", p=128), allow_slow_non_contiguous=True)
        om_t = sbp("om_t", [128, 14]); hm_t = sbp("hm_t", [128, 14])
        kb.op('dve', lambda e: e.tensor_scalar(out=om_t[:], in0=mu_t[:], scalar1=-1.0, scalar2=1.0, op0=ALU.mult, op1=ALU.add), reads=["mu_t"], writes=["om_t"])
        kb.op('dve', lambda e: e.tensor_scalar(out=hm_t[:], in0=mu_t[:], scalar1=0.5, scalar2=None, op0=ALU.mult), reads=["mu_t"], writes=["hm_t"])
        w0_t = ld("w0_t", [128, 2, 4], C["w0"].rearrange("d (c p) -> p d c", p=128), allow_slow_non_contiguous=True)
        a0_t = ld("a0_t", [128, 2, 4], C["a0"].rearrange("d (c p) -> p d c", p=128), allow_slow_non_contiguous=True)
        W2A2 = sbp("W2A2", [128, 2, 512])
        kb.dma('sp', W2A2[0:64], C["w2"].rearrange("d l c -> l d c"), writes=["W2A2"])
        kb.dma('sp', W2A2[64:128], C["a2"].rearrange("d l c -> l d c"), writes=["W2A2"])
        kk_t = ld("kk_t", [128, 4], C["k_k"].rearrange("(c p) -> p c", p=128), allow_slow_non_contiguous=True)
        ka_t = ld("ka_t", [128, 4], C["k_a"].rearrange("(c p) -> p c", p=128), allow_slow_non_contiguous=True)
        oka_t = sbp("oka_t", [128, 4])
        kb.op('dve', lambda e: e.tensor_scalar(out=oka_t[:], in0=ka_t[:], scalar1=-1.0, scalar2=1.0, op0=ALU.mult, op1=ALU.add), reads=["ka_t"], writes=["oka_t"])
        rk_t = ld("rk_t", [128, 4], C["r_k"].rearrange("(c p) -> p c", p=128), allow_slow_non_contiguous=True)
        lnw_t = ld("lnw_t", [128, 4], C["ln_w"].rearrange("(c p) -> p c", p=128), allow_slow_non_contiguous=True)
        lnb_t = ld("lnb_t", [128, 4], C["ln_b"].rearrange("(c p) -> p c", p=128), allow_slow_non_contiguous=True)
        g2_t = ld("g2_t", [128, 512], C["g2"])
        MASK4 = ld("MASK4", [128, 2, 512], C["mask4"].rearrange("d p n -> p d n"))
        MASKL = ld("MASKL", [128, 2, 128], C["maskl"].rearrange("d p n -> p d n"))
        BLK = ld("BLK", [128, 128], C["blk"])
        UU = ld("UU", [128, 64], C["uu"])
        SEL = ld("SEL", [128, 64], C["sel"])
        selF = ld("selF", [128, 4], C["selF"])
        selB = ld("selB", [128, 4], C["selB"])
        hal = ld("hal", [128, 2], C["hal"])
        if os.environ.get('KRW_STOP') == '1':
            kb.barrier()
            return
        P_r = Rot(nc, ph, "Pl", [128, 514], 3)
        sh_r = Rot(nc, ph, "shf", [128, 512], 4)
        RS_r = Rot(nc, ph, "RSs", [128, 512], 2)
        KS_r = Rot(nc, ph, "KSs", [128, 512], 2)
        VS_r = Rot(nc, ph, "VSs", [128, 512], 2)
        KK_r = Rot(nc, ph, "KKs", [128, 512], 2)
        X12_r = Rot(nc, ph, "X12", [128, 512], 2)
        TX_r = Rot(nc, ph, "TXs", [128, 512], 2)
        dA_r = Rot(nc, ph, "dA", [128, 512], 14)
        bs_r = Rot(nc, ph, "bsr", [128, 512], 2)
        pl_r = Rot(nc, ph, "plr", [128, 8], 4)
        ex_r = {nm: Rot(nc, ph, "ex" + nm, [128, 8, 2, 64], 2) for nm in ("A", "R", "B", "K", "Bp", "Kp")}
        exV_r = Rot(nc, ph, "exV", [128, 8, 2, 64], 2)
        for r_ in list(ex_r.values()) + [exV_r]:
            for j_, t_ in enumerate(r_.t):
                kb.op('pool', lambda e, t_=t_: e.memset(t_[:], 0.0), writes=[(r_.name, j_)])
        psA = Rot(nc, ph, "psRA", [128, 512], 3, psum=True)
        psB = Rot(nc, ph, "psRB", [128, 512], 5, psum=True)
        AT4_r = Rot(nc, ph, "AT4", [128, 4, 128], 2)
        AB_r = Rot(nc, ph, "ABk", [128, 2, 128], 4)
        XT_r = Rot(nc, ph, "XTk", [128, 128], 3)
        TM_r = Rot(nc, ph, "TMk", [128, 4, 128], 2)
        AU_r = Rot(nc, ph, "AUk", [128, 2, 128], 2)
        stG_r = Rot(nc, ph, "stG", [128, 8, 128], 1)
        stN_r = Rot(nc, ph, "stN", [128, 8, 128], 1)
        stR_r = Rot(nc, ph, "stR", [128, 8, 128], 1)
        stO_r = Rot(nc, ph, "stO", [128, 8, 128], 1)
        print('RWKV region sbuf remaining', nc.sbuf_bytes_remaining, nc.SBUF_PARTITION_SIZE_BYTES)
        cnt = {"ev": 0}

        def evac(out_ap, in_ap, reads, writes):
            cnt["ev"] += 1
            if True:
                kb.op('dve', lambda e: e.tensor_copy(out=out_ap, in_=in_ap), reads=reads, writes=writes)
            else:
                kb.op('act', lambda e: e.copy(out=out_ap, in_=in_ap), reads=reads, writes=writes)

        def load_shift(src, row0, c0, n, lb, rb, own, dst, dk, cc):
            P, pk = P_r.next()
            lo = c0 - (0 if lb else 1)
            hi = c0 + n + (0 if rb else 1)
            doff = 1 if lb else 0
            kb.dma('sp', P[:, doff:doff + (hi - lo)], src[row0:row0 + 128, lo:hi], writes=[pk])
            if lb:
                if own:
                    kb.dma('sp', P[:, 0:1], src[row0:row0 + 128, QT:QT + 1], writes=[pk], allow_slow_non_contiguous=True)
                    kb.op('dve', lambda e: e.tensor_scalar(out=P[:, 0:1], in0=P[:, 0:1], scalar1=hal[:, 0:1], scalar2=None, op0=ALU.mult), reads=[pk, "hal"], writes=[pk])
                else:
                    kb.op('pool', lambda e: e.memset(P[:, 0:1], 0.0), writes=[pk])
            if rb:
                if own:
                    kb.dma('sp', P[:, n + 1:n + 2], src[row0:row0 + 128, QT + 1:QT + 2], writes=[pk], allow_slow_non_contiguous=True)
                    kb.op('dve', lambda e: e.tensor_scalar(out=P[:, n + 1:n + 2], in0=P[:, n + 1:n + 2], scalar1=hal[:, 1:2], scalar2=None, op0=ALU.mult), reads=[pk, "hal"], writes=[pk])
                else:
                    kb.op('pool', lambda e: e.memset(P[:, n + 1:n + 2], 0.0), writes=[pk])
            t, tk = sh_r.next()
            kb.op('pool', lambda e: e.tensor_tensor(out=t[:, :n], in0=P[:, 0:n], in1=P[:, 2:n + 2], op=ALU.add), reads=[pk], writes=[tk])
            u, uk = sh_r.next()
            kb.op('dve', lambda e: e.tensor_scalar(out=u[:, :n], in0=P[:, 1:n + 1], scalar1=om_t[:, cc:cc + 1], scalar2=None, op0=ALU.mult), reads=[pk, "om_t"], writes=[uk])
            kb.op('dve', lambda e: e.scalar_tensor_tensor(out=dst[:, :n], in0=t[:, :n], scalar=hm_t[:, cc:cc + 1], in1=u[:, :n], op0=ALU.mult, op1=ALU.add),
                  reads=[tk, uk, "hm_t"], writes=[dk])

        def c3(ap, n):
            return ap.rearrange("p (c t) -> p c t", t=64)

        def exp_write(eng, dst, dk, nch, n, fn, reads):
            for hh in range(2):
                sl = slice(hh * 64, hh * 64 + 64)
                kb.op(eng, lambda e, hh=hh, sl=sl: fn(e, dst[sl, :nch, hh, :], sl), reads=reads, writes=[dk])

        def region(src, blocks, own, GTd, Nd, RhTd, Ohd, chunk0_of_block):
            for bi, (c0, n, lb, rb) in list(enumerate(blocks))[KRW_BLK0:KRW_BLK0 + KRW_BLKS]:
                nch = n // 64
                ch0 = chunk0_of_block(bi)
                X12, xk = X12_r.next()
                load_shift(src, 1536, c0, n, lb, rb, own, X12, xk, 12)
                TX, txk = TX_r.next()
                kb.op('act', lambda e: e.activation(out=TX[0:64, :n], in_=X12[0:64, :n], func=AF.Tanh), reads=[xk], writes=[txk])
                mark(1)
                if own:
                    XG, xgk = dA_r.next()
                    load_shift(src, 1664, c0, n, lb, rb, own, XG, xgk, 13)
                    SGg, sggk = TX_r.next()
                    kb.op('act', lambda e: e.activation(out=SGg[:, :n], in_=XG[:, :n], func=AF.Sigmoid), reads=[xgk], writes=[sggk])
                for hp in range(4):
                    RS, rsk = RS_r.next(); KS, ksk = KS_r.next(); VS, vsk = VS_r.next(); KK, kkk = KK_r.next()
                    load_shift(src, hp * 128, c0, n, lb, rb, own, RS, rsk, hp)
                    load_shift(src, 512 + hp * 128, c0, n, lb, rb, own, KS, ksk, 4 + hp)
                    load_shift(src, 1024 + hp * 128, c0, n, lb, rb, own, VS, vsk, 8 + hp)
                    mark(2)
                    kkr, kkrk = dA_r.next()
                    kb.op('dve', lambda e: e.tensor_scalar(out=kkr[:, :n], in0=KS[:, :n], scalar1=kk_t[:, hp:hp + 1], scalar2=None, op0=ALU.mult), reads=[ksk, "kk_t"], writes=[kkrk])
                    sq, sqk = dA_r.next()
                    kb.op('pool', lambda e: e.tensor_tensor(out=sq[:, :n], in0=kkr[:, :n], in1=kkr[:, :n], op=ALU.mult), reads=[kkrk], writes=[sqk])
                    pss, pssk = psA.next()
                    kb.op('pe', lambda e: e.matmul(pss[:, :n], lhsT=BLK[:], rhs=sq[:, :n], start=True, stop=True), reads=["BLK", sqk], writes=[pssk])
                    kb.op('dve', lambda e: e.tensor_scalar(out=sq[:, :n], in0=pss[:, :n], scalar1=1e-24, scalar2=None, op0=ALU.max), reads=[pssk], writes=[sqk])
                    kb.op('act', lambda e: e.activation(out=sq[:, :n], in_=sq[:, :n], func=AF.Sqrt), reads=[sqk], writes=[sqk])
                    kb.op('dve', lambda e: e.reciprocal(out=sq[:, :n], in_=sq[:, :n]), reads=[sqk], writes=[sqk])
                    kb.op('pool', lambda e: e.tensor_tensor(out=KK[:, :n], in0=kkr[:, :n], in1=sq[:, :n], op=ALU.mult), reads=[kkrk, sqk], writes=[kkk])
                    mark(3)
                    Vd, vdk = exV_r.next()
                    exp_write('pool', Vd, vdk, nch, n, lambda e, o, sl: e.tensor_copy(out=o, in_=c3(VS[sl, :n], n)), [vsk])
                    mark(4)
                    if own:
                        bsum, bsk = bs_r.next()
                    for d in range(2):
                        psw, pswk = psA.next()
                        kb.op('pe', lambda e: e.matmul(psw[:, :n], lhsT=W2A2[0:64, d, hp * 128:(hp + 1) * 128], rhs=TX[0:64, :n], start=True, stop=True),
                              reads=["W2A2", txk], writes=[pswk])
                        SGM, sgk = dA_r.next()
                        kb.op('act', lambda e: e.activation(out=SGM[:, :n], in_=psw[:, :n], func=AF.Sigmoid, bias=w0_t[:, d, hp:hp + 1], scale=1.0), reads=[pswk, "w0_t"], writes=[sgk])
                        psa, psak = psA.next()
                        kb.op('pe', lambda e: e.matmul(psa[:, :n], lhsT=W2A2[64:128, d, hp * 128:(hp + 1) * 128], rhs=X12[64:128, :n], start=True, stop=True),
                              reads=["W2A2", xk], writes=[psak])
                        AA, aak = dA_r.next()
                        kb.op('act', lambda e: e.activation(out=AA[:, :n], in_=psa[:, :n], func=AF.Sigmoid, bias=a0_t[:, d, hp:hp + 1], scale=1.0), reads=[psak, "a0_t"], writes=[aak])
                        mark(5)
                        CIN, cik = dA_r.next()
                        T1, t1k = sh_r.next()
                        src_, srck_ = SGM, sgk
                        for si, s_ in enumerate((1, 2, 4, 8, 16, 32)):
                            dst_, dstk_ = (T1, t1k) if si % 2 == 0 else (CIN, cik)
                            kb.op('pool', lambda e, s_=s_, src_=src_, dst_=dst_: e.tensor_tensor(out=c3(dst_[:, :n], n)[:, :, s_:], in0=c3(src_[:, :n], n)[:, :, s_:],
                                                                                                in1=c3(src_[:, :n], n)[:, :, :64 - s_], op=ALU.add),
                                  reads=[srck_], writes=[dstk_])
                            kb.op('act', lambda e, s_=s_, src_=src_, dst_=dst_: e.copy(out=c3(dst_[:, :n], n)[:, :, :s_], in_=c3(src_[:, :n], n)[:, :, :s_]),
                                  reads=[srck_], writes=[dstk_])
                            src_, srck_ = dst_, dstk_
                        mark(6)
                        CEX, cek = dA_r.next()
                        kb.op('pool', lambda e: e.tensor_tensor(out=CEX[:, :n], in0=CIN[:, :n], in1=SGM[:, :n], op=ALU.subtract), reads=[cik, sgk], writes=[cek])
                        totb = c3(CIN[:, :n], n)[:, :, 63:64].to_broadcast([128, nch, 64])
                        Dm, dmk = dA_r.next()
                        kb.op('dve', lambda e: e.tensor_tensor(out=c3(Dm[:, :n], n), in0=totb, in1=c3(CIN[:, :n], n), op=ALU.subtract), reads=[cik], writes=[dmk])
                        DX, dxk = dA_r.next()
                        kb.op('dve', lambda e: e.tensor_tensor(out=c3(DX[:, :n], n), in0=totb, in1=c3(CEX[:, :n], n), op=ALU.subtract), reads=[cik, cek], writes=[dxk])
                        srcs = {0: ((CIN, cik, -CDEC), (CEX, cek, -CDEC), (CIN, cik, CDEC), (Dm, dmk, -CDEC)),
                                1: ((DX, dxk, -CDEC), (Dm, dmk, -CDEC), (DX, dxk, CDEC), (CEX, cek, -CDEC))}[d]
                        E4 = []
                        for (s_, sk_, sc_) in srcs:
                            o_, ok_ = dA_r.next()
                            kb.op('act', lambda e, s_=s_, o_=o_, sc_=sc_: e.activation(out=o_[:, :n], in_=s_[:, :n], func=AF.Exp, scale=sc_), reads=[sk_], writes=[ok_])
                            E4.append((o_, ok_))
                        (PIN, pik), (PEX, pek), (INV, ink), (EEND, eek) = E4
                        PL, plk = pl_r.next()
                        kb.op('act', lambda e: e.activation(out=PL[:, :nch], in_=CIN[:, 63:n:64], func=AF.Exp, scale=-CDEC), reads=[cik], writes=[plk])
                        mark(7)
                        KD, kdk = dA_r.next()
                        kb.op('dve', lambda e: e.tensor_scalar(out=KD[:, :n], in0=AA[:, :n], scalar1=ka_t[:, hp:hp + 1], scalar2=oka_t[:, hp:hp + 1], op0=ALU.mult, op1=ALU.add),
                              reads=[aak, "ka_t", "oka_t"], writes=[kdk])
                        kb.op('pool', lambda e: e.tensor_tensor(out=KD[:, :n], in0=KD[:, :n], in1=KS[:, :n], op=ALU.mult), reads=[kdk, ksk], writes=[kdk])
                        Bv, bvk = dA_r.next()
                        kb.op('pool', lambda e: e.tensor_tensor(out=Bv[:, :n], in0=KK[:, :n], in1=AA[:, :n], op=ALU.mult), reads=[kkk, aak], writes=[bvk])
                        if own:
                            if d == 0:
                                kb.op('pool', lambda e: e.tensor_tensor(out=bsum[:, :n], in0=RS[:, :n], in1=KD[:, :n], op=ALU.mult), reads=[rsk, kdk], writes=[bsk])
                            else:
                                t_, tk_ = sh_r.next()
                                kb.op('pool', lambda e: e.tensor_tensor(out=t_[:, :n], in0=RS[:, :n], in1=KD[:, :n], op=ALU.mult), reads=[rsk, kdk], writes=[tk_])
                                kb.op('pool', lambda e: e.tensor_tensor(out=bsum[:, :n], in0=bsum[:, :n], in1=t_[:, :n], op=ALU.add), reads=[bsk, tk_], writes=[bsk])
                        mark(8)
                        ex = {nm: ex_r[nm].next() for nm in ex_r}
                        exp_write('dve', ex["A"][0], ex["A"][1], nch, n,
                                  lambda e, o, sl: e.scalar_tensor_tensor(out=o, in0=c3(KK[sl, :n], n), scalar=-1.0, in1=c3(PEX[sl, :n], n), op0=ALU.mult, op1=ALU.mult), [kkk, pek])
                        exp_write('pool', ex["R"][0], ex["R"][1], nch, n, lambda e, o, sl: e.tensor_tensor(out=o, in0=c3(RS[sl, :n], n), in1=c3(PIN[sl, :n], n), op=ALU.mult), [rsk, pik])
                        exp_write('dve', ex["B"][0], ex["B"][1], nch, n, lambda e, o, sl: e.tensor_tensor(out=o, in0=c3(Bv[sl, :n], n), in1=c3(INV[sl, :n], n), op=ALU.mult), [bvk, ink])
                        exp_write('pool', ex["K"][0], ex["K"][1], nch, n, lambda e, o, sl: e.tensor_tensor(out=o, in0=c3(KD[sl, :n], n), in1=c3(INV[sl, :n], n), op=ALU.mult), [kdk, ink])
                        exp_write('dve', ex["Bp"][0], ex["Bp"][1], nch, n, lambda e, o, sl: e.tensor_tensor(out=o, in0=c3(Bv[sl, :n], n), in1=c3(EEND[sl, :n], n), op=ALU.mult), [bvk, eek])
                        exp_write('pool', ex["Kp"][0], ex["Kp"][1], nch, n, lambda e, o, sl: e.tensor_tensor(out=o, in0=c3(KD[sl, :n], n), in1=c3(EEND[sl, :n], n), op=ALU.mult), [kdk, eek])
                        mark(9)
                        stG, stGk = stG_r.next(); stN, stNk = stN_r.next()
                        if own:
                            stR, stRk = stR_r.next(); stO, stOk = stO_r.next()
                        for ci in range(0 if KRW_NOB else nch):
                            f2 = lambda t_: t_[:, ci].rearrange("p a b -> p (a b)")
                            Ad, Rd, Bd, Kd, Bpd, Kpd, Vdd = f2(ex["A"][0]), f2(ex["R"][0]), f2(ex["B"][0]), f2(ex["K"][0]), f2(ex["Bp"][0]), f2(ex["Kp"][0]), f2(Vd)
                            exk = [ex[nm][1] for nm in ("A", "R", "B", "K")]
                            ps1, ps1k = psB.next()
                            for qi, (l_, r_) in enumerate(((Bd, Ad), (Kd, Ad), (Bd, Rd), (Kd, Rd))):
                                kb.op('pe', lambda e, qi=qi, l_=l_, r_=r_: e.matmul(ps1[:, qi * 128:(qi + 1) * 128], lhsT=l_, rhs=r_, start=True, stop=True),
                                      reads=exk, writes=[ps1k], inc=(qi == 3))
                            AT4, atk = AT4_r.next()
                            kb.op('dve', lambda e: e.tensor_tensor(out=AT4[:].rearrange("p a b -> p (a b)"), in0=ps1[:], in1=MASK4[:, d, :], op=ALU.mult), reads=[ps1k, "MASK4"], writes=[atk])
                            if KRW_BSTOP <= 1:
                                continue
                            ps2, ps2k = psB.next()
                            kb.op('pe', lambda e: e.matmul(ps2[:, 0:128], lhsT=Ad, rhs=Bd, start=True, stop=True), reads=exk, writes=[ps2k])
                            AB, abk = AB_r.next()
                            kb.op('dve', lambda e: e.tensor_tensor(out=AB[:, 0, :], in0=ps2[:, 0:128], in1=MASKL[:, d, :], op=ALU.mult), reads=[ps2k, "MASKL"], writes=[abk])
                            kb.op('pool', lambda e: e.tensor_copy(out=AB[:, 1, :], in_=AT4[:, 0, :]), reads=[atk], writes=[abk])
                            XT, xtk = XT_r.next()
                            kb.op('pool', lambda e: e.tensor_tensor(out=XT[:], in0=AT4[:, 0, :], in1=idt[:], op=ALU.add), reads=[atk, "idt"], writes=[xtk])
                            if KRW_BSTOP <= 2:
                                continue
                            for it in range(5):
                                psk_, pskk_ = psB.next()
                                kb.op('pe', lambda e: e.matmul(psk_[:, 0:128], lhsT=AB[:, 1, :], rhs=AB[:, 0, :], start=True, stop=True), reads=[abk], writes=[pskk_], inc=False)
                                kb.op('pe', lambda e: e.matmul(psk_[:, 128:256], lhsT=AB[:, 0, :], rhs=AB[:, 1, :], start=True, stop=True), reads=[abk], writes=[pskk_])
                                AB2, ab2k = AB_r.next()
                                evac(AB2[:].rearrange("p a b -> p (a b)"), psk_[:, 0:256], [pskk_], [ab2k])
                                psx, psxk = psB.next()
                                kb.op('pe', lambda e: e.matmul(psx[:, 0:128], lhsT=AB2[:, 0, :], rhs=XT[:], start=True, stop=True), reads=[ab2k, xtk], writes=[psxk])
                                XT2, xt2k = XT_r.next()
                                kb.op('dve', lambda e: e.tensor_tensor(out=XT2[:], in0=psx[:, 0:128], in1=XT[:], op=ALU.add), reads=[psxk, xtk], writes=[xt2k])
                                AB, abk, XT, xtk = AB2, ab2k, XT2, xt2k
                            WT, wtk = XT, xtk
                            if KRW_BSTOP <= 3:
                                continue
                            pst, pstk = psB.next()
                            exk2 = [ex["A"][1], ex["Bp"][1], ex["Kp"][1], vdk]
                            for qi, s_ in enumerate((Ad, Bpd, Kpd, Vdd)):
                                kb.op('pe', lambda e, qi=qi, s_=s_: e.matmul(pst[:, qi * 128:(qi + 1) * 128], lhsT=s_, rhs=idt[:], start=True, stop=True), reads=exk2 + ["idt"], writes=[pstk], inc=(qi == 3))
                            if os.environ.get('KRW_X') == 'noevac':
                                continue
                            TM, tmk = TM_r.next()
                            AU, auk = AU_r.next()
                            Vtm, vtk = XT_r.next()
                            evac(TM[:, 0, :], pst[:, 0:128], [pstk], [tmk])
                            if os.environ.get('KRW_X') == 'split':
                                evac(TM[:, 2, :], pst[:, 128:256], [pstk], [tmk])
                                evac(TM[:, 3, :], pst[:, 256:384], [pstk], [tmk])
                            else:
                                evac(TM[:, 2:4, :].rearrange("p a b -> p (a b)"), pst[:, 128:384], [pstk], [tmk])
                            evac(Vtm[:], pst[:, 384:512], [pstk], [vtk])
                            if KRW_BSTOP <= 4:
                                continue
                            psx_, psxk_ = psB.next()
                            kb.op('pe', lambda e: e.matmul(psx_[:, 0:128], lhsT=AT4[:, 1, :], rhs=Vtm[:], start=True, stop=True), reads=[atk, vtk], writes=[psxk_])
                            evac(TM[:, 1, :], psx_[:, 0:128], [psxk_], [tmk])
                            psau, psauk = psB.next()
                            kb.op('pe', lambda e: e.matmul(psau[:, 0:256], lhsT=WT[:], rhs=TM[:, 0:2, :].rearrange("p a b -> p (a b)"), start=True, stop=True), reads=[wtk, tmk], writes=[psauk])
                            evac(AU[:].rearrange("p a b -> p (a b)"), psau[:, 0:256], [psauk], [auk])
                            if KRW_BSTOP <= 5:
                                continue
                            psg, psgk = psB.next()
                            kb.op('pe', lambda e: e.matmul(psg[:, 0:128], lhsT=AU[:, 0, :], rhs=TM[:, 2, :], start=True, stop=True), reads=[auk, tmk], writes=[psgk])
                            kb.op('dve', lambda e: e.scalar_tensor_tensor(out=stG[:, ci, :], in0=idt[:], scalar=PL[:, ci:ci + 1], in1=psg[:, 0:128], op0=ALU.mult, op1=ALU.add),
                                  reads=[psgk, plk, "idt"], writes=[stGk])
                            if KRW_BSTOP <= 6:
                                continue
                            psn, psnk = psB.next()
                            kb.op('pe', lambda e: e.matmul(psn[:, 0:128], lhsT=TM[:, 2, :], rhs=AU[:, 1, :], start=True, stop=False), reads=[auk, tmk], writes=[psnk], inc=False)
                            kb.op('pe', lambda e: e.matmul(psn[:, 0:128], lhsT=TM[:, 3, :], rhs=Vtm[:], start=False, stop=True), reads=[tmk, vtk], writes=[psnk])
                            evac(stN[:, ci, :], psn[:, 0:128], [psnk], [stNk])
                            if KRW_BSTOP <= 7:
                                continue
                            if own:
                                psr, psrk = psB.next()
                                kb.op('pe', lambda e: e.matmul(psr[:, 0:128], lhsT=AU[:, 0, :], rhs=AT4[:, 2, :], start=True, stop=True), reads=[auk, atk], writes=[psrk])
                                kb.op('dve', lambda e: e.tensor_tensor(out=stR[:, ci, :], in0=psr[:, 0:128], in1=Rd, op=ALU.add), reads=[psrk, ex["R"][1]], writes=[stRk])
                                if KRW_BSTOP <= 8:
                                    continue
                                pso, psok = psB.next()
                                kb.op('pe', lambda e: e.matmul(pso[:, 0:128], lhsT=AT4[:, 2, :], rhs=AU[:, 1, :], start=True, stop=False), reads=[auk, atk], writes=[psok], inc=False)
                                kb.op('pe', lambda e: e.matmul(pso[:, 0:128], lhsT=AT4[:, 3, :], rhs=Vtm[:], start=False, stop=True), reads=[atk, vtk], writes=[psok])
                                evac(stO[:, ci, :], pso[:, 0:128], [psok], [stOk])
                        if os.environ.get('KRW_NOST'):
                            continue
                        kb.dma('sp', GTd[d, hp, :, ch0:ch0 + nch, :], stG[:, :nch, :], reads=[stGk])
                        kb.dma('sp', Nd[d, hp, :, ch0:ch0 + nch, :], stN[:, :nch, :], reads=[stNk])
                        if own:
                            kb.dma('sp', RhTd[d, hp, :, ch0:ch0 + nch, :], stR[:, :nch, :], reads=[stRk])
                            kb.dma('sp', Ohd[d, hp, :, ch0:ch0 + nch, :], stO[:, :nch, :], reads=[stOk])
                    if own:
                        kb.op('dve', lambda e: e.tensor_scalar(out=bsum[:, :n], in0=bsum[:, :n], scalar1=rk_t[:, hp:hp + 1], scalar2=None, op0=ALU.mult), reads=[bsk, "rk_t"], writes=[bsk])
                        psb_, psbk_ = psA.next()
                        kb.op('pe', lambda e: e.matmul(psb_[:, :n], lhsT=BLK[:], rhs=bsum[:, :n], start=True, stop=True), reads=["BLK", bsk], writes=[psbk_])
                        bo, bok = sh_r.next()
                        kb.op('dve', lambda e: e.tensor_tensor(out=bo[:, :n], in0=psb_[:, :n], in1=VS[:, :n], op=ALU.mult), reads=[psbk_, vsk], writes=[bok])
                        kb.dma('sp', BONs[hp, :, c0:c0 + n], bo[:, :n], reads=[bok])
                        psg_, psgk_ = psA.next()
                        kb.op('pe', lambda e: e.matmul(psg_[:, :n], lhsT=g2_t[:, hp * 128:(hp + 1) * 128], rhs=SGg[:, :n], start=True, stop=True), reads=["g2_t", sggk], writes=[psgk_])
                        go, gok = sh_r.next()
                        evac(go[:, :n], psg_[:, :n], [psgk_], [gok])
                        kb.dma('sp', Gsc[hp, :, c0:c0 + n], go[:, :n], reads=[gok])

        KREG = os.environ.get('KRW_REG', 'both')
        if KRW_MARK < 99:
            try:
                region(pT, FULL_BLKS, False, GTs, Nsc, None, None, lambda bi: 0)
            except StopRegion:
                pass
            kb.barrier()
            return
        if KREG in ('both', 'full'):
            region(pT, FULL_BLKS, False, GTs, Nsc, None, None, lambda bi: 0 if bi == 0 else 4 + (bi - 1) * 8)
        if KREG in ('both', 'own'):
            region(poT, OWN_RBLKS, True, GTo, Nso, RhTo, Oho, lambda bi: bi * 8)
        kb.barrier()

    if KRW_NOCHAIN:
        return
    with ExitStack() as ph:
        sbp = lambda name, shape: ph.enter_context(nc.sbuf_tensor(name, shape, F32))
        idt_ = idt
        selF = sbp("selF2", [128, 4]); kb.dma('sp', selF[:], C["selF"], writes=["selF2"])
        selB = sbp("selB2", [128, 4]); kb.dma('sp', selB[:], C["selB"], writes=["selB2"])
        SEL = sbp("SEL2", [128, 64]); kb.dma('sp', SEL[:], C["sel"], writes=["SEL2"])
        lnw_t = sbp("lnw2", [128, 4]); kb.dma('sp', lnw_t[:], C["ln_w"].rearrange("(c p) -> p c", p=128), writes=["lnw2"], allow_slow_non_contiguous=True)
        lnb_t = sbp("lnb2", [128, 4]); kb.dma('sp', lnb_t[:], C["ln_b"].rearrange("(c p) -> p c", p=128), writes=["lnb2"], allow_slow_non_contiguous=True)
        chains = [(d, hp) for d in range(2) for hp in range(4)]
        S = {}
        for (d, hp) in chains:
            S[(d, hp)] = [sbp("S%d%d_%d" % (d, hp, i), [128, 128]) for i in range(2)]
            kb.op('pool', lambda e: e.memset(S[(d, hp)][0][:], 0.0), writes=[("S", d, hp, 0)])
        CAND = {(d, hp): sbp("CA%d%d" % (d, hp), [128, 4, 128]) for (d, hp) in chains}
        ph1 = ExitStack()
        gl_r = {ch: Rot(nc, ph1, "gl%d%d" % ch, [128, 8, 128], 1) for ch in chains}
        nl_r = {ch: Rot(nc, ph1, "nl%d%d" % ch, [128, 8, 128], 1) for ch in chains}
        psC = Rot(nc, ph, "psC", [128, 512], 8, psum=True)
        cur = {ch: 0 for ch in chains}
        def groups(d):
            if d == 0:
                g = [list(range(0, 4))] + [list(range(4 + i * 8, 12 + i * 8)) for i in range(12)]
            else:
                g = [list(range(3, -1, -1))] + [list(range(4 + i * 8 + 7, 4 + i * 8 - 1, -1)) for i in range(15, 3, -1)]
            return g
        G = {0: groups(0), 1: groups(1)}
        ngroups = len(G[0])
        for gi in range(ngroups):
            loaded = {}
            for ch in chains:
                d, hp = ch
                g = G[d][gi]
                lo = min(g)
                gl, glk = gl_r[ch].next(); nl, nlk = nl_r[ch].next()
                kb.dma('sp', gl[:, :len(g), :], GTs[d, hp, :, lo:lo + len(g), :], writes=[glk])
                kb.dma('sp', nl[:, :len(g), :], Nsc[d, hp, :, lo:lo + len(g), :], writes=[nlk])
                loaded[ch] = (gl, glk, nl, nlk, lo)
            for step in range(len(G[0][gi])):
                for ch in chains:
                    d, hp = ch
                    gl, glk, nl, nlk, lo = loaded[ch]
                    c = G[d][gi][step] - lo
                    i0 = cur[ch]; i1 = 1 - i0
                    ps, pk = psC.next()
                    kb.op('pe', lambda e: e.matmul(ps[:, 0:128], lhsT=gl[:, c, :], rhs=S[ch][i0][:], start=True, stop=True), reads=[glk, ("S", d, hp, i0)], writes=[pk])
                    kb.op('dve', lambda e: e.tensor_tensor(out=S[ch][i1][:], in0=ps[:, 0:128], in1=nl[:, c, :], op=ALU.add), reads=[pk, nlk], writes=[("S", d, hp, i1)])
                    cur[ch] = i1
            if gi in (0, 4, 8, 12):
                ci_ = {0: 0, 4: 1, 8: 2, 12: 3}[gi]
                for ch in chains:
                    d, hp = ch
                    kb.op('dve', lambda e: e.tensor_copy(out=CAND[ch][:, ci_, :], in_=S[ch][cur[ch]][:]), reads=[("S", d, hp, cur[ch])], writes=[("CAND", d, hp)])
        for ch in chains:
            d, hp = ch
            sel = selF if d == 0 else selB
            seln = "selF2" if d == 0 else "selB2"
            i0 = cur[ch]
            kb.op('dve', lambda e: e.tensor_scalar(out=S[ch][i0][:], in0=CAND[ch][:, 0, :], scalar1=sel[:, 0:1], scalar2=None, op0=ALU.mult),
                  reads=[("CAND", d, hp), seln], writes=[("S", d, hp, i0)])
            for i in range(1, 4):
                kb.op('dve', lambda e: e.scalar_tensor_tensor(out=S[ch][i0][:], in0=CAND[ch][:, i, :], scalar=sel[:, i:i + 1], in1=S[ch][i0][:], op0=ALU.mult, op1=ALU.add),
                      reads=[("CAND", d, hp), seln, ("S", d, hp, i0)], writes=[("S", d, hp, i0)])
        kb.barrier()
        ph1.close()
        OD = {ch: sbp("OD%d%d" % ch, [128, NCH_OWN, 64]) for ch in chains}
        gl_r = {ch: Rot(nc, ph, "g2l%d%d" % ch, [128, 4, 128], 1) for ch in chains}
        nl_r = {ch: Rot(nc, ph, "n2l%d%d" % ch, [128, 4, 128], 1) for ch in chains}
        rl_r = {ch: Rot(nc, ph, "rl%d%d" % ch, [128, 4, 128], 1) for ch in chains}
        ol_r = {ch: Rot(nc, ph, "ol%d%d" % ch, [128, 4, 128], 1) for ch in chains}
        tmp_r = Rot(nc, ph, "ctmp", [128, 128], 4)
        for gi in range(8):
            loaded = {}
            for ch in chains:
                d, hp = ch
                g = list(range(gi * 4, gi * 4 + 4)) if d == 0 else list(range(31 - gi * 4, 27 - gi * 4, -1))
                lo = min(g)
                gl, glk = gl_r[ch].next(); nl, nlk = nl_r[ch].next(); rl, rlk = rl_r[ch].next(); ol, olk = ol_r[ch].next()
                for (t_, k_, src_) in ((gl, glk, GTo), (nl, nlk, Nso), (rl, rlk, RhTo), (ol, olk, Oho)):
                    kb.dma('sp', t_[:], src_[d, hp, :, lo:lo + 4, :], writes=[k_])
                loaded[ch] = (gl, glk, nl, nlk, rl, rlk, ol, olk, lo, g)
            for step in range(4):
                for ch in chains:
                    d, hp = ch
                    gl, glk, nl, nlk, rl, rlk, ol, olk, lo, g = loaded[ch]
                    cg = g[step]; c = cg - lo
                    i0 = cur[ch]; i1 = 1 - i0
                    pso, psok = psC.next()
                    kb.op('pe', lambda e: e.matmul(pso[:, 0:128], lhsT=rl[:, c, :], rhs=S[ch][i0][:], start=True, stop=True), reads=[rlk, ("S", d, hp, i0)], writes=[psok], inc=False)
                    kb.op('pe', lambda e: e.matmul(pso[:, 128:256], lhsT=gl[:, c, :], rhs=S[ch][i0][:], start=True, stop=True), reads=[glk, ("S", d, hp, i0)], writes=[psok])
                    kb.op('dve', lambda e: e.tensor_tensor(out=S[ch][i1][:], in0=pso[:, 128:256], in1=nl[:, c, :], op=ALU.add), reads=[psok, nlk], writes=[("S", d, hp, i1)])
                    tt, ttk = tmp_r.next()
                    kb.op('dve', lambda e: e.tensor_tensor(out=tt[:], in0=pso[:, 0:128], in1=ol[:, c, :], op=ALU.add), reads=[psok, olk], writes=[ttk])
                    kb.op('pool', lambda e: e.tensor_tensor(out=OD[ch][:, cg, :], in0=tt[:, 0:64], in1=tt[:, 64:128], op=ALU.add), reads=[ttk], writes=[("OD", d, hp)])
                    cur[ch] = i1
        ye_r = Rot(nc, ph, "yexp", [128, 8, 2, 64], 2)
        for j_, t_ in enumerate(ye_r.t):
            kb.op('pool', lambda e, t_=t_: e.memset(t_[:], 0.0), writes=[("yexp", j_)])
        os_r = Rot(nc, ph, "osum", [128, 8, 64], 2)
        sq_r = Rot(nc, ph, "osq", [128, 8, 64], 2)
        stt_r = Rot(nc, ph, "ostt", [128, 4, 8], 2)
        yn_r = Rot(nc, ph, "ynr", [128, 512], 2)
        bg_r = Rot(nc, ph, "bgr", [128, 2, 512], 2)
        for hp in range(4):
            for bi in range(4):
                OS, osk = os_r.next()
                kb.op('pool', lambda e: e.tensor_tensor(out=OS[:], in0=OD[(0, hp)][:, bi * 8:(bi + 1) * 8, :], in1=OD[(1, hp)][:, bi * 8:(bi + 1) * 8, :], op=ALU.add),
                      reads=[("OD", 0, hp), ("OD", 1, hp)], writes=[osk])
                stt, sk = stt_r.next()
                kb.op('dve', lambda e: e.reduce_sum(out=stt[:, 0, :], in_=OS[:], axis=AX.X), reads=[osk], writes=[sk])
                SQ, sqk = sq_r.next()
                kb.op('pool', lambda e: e.tensor_tensor(out=SQ[:], in0=OS[:], in1=OS[:], op=ALU.mult), reads=[osk], writes=[sqk])
                kb.op('dve', lambda e: e.reduce_sum(out=stt[:, 1, :], in_=SQ[:], axis=AX.X), reads=[sqk], writes=[sk])
                kb.op('dve', lambda e: e.tensor_scalar(out=stt[:, 0, :], in0=stt[:, 0, :], scalar1=1.0 / 64, scalar2=None, op0=ALU.mult), reads=[sk], writes=[sk])
                kb.op('dve', lambda e: e.tensor_tensor(out=stt[:, 2, :], in0=stt[:, 0, :], in1=stt[:, 0, :], op=ALU.mult), reads=[sk], writes=[sk])
                kb.op('dve', lambda e: e.scalar_tensor_tensor(out=stt[:, 3, :], in0=stt[:, 1, :], scalar=1.0 / 64, in1=stt[:, 2, :], op0=ALU.mult, op1=ALU.subtract), reads=[sk], writes=[sk])
                kb.op('dve', lambda e: e.tensor_scalar(out=stt[:, 3, :], in0=stt[:, 3, :], scalar1=GN_EPS, scalar2=None, op0=ALU.add), reads=[sk], writes=[sk])
                kb.op('act', lambda e: e.activation(out=stt[:, 3, :], in_=stt[:, 3, :], func=AF.Sqrt), reads=[sk], writes=[sk])
                kb.op('dve', lambda e: e.reciprocal(out=stt[:, 3, :], in_=stt[:, 3, :]), reads=[sk], writes=[sk])
                kb.op('dve', lambda e: e.tensor_tensor(out=OS[:], in0=OS[:], in1=stt[:, 0, :].unsqueeze(2).to_broadcast([128, 8, 64]), op=ALU.subtract), reads=[osk, sk], writes=[osk])
                ye, yek = ye_r.next()
                for hh in range(2):
                    sl = slice(hh * 64, hh * 64 + 64)
                    kb.op('dve', lambda e, hh=hh, sl=sl: e.tensor_tensor(out=ye[sl, :, hh, :], in0=OS[sl], in1=stt[sl, 3, :].unsqueeze(2).to_broadcast([64, 8, 64]), op=ALU.mult),
                          reads=[osk, sk], writes=[yek])
                ps, pk = psC.next()
                for ci in range(8):
                    kb.op('pe', lambda e, ci=ci: e.matmul(ps[:, ci * 64:(ci + 1) * 64], lhsT=ye[:, ci].rearrange("p a b -> p (a b)"), rhs=SEL[:], start=True, stop=True),
                          reads=[yek, "SEL2"], writes=[pk], inc=(ci == 7))
                bg, bgk = bg_r.next()
                kb.dma('sp', bg[:, 0, :], BONs[hp, :, bi * 512:(bi + 1) * 512], writes=[bgk])
                kb.dma('sp', bg[:, 1, :], Gsc[hp, :, bi * 512:(bi + 1) * 512], writes=[bgk])
                yn, ynk = yn_r.next()
                kb.op('dve', lambda e: e.tensor_scalar(out=yn[:], in0=ps[:], scalar1=lnw_t[:, hp:hp + 1], scalar2=lnb_t[:, hp:hp + 1], op0=ALU.mult, op1=ALU.add),
                      reads=[pk, "lnw2", "lnb2"], writes=[ynk])
                kb.op('pool', lambda e: e.tensor_tensor(out=yn[:], in0=yn[:], in1=bg[:, 0, :], op=ALU.add), reads=[ynk, bgk], writes=[ynk])
                kb.op('pool', lambda e: e.tensor_tensor(out=yn[:], in0=yn[:], in1=bg[:, 1, :], op=ALU.mult), reads=[ynk, bgk], writes=[ynk])
                kb.dma('sp', yT[hp * 128:(hp + 1) * 128, bi * 512:(bi + 1) * 512], yn[:], reads=[ynk])
        kb.barrier()


def rwkv_consts(q):
    tt = np.arange(64)
    lowS = (tt[:, None] < tt[None, :]).astype(np.float32)
    lowI = (tt[:, None] <= tt[None, :]).astype(np.float32)
    def bd(m):
        z = np.zeros((128, 128), np.float32)
        z[:64, :64] = m
        z[64:, 64:] = m
        return z
    mask4 = np.zeros((2, 128, 512), np.float32)
    maskl = np.zeros((2, 128, 128), np.float32)
    for d in range(2):
        S_, I_ = (lowS, lowI) if d == 0 else (lowS.T, lowI.T)
        mask4[d] = np.concatenate([bd(S_), bd(S_), bd(I_), bd(I_)], axis=1)
        maskl[d] = bd(S_.T)
    blk = bd(np.ones((64, 64), np.float32))
    uu = np.concatenate([(tt[:, None] <= tt[None, :]).astype(np.float32)] * 2, axis=0)
    sel = np.concatenate([np.eye(64, dtype=np.float32)] * 2, axis=0)
    selF = np.zeros((128, 4), np.float32); selF[:, q] = 1.0
    selB = np.zeros((128, 4), np.float32); selB[:, 3 - q] = 1.0
    hal = np.zeros((128, 2), np.float32)
    hal[:, 0] = 1.0 if q > 0 else 0.0
    hal[:, 1] = 1.0 if q < 3 else 0.0
    return dict(mask4=mask4, maskl=maskl, blk=blk, uu=uu, sel=sel, selF=selF, selB=selB, hal=hal)


OWN_BLKS = [(0, 512), (512, 512), (1024, 512), (1536, 512), (2048, 128)]
XO_ROWS = QT + 128
NE = 32
ATT_SCALE = 192.0 ** -0.5


RUN = os.environ.get("KSTAGES", "123456")
SKIP = os.environ.get("KSKIP", "").split(",")


class Stage:
    def __init__(self, s):
        self.s = s

    def __enter__(self):
        return self.s in RUN

    def __exit__(self, *a):
        return False


def build():
    nc = bass.Bass("TRN2", target_bir_lowering=False)
    dt = nc.dram_tensor

    def din(name, shape):
        return dt(name, shape, F32, kind="ExternalInput").ap()

    def dscr(name, shape):
        if DEBUG and name in DEBUG.split(","):
            return dt("dbg_" + name, shape, F32, kind="ExternalOutput").ap()
        return dt(name, shape, F32).ap()

    xs = din("xs", [TS, D])
    xo = din("xo", [XO_ROWS, D])
    cvec = din("cvec", [128, 16])
    mod_w = din("mod_w", [D, 6 * D])
    mod_b = din("mod_b", [6 * D])
    g1 = din("g1", [D])
    g2n = din("g2n", [D])
    gfin = din("gfin", [D])
    w_in = din("w_in", [D, WCOLS])
    ident = din("ident", [128, 128])
    ropeC = din("ropeC", [64, T])
    ropeS = din("ropeS", [64, T])
    ropeCo = din("ropeCo", [64, QT])
    ropeSo = din("ropeSo", [64, QT])
    kvg = din("kvg", [128, 1])
    qg = din("qg", [128, 2])
    wukv_k = din("wukv_k", [128, 512])
    wukv_v = din("wukv_v", [128, 512])
    wuq = din("wuq", [256, 1024])
    w_out = din("w_out", [D, D])
    router_w = din("router_w", [D, NE])
    router_b = din("router_b", [NE])
    w1 = din("w1", [NE, D, 2 * D]) if "5" in RUN else None
    b1fm = din("b1fm", [128, NE * 16])
    w2 = din("w2", [NE, D, D]) if "5" in RUN else None
    b2 = din("b2", [NE, D])
    yrw_in = din("yrw_in", [512, QT]) if (DEBUG and '2' not in RUN) else None
    rw = {}
    if '2' in RUN:
        for nm, shp in (("mu", [1792]), ("w0", [2, 512]), ("a0", [2, 512]), ("w2", [2, 64, 512]), ("a2", [2, 64, 512]), ("k_k", [512]), ("k_a", [512]),
                        ("r_k", [512]), ("ln_w", [512]), ("ln_b", [512]), ("g2", [128, 512]), ("mask4", [2, 128, 512]), ("maskl", [2, 128, 128]),
                        ("blk", [128, 128]), ("uu", [128, 64]), ("sel", [128, 64]), ("selF", [128, 4]), ("selB", [128, 4]), ("hal", [128, 2])):
            rw[nm] = din("rw_" + nm, shp)
    out = dt("out", [QT, D], F32, kind="ExternalOutput").ap()

    pT = dscr("pT", [1792, TS])
    poT = dscr("poT", [1792, XO_ROWS])
    KnT = dscr("KnT", [4, 128, TS])
    KrT = dscr("KrT", [64, TS])
    Vs = dscr("Vs", [TS, 4 * 129])
    QnT = dscr("QnT", [4, 128, QT])
    QrT = dscr("QrT", [4, 64, QT])
    yT = dscr("yT", [D, QT])
    X1 = dscr("X1", [QT, D])
    LG = dscr("LG", [QT, 2 * NE])
    FF = dscr("FF", [QT, D])

    with ExitStack() as st:
        kb = KB(nc, st)
        sb = lambda name, shape: st.enter_context(nc.sbuf_tensor(name, shape, F32))
        idt = sb("idt", [128, 128])
        kb.dma('sp', idt[:], ident, writes=["idt"])
        ones = sb("ones", [128, 128])
        kb.op('pool', lambda e: e.memset(ones[:], 1.0), writes=["ones"])
        cv = sb("cv", [128, 16])
        sc = sb("sc", [128, 16])
        kb.dma('sp', cv[:], cvec, writes=["cv"])
        kb.op('act', lambda e: e.activation(out=sc[:], in_=cv[:], func=AF.Silu), reads=["cv"], writes=["sc"])
        modfm = sb("modfm", [128, 4, 8, 2])
        mbfm = sb("mbfm", [128, 6, 8])
        kb.dma('sp', mbfm[:], mod_b.rearrange("(v k p) -> p v k", p=128, k=8), writes=["mbfm"], allow_slow_non_contiguous=True)
        g1t = sb("g1t", [128, 8])
        kb.dma('sp', g1t[:], g1.rearrange("(k p) -> p k", p=128), writes=["g1t"], allow_slow_non_contiguous=True)
        g2t = sb("g2t", [128, 8])
        kb.dma('sp', g2t[:], g2n.rearrange("(k p) -> p k", p=128), writes=["g2t"], allow_slow_non_contiguous=True)
        GT = sb("GT", [128, 2, 1024])
        with ExitStack() as ph:
            wv = Rot(nc, ph, "wv", [128, 8, 1024], 2)
            psm = Rot(nc, ph, "psm", [128, 16], 2, psum=True)
            psg = Rot(nc, ph, "psg", [128, 512], 2, psum=True)
            SCB = ph.enter_context(nc.sbuf_tensor("SCB", [128, 8, 128], F32))
            mbb = ph.enter_context(nc.sbuf_tensor("mbb", [128, 2, 1024], F32))
            for k in range(8):
                kb.op('dve', lambda e, k=k: e.tensor_copy(out=SCB[:, k, :], in_=sc[:, 2 * k:2 * k + 1].to_broadcast([128, 128])),
                      reads=["sc"], writes=["SCB"])
            for gi, v in enumerate((2, 5)):
                kb.dma('sp', mbb[:, gi, :], mod_b[v * 1024:(v + 1) * 1024].partition_broadcast(128), writes=["mbb"])
            for vi, v in enumerate((0, 1, 3, 4)):
                wt, wk = wv.next()
                kb.dma('sp', wt[:], mod_w[:, v * 1024:(v + 1) * 1024].rearrange("(k p) n -> p k n", p=128), writes=[wk])
                ps, pk = psm.next()
                for j in range(8):
                    for k in range(8):
                        kb.op('pe', lambda e, j=j, k=k: e.matmul(ps[:, 2 * j:2 * j + 2], lhsT=wt[:, k, j * 128:(j + 1) * 128],
                                                                 rhs=sc[:, 2 * k:2 * k + 2], start=(k == 0), stop=(k == 7)),
                              reads=[wk, "sc"], writes=[pk], inc=(j == 7 and k == 7))
                kb.op('dve', lambda e, vi=vi, v=v: e.tensor_tensor(out=modfm[:, vi], in0=ps[:].rearrange("p (j c) -> p j c", c=2),
                                                                   in1=mbfm[:, v, :].unsqueeze(2).to_broadcast([128, 8, 2]), op=ALU.add),
                      reads=[pk, "mbfm"], writes=["modfm"])
            for gi, v in enumerate((2, 5)):
                wt, wk = wv.next()
                kb.dma('sp', wt[:], mod_w[:, v * 1024:(v + 1) * 1024].rearrange("(k p) n -> p k n", p=128), writes=[wk])
                for hf in range(2):
                    ps, pk = psg.next()
                    for k in range(8):
                        kb.op('pe', lambda e, k=k, hf=hf: e.matmul(ps[:], lhsT=SCB[:, k, :], rhs=wt[:, k, hf * 512:(hf + 1) * 512],
                                                                   start=(k == 0), stop=(k == 7)),
                              reads=[wk, "SCB"], writes=[pk], inc=(k == 7))
                    kb.op('dve', lambda e, gi=gi, hf=hf: e.tensor_tensor(out=GT[:, gi, hf * 512:(hf + 1) * 512], in0=ps[:],
                                                                         in1=mbb[:, gi, hf * 512:(hf + 1) * 512], op=ALU.add),
                          reads=[pk, "mbb"], writes=["GT"])
            kb.barrier()
        G1 = sb("G1", [128, 8, 2])
        G2 = sb("G2", [128, 8, 2])
        for Gt, gt_, mrow, nm in ((G1, g1t, 1, "G1"), (G2, g2t, 3, "G2")):
            kb.op('dve', lambda e: e.tensor_scalar(out=Gt[:], in0=modfm[:, mrow], scalar1=1.0, scalar2=None, op0=ALU.add), reads=["modfm"], writes=[nm])
            kb.op('dve', lambda e: e.tensor_tensor(out=Gt[:], in0=Gt[:], in1=gt_[:].unsqueeze(2).to_broadcast([128, 8, 2]), op=ALU.mult),
                  reads=[nm, "g1t", "g2t"], writes=[nm])

        def rms_rstd(xt, xk, stt, sk, junk, n_feat):
            kb.op('pool', lambda e: e.memset(stt[:], 0.0), writes=[sk])
            kb.op('act', lambda e: e.activation(out=junk[:], in_=xt, func=AF.Square, accum_out=stt[:, 0:1]), reads=[xk], writes=["junk", sk])
            kb.op('dve', lambda e: e.tensor_scalar(out=stt[:, 1:2], in0=stt[:, 0:1], scalar1=1.0 / n_feat, scalar2=1e-6, op0=ALU.mult, op1=ALU.add),
                  reads=[sk], writes=[sk])
            kb.op('act', lambda e: e.activation(out=stt[:, 2:3], in_=stt[:, 1:2], func=AF.Sqrt), reads=[sk], writes=[sk])
            kb.op('dve', lambda e: e.reciprocal(out=stt[:, 3:4], in_=stt[:, 2:3]), reads=[sk], writes=[sk])

        def cm_rstd(ps_ss, pk, dst, dk, n, n_feat, extra_scale=1.0):
            kb.op('dve', lambda e: e.tensor_scalar(out=dst[:, :n], in0=ps_ss[:, :n], scalar1=1.0 / n_feat, scalar2=1e-6, op0=ALU.mult, op1=ALU.add),
                  reads=[pk], writes=[dk])
            kb.op('act', lambda e: e.activation(out=dst[:, :n], in_=dst[:, :n], func=AF.Sqrt), reads=[dk], writes=[dk])
            kb.op('dve', lambda e: e.reciprocal(out=dst[:, :n], in_=dst[:, :n]), reads=[dk], writes=[dk])
            if extra_scale != 1.0:
                kb.op('dve', lambda e: e.tensor_scalar(out=dst[:, :n], in0=dst[:, :n], scalar1=extra_scale, scalar2=None, op0=ALU.mult), reads=[dk], writes=[dk])

        with ExitStack() as ph, Stage('1') as go:
          if go:
            sbp = lambda name, shape: ph.enter_context(nc.sbuf_tensor(name, shape, F32))
            WB = sbp("WB", [128, 8, WCOLS])
            for k in range(8):
                kb.dma('sp', WB[:, k, :], w_in[k * 128:(k + 1) * 128, :], writes=[("WB", k)])
            wk_t = sbp("wk_t", [128, 512]); kb.dma('sp', wk_t[:], wukv_k, writes=["wk_t"])
            wv_t = sbp("wv_t", [128, 512]); kb.dma('sp', wv_t[:], wukv_v, writes=["wv_t"])
            wq_t = sbp("wq_t", [128, 2, 1024]); kb.dma('sp', wq_t[:], wuq.rearrange("(k p) n -> p k n", p=128), writes=["wq_t"])
            kvg_t = sbp("kvg_t", [128, 1]); kb.dma('sp', kvg_t[:], kvg, writes=["kvg_t"])
            qg_t = sbp("qg_t", [128, 2]); kb.dma('sp', qg_t[:], qg, writes=["qg_t"])
            xt_r = Rot(nc, ph, "xt", [128, 1024], 2)
            xn_r = Rot(nc, ph, "xn", [128, 1024], 1)
            st_r = Rot(nc, ph, "stat", [128, 4], 4)
            xmT_r = Rot(nc, ph, "xmT", [128, 8, 512], 2)
            pst_r = Rot(nc, ph, "pst", [128, 8, 128], 1, psum=True)
            psp_r = Rot(nc, ph, "psp", [128, 512], 6, psum=True)
            stg_r = Rot(nc, ph, "stg", [128, 512], 3)
            tmp_r = Rot(nc, ph, "tmp", [128, 512], 3)
            ql_r = Rot(nc, ph, "qlr", [128, 512], 2)
            rs_r = Rot(nc, ph, "rsr", [128, 512], 1)
            ckn_r = Rot(nc, ph, "cknr", [128, 512], 1)
            rp_r = Rot(nc, ph, "rp", [64, 2, 512], 2)
            vst_r = Rot(nc, ph, "vst", [128, 4, 129], 2)
            for j_, t_ in enumerate(vst_r.t):
                kb.op('pool', lambda e, t_=t_: e.memset(t_[:], 1.0), writes=[("vst", j_)])
            junk = sbp("junk", [128, 1024])
            cnt = {"ev": 0}

            def evac(out_ap, in_ap, reads, writes):
                cnt["ev"] += 1
                if cnt["ev"] % 2:
                    kb.op('dve', lambda e: e.tensor_copy(out=out_ap, in_=in_ap), reads=reads, writes=writes)
                else:
                    kb.op('act', lambda e: e.copy(out=out_ap, in_=in_ap), reads=reads, writes=writes)

            def proj_pass(src, blocks, dstT, own):
                for (s0, n) in blocks:
                    mi = 1 if (not own and s0 < CTX) else 0
                    xmT, xmk = xmT_r.next()
                    for i in range(n // 128):
                        xt, xk = xt_r.next()
                        kb.dma('sp', xt[:], src[s0 + i * 128: s0 + (i + 1) * 128, :], writes=[xk])
                        stt, sk = st_r.next()
                        rms_rstd(xt[:], xk, stt, sk, junk, D)
                        xn, nk = xn_r.next()
                        kb.op('dve', lambda e: e.tensor_scalar(out=xn[:], in0=xt[:], scalar1=stt[:, 3:4], scalar2=None, op0=ALU.mult),
                              reads=[xk, sk], writes=[nk])
                        pt, ptk = pst_r.next()
                        for k in range(8):
                            kb.op('pe', lambda e, k=k: e.transpose(out=pt[:, k, :], in_=xn[:, k * 128:(k + 1) * 128], identity=idt[:]),
                                  reads=[nk, "idt"], writes=[ptk], inc=(k == 7))
                        for k in range(8):
                            if k % 2 == 0:
                                kb.op('dve', lambda e, k=k, i=i: e.tensor_scalar(out=xmT[:, k, i * 128:(i + 1) * 128], in0=pt[:, k, :],
                                                                                  scalar1=G1[:, k, mi:mi + 1], scalar2=modfm[:, 0, k, mi:mi + 1],
                                                                                  op0=ALU.mult, op1=ALU.add),
                                      reads=[ptk, "G1", "modfm"], writes=[xmk])
                            else:
                                kb.op('act', lambda e, k=k, i=i: e.activation(out=xmT[:, k, i * 128:(i + 1) * 128], in_=pt[:, k, :], func=AF.Identity,
                                                                               scale=G1[:, k, mi:mi + 1], bias=modfm[:, 0, k, mi:mi + 1]),
                                      reads=[ptk, "G1", "modfm"], writes=[xmk])

                    def proj_cols(c0, m):
                        ps, pk = psp_r.next()
                        for k in range(8):
                            kb.op('pe', lambda e, k=k: e.matmul(ps[:m, :n], lhsT=WB[:, k, c0:c0 + m], rhs=xmT[:, k, :n], start=(k == 0), stop=(k == 7)),
                                  reads=[("WB", k), xmk], writes=[pk], inc=(k == 7))
                        return ps, pk

                    for cc in range(14):
                        ps, pk = proj_cols(cc * 128, 128)
                        sg, sgk = stg_r.next()
                        evac(sg[:, :n], ps[:, :n], [pk], [sgk])
                        kb.dma('sp', dstT[cc * 128:(cc + 1) * 128, s0:s0 + n], sg[:, :n], reads=[sgk])
                    if not own and 'kv' not in SKIP:
                        ps, pk = proj_cols(1792, 128)
                        ckv, ck = tmp_r.next()
                        evac(ckv[:, :n], ps[:, :n], [pk], [ck])
                        sq, sqk = tmp_r.next()
                        kb.op('pool', lambda e: e.tensor_tensor(out=sq[:, :n], in0=ckv[:, :n], in1=ckv[:, :n], op=ALU.mult), reads=[ck], writes=[sqk])
                        pss, pssk = psp_r.next()
                        kb.op('pe', lambda e: e.matmul(pss[:, :n], lhsT=ones[:], rhs=sq[:, :n], start=True, stop=True), reads=["ones", sqk], writes=[pssk])
                        rs, rsk = rs_r.next()
                        cm_rstd(pss, pssk, rs, rsk, n, 128.0)
                        ckn, cnk = ckn_r.next()
                        kb.op('dve', lambda e: e.scalar_tensor_tensor(out=ckn[:, :n], in0=ckv[:, :n], scalar=kvg_t[:, 0:1], in1=rs[:, :n],
                                                                       op0=ALU.mult, op1=ALU.mult), reads=[ck, rsk, "kvg_t"], writes=[cnk])
                        for h in range(0 if 'kvK' in SKIP else 4):
                            psk, pskk = psp_r.next()
                            kb.op('pe', lambda e, h=h: e.matmul(psk[:, :n], lhsT=wk_t[:, h * 128:(h + 1) * 128], rhs=ckn[:, :n], start=True, stop=True),
                                  reads=["wk_t", cnk], writes=[pskk])
                            sg, sgk = stg_r.next()
                            evac(sg[:, :n], psk[:, :n], [pskk], [sgk])
                            kb.dma('sp', KnT[h, :, s0:s0 + n], sg[:, :n], reads=[sgk])
                        for i in range(0 if 'kvV' in SKIP else n // 128):
                            psv, psvk = psp_r.next()
                            kb.op('pe', lambda e, i=i: e.matmul(psv[:, :], lhsT=ckn[:, i * 128:(i + 1) * 128], rhs=wv_t[:], start=True, stop=True),
                                  reads=["wv_t", cnk], writes=[psvk])
                            vt, vk = vst_r.next()
                            evac(vt[:, :, 0:128], psv[:].rearrange("p (h d) -> p h d", h=4), [psvk], [vk])
                            kb.dma('sp', Vs[s0 + i * 128:s0 + (i + 1) * 128, :], vt[:].rearrange("p h d -> p (h d)"), reads=[vk])
                        if 'kvR' in SKIP:
                            continue
                        psa, pak = proj_cols(1920, 64)
                        sg, sgk = stg_r.next()
                        if s0 < CTX:
                            evac(sg[:64, :n], psa[:64, :n], [pak], [sgk])
                        else:
                            psb, pbk = proj_cols(1984, 64)
                            rp, rpk = rp_r.next()
                            kb.dma('sp', rp[:, 0, :n], ropeC[:, s0 - CTX:s0 - CTX + n], writes=[rpk])
                            kb.dma('sp', rp[:, 1, :n], ropeS[:, s0 - CTX:s0 - CTX + n], writes=[rpk])
                            t1, t1k = tmp_r.next()
                            kb.op('dve', lambda e: e.tensor_tensor(out=t1[:64, :n], in0=psa[:64, :n], in1=rp[:, 0, :n], op=ALU.mult), reads=[pak, rpk], writes=[t1k])
                            t2, t2k = tmp_r.next()
                            kb.op('dve', lambda e: e.tensor_tensor(out=t2[:64, :n], in0=psb[:64, :n], in1=rp[:, 1, :n], op=ALU.mult), reads=[pbk, rpk], writes=[t2k])
                            kb.op('pool', lambda e: e.tensor_tensor(out=sg[:64, :n], in0=t1[:64, :n], in1=t2[:64, :n], op=ALU.add), reads=[t1k, t2k], writes=[sgk])
                        kb.dma('sp', KrT[:, s0:s0 + n], sg[:64, :n], reads=[sgk])
                    elif own and s0 < QT and 'q' not in SKIP:
                        ql = []
                        pss, pssk = psp_r.next()
                        for kc in range(2):
                            ps, pk = proj_cols(2048 + kc * 128, 128)
                            qn_, qnk = ql_r.next()
                            evac(qn_[:, :n], ps[:, :n], [pk], [qnk])
                            sq, sqk = tmp_r.next()
                            kb.op('pool', lambda e: e.tensor_tensor(out=sq[:, :n], in0=qn_[:, :n], in1=qn_[:, :n], op=ALU.mult), reads=[qnk], writes=[sqk])
                            kb.op('dve', lambda e, kc=kc: e.tensor_scalar(out=qn_[:, :n], in0=qn_[:, :n], scalar1=qg_t[:, kc:kc + 1], scalar2=None, op0=ALU.mult),
                                  reads=[qnk, sqk, "qg_t"], writes=[qnk])
                            kb.op('pe', lambda e, kc=kc: e.matmul(pss[:, :n], lhsT=ones[:], rhs=sq[:, :n], start=(kc == 0), stop=(kc == 1)),
                                  reads=["ones", sqk], writes=[pssk], inc=(kc == 1))
                            ql.append((qn_, qnk))
                        rs, rsk = rs_r.next()
                        cm_rstd(pss, pssk, rs, rsk, n, 256.0, ATT_SCALE)
                        rp, rpk = rp_r.next()
                        kb.dma('sp', rp[:, 0, :n], ropeCo[:, s0:s0 + n], writes=[rpk])
                        kb.dma('sp', rp[:, 1, :n], ropeSo[:, s0:s0 + n], writes=[rpk])
                        for h in range(4):
                            def qmm(c0, m):
                                ps, pk = psp_r.next()
                                for kc in range(2):
                                    kb.op('pe', lambda e, kc=kc: e.matmul(ps[:m, :n], lhsT=wq_t[:, kc, h * 256 + c0:h * 256 + c0 + m], rhs=ql[kc][0][:, :n],
                                                                          start=(kc == 0), stop=(kc == 1)),
                                          reads=["wq_t", ql[kc][1]], writes=[pk], inc=(kc == 1))
                                return ps, pk
                            ps, pk = qmm(0, 128)
                            sg, sgk = stg_r.next()
                            kb.op('dve', lambda e: e.tensor_tensor(out=sg[:, :n], in0=ps[:, :n], in1=rs[:, :n], op=ALU.mult), reads=[pk, rsk], writes=[sgk])
                            kb.dma('sp', QnT[h, :, s0:s0 + n], sg[:, :n], reads=[sgk])
                            psa, pak = qmm(128, 64)
                            psb, pbk = qmm(192, 64)
                            t1, t1k = tmp_r.next()
                            kb.op('dve', lambda e: e.tensor_tensor(out=t1[:64, :n], in0=psa[:64, :n], in1=rp[:, 0, :n], op=ALU.mult), reads=[pak, rpk], writes=[t1k])
                            t2, t2k = tmp_r.next()
                            kb.op('dve', lambda e: e.tensor_tensor(out=t2[:64, :n], in0=psb[:64, :n], in1=rp[:, 1, :n], op=ALU.mult), reads=[pbk, rpk], writes=[t2k])
                            kb.op('pool', lambda e: e.tensor_tensor(out=t1[:64, :n], in0=t1[:64, :n], in1=t2[:64, :n], op=ALU.add), reads=[t1k, t2k], writes=[t1k])
                            sg, sgk = stg_r.next()
                            kb.op('dve', lambda e: e.tensor_tensor(out=sg[:64, :n], in0=t1[:64, :n], in1=rs[:64, :n], op=ALU.mult), reads=[t1k, rsk], writes=[sgk])
                            kb.dma('sp', QrT[h, :, s0:s0 + n], sg[:64, :n], reads=[sgk])

            proj_pass(xs, NBLK_FULL, pT, False)
            if 'own' not in SKIP:
                proj_pass(xo, OWN_BLKS, poT, True)
            kb.barrier()

        if '2' in RUN:
            C = dict(rw)
            C["idt"] = idt
            rwkv_stage(nc, kb, st, pT, poT, yT, C, dscr)
        elif DEBUG:
            with ExitStack() as ph:
                t = ph.enter_context(nc.sbuf_tensor("yin", [128, 4, QT], F32))
                kb.dma('sp', t[:], yrw_in.rearrange("(k p) n -> p k n", p=128), writes=["yin"])
                kb.dma('sp', yT[0:512, :].rearrange("(k p) n -> p k n", p=128), t[:], reads=["yin"])
                kb.barrier()

        with ExitStack() as ph, Stage('3') as go:
          if go:
            sbp = lambda name, shape: ph.enter_context(nc.sbuf_tensor(name, shape, F32))
            Kn = sbp("Kn", [128, TS])
            Kr = sbp("Kr", [64, TS])
            Vh = sbp("Vh", [128, 66, 129])
            kb.dma('sp', Kr[:], KrT, writes=["Kr"])
            qn_r = Rot(nc, ph, "qn", [128, 512], 2)
            qr_r = Rot(nc, ph, "qr", [64, 512], 2)
            pT_r = Rot(nc, ph, "pTt", [128, 512], 3)
            pss_r = Rot(nc, ph, "pss", [128, 512], 3, psum=True)
            pso = [ph.enter_context(nc.psum_tensor("pso%d" % i, [128, 512], F32)) for i in range(4)]
            ptr_r = Rot(nc, ph, "ptr", [128, 128], 1, psum=True)
            yv_r = Rot(nc, ph, "yv", [128, 132], 3)
            yt_r = Rot(nc, ph, "ytt", [128, 512], 2)
            for h in range(4):
                for part in range(4):
                    c0 = part * 2112
                    kb.dma('sp', Kn[:, c0:c0 + 2112], KnT[h, :, c0:c0 + 2112], writes=["Kn"])
                kb.dma('sp', Vh[:], Vs[:, h * 129:(h + 1) * 129].rearrange("(t p) d -> p t d", p=128), writes=["Vh"])
                for qb in range(4):
                    qn_, qnk = qn_r.next()
                    qr_, qrk = qr_r.next()
                    kb.dma('sp', qn_[:], QnT[h, :, qb * 512:(qb + 1) * 512], writes=[qnk])
                    kb.dma('sp', qr_[:], QrT[h, :, qb * 512:(qb + 1) * 512], writes=[qrk])
                    pend = None
                    for kt in range(67):
                        if kt < 66:
                            ps, pk = pss_r.next()
                            kb.op('pe', lambda e: e.matmul(ps[:], lhsT=Kn[:, kt * 128:(kt + 1) * 128], rhs=qn_[:], start=True, stop=False),
                                  reads=["Kn", qnk], writes=[pk], inc=False)
                            kb.op('pe', lambda e: e.matmul(ps[:], lhsT=Kr[:, kt * 128:(kt + 1) * 128], rhs=qr_[:], start=False, stop=True),
                                  reads=["Kr", qrk], writes=[pk])
                            pt_, ptk = pT_r.next()
                            kb.op('act', lambda e: e.activation(out=pt_[:], in_=ps[:], func=AF.Exp), reads=[pk], writes=[ptk])
                            cur = (pt_, ptk, kt)
                        else:
                            cur = None
                        if pend is not None:
                            ppt, pptk, pkt = pend
                            for qi in range(4):
                                kb.op('pe', lambda e, qi=qi: e.matmul(pso[qi][:, 0:129], lhsT=ppt[:, qi * 128:(qi + 1) * 128], rhs=Vh[:, pkt, :],
                                                                      start=(pkt == 0), stop=(pkt == 65)),
                                      reads=[pptk, "Vh"], writes=[("pso", qi)], inc=(qi == 3))
                        pend = cur
                    ytt, ytk = yt_r.next()
                    for qi in range(4):
                        yv, yvk = yv_r.next()
                        kb.op('dve', lambda e: e.reciprocal(out=yv[:, 129:130], in_=pso[qi][:, 128:129]), reads=[("pso", qi)], writes=[yvk])
                        kb.op('dve', lambda e: e.tensor_scalar(out=yv[:, 0:128], in0=pso[qi][:, 0:128], scalar1=yv[:, 129:130], scalar2=None, op0=ALU.mult),
                              reads=[("pso", qi), yvk], writes=[yvk])
                        ptr, ptrk = ptr_r.next()
                        kb.op('pe', lambda e: e.transpose(out=ptr[:], in_=yv[:, 0:128], identity=idt[:]), reads=[yvk, "idt"], writes=[ptrk])
                        kb.op('act', lambda e: e.copy(out=ytt[:, qi * 128:(qi + 1) * 128], in_=ptr[:]), reads=[ptrk], writes=[ytk])
                    kb.dma('sp', yT[512 + h * 128:512 + (h + 1) * 128, qb * 512:(qb + 1) * 512], ytt[:], reads=[ytk])
            kb.barrier()

        with ExitStack() as ph, Stage('4') as go:
          if go:
            sbp = lambda name, shape: ph.enter_context(nc.sbuf_tensor(name, shape, F32))
            Wo = sbp("Wo", [128, 8, D])
            kb.dma('sp', Wo[:], w_out.rearrange("(k p) n -> p k n", p=128), writes=["Wo"])
            Wr = sbp("Wr", [128, 8, NE])
            kb.dma('sp', Wr[:], router_w.rearrange("(k p) n -> p k n", p=128), writes=["Wr"])
            rbb = sbp("rbb", [128, NE])
            kb.dma('sp', rbb[:], router_b.partition_broadcast(128), writes=["rbb"])
            junk = sbp("junk4", [128, 1024])
            yt_r = Rot(nc, ph, "yt4", [128, 8, 128], 2)
            xo_r = Rot(nc, ph, "xo4", [128, 1024], 2)
            x1_r = Rot(nc, ph, "x14", [128, 1024], 2)
            xn_r = Rot(nc, ph, "xn4", [128, 1024], 2)
            st_r = Rot(nc, ph, "st4", [128, 4], 3)
            h2_r = Rot(nc, ph, "h24", [128, 8, 128], 2)
            lg_r = Rot(nc, ph, "lg4", [128, 2 * NE], 2)
            mx_r = Rot(nc, ph, "mx4", [128, 16], 2)
            psA = Rot(nc, ph, "psA", [128, 512], 2, psum=True)
            psT = Rot(nc, ph, "psT", [128, 8, 128], 1, psum=True)
            psL = Rot(nc, ph, "psL", [128, NE], 1, psum=True)
            H2T = dscr("H2T", [D, QT])
            for i in range(QT // 128):
                yt, ytk = yt_r.next()
                kb.dma('sp', yt[:], yT[:, i * 128:(i + 1) * 128].rearrange("(k p) n -> p k n", p=128), writes=[ytk])
                xt, xk = xo_r.next()
                kb.dma('sp', xt[:], xo[i * 128:(i + 1) * 128, :], writes=[xk])
                x1, x1k = x1_r.next()
                for hf in range(2):
                    ps, pk = psA.next()
                    for k in range(8):
                        kb.op('pe', lambda e, k=k: e.matmul(ps[:], lhsT=yt[:, k, :], rhs=Wo[:, k, hf * 512:(hf + 1) * 512], start=(k == 0), stop=(k == 7)),
                              reads=[ytk, "Wo"], writes=[pk], inc=(k == 7))
                    kb.op('dve', lambda e: e.tensor_tensor(out=x1[:, hf * 512:(hf + 1) * 512], in0=ps[:], in1=GT[:, 0, hf * 512:(hf + 1) * 512], op=ALU.mult),
                          reads=[pk, "GT"], writes=[x1k])
                kb.op('pool', lambda e: e.tensor_tensor(out=x1[:], in0=x1[:], in1=xt[:], op=ALU.add), reads=[x1k, xk], writes=[x1k])
                kb.dma('sp', X1[i * 128:(i + 1) * 128, :], x1[:], reads=[x1k])
                stt, sk = st_r.next()
                rms_rstd(x1[:], x1k, stt, sk, junk, D)
                xn, nk = xn_r.next()
                kb.op('dve', lambda e: e.tensor_scalar(out=xn[:], in0=x1[:], scalar1=stt[:, 3:4], scalar2=None, op0=ALU.mult), reads=[x1k, sk], writes=[nk])
                pt, ptk = psT.next()
                for k in range(8):
                    kb.op('pe', lambda e, k=k: e.transpose(out=pt[:, k, :], in_=xn[:, k * 128:(k + 1) * 128], identity=idt[:]),
                          reads=[nk, "idt"], writes=[ptk], inc=(k == 7))
                h2, h2k = h2_r.next()
                for k in range(8):
                    if k % 2 == 0:
                        kb.op('dve', lambda e, k=k: e.tensor_scalar(out=h2[:, k, :], in0=pt[:, k, :], scalar1=G2[:, k, 0:1], scalar2=modfm[:, 2, k, 0:1],
                                                                     op0=ALU.mult, op1=ALU.add), reads=[ptk, "G2", "modfm"], writes=[h2k])
                    else:
                        kb.op('act', lambda e, k=k: e.activation(out=h2[:, k, :], in_=pt[:, k, :], func=AF.Identity, scale=G2[:, k, 0:1],
                                                                  bias=modfm[:, 2, k, 0:1]), reads=[ptk, "G2", "modfm"], writes=[h2k])
                kb.dma('sp', H2T[:, i * 128:(i + 1) * 128].rearrange("(k p) n -> p k n", p=128), h2[:], reads=[h2k])
                pl, plk = psL.next()
                for k in range(8):
                    kb.op('pe', lambda e, k=k: e.matmul(pl[:], lhsT=h2[:, k, :], rhs=Wr[:, k, :], start=(k == 0), stop=(k == 7)),
                          reads=[h2k, "Wr"], writes=[plk], inc=(k == 7))
                lg, lgk = lg_r.next()
                mx, mxk = mx_r.next()
                kb.op('dve', lambda e: e.tensor_tensor(out=lg[:, 0:NE], in0=pl[:], in1=rbb[:], op=ALU.add), reads=[plk, "rbb"], writes=[lgk])
                kb.op('dve', lambda e: e.max(out=mx[:, 0:8], in_=lg[:, 0:NE]), reads=[lgk], writes=[mxk])
                kb.op('dve', lambda e: e.tensor_scalar(out=mx[:, 8:9], in0=mx[:, 0:1], scalar1=-1.0, scalar2=None, op0=ALU.mult), reads=[mxk], writes=[mxk])
                kb.op('dve', lambda e: e.tensor_scalar(out=lg[:, NE:2 * NE], in0=lg[:, 0:NE], scalar1=mx[:, 3:4], scalar2=None, op0=ALU.is_ge),
                      reads=[lgk, mxk], writes=[lgk])
                kb.op('act', lambda e: e.activation(out=lg[:, 0:NE], in_=lg[:, 0:NE], func=AF.Exp, bias=mx[:, 8:9], scale=1.0), reads=[lgk, mxk], writes=[lgk])
                kb.op('dve', lambda e: e.tensor_tensor(out=lg[:, 0:NE], in0=lg[:, 0:NE], in1=lg[:, NE:2 * NE], op=ALU.mult), reads=[lgk], writes=[lgk])
                kb.op('dve', lambda e: e.reduce_sum(out=mx[:, 9:10], in_=lg[:, 0:NE], axis=AX.X), reads=[lgk], writes=[mxk])
                kb.op('dve', lambda e: e.reciprocal(out=mx[:, 10:11], in_=mx[:, 9:10]), reads=[mxk], writes=[mxk])
                kb.op('dve', lambda e: e.tensor_scalar(out=lg[:, 0:NE], in0=lg[:, 0:NE], scalar1=mx[:, 10:11], scalar2=None, op0=ALU.mult), reads=[lgk, mxk], writes=[lgk])
                kb.dma('sp', LG[i * 128:(i + 1) * 128, :], lg[:], reads=[lgk])
            kb.barrier()

        with ExitStack() as ph, Stage('5') as go:
          if go:
            sbp = lambda name, shape: ph.enter_context(nc.sbuf_tensor(name, shape, F32))
            HT = 512
            h2T = sbp("h2T", [128, 8, HT])
            acc = sbp("acc", [128, 4, D])
            gts = sbp("gts", [128, 4, NE])
            gT = sbp("gT", [NE, 4, 128])
            b1t = sbp("b1t", [128, NE * 16])
            kb.dma('sp', b1t[:], b1fm, writes=["b1t"])
            b2t = sbp("b2t", [NE, D])
            kb.dma('sp', b2t[:], b2, writes=["b2t"])
            wp_r = Rot(nc, ph, "wp", [128, 8, 512], 2)
            w2_r = Rot(nc, ph, "w2p", [128, 4, D], 2)
            actT = sbp("actT", [128, 8, 512])
            ga_r = Rot(nc, ph, "ga", [128, 512], 3)
            sg_r = Rot(nc, ph, "sgm", [128, 512], 3)
            li_r = Rot(nc, ph, "li", [128, 512], 3)
            psU = Rot(nc, ph, "psU", [128, 512], 4, psum=True)
            psY = Rot(nc, ph, "psY", [128, 512], 3, psum=True)
            psG = Rot(nc, ph, "psG", [NE, 128], 1, psum=True)
            for half in range(QT // HT):
                t0 = half * HT
                kb.dma('sp', h2T[:], H2T[:, t0:t0 + HT].rearrange("(k p) n -> p k n", p=128), writes=["h2T"])
                for i in range(4):
                    kb.dma('sp', gts[:, i, :], LG[t0 + i * 128:t0 + (i + 1) * 128, 0:NE], writes=["gts"])
                for i in range(4):
                    pg, pgk = psG.next()
                    kb.op('pe', lambda e: e.transpose(out=pg[:], in_=gts[:, i, :], identity=idt[:]), reads=["gts", "idt"], writes=[pgk])
                    kb.op('act', lambda e: e.copy(out=gT[:, i, :], in_=pg[:]), reads=[pgk], writes=["gT"])
                for i in range(4):
                    for hf in range(2):
                        ps, pk = psY.next()
                        kb.op('pe', lambda e: e.matmul(ps[:], lhsT=gT[:, i, :], rhs=b2t[:, hf * 512:(hf + 1) * 512], start=True, stop=True),
                              reads=["gT", "b2t"], writes=[pk])
                        kb.op('dve', lambda e: e.tensor_copy(out=acc[:, i, hf * 512:(hf + 1) * 512], in_=ps[:]), reads=[pk], writes=[("acc", i)])
                for ex in range(NE):
                    w2p = []
                    for j2 in range(2):
                        wt, wk = w2_r.next()
                        kb.dma('sp', wt[:], w2[ex, j2 * 512:(j2 + 1) * 512, :].rearrange("(k p) n -> p k n", p=128), writes=[wk])
                        w2p.append((wt, wk))
                    for tb in range(1):
                        for pc in range(4):
                            wt, wk = wp_r.next()
                            kb.dma('sp', wt[:, :, 0:256], w1[ex, :, pc * 256:(pc + 1) * 256].rearrange("(k p) n -> p k n", p=128), writes=[wk])
                            kb.dma('sp', wt[:, :, 256:512], w1[ex, :, D + pc * 256:D + (pc + 1) * 256].rearrange("(k p) n -> p k n", p=128), writes=[wk])
                            for jj in range(2):
                                j = pc * 2 + jj
                                pgl, pglk = psU.next()
                                pli, plik = psU.next()
                                for k in range(8):
                                    kb.op('pe', lambda e, k=k: e.matmul(pgl[:], lhsT=wt[:, k, jj * 128:(jj + 1) * 128], rhs=h2T[:, k, tb * 512:(tb + 1) * 512],
                                                                        start=(k == 0), stop=(k == 7)), reads=[wk, "h2T"], writes=[pglk], inc=(k == 7))
                                for k in range(8):
                                    kb.op('pe', lambda e, k=k: e.matmul(pli[:], lhsT=wt[:, k, 256 + jj * 128:256 + (jj + 1) * 128], rhs=h2T[:, k, tb * 512:(tb + 1) * 512],
                                                                        start=(k == 0), stop=(k == 7)), reads=[wk, "h2T"], writes=[plik], inc=(k == 7))
                                bg = b1t[:, ex * 16 + j:ex * 16 + j + 1]
                                bl = b1t[:, ex * 16 + 8 + j:ex * 16 + 8 + j + 1]
                                ga, gak = ga_r.next()
                                kb.op('dve', lambda e: e.tensor_scalar(out=ga[:], in0=pgl[:], scalar1=bg, scalar2=7.0, op0=ALU.add, op1=ALU.min),
                                      reads=[pglk, "b1t"], writes=[gak])
                                sg, sgk = sg_r.next()
                                kb.op('act', lambda e: e.activation(out=sg[:], in_=ga[:], func=AF.Sigmoid, scale=1.702), reads=[gak], writes=[sgk])
                                li, lik = li_r.next()
                                kb.op('dve', lambda e: e.tensor_scalar(out=li[:], in0=pli[:], scalar1=bl, scalar2=7.0, op0=ALU.add, op1=ALU.min),
                                      reads=[plik, "b1t"], writes=[lik])
                                kb.op('pool', lambda e: e.tensor_scalar(out=li[:], in0=li[:], scalar1=-7.0, scalar2=1.0, op0=ALU.max, op1=ALU.add),
                                      reads=[lik], writes=[lik])
                                kb.op('pool', lambda e: e.tensor_tensor(out=ga[:], in0=ga[:], in1=sg[:], op=ALU.mult), reads=[gak, sgk], writes=[gak])
                                kb.op('pool', lambda e, j=j: e.tensor_tensor(out=actT[:, j, :], in0=ga[:], in1=li[:], op=ALU.mult), reads=[gak, lik], writes=[("actT", j)])
                        for ti in range(4):
                            i = tb * 4 + ti
                            for hf in range(2):
                                ps, pk = psY.next()
                                for j in range(8):
                                    wt2, wk2 = w2p[j // 4]
                                    kb.op('pe', lambda e, j=j: e.matmul(ps[:], lhsT=actT[:, j, ti * 128:(ti + 1) * 128], rhs=wt2[:, j % 4, hf * 512:(hf + 1) * 512],
                                                                        start=(j == 0), stop=(j == 7)), reads=[("actT", j), wk2], writes=[pk], inc=(j == 7))
                                kb.op('dve', lambda e: e.scalar_tensor_tensor(out=acc[:, i, hf * 512:(hf + 1) * 512], in0=ps[:], scalar=gts[:, i, ex:ex + 1],
                                                                               in1=acc[:, i, hf * 512:(hf + 1) * 512], op0=ALU.mult, op1=ALU.add),
                                      reads=[pk, "gts", ("acc", i)], writes=[("acc", i)])
                for i in range(4):
                    kb.dma('sp', FF[t0 + i * 128:t0 + (i + 1) * 128, :], acc[:, i, :], reads=[("acc", i)])
            kb.barrier()

        with ExitStack() as ph, Stage('6') as go:
          if go:
            sbp = lambda name, shape: ph.enter_context(nc.sbuf_tensor(name, shape, F32))
            gfb = sbp("gfb", [128, D])
            kb.dma('sp', gfb[:], gfin.partition_broadcast(128), writes=["gfb"])
            junk = sbp("junk6", [128, 1024])
            x1_r = Rot(nc, ph, "x16", [128, 1024], 2)
            ff_r = Rot(nc, ph, "ff6", [128, 1024], 2)
            st_r = Rot(nc, ph, "st6", [128, 4], 3)
            o_r = Rot(nc, ph, "o6", [128, 1024], 2)
            for i in range(QT // 128):
                x1, x1k = x1_r.next()
                ff, ffk = ff_r.next()
                kb.dma('sp', x1[:], X1[i * 128:(i + 1) * 128, :], writes=[x1k])
                kb.dma('sp', ff[:], FF[i * 128:(i + 1) * 128, :], writes=[ffk])
                kb.op('dve', lambda e: e.tensor_tensor(out=ff[:], in0=ff[:], in1=GT[:, 1, :], op=ALU.mult), reads=[ffk, "GT"], writes=[ffk])
                kb.op('pool', lambda e: e.tensor_tensor(out=x1[:], in0=x1[:], in1=ff[:], op=ALU.add), reads=[ffk, x1k], writes=[x1k])
                stt, sk = st_r.next()
                rms_rstd(x1[:], x1k, stt, sk, junk, D)
                o, ok = o_r.next()
                kb.op('dve', lambda e: e.scalar_tensor_tensor(out=o[:], in0=x1[:], scalar=stt[:, 3:4], in1=gfb[:], op0=ALU.mult, op1=ALU.mult),
                      reads=[x1k, sk, "gfb"], writes=[ok])
                kb.dma('sp', out[i * 128:(i + 1) * 128, :], o[:], reads=[ok])
            kb.barrier()
        print("instructions", kb.nins, "dmas", kb.ndma, "cnt", kb.cnt)
    return nc


def rope_tables(tok):
    fr = (10000.0 ** (-np.arange(16, dtype=np.float32) / 16)).astype(np.float32)
    pos = [(tok // 64).astype(np.float32), (tok % 64).astype(np.float32)]
    C = np.zeros((64, len(tok)), np.float32)
    S = np.zeros((64, len(tok)), np.float32)
    for a in range(2):
        ang = (pos[a][None, :] * fr[:, None]).astype(np.float32)
        for hf in range(2):
            r0 = a * 32 + hf * 16
            C[r0:r0 + 16] = np.cos(ang)
            S[r0:r0 + 16] = np.sin(ang) * (-1.0 if hf == 0 else 1.0)
    return C, S


def make_inputs(inp, core):
    b, q = core // 4, core % 4
    f = lambda a: np.ascontiguousarray(a, dtype=np.float32)
    w = inp["w_in"][0]
    perm = np.arange(64).reshape(2, 2, 16)[:, ::-1, :].reshape(64)
    kr = w[:, 1792 + 384:1792 + 448]
    wcat = np.concatenate([w[:, :1792], w[:, 1792 + 256:1792 + 384], kr, kr[:, perm], w[:, 1792:1792 + 256]], axis=1)
    cv = np.stack([inp["c"][b].reshape(8, 128).T, inp["c_ctx"].reshape(8, 128).T], axis=2).reshape(128, 16)
    xb = inp["x"][b]
    xo = np.zeros((XO_ROWS, D), np.float32)
    xo[:QT] = xb[q * QT:(q + 1) * QT]
    if q > 0:
        xo[QT] = xb[q * QT - 1]
    if q < 3:
        xo[QT + 1] = xb[(q + 1) * QT]
    C, S = rope_tables(np.arange(T))
    wukv = inp["mla_w_ukv"][0].reshape(128, 4, 256)
    wuq = inp["mla_w_uq"][0].reshape(256, 4, 192)
    wuq_p = np.concatenate([wuq[:, :, :128], wuq[:, :, 128:], wuq[:, :, 128:][:, :, perm]], axis=2).reshape(256, 1024)
    w1 = inp["exp_w1"][0]
    w1d = np.concatenate([w1[:, :, 0::2], w1[:, :, 1::2]], axis=2)
    b1 = inp["exp_b1"][0]
    b1d = np.concatenate([b1[:, 0::2], b1[:, 1::2]], axis=1)
    b1fm = b1d.reshape(NE, 16, 128).transpose(2, 0, 1).reshape(128, NE * 16)
    d = {
        "xs": f(np.concatenate([inp["ctx"][b], xb], axis=0)),
        "xo": f(xo),
        "cvec": f(cv),
        "mod_w": f(inp["mod_w"][0]),
        "mod_b": f(inp["mod_b"][0]),
        "g1": f(inp["norm1_g"][0]),
        "g2n": f(inp["norm2_g"][0]),
        "gfin": f(inp["final_norm_g"]),
        "w_in": f(wcat),
        "ident": np.eye(128, dtype=np.float32),
        "ropeC": f(C), "ropeS": f(S),
        "ropeCo": f(C[:, q * QT:(q + 1) * QT]), "ropeSo": f(S[:, q * QT:(q + 1) * QT]),
        "kvg": f(inp["mla_kv_norm"][0].reshape(128, 1)),
        "qg": f(inp["mla_q_norm"][0].reshape(2, 128).T),
        "wukv_k": f(wukv[:, :, :128].reshape(128, 512)),
        "wukv_v": f(wukv[:, :, 128:].reshape(128, 512)),
        "wuq": f(wuq_p),
        "w_out": f(inp["w_out"][0]),
        "router_w": f(inp["router_w"][0]),
        "router_b": f(inp["router_b"][0]),
        "w1": f(w1d) if "5" in RUN else None,
        "b1fm": f(b1fm),
        "w2": f(inp["exp_w2"][0]) if "5" in RUN else None,
        "b2": f(inp["exp_b2"][0]),
    }
    if '2' in RUN:
        rc = rwkv_consts(q)
        rc.update(mu=inp["rwkv_mu"][0], w0=inp["rwkv_w0"][0], a0=inp["rwkv_a0"][0], w2=inp["rwkv_w2"][0], a2=inp["rwkv_a2"][0],
                  k_k=inp["rwkv_k_k"][0], k_a=inp["rwkv_k_a"][0], r_k=inp["rwkv_r_k"][0].reshape(512), ln_w=inp["rwkv_ln_w"][0],
                  ln_b=inp["rwkv_ln_b"][0], g2=inp["rwkv_g2"][0])
        for k_, v_ in rc.items():
            d["rw_" + k_] = f(v_)
    return {k: v for k, v in d.items() if v is not None}


def kernel(**inputs):
    inp = {k: np.asarray(v) for k, v in inputs.items()}
    nc = build()
    in_maps = [make_inputs(inp, c) for c in range(8)]
    if DEBUG:
        for c in range(8):
            if '2' not in RUN:
                in_maps[c]["yrw_in"] = kernel.dbg_yrw[c]
    res = run_bass_kernel_spmd(nc, in_maps, core_ids=list(range(8)))
    outp = np.zeros((2, T, D), np.float32)
    for c in range(8):
        b, q = c // 4, c % 4
        outp[b, q * QT:(q + 1) * QT] = res.results[c]["out"]
    if DEBUG:
        kernel.debug = res.results
    return outp
```

```python
import os
import numpy as np
from contextlib import ExitStack
import concourse.bass as bass
import concourse.mybir as mybir
from concourse.bass_utils import run_bass_kernel_spmd

F32 = mybir.dt.float32
AF = mybir.ActivationFunctionType
ALU = mybir.AluOpType
AX = mybir.AxisListType

D = 1024
T = 8192
CTX = 256
TS = CTX + T
QT = 2048
NBLK_FULL = [(0, 256)] + [(256 + i * 512, 512) for i in range(16)]
WCOLS = 2304
DEBUG = os.environ.get("KDEBUG", "")


class KB:
    NRING = 12

    def __init__(self, nc, stack, same_engine_sync=True):
        self.nc = nc
        self.stack = stack
        self.E = {'pe': nc.tensor, 'dve': nc.vector, 'act': nc.scalar, 'pool': nc.gpsimd, 'sp': nc.sync}
        self.sem = {e: stack.enter_context(nc.semaphore("s_" + e)) for e in ('pe', 'dve', 'act', 'pool')}
        self.cnt = {e: 0 for e in self.sem}
        self.ring = [stack.enter_context(nc.semaphore("d%d" % i)) for i in range(self.NRING)]
        self.ndma = 0
        self.seen = {e: {} for e in self.E}
        self.res = {}
        self.pend = {e: ([], []) for e in self.E}
        self.ses = same_engine_sync
        self.nins = 0
        for s_ in list(self.sem.values()) + self.ring:
            nc.gpsimd.sem_clear(s_)
        nc.all_engine_barrier()

    def _wait(self, eng, sem, val):
        k = sem.name
        if self.seen[eng].get(k, 0) >= val:
            return
        self.E[eng].wait_ge(sem, val)
        self.seen[eng][k] = val

    def _deps(self, eng, reads, writes):
        deps = []
        for r in reads:
            st = self.res.get(r)
            if st and st[0]:
                deps.append(st[0])
        for w in writes:
            st = self.res.get(w)
            if st:
                if st[0]:
                    deps.append(st[0])
                deps.extend(st[1].values())
        own = self.sem.get(eng)
        for (sem, val) in deps:
            if own is not None and sem.name == own.name and (eng == 'pe' or not self.ses):
                continue
            self._wait(eng, sem, val)

    def _record(self, tok, reads, writes):
        for r in reads:
            st = self.res.setdefault(r, [None, {}])
            old = st[1].get(tok[0].name)
            if old is None or old[1] < tok[1]:
                st[1][tok[0].name] = tok
        for w in writes:
            self.res[w] = [tok, {}]

    def op(self, eng, fn, reads=(), writes=(), inc=True):
        self._deps(eng, reads, writes)
        ins = fn(self.E[eng])
        self.nins += 1
        pr, pw = self.pend[eng]
        pr.extend(reads)
        pw.extend(writes)
        if inc:
            self.cnt[eng] += 1
            ins.then_inc(self.sem[eng], 1)
            self._record((self.sem[eng], self.cnt[eng]), pr, pw)
            self.pend[eng] = ([], [])
        return ins

    def dma(self, q, out, in_, reads=(), writes=(), **kw):
        i = self.ndma
        self.ndma += 1
        sem = self.ring[i % self.NRING]
        val = 16 * (i // self.NRING + 1)
        if val > 16:
            self._wait(q, sem, val - 16)
        self._deps(q, reads, writes)
        ins = self.E[q].dma_start(out=out, in_=in_, **kw).then_inc(sem, 16)
        self.nins += 1
        self._record((sem, val), list(reads), list(writes))
        return ins

    def barrier(self, engines=('pe', 'dve', 'act', 'pool', 'sp')):
        for e in engines:
            for o, s in self.sem.items():
                if o != e and self.cnt[o] > 0:
                    self._wait(e, s, self.cnt[o])
            for j, s in enumerate(self.ring):
                n = (self.ndma - 1 - j) // self.NRING + 1 if self.ndma > j else 0
                if n > 0:
                    self._wait(e, s, 16 * n)


class Rot:
    def __init__(self, nc, st, name, shape, n, dtype=F32, psum=False):
        mk = nc.psum_tensor if psum else nc.sbuf_tensor
        self.t = [st.enter_context(mk("%s%d" % (name, i), shape, dtype)) for i in range(n)]
        self.name = name
        self.i = 0

    def next(self):
        j = self.i % len(self.t)
        self.i += 1
        return self.t[j], (self.name, j)


KRW_BLKS = int(os.environ.get('KRW_BLKS', '99'))
KRW_BLK0 = int(os.environ.get('KRW_BLK0', '0'))
KRW_BSTOP = int(os.environ.get('KRW_BSTOP', '99'))
KRW_NOB = int(os.environ.get('KRW_NOB', '0'))
KRW_NOCHAIN = int(os.environ.get('KRW_NOCHAIN', '0'))
KRW_MARK = int(os.environ.get('KRW_MARK', '99'))


class StopRegion(Exception):
    pass


def mark(i):
    if i >= KRW_MARK:
        raise StopRegion()


CDEC = 0.6065306597126334
GN_EPS = 64e-5
FULL_BLKS = [(0, 256, True, True)] + [(256 + i * 512, 512, i == 0, i == 15) for i in range(16)]
OWN_RBLKS = [(i * 512, 512, i == 0, i == 3) for i in range(4)]
NCH_FULL = TS // 64
NCH_OWN = QT // 64


def rwkv_stage(nc, kb, st, pT, poT, yT, C, dscr):
    idt = C["idt"]
    GTs = dscr("GTs", [2, 4, 128, NCH_FULL, 128])
    Nsc = dscr("Nsc", [2, 4, 128, NCH_FULL, 128])
    GTo = dscr("GTo", [2, 4, 128, NCH_OWN, 128])
    Nso = dscr("Nso", [2, 4, 128, NCH_OWN, 128])
    RhTo = dscr("RhTo", [2, 4, 128, NCH_OWN, 128])
    Oho = dscr("Oho", [2, 4, 128, NCH_OWN, 128])
    BONs = dscr("BONs", [4, 128, QT])
    Gsc = dscr("Gsc", [4, 128, QT])
    if os.environ.get('KRW_ALLOC_ONLY'):
        return
    with ExitStack() as ph:
        sbp = lambda name, shape: ph.enter_context(nc.sbuf_tensor(name, shape, F32))
        def ld(name, shape, src, **kw):
            t = sbp(name, shape)
            kb.dma('sp', t[:], src, writes=[name], **kw)
            return t
        mu_t = ld("mu_t", [128, 14], C["mu"].rearrange("(c p) -> p c", p=128), allow_slow_non_contiguous=True)
        om_t = sbp("om_t", [128, 14]); hm_t = sbp("hm_t", [128, 14])
        kb.op('dve', lambda e: e.tensor_scalar(out=om_t[:], in0=mu_t[:], scalar1=-1.0, scalar2=1.0, op0=ALU.mult, op1=ALU.add), reads=["mu_t"], writes=["om_t"])
        kb.op('dve', lambda e: e.tensor_scalar(out=hm_t[:], in0=mu_t[:], scalar1=0.5, scalar2=None, op0=ALU.mult), reads=["mu_t"], writes=["hm_t"])
        w0_t = ld("w0_t", [128, 2, 4], C["w0"].rearrange("d (c p) -> p d c", p=128), allow_slow_non_contiguous=True)
        a0_t = ld("a0_t", [128, 2, 4], C["a0"].rearrange("d (c p) -> p d c", p=128), allow_slow_non_contiguous=True)
        W2A2 = sbp("W2A2", [128, 2, 512])
        kb.dma('sp', W2A2[0:64], C["w2"].rearrange("d l c -> l d c"), writes=["W2A2"])
        kb.dma('sp', W2A2[64:128], C["a2"].rearrange("d l c -> l d c"), writes=["W2A2"])
        kk_t = ld("kk_t", [128, 4], C["k_k"].rearrange("(c p) -> p c", p=128), allow_slow_non_contiguous=True)
        ka_t = ld("ka_t", [128, 4], C["k_a"].rearrange("(c p) -> p c", p=128), allow_slow_non_contiguous=True)
        oka_t = sbp("oka_t", [128, 4])
        kb.op('dve', lambda e: e.tensor_scalar(out=oka_t[:], in0=ka_t[:], scalar1=-1.0, scalar2=1.0, op0=ALU.mult, op1=ALU.add), reads=["ka_t"], writes=["oka_t"])
        rk_t = ld("rk_t", [128, 4], C["r_k"].rearrange("(c p) -> p c", p=128), allow_slow_non_contiguous=True)
        lnw_t = ld("lnw_t", [128, 4], C["ln_w"].rearrange("(c p) -> p c", p=128), allow_slow_non_contiguous=True)
        lnb_t = ld("lnb_t", [128, 4], C["ln_b"].rearrange("(c p) -> p c", p=128), allow_slow_non_contiguous=True)
        g2_t = ld("g2_t", [128, 512], C["g2"])
        MASK4 = ld("MASK4", [128, 2, 512], C["mask4"].rearrange("d p n -> p d n"))
        MASKL = ld("MASKL", [128, 2, 128], C["maskl"].rearrange("d p n -> p d n"))
        BLK = ld("BLK", [128, 128], C["blk"])
        UU = ld("UU", [128, 64], C["uu"])
        SEL = ld("SEL", [128, 64], C["sel"])
        selF = ld("selF", [128, 4], C["selF"])
        selB = ld("selB", [128, 4], C["selB"])
        hal = ld("hal", [128, 2], C["hal"])
        if os.environ.get('KRW_STOP') == '1':
            kb.barrier()
            return
        P_r = Rot(nc, ph, "Pl", [128, 514], 3)
        sh_r = Rot(nc, ph, "shf", [128, 512], 4)
        RS_r = Rot(nc, ph, "RSs", [128, 512], 2)
        KS_r = Rot(nc, ph, "KSs", [128, 512], 2)
        VS_r = Rot(nc, ph, "VSs", [128, 512], 2)
        KK_r = Rot(nc, ph, "KKs", [128, 512], 2)
        X12_r = Rot(nc, ph, "X12", [128, 512], 2)
        TX_r = Rot(nc, ph, "TXs", [128, 512], 2)
        dA_r = Rot(nc, ph, "dA", [128, 512], 14)
        bs_r = Rot(nc, ph, "bsr", [128, 512], 2)
        pl_r = Rot(nc, ph, "plr", [128, 8], 4)
        ex_r = {nm: Rot(nc, ph, "ex" + nm, [128, 8, 2, 64], 2) for nm in ("A", "R", "B", "K", "Bp", "Kp")}
        exV_r = Rot(nc, ph, "exV", [128, 8, 2, 64], 2)
        for r_ in list(ex_r.values()) + [exV_r]:
            for j_, t_ in enumerate(r_.t):
                kb.op('pool', lambda e, t_=t_: e.memset(t_[:], 0.0), writes=[(r_.name, j_)])
        psA = Rot(nc, ph, "psRA", [128, 512], 3, psum=True)
        psB = Rot(nc, ph, "psRB", [128, 512], 5, psum=True)
        AT4_r = Rot(nc, ph, "AT4", [128, 4, 128], 2)
        AB_r = Rot(nc, ph, "ABk", [128, 2, 128], 4)
        XT_r = Rot(nc, ph, "XTk", [128, 128], 3)
        TM_r = Rot(nc, ph, "TMk", [128, 4, 128], 2)
        AU_r = Rot(nc, ph, "AUk", [128, 2, 128], 2)
        stG_r = Rot(nc, ph, "stG", [128, 8, 128], 1)
        stN_r = Rot(nc, ph, "stN", [128, 8, 128], 1)
        stR_r = Rot(nc, ph, "stR", [128, 8, 128], 1)
        stO_r = Rot(nc, ph, "stO", [128, 8, 128], 1)
        print('RWKV region sbuf remaining', nc.sbuf_bytes_remaining, nc.SBUF_PARTITION_SIZE_BYTES)
        cnt = {"ev": 0}

        def evac(out_ap, in_ap, reads, writes):
            cnt["ev"] += 1
            if True:
                kb.op('dve', lambda e: e.tensor_copy(out=out_ap, in_=in_ap), reads=reads, writes=writes)
            else:
                kb.op('act', lambda e: e.copy(out=out_ap, in_=in_ap), reads=reads, writes=writes)

        def load_shift(src, row0, c0, n, lb, rb, own, dst, dk, cc):
            P, pk = P_r.next()
            lo = c0 - (0 if lb else 1)
            hi = c0 + n + (0 if rb else 1)
            doff = 1 if lb else 0
            kb.dma('sp', P[:, doff:doff + (hi - lo)], src[row0:row0 + 128, lo:hi], writes=[pk])
            if lb:
                if own:
                    kb.dma('sp', P[:, 0:1], src[row0:row0 + 128, QT:QT + 1], writes=[pk], allow_slow_non_contiguous=True)
                    kb.op('dve', lambda e: e.tensor_scalar(out=P[:, 0:1], in0=P[:, 0:1], scalar1=hal[:, 0:1], scalar2=None, op0=ALU.mult), reads=[pk, "hal"], writes=[pk])
                else:
                    kb.op('pool', lambda e: e.memset(P[:, 0:1], 0.0), writes=[pk])
            if rb:
                if own:
                    kb.dma('sp', P[:, n + 1:n + 2], src[row0:row0 + 128, QT + 1:QT + 2], writes=[pk], allow_slow_non_contiguous=True)
                    kb.op('dve', lambda e: e.tensor_scalar(out=P[:, n + 1:n + 2], in0=P[:, n + 1:n + 2], scalar1=hal[:, 1:2], scalar2=None, op0=ALU.mult), reads=[pk, "hal"], writes=[pk])
                else:
                    kb.op('pool', lambda e: e.memset(P[:, n + 1:n + 2], 0.0), writes=[pk])
            t, tk = sh_r.next()
            kb.op('pool', lambda e: e.tensor_tensor(out=t[:, :n], in0=P[:, 0:n], in1=P[:, 2:n + 2], op=ALU.add), reads=[pk], writes=[tk])
            u, uk = sh_r.next()
            kb.op('dve', lambda e: e.tensor_scalar(out=u[:, :n], in0=P[:, 1:n + 1], scalar1=om_t[:, cc:cc + 1], scalar2=None, op0=ALU.mult), reads=[pk, "om_t"], writes=[uk])
            kb.op('dve', lambda e: e.scalar_tensor_tensor(out=dst[:, :n], in0=t[:, :n], scalar=hm_t[:, cc:cc + 1], in1=u[:, :n], op0=ALU.mult, op1=ALU.add),
                  reads=[tk, uk, "hm_t"], writes=[dk])

        def c3(ap, n):
            return ap.rearrange("p (c t) -> p c t", t=64)

        def exp_write(eng, dst, dk, nch, n, fn, reads):
            for hh in range(2):
                sl = slice(hh * 64, hh * 64 + 64)
                kb.op(eng, lambda e, hh=hh, sl=sl: fn(e, dst[sl, :nch, hh, :], sl), reads=reads, writes=[dk])

        def region(src, blocks, own, GTd, Nd, RhTd, Ohd, chunk0_of_block):
            for bi, (c0, n, lb, rb) in list(enumerate(blocks))[KRW_BLK0:KRW_BLK0 + KRW_BLKS]:
                nch = n // 64
                ch0 = chunk0_of_block(bi)
                X12, xk = X12_r.next()
                load_shift(src, 1536, c0, n, lb, rb, own, X12, xk, 12)
                TX, txk = TX_r.next()
                kb.op('act', lambda e: e.activation(out=TX[0:64, :n], in_=X12[0:64, :n], func=AF.Tanh), reads=[xk], writes=[txk])
                mark(1)
                if own:
                    XG, xgk = dA_r.next()
                    load_shift(src, 1664, c0, n, lb, rb, own, XG, xgk, 13)
                    SGg, sggk = TX_r.next()
                    kb.op('act', lambda e: e.activation(out=SGg[:, :n], in_=XG[:, :n], func=AF.Sigmoid), reads=[xgk], writes=[sggk])
                for hp in range(4):
                    RS, rsk = RS_r.next(); KS, ksk = KS_r.next(); VS, vsk = VS_r.next(); KK, kkk = KK_r.next()
                    load_shift(src, hp * 128, c0, n, lb, rb, own, RS, rsk, hp)
                    load_shift(src, 512 + hp * 128, c0, n, lb, rb, own, KS, ksk, 4 + hp)
                    load_shift(src, 1024 + hp * 128, c0, n, lb, rb, own, VS, vsk, 8 + hp)
                    mark(2)
                    kkr, kkrk = dA_r.next()
                    kb.op('dve', lambda e: e.tensor_scalar(out=kkr[:, :n], in0=KS[:, :n], scalar1=kk_t[:, hp:hp + 1], scalar2=None, op0=ALU.mult), reads=[ksk, "kk_t"], writes=[kkrk])
                    sq, sqk = dA_r.next()
                    kb.op('pool', lambda e: e.tensor_tensor(out=sq[:, :n], in0=kkr[:, :n], in1=kkr[:, :n], op=ALU.mult), reads=[kkrk], writes=[sqk])
                    pss, pssk = psA.next()
                    kb.op('pe', lambda e: e.matmul(pss[:, :n], lhsT=BLK[:], rhs=sq[:, :n], start=True, stop=True), reads=["BLK", sqk], writes=[pssk])
                    kb.op('dve', lambda e: e.tensor_scalar(out=sq[:, :n], in0=pss[:, :n], scalar1=1e-24, scalar2=None, op0=ALU.max), reads=[pssk], writes=[sqk])
                    kb.op('act', lambda e: e.activation(out=sq[:, :n], in_=sq[:, :n], func=AF.Sqrt), reads=[sqk], writes=[sqk])
                    kb.op('dve', lambda e: e.reciprocal(out=sq[:, :n], in_=sq[:, :n]), reads=[sqk], writes=[sqk])
                    kb.op('pool', lambda e: e.tensor_tensor(out=KK[:, :n], in0=kkr[:, :n], in1=sq[:, :n], op=ALU.mult), reads=[kkrk, sqk], writes=[kkk])
                    mark(3)
                    Vd, vdk = exV_r.next()
                    exp_write('pool', Vd, vdk, nch, n, lambda e, o, sl: e.tensor_copy(out=o, in_=c3(VS[sl, :n], n)), [vsk])
                    mark(4)
                    if own:
                        bsum, bsk = bs_r.next()
                    for d in range(2):
                        psw, pswk = psA.next()
                        kb.op('pe', lambda e: e.matmul(psw[:, :n], lhsT=W2A2[0:64, d, hp * 128:(hp + 1) * 128], rhs=TX[0:64, :n], start=True, stop=True),
                              reads=["W2A2", txk], writes=[pswk])
                        SGM, sgk = dA_r.next()
                        kb.op('act', lambda e: e.activation(out=SGM[:, :n], in_=psw[:, :n], func=AF.Sigmoid, bias=w0_t[:, d, hp:hp + 1], scale=1.0), reads=[pswk, "w0_t"], writes=[sgk])
                        psa, psak = psA.next()
                        kb.op('pe', lambda e: e.matmul(psa[:, :n], lhsT=W2A2[64:128, d, hp * 128:(hp + 1) * 128], rhs=X12[64:128, :n], start=True, stop=True),
                              reads=["W2A2", xk], writes=[psak])
                        AA, aak = dA_r.next()
                        kb.op('act', lambda e: e.activation(out=AA[:, :n], in_=psa[:, :n], func=AF.Sigmoid, bias=a0_t[:, d, hp:hp + 1], scale=1.0), reads=[psak, "a0_t"], writes=[aak])
                        mark(5)
                        CIN, cik = dA_r.next()
                        T1, t1k = sh_r.next()
                        src_, srck_ = SGM, sgk
                        for si, s_ in enumerate((1, 2, 4, 8, 16, 32)):
                            dst_, dstk_ = (T1, t1k) if si % 2 == 0 else (CIN, cik)
                            kb.op('pool', lambda e, s_=s_, src_=src_, dst_=dst_: e.tensor_tensor(out=c3(dst_[:, :n], n)[:, :, s_:], in0=c3(src_[:, :n], n)[:, :, s_:],
                                                                                                in1=c3(src_[:, :n], n)[:, :, :64 - s_], op=ALU.add),
                                  reads=[srck_], writes=[dstk_])
                            kb.op('act', lambda e, s_=s_, src_=src_, dst_=dst_: e.copy(out=c3(dst_[:, :n], n)[:, :, :s_], in_=c3(src_[:, :n], n)[:, :, :s_]),
                                  reads=[srck_], writes=[dstk_])
                            src_, srck_ = dst_, dstk_
                        mark(6)
                        CEX, cek = dA_r.next()
                        kb.op('pool', lambda e: e.tensor_tensor(out=CEX[:, :n], in0=CIN[:, :n], in1=SGM[:, :n], op=ALU.subtract), reads=[cik, sgk], writes=[cek])
                        totb = c3(CIN[:, :n], n)[:, :, 63:64].to_broadcast([128, nch, 64])
                        Dm, dmk = dA_r.next()
                        kb.op('dve', lambda e: e.tensor_tensor(out=c3(Dm[:, :n], n), in0=totb, in1=c3(CIN[:, :n], n), op=ALU.subtract), reads=[cik], writes=[dmk])
                        DX, dxk = dA_r.next()
                        kb.op('dve', lambda e: e.tensor_tensor(out=c3(DX[:, :n], n), in0=totb, in1=c3(CEX[:, :n], n), op=ALU.subtract), reads=[cik, cek], writes=[dxk])
                        srcs = {0: ((CIN, cik, -CDEC), (CEX, cek, -CDEC), (CIN, cik, CDEC), (Dm, dmk, -CDEC)),
                                1: ((DX, dxk, -CDEC), (Dm, dmk, -CDEC), (DX, dxk, CDEC), (CEX, cek, -CDEC))}[d]
                        E4 = []
                        for (s_, sk_, sc_) in srcs:
                            o_, ok_ = dA_r.next()
                            kb.op('act', lambda e, s_=s_, o_=o_, sc_=sc_: e.activation(out=o_[:, :n], in_=s_[:, :n], func=AF.Exp, scale=sc_), reads=[sk_], writes=[ok_])
                            E4.append((o_, ok_))
                        (PIN, pik), (PEX, pek), (INV, ink), (EEND, eek) = E4
                        PL, plk = pl_r.next()
                        kb.op('act', lambda e: e.activation(out=PL[:, :nch], in_=CIN[:, 63:n:64], func=AF.Exp, scale=-CDEC), reads=[cik], writes=[plk])
                        mark(7)
                        KD, kdk = dA_r.next()
                        kb.op('dve', lambda e: e.tensor_scalar(out=KD[:, :n], in0=AA[:, :n], scalar1=ka_t[:, hp:hp + 1], scalar2=oka_t[:, hp:hp + 1], op0=ALU.mult, op1=ALU.add),
                              reads=[aak, "ka_t", "oka_t"], writes=[kdk])
                        kb.op('pool', lambda e: e.tensor_tensor(out=KD[:, :n], in0=KD[:, :n], in1=KS[:, :n], op=ALU.mult), reads=[kdk, ksk], writes=[kdk])
                        Bv, bvk = dA_r.next()
                        kb.op('pool', lambda e: e.tensor_tensor(out=Bv[:, :n], in0=KK[:, :n], in1=AA[:, :n], op=ALU.mult), reads=[kkk, aak], writes=[bvk])
                        if own:
                            if d == 0:
                                kb.op('pool', lambda e: e.tensor_tensor(out=bsum[:, :n], in0=RS[:, :n], in1=KD[:, :n], op=ALU.mult), reads=[rsk, kdk], writes=[bsk])
                            else:
                                t_, tk_ = sh_r.next()
                                kb.op('pool', lambda e: e.tensor_tensor(out=t_[:, :n], in0=RS[:, :n], in1=KD[:, :n], op=ALU.mult), reads=[rsk, kdk], writes=[tk_])
                                kb.op('pool', lambda e: e.tensor_tensor(out=bsum[:, :n], in0=bsum[:, :n], in1=t_[:, :n], op=ALU.add), reads=[bsk, tk_], writes=[bsk])
                        mark(8)
                        ex = {nm: ex_r[nm].next() for nm in ex_r}
                        exp_write('dve', ex["A"][0], ex["A"][1], nch, n,
                                  lambda e, o, sl: e.scalar_tensor_tensor(out=o, in0=c3(KK[sl, :n], n), scalar=-1.0, in1=c3(PEX[sl, :n], n), op0=ALU.mult, op1=ALU.mult), [kkk, pek])
                        exp_write('pool', ex["R"][0], ex["R"][1], nch, n, lambda e, o, sl: e.tensor_tensor(out=o, in0=c3(RS[sl, :n], n), in1=c3(PIN[sl, :n], n), op=ALU.mult), [rsk, pik])
                        exp_write('dve', ex["B"][0], ex["B"][1], nch, n, lambda e, o, sl: e.tensor_tensor(out=o, in0=c3(Bv[sl, :n], n), in1=c3(INV[sl, :n], n), op=ALU.mult), [bvk, ink])
                        exp_write('pool', ex["K"][0], ex["K"][1], nch, n, lambda e, o, sl: e.tensor_tensor(out=o, in0=c3(KD[sl, :n], n), in1=c3(INV[sl, :n], n), op=ALU.mult), [kdk, ink])
                        exp_write('dve', ex["Bp"][0], ex["Bp"][1], nch, n, lambda e, o, sl: e.tensor_tensor(out=o, in0=c3(Bv[sl, :n], n), in1=c3(EEND[sl, :n], n), op=ALU.mult), [bvk, eek])
                        exp_write('pool', ex["Kp"][0], ex["Kp"][1], nch, n, lambda e, o, sl: e.tensor_tensor(out=o, in0=c3(KD[sl, :n], n), in1=c3(EEND[sl, :n], n), op=ALU.mult), [kdk, eek])
                        mark(9)
                        stG, stGk = stG_r.next(); stN, stNk = stN_r.next()
                        if own:
                            stR, stRk = stR_r.next(); stO, stOk = stO_r.next()
                        for ci in range(0 if KRW_NOB else nch):
                            f2 = lambda t_: t_[:, ci].rearrange("p a b -> p (a b)")
                            Ad, Rd, Bd, Kd, Bpd, Kpd, Vdd = f2(ex["A"][0]), f2(ex["R"][0]), f2(ex["B"][0]), f2(ex["K"][0]), f2(ex["Bp"][0]), f2(ex["Kp"][0]), f2(Vd)
                            exk = [ex[nm][1] for nm in ("A", "R", "B", "K")]
                            ps1, ps1k = psB.next()
                            for qi, (l_, r_) in enumerate(((Bd, Ad), (Kd, Ad), (Bd, Rd), (Kd, Rd))):
                                kb.op('pe', lambda e, qi=qi, l_=l_, r_=r_: e.matmul(ps1[:, qi * 128:(qi + 1) * 128], lhsT=l_, rhs=r_, start=True, stop=True),
                                      reads=exk, writes=[ps1k], inc=(qi == 3))
                            AT4, atk = AT4_r.next()
                            kb.op('dve', lambda e: e.tensor_tensor(out=AT4[:].rearrange("p a b -> p (a b)"), in0=ps1[:], in1=MASK4[:, d, :], op=ALU.mult), reads=[ps1k, "MASK4"], writes=[atk])
                            if KRW_BSTOP <= 1:
                                continue
                            ps2, ps2k = psB.next()
                            kb.op('pe', lambda e: e.matmul(ps2[:, 0:128], lhsT=Ad, rhs=Bd, start=True, stop=True), reads=exk, writes=[ps2k])
                            AB, abk = AB_r.next()
                            kb.op('dve', lambda e: e.tensor_tensor(out=AB[:, 0, :], in0=ps2[:, 0:128], in1=MASKL[:, d, :], op=ALU.mult), reads=[ps2k, "MASKL"], writes=[abk])
                            kb.op('pool', lambda e: e.tensor_copy(out=AB[:, 1, :], in_=AT4[:, 0, :]), reads=[atk], writes=[abk])
                            XT, xtk = XT_r.next()
                            kb.op('pool', lambda e: e.tensor_tensor(out=XT[:], in0=AT4[:, 0, :], in1=idt[:], op=ALU.add), reads=[atk, "idt"], writes=[xtk])
                            if KRW_BSTOP <= 2:
                                continue
                            for it in range(5):
                                psk_, pskk_ = psB.next()
                                kb.op('pe', lambda e: e.matmul(psk_[:, 0:128], lhsT=AB[:, 1, :], rhs=AB[:, 0, :], start=True, stop=True), reads=[abk], writes=[pskk_], inc=False)
                                kb.op('pe', lambda e: e.matmul(psk_[:, 128:256], lhsT=AB[:, 0, :], rhs=AB[:, 1, :], start=True, stop=True), reads=[abk], writes=[pskk_])
                                AB2, ab2k = AB_r.next()
                                evac(AB2[:].rearrange("p a b -> p (a b)"), psk_[:, 0:256], [pskk_], [ab2k])
                                psx, psxk = psB.next()
                                kb.op('pe', lambda e: e.matmul(psx[:, 0:128], lhsT=AB2[:, 0, :], rhs=XT[:], start=True, stop=True), reads=[ab2k, xtk], writes=[psxk])
                                XT2, xt2k = XT_r.next()
                                kb.op('dve', lambda e: e.tensor_tensor(out=XT2[:], in0=psx[:, 0:128], in1=XT[:], op=ALU.add), reads=[psxk, xtk], writes=[xt2k])
                                AB, abk, XT, xtk = AB2, ab2k, XT2, xt2k
                            WT, wtk = XT, xtk
                            if KRW_BSTOP <= 3:
                                continue
                            pst, pstk = psB.next()
                            exk2 = [ex["A"][1], ex["Bp"][1], ex["Kp"][1], vdk]
                            for qi, s_ in enumerate((Ad, Bpd, Kpd, Vdd)):
                                kb.op('pe', lambda e, qi=qi, s_=s_: e.matmul(pst[:, qi * 128:(qi + 1) * 128], lhsT=s_, rhs=idt[:], start=True, stop=True), reads=exk2 + ["idt"], writes=[pstk], inc=(qi == 3))
                            if os.environ.get('KRW_X') == 'noevac':
                                continue
                            TM, tmk = TM_r.next()
                            AU, auk = AU_r.next()
                            Vtm, vtk = XT_r.next()
                            evac(TM[:, 0, :], pst[:, 0:128], [pstk], [tmk])
                            if os.environ.get('KRW_X') == 'split':
                                evac(TM[:, 2, :], pst[:, 128:256], [pstk], [tmk])
                                evac(TM[:, 3, :], pst[:, 256:384], [pstk], [tmk])
                            else:
                                evac(TM[:, 2:4, :].rearrange("p a b -> p (a b)"), pst[:, 128:384], [pstk], [tmk])
                            evac(Vtm[:], pst[:, 384:512], [pstk], [vtk])
                            if KRW_BSTOP <= 4:
                                continue
                            psx_, psxk_ = psB.next()
                            kb.op('pe', lambda e: e.matmul(psx_[:, 0:128], lhsT=AT4[:, 1, :], rhs=Vtm[:], start=True, stop=True), reads=[atk, vtk], writes=[psxk_])
                            evac(TM[:, 1, :], psx_[:, 0:128], [psxk_], [tmk])
                            psau, psauk = psB.next()
                            kb.op('pe', lambda e: e.matmul(psau[:, 0:256], lhsT=WT[:], rhs=TM[:, 0:2, :].rearrange("p a b -> p (a b)"), start=True, stop=True), reads=[wtk, tmk], writes=[psauk])
                            evac(AU[:].rearrange("p a b -> p (a b)"), psau[:, 0:256], [psauk], [auk])
                            if KRW_BSTOP <= 5:
                                continue
                            psg, psgk = psB.next()
                            kb.op('pe', lambda e: e.matmul(psg[:, 0:128], lhsT=AU[:, 0, :], rhs=TM[:, 2, :], start=True, stop=True), reads=[auk, tmk], writes=[psgk])
                            kb.op('dve', lambda e: e.scalar_tensor_tensor(out=stG[:, ci, :], in0=idt[:], scalar=PL[:, ci:ci + 1], in1=psg[:, 0:128], op0=ALU.mult, op1=ALU.add),
                                  reads=[psgk, plk, "idt"], writes=[stGk])
                            if KRW_BSTOP <= 6:
                                continue
                            psn, psnk = psB.next()
                            kb.op('pe', lambda e: e.matmul(psn[:, 0:128], lhsT=TM[:, 2, :], rhs=AU[:, 1, :], start=True, stop=False), reads=[auk, tmk], writes=[psnk], inc=False)
                            kb.op('pe', lambda e: e.matmul(psn[:, 0:128], lhsT=TM[:, 3, :], rhs=Vtm[:], start=False, stop=True), reads=[tmk, vtk], writes=[psnk])
                            evac(stN[:, ci, :], psn[:, 0:128], [psnk], [stNk])
                            if KRW_BSTOP <= 7:
                                continue
                            if own:
                                psr, psrk = psB.next()
                                kb.op('pe', lambda e: e.matmul(psr[:, 0:128], lhsT=AU[:, 0, :], rhs=AT4[:, 2, :], start=True, stop=True), reads=[auk, atk], writes=[psrk])
                                kb.op('dve', lambda e: e.tensor_tensor(out=stR[:, ci, :], in0=psr[:, 0:128], in1=Rd, op=ALU.add), reads=[psrk, ex["R"][1]], writes=[stRk])
                                if KRW_BSTOP <= 8:
                                    continue
                                pso, psok = psB.next()
                                kb.op('pe', lambda e: e.matmul(pso[:, 0:128], lhsT=AT4[:, 2, :], rhs=AU[:, 1, :], start=True, stop=False), reads=[auk, atk], writes=[psok], inc=False)
                                kb.op('pe', lambda e: e.matmul(pso[:, 0:128], lhsT=AT4[:, 3, :], rhs=Vtm[:], start=False, stop=True), reads=[atk, vtk], writes=[psok])
                                evac(stO[:, ci, :], pso[:, 0:128], [psok], [stOk])
                        if os.environ.get('KRW_NOST'):
                            continue
                        kb.dma('sp', GTd[d, hp, :, ch0:ch0 + nch, :], stG[:, :nch, :], reads=[stGk])
                        kb.dma('sp', Nd[d, hp, :, ch0:ch0 + nch, :], stN[:, :nch, :], reads=[stNk])
                        if own:
                            kb.dma('sp', RhTd[d, hp, :, ch0:ch0 + nch, :], stR[:, :nch, :], reads=[stRk])
                            kb.dma('sp', Ohd[d, hp, :, ch0:ch0 + nch, :], stO[:, :nch, :], reads=[stOk])
                    if own:
                        kb.op('dve', lambda e: e.tensor_scalar(out=bsum[:, :n], in0=bsum[:, :n], scalar1=rk_t[:, hp:hp + 1], scalar2=None, op0=ALU.mult), reads=[bsk, "rk_t"], writes=[bsk])
                        psb_, psbk_ = psA.next()
                        kb.op('pe', lambda e: e.matmul(psb_[:, :n], lhsT=BLK[:], rhs=bsum[:, :n], start=True, stop=True), reads=["BLK", bsk], writes=[psbk_])
                        bo, bok = sh_r.next()
                        kb.op('dve', lambda e: e.tensor_tensor(out=bo[:, :n], in0=psb_[:, :n], in1=VS[:, :n], op=ALU.mult), reads=[psbk_, vsk], writes=[bok])
                        kb.dma('sp', BONs[hp, :, c0:c0 + n], bo[:, :n], reads=[bok])
                        psg_, psgk_ = psA.next()
                        kb.op('pe', lambda e: e.matmul(psg_[:, :n], lhsT=g2_t[:, hp * 128:(hp + 1) * 128], rhs=SGg[:, :n], start=True, stop=True), reads=["g2_t", sggk], writes=[psgk_])
                        go, gok = sh_r.next()
                        evac(go[:, :n], psg_[:, :n], [psgk_], [gok])
                        kb.dma('sp', Gsc[hp, :, c0:c0 + n], go[:, :n], reads=[gok])

        KREG = os.environ.get('KRW_REG', 'both')
        if KRW_MARK < 99:
            try:
                region(pT, FULL_BLKS, False, GTs, Nsc, None, None, lambda bi: 0)
            except StopRegion:
                pass
            kb.barrier()
            return
        if KREG in ('both', 'full'):
            region(pT, FULL_BLKS, False, GTs, Nsc, None, None, lambda bi: 0 if bi == 0 else 4 + (bi - 1) * 8)
        if KREG in ('both', 'own'):
            region(poT, OWN_RBLKS, True, GTo, Nso, RhTo, Oho, lambda bi: bi * 8)
        kb.barrier()

    if KRW_NOCHAIN:
        return
    with ExitStack() as ph:
        sbp = lambda name, shape: ph.enter_context(nc.sbuf_tensor(name, shape, F32))
        idt_ = idt
        selF = sbp("selF2", [128, 4]); kb.dma('sp', selF[:], C["selF"], writes=["selF2"])
        selB = sbp("selB2", [128, 4]); kb.dma('sp', selB[:], C["selB"], writes=["selB2"])
        SEL = sbp("SEL2", [128, 64]); kb.dma('sp', SEL[:], C["sel"], writes=["SEL2"])
        lnw_t = sbp("lnw2", [128, 4]); kb.dma('sp', lnw_t[:], C["ln_w"].rearrange("(c p) -> p c", p=128), writes=["lnw2"], allow_slow_non_contiguous=True)
        lnb_t = sbp("lnb2", [128, 4]); kb.dma('sp', lnb_t[:], C["ln_b"].rearrange("(c p) -> p c", p=128), writes=["lnb2"], allow_slow_non_contiguous=True)
        chains = [(d, hp) for d in range(2) for hp in range(4)]
        S = {}
        for (d, hp) in chains:
            S[(d, hp)] = [sbp("S%d%d_%d" % (d, hp, i), [128, 128]) for i in range(2)]
            kb.op('pool', lambda e: e.memset(S[(d, hp)][0][:], 0.0), writes=[("S", d, hp, 0)])
        CAND = {(d, hp): sbp("CA%d%d" % (d, hp), [128, 4, 128]) for (d, hp) in chains}
        ph1 = ExitStack()
        gl_r = {ch: Rot(nc, ph1, "gl%d%d" % ch, [128, 8, 128], 1) for ch in chains}
        nl_r = {ch: Rot(nc, ph1, "nl%d%d" % ch, [128, 8, 128], 1) for ch in chains}
        psC = Rot(nc, ph, "psC", [128, 512], 8, psum=True)
        cur = {ch: 0 for ch in chains}
        def groups(d):
            if d == 0:
                g = [list(range(0, 4))] + [list(range(4 + i * 8, 12 + i * 8)) for i in range(12)]
            else:
                g = [list(range(3, -1, -1))] + [list(range(4 + i * 8 + 7, 4 + i * 8 - 1, -1)) for i in range(15, 3, -1)]
            return g
        G = {0: groups(0), 1: groups(1)}
        ngroups = len(G[0])
        for gi in range(ngroups):
            loaded = {}
            for ch in chains:
                d, hp = ch
                g = G[d][gi]
                lo = min(g)
                gl, glk = gl_r[ch].next(); nl, nlk = nl_r[ch].next()
                kb.dma('sp', gl[:, :len(g), :], GTs[d, hp, :, lo:lo + len(g), :], writes=[glk])
                kb.dma('sp', nl[:, :len(g), :], Nsc[d, hp, :, lo:lo + len(g), :], writes=[nlk])
                loaded[ch] = (gl, glk, nl, nlk, lo)
            for step in range(len(G[0][gi])):
                for ch in chains:
                    d, hp = ch
                    gl, glk, nl, nlk, lo = loaded[ch]
                    c = G[d][gi][step] - lo
                    i0 = cur[ch]; i1 = 1 - i0
                    ps, pk = psC.next()
                    kb.op('pe', lambda e: e.matmul(ps[:, 0:128], lhsT=gl[:, c, :], rhs=S[ch][i0][:], start=True, stop=True), reads=[glk, ("S", d, hp, i0)], writes=[pk])
                    kb.op('dve', lambda e: e.tensor_tensor(out=S[ch][i1][:], in0=ps[:, 0:128], in1=nl[:, c, :], op=ALU.add), reads=[pk, nlk], writes=[("S", d, hp, i1)])
                    cur[ch] = i1
            if gi in (0, 4, 8, 12):
                ci_ = {0: 0, 4: 1, 8: 2, 12: 3}[gi]
                for ch in chains:
                    d, hp = ch
                    kb.op('dve', lambda e: e.tensor_copy(out=CAND[ch][:, ci_, :], in_=S[ch][cur[ch]][:]), reads=[("S", d, hp, cur[ch])], writes=[("CAND", d, hp)])
        for ch in chains:
            d, hp = ch
            sel = selF if d == 0 else selB
            seln = "selF2" if d == 0 else "selB2"
            i0 = cur[ch]
            kb.op('dve', lambda e: e.tensor_scalar(out=S[ch][i0][:], in0=CAND[ch][:, 0, :], scalar1=sel[:, 0:1], scalar2=None, op0=ALU.mult),
                  reads=[("CAND", d, hp), seln], writes=[("S", d, hp, i0)])
            for i in range(1, 4):
                kb.op('dve', lambda e: e.scalar_tensor_tensor(out=S[ch][i0][:], in0=CAND[ch][:, i, :], scalar=sel[:, i:i + 1], in1=S[ch][i0][:], op0=ALU.mult, op1=ALU.add),
                      reads=[("CAND", d, hp), seln, ("S", d, hp, i0)], writes=[("S", d, hp, i0)])
        kb.barrier()
        ph1.close()
        OD = {ch: sbp("OD%d%d" % ch, [128, NCH_OWN, 64]) for ch in chains}
        gl_r = {ch: Rot(nc, ph, "g2l%d%d" % ch, [128, 4, 128], 1) for ch in chains}
        nl_r = {ch: Rot(nc, ph, "n2l%d%d" % ch, [128, 4, 128], 1) for ch in chains}
        rl_r = {ch: Rot(nc, ph, "rl%d%d" % ch, [128, 4, 128], 1) for ch in chains}
        ol_r = {ch: Rot(nc, ph, "ol%d%d" % ch, [128, 4, 128], 1) for ch in chains}
        tmp_r = Rot(nc, ph, "ctmp", [128, 128], 4)
        for gi in range(8):
            loaded = {}
            for ch in chains:
                d, hp = ch
                g = list(range(gi * 4, gi * 4 + 4)) if d == 0 else list(range(31 - gi * 4, 27 - gi * 4, -1))
                lo = min(g)
                gl, glk = gl_r[ch].next(); nl, nlk = nl_r[ch].next(); rl, rlk = rl_r[ch].next(); ol, olk = ol_r[ch].next()
                for (t_, k_, src_) in ((gl, glk, GTo), (nl, nlk, Nso), (rl, rlk, RhTo), (ol, olk, Oho)):
                    kb.dma('sp', t_[:], src_[d, hp, :, lo:lo + 4, :], writes=[k_])
                loaded[ch] = (gl, glk, nl, nlk, rl, rlk, ol, olk, lo, g)
            for step in range(4):
                for ch in chains:
                    d, hp = ch
                    gl, glk, nl, nlk, rl, rlk, ol, olk, lo, g = loaded[ch]
                    cg = g[step]; c = cg - lo
                    i0 = cur[ch]; i1 = 1 - i0
                    pso, psok = psC.next()
                    kb.op('pe', lambda e: e.matmul(pso[:, 0:128], lhsT=rl[:, c, :], rhs=S[ch][i0][:], start=True, stop=True), reads=[rlk, ("S", d, hp, i0)], writes=[psok], inc=False)
                    kb.op('pe', lambda e: e.matmul(pso[:, 128:256], lhsT=gl[:, c, :], rhs=S[ch][i0][:], start=True, stop=True), reads=[glk, ("S", d, hp, i0)], writes=[psok])
                    kb.op('dve', lambda e: e.tensor_tensor(out=S[ch][i1][:], in0=pso[:, 128:256], in1=nl[:, c, :], op=ALU.add), reads=[psok, nlk], writes=[("S", d, hp, i1)])
                    tt, ttk = tmp_r.next()
                    kb.op('dve', lambda e: e.tensor_tensor(out=tt[:], in0=pso[:, 0:128], in1=ol[:, c, :], op=ALU.add), reads=[psok, olk], writes=[ttk])
                    kb.op('pool', lambda e: e.tensor_tensor(out=OD[ch][:, cg, :], in0=tt[:, 0:64], in1=tt[:, 64:128], op=ALU.add), reads=[ttk], writes=[("OD", d, hp)])
                    cur[ch] = i1
        ye_r = Rot(nc, ph, "yexp", [128, 8, 2, 64], 2)
        for j_, t_ in enumerate(ye_r.t):
            kb.op('pool', lambda e, t_=t_: e.memset(t_[:], 0.0), writes=[("yexp", j_)])
        os_r = Rot(nc, ph, "osum", [128, 8, 64], 2)
        sq_r = Rot(nc, ph, "osq", [128, 8, 64], 2)
        stt_r = Rot(nc, ph, "ostt", [128, 4, 8], 2)
        yn_r = Rot(nc, ph, "ynr", [128, 512], 2)
        bg_r = Rot(nc, ph, "bgr", [128, 2, 512], 2)
        for hp in range(4):
            for bi in range(4):
                OS, osk = os_r.next()
                kb.op('pool', lambda e: e.tensor_tensor(out=OS[:], in0=OD[(0, hp)][:, bi * 8:(bi + 1) * 8, :], in1=OD[(1, hp)][:, bi * 8:(bi + 1) * 8, :], op=ALU.add),
                      reads=[("OD", 0, hp), ("OD", 1, hp)], writes=[osk])
                stt, sk = stt_r.next()
                kb.op('dve', lambda e: e.reduce_sum(out=stt[:, 0, :], in_=OS[:], axis=AX.X), reads=[osk], writes=[sk])
                SQ, sqk = sq_r.next()
                kb.op('pool', lambda e: e.tensor_tensor(out=SQ[:], in0=OS[:], in1=OS[:], op=ALU.mult), reads=[osk], writes=[sqk])
                kb.op('dve', lambda e: e.reduce_sum(out=stt[:, 1, :], in_=SQ[:], axis=AX.X), reads=[sqk], writes=[sk])
                kb.op('dve', lambda e: e.tensor_scalar(out=stt[:, 0, :], in0=stt[:, 0, :], scalar1=1.0 / 64, scalar2=None, op0=ALU.mult), reads=[sk], writes=[sk])
                kb.op('dve', lambda e: e.tensor_tensor(out=stt[:, 2, :], in0=stt[:, 0, :], in1=stt[:, 0, :], op=ALU.mult), reads=[sk], writes=[sk])
                kb.op('dve', lambda e: e.scalar_tensor_tensor(out=stt[:, 3, :], in0=stt[:, 1, :], scalar=1.0 / 64, in1=stt[:, 2, :], op0=ALU.mult, op1=ALU.subtract), reads=[sk], writes=[sk])
                kb.op('dve', lambda e: e.tensor_scalar(out=stt[:, 3, :], in0=stt[:, 3, :], scalar1=GN_EPS, scalar2=None, op0=ALU.add), reads=[sk], writes=[sk])
                kb.op('act', lambda e: e.activation(out=stt[:, 3, :], in_=stt[:, 3, :], func=AF.Sqrt), reads=[sk], writes=[sk])
                kb.op('dve', lambda e: e.reciprocal(out=stt[:, 3, :], in_=stt[:, 3, :]), reads=[sk], writes=[sk])
                kb.op('dve', lambda e: e.tensor_tensor(out=OS[:], in0=OS[:], in1=stt[:, 0, :].unsqueeze(2).to_broadcast([128, 8, 64]), op=ALU.subtract), reads=[osk, sk], writes=[osk])
                ye, yek = ye_r.next()
                for hh in range(2):
                    sl = slice(hh * 64, hh * 64 + 64)
                    kb.op('dve', lambda e, hh=hh, sl=sl: e.tensor_tensor(out=ye[sl, :, hh, :], in0=OS[sl], in1=stt[sl, 3, :].unsqueeze(2).to_broadcast([64, 8, 64]), op=ALU.mult),
                          reads=[osk, sk], writes=[yek])
                ps, pk = psC.next()
                for ci in range(8):
                    kb.op('pe', lambda e, ci=ci: e.matmul(ps[:, ci * 64:(ci + 1) * 64], lhsT=ye[:, ci].rearrange("p a b -> p (a b)"), rhs=SEL[:], start=True, stop=True),
                          reads=[yek, "SEL2"], writes=[pk], inc=(ci == 7))
                bg, bgk = bg_r.next()
                kb.dma('sp', bg[:, 0, :], BONs[hp, :, bi * 512:(bi + 1) * 512], writes=[bgk])
                kb.dma('sp', bg[:, 1, :], Gsc[hp, :, bi * 512:(bi + 1) * 512], writes=[bgk])
                yn, ynk = yn_r.next()
                kb.op('dve', lambda e: e.tensor_scalar(out=yn[:], in0=ps[:], scalar1=lnw_t[:, hp:hp + 1], scalar2=lnb_t[:, hp:hp + 1], op0=ALU.mult, op1=ALU.add),
                      reads=[pk, "lnw2", "lnb2"], writes=[ynk])
                kb.op('pool', lambda e: e.tensor_tensor(out=yn[:], in0=yn[:], in1=bg[:, 0, :], op=ALU.add), reads=[ynk, bgk], writes=[ynk])
                kb.op('pool', lambda e: e.tensor_tensor(out=yn[:], in0=yn[:], in1=bg[:, 1, :], op=ALU.mult), reads=[ynk, bgk], writes=[ynk])
                kb.dma('sp', yT[hp * 128:(hp + 1) * 128, bi * 512:(bi + 1) * 512], yn[:], reads=[ynk])
        kb.barrier()


def rwkv_consts(q):
    tt = np.arange(64)
    lowS = (tt[:, None] < tt[None, :]).astype(np.float32)
    lowI = (tt[:, None] <= tt[None, :]).astype(np.float32)
    def bd(m):
        z = np.zeros((128, 128), np.float32)
        z[:64, :64] = m
        z[64:, 64:] = m
        return z
    mask4 = np.zeros((2, 128, 512), np.float32)
    maskl = np.zeros((2, 128, 128), np.float32)
    for d in range(2):
        S_, I_ = (lowS, lowI) if d == 0 else (lowS.T, lowI.T)
        mask4[d] = np.concatenate([bd(S_), bd(S_), bd(I_), bd(I_)], axis=1)
        maskl[d] = bd(S_.T)
    blk = bd(np.ones((64, 64), np.float32))
    uu = np.concatenate([(tt[:, None] <= tt[None, :]).astype(np.float32)] * 2, axis=0)
    sel = np.concatenate([np.eye(64, dtype=np.float32)] * 2, axis=0)
    selF = np.zeros((128, 4), np.float32); selF[:, q] = 1.0
    selB = np.zeros((128, 4), np.float32); selB[:, 3 - q] = 1.0
    hal = np.zeros((128, 2), np.float32)
    hal[:, 0] = 1.0 if q > 0 else 0.0
    hal[:, 1] = 1.0 if q < 3 else 0.0
    return dict(mask4=mask4, maskl=maskl, blk=blk, uu=uu, sel=sel, selF=selF, selB=selB, hal=hal)


OWN_BLKS = [(0, 512), (512, 512), (1024, 512), (1536, 512), (2048, 128)]
XO_ROWS = QT + 128
NE = 32
ATT_SCALE = 192.0 ** -0.5


RUN = os.environ.get("KSTAGES", "123456")
SKIP = os.environ.get("KSKIP", "").split(",")


class Stage:
    def __init__(self, s):
        self.s = s

    def __enter__(self):
        return self.s in RUN

    def __exit__(self, *a):
        return False


def build():
    nc = bass.Bass("TRN2", target_bir_lowering=False)
    dt = nc.dram_tensor

    def din(name, shape):
        return dt(name, shape, F32, kind="ExternalInput").ap()

    def dscr(name, shape):
        if DEBUG and name in DEBUG.split(","):
            return dt("dbg_" + name, shape, F32, kind="ExternalOutput").ap()
        return dt(name, shape, F32).ap()

    xs = din("xs", [TS, D])
    xo = din("xo", [XO_ROWS, D])
    cvec = din("cvec", [128, 16])
    mod_w = din("mod_w", [D, 6 * D])
    mod_b = din("mod_b", [6 * D])
    g1 = din("g1", [D])
    g2n = din("g2n", [D])
    gfin = din("gfin", [D])
    w_in = din("w_in", [D, WCOLS])
    ident = din("ident", [128, 128])
    ropeC = din("ropeC", [64, T])
    ropeS = din("ropeS", [64, T])
    ropeCo = din("ropeCo", [64, QT])
    ropeSo = din("ropeSo", [64, QT])
    kvg = din("kvg", [128, 1])
    qg = din("qg", [128, 2])
    wukv_k = din("wukv_k", [128, 512])
    wukv_v = din("wukv_v", [128, 512])
    wuq = din("wuq", [256, 1024])
    w_out = din("w_out", [D, D])
    router_w = din("router_w", [D, NE])
    router_b = din("router_b", [NE])
    w1 = din("w1", [NE, D, 2 * D]) if "5" in RUN else None
    b1fm = din("b1fm", [128, NE * 16])
    w2 = din("w2", [NE, D, D]) if "5" in RUN else None
    b2 = din("b2", [NE, D])
    yrw_in = din("yrw_in", [512, QT]) if (DEBUG and '2' not in RUN) else None
    rw = {}
    if '2' in RUN:
        for nm, shp in (("mu", [1792]), ("w0", [2, 512]), ("a0", [2, 512]), ("w2", [2, 64, 512]), ("a2", [2, 64, 512]), ("k_k", [512]), ("k_a", [512]),
                        ("r_k", [512]), ("ln_w", [512]), ("ln_b", [512]), ("g2", [128, 512]), ("mask4", [2, 128, 512]), ("maskl", [2, 128, 128]),
                        ("blk", [128, 128]), ("uu", [128, 64]), ("sel", [128, 64]), ("selF", [128, 4]), ("selB", [128, 4]), ("hal", [128, 2])):
            rw[nm] = din("rw_" + nm, shp)
    out = dt("out", [QT, D], F32, kind="ExternalOutput").ap()

    pT = dscr("pT", [1792, TS])
    poT = dscr("poT", [1792, XO_ROWS])
    KnT = dscr("KnT", [4, 128, TS])
    KrT = dscr("KrT", [64, TS])
    Vs = dscr("Vs", [TS, 4 * 129])
    QnT = dscr("QnT", [4, 128, QT])
    QrT = dscr("QrT", [4, 64, QT])
    yT = dscr("yT", [D, QT])
    X1 = dscr("X1", [QT, D])
    LG = dscr("LG", [QT, 2 * NE])
    FF = dscr("FF", [QT, D])

    with ExitStack() as st:
        kb = KB(nc, st)
        sb = lambda name, shape: st.enter_context(nc.sbuf_tensor(name, shape, F32))
        idt = sb("idt", [128, 128])
        kb.dma('sp', idt[:], ident, writes=["idt"])
        ones = sb("ones", [128, 128])
        kb.op('pool', lambda e: e.memset(ones[:], 1.0), writes=["ones"])
        cv = sb("cv", [128, 16])
        sc = sb("sc", [128, 16])
        kb.dma('sp', cv[:], cvec, writes=["cv"])
        kb.op('act', lambda e: e.activation(out=sc[:], in_=cv[:], func=AF.Silu), reads=["cv"], writes=["sc"])
        modfm = sb("modfm", [128, 4, 8, 2])
        mbfm = sb("mbfm", [128, 6, 8])
        kb.dma('sp', mbfm[:], mod_b.rearrange("(v k p) -> p v k", p=128, k=8), writes=["mbfm"], allow_slow_non_contiguous=True)
        g1t = sb("g1t", [128, 8])
        kb.dma('sp', g1t[:], g1.rearrange("(k p) -> p k", p=128), writes=["g1t"], allow_slow_non_contiguous=True)
        g2t = sb("g2t", [128, 8])
        kb.dma('sp', g2t[:], g2n.rearrange("(k p) -> p k", p=128), writes=["g2t"], allow_slow_non_contiguous=True)
        GT = sb("GT", [128, 2, 1024])
        with ExitStack() as ph:
            wv = Rot(nc, ph, "wv", [128, 8, 1024], 2)
            psm = Rot(nc, ph, "psm", [128, 16], 2, psum=True)
            psg = Rot(nc, ph, "psg", [128, 512], 2, psum=True)
            SCB = ph.enter_context(nc.sbuf_tensor("SCB", [128, 8, 128], F32))
            mbb = ph.enter_context(nc.sbuf_tensor("mbb", [128, 2, 1024], F32))
            for k in range(8):
                kb.op('dve', lambda e, k=k: e.tensor_copy(out=SCB[:, k, :], in_=sc[:, 2 * k:2 * k + 1].to_broadcast([128, 128])),
                      reads=["sc"], writes=["SCB"])
            for gi, v in enumerate((2, 5)):
                kb.dma('sp', mbb[:, gi, :], mod_b[v * 1024:(v + 1) * 1024].partition_broadcast(128), writes=["mbb"])
            for vi, v in enumerate((0, 1, 3, 4)):
                wt, wk = wv.next()
                kb.dma('sp', wt[:], mod_w[:, v * 1024:(v + 1) * 1024].rearrange("(k p) n -> p k n", p=128), writes=[wk])
                ps, pk = psm.next()
                for j in range(8):
                    for k in range(8):
                        kb.op('pe', lambda e, j=j, k=k: e.matmul(ps[:, 2 * j:2 * j + 2], lhsT=wt[:, k, j * 128:(j + 1) * 128],
                                                                 rhs=sc[:, 2 * k:2 * k + 2], start=(k == 0), stop=(k == 7)),
                              reads=[wk, "sc"], writes=[pk], inc=(j == 7 and k == 7))
                kb.op('dve', lambda e, vi=vi, v=v: e.tensor_tensor(out=modfm[:, vi], in0=ps[:].rearrange("p (j c) -> p j c", c=2),
                                                                   in1=mbfm[:, v, :].unsqueeze(2).to_broadcast([128, 8, 2]), op=ALU.add),
                      reads=[pk, "mbfm"], writes=["modfm"])
            for gi, v in enumerate((2, 5)):
                wt, wk = wv.next()
                kb.dma('sp', wt[:], mod_w[:, v * 1024:(v + 1) * 1024].rearrange("(k p) n -> p k n", p=128), writes=[wk])
                for hf in range(2):
                    ps, pk = psg.next()
                    for k in range(8):
                        kb.op('pe', lambda e, k=k, hf=hf: e.matmul(ps[:], lhsT=SCB[:, k, :], rhs=wt[:, k, hf * 512:(hf + 1) * 512],
                                                                   start=(k == 0), stop=(k == 7)),
                              reads=[wk, "SCB"], writes=[pk], inc=(k == 7))
                    kb.op('dve', lambda e, gi=gi, hf=hf: e.tensor_tensor(out=GT[:, gi, hf * 512:(hf + 1) * 512], in0=ps[:],
                                                                         in1=mbb[:, gi, hf * 512:(hf + 1) * 512], op=ALU.add),
                          reads=[pk, "mbb"], writes=["GT"])
            kb.barrier()
        G1 = sb("G1", [128, 8, 2])
        G2 = sb("G2", [128, 8, 2])
        for Gt, gt_, mrow, nm in ((G1, g1t, 1, "G1"), (G2, g2t, 3, "G2")):
            kb.op('dve', lambda e: e.tensor_scalar(out=Gt[:], in0=modfm[:, mrow], scalar1=1.0, scalar2=None, op0=ALU.add), reads=["modfm"], writes=[nm])
            kb.op('dve', lambda e: e.tensor_tensor(out=Gt[:], in0=Gt[:], in1=gt_[:].unsqueeze(2).to_broadcast([128, 8, 2]), op=ALU.mult),
                  reads=[nm, "g1t", "g2t"], writes=[nm])

        def rms_rstd(xt, xk, stt, sk, junk, n_feat):
            kb.op('pool', lambda e: e.memset(stt[:], 0.0), writes=[sk])
            kb.op('act', lambda e: e.activation(out=junk[:], in_=xt, func=AF.Square, accum_out=stt[:, 0:1]), reads=[xk], writes=["junk", sk])
            kb.op('dve', lambda e: e.tensor_scalar(out=stt[:, 1:2], in0=stt[:, 0:1], scalar1=1.0 / n_feat, scalar2=1e-6, op0=ALU.mult, op1=ALU.add),
                  reads=[sk], writes=[sk])
            kb.op('act', lambda e: e.activation(out=stt[:, 2:3], in_=stt[:, 1:2], func=AF.Sqrt), reads=[sk], writes=[sk])
            kb.op('dve', lambda e: e.reciprocal(out=stt[:, 3:4], in_=stt[:, 2:3]), reads=[sk], writes=[sk])

        def cm_rstd(ps_ss, pk, dst, dk, n, n_feat, extra_scale=1.0):
            kb.op('dve', lambda e: e.tensor_scalar(out=dst[:, :n], in0=ps_ss[:, :n], scalar1=1.0 / n_feat, scalar2=1e-6, op0=ALU.mult, op1=ALU.add),
                  reads=[pk], writes=[dk])
            kb.op('act', lambda e: e.activation(out=dst[:, :n], in_=dst[:, :n], func=AF.Sqrt), reads=[dk], writes=[dk])
            kb.op('dve', lambda e: e.reciprocal(out=dst[:, :n], in_=dst[:, :n]), reads=[dk], writes=[dk])
            if extra_scale != 1.0:
                kb.op('dve', lambda e: e.tensor_scalar(out=dst[:, :n], in0=dst[:, :n], scalar1=extra_scale, scalar2=None, op0=ALU.mult), reads=[dk], writes=[dk])

        with ExitStack() as ph, Stage('1') as go:
          if go:
            sbp = lambda name, shape: ph.enter_context(nc.sbuf_tensor(name, shape, F32))
            WB = sbp("WB", [128, 8, WCOLS])
            for k in range(8):
                kb.dma('sp', WB[:, k, :], w_in[k * 128:(k + 1) * 128, :], writes=[("WB", k)])
            wk_t = sbp("wk_t", [128, 512]); kb.dma('sp', wk_t[:], wukv_k, writes=["wk_t"])
            wv_t = sbp("wv_t", [128, 512]); kb.dma('sp', wv_t[:], wukv_v, writes=["wv_t"])
            wq_t = sbp("wq_t", [128, 2, 1024]); kb.dma('sp', wq_t[:], wuq.rearrange("(k p) n -> p k n", p=128), writes=["wq_t"])
            kvg_t = sbp("kvg_t", [128, 1]); kb.dma('sp', kvg_t[:], kvg, writes=["kvg_t"])
            qg_t = sbp("qg_t", [128, 2]); kb.dma('sp', qg_t[:], qg, writes=["qg_t"])
            xt_r = Rot(nc, ph, "xt", [128, 1024], 2)
            xn_r = Rot(nc, ph, "xn", [128, 1024], 1)
            st_r = Rot(nc, ph, "stat", [128, 4], 4)
            xmT_r = Rot(nc, ph, "xmT", [128, 8, 512], 2)
            pst_r = Rot(nc, ph, "pst", [128, 8, 128], 1, psum=True)
            psp_r = Rot(nc, ph, "psp", [128, 512], 6, psum=True)
            stg_r = Rot(nc, ph, "stg", [128, 512], 3)
            tmp_r = Rot(nc, ph, "tmp", [128, 512], 3)
            ql_r = Rot(nc, ph, "qlr", [128, 512], 2)
            rs_r = Rot(nc, ph, "rsr", [128, 512], 1)
            ckn_r = Rot(nc, ph, "cknr", [128, 512], 1)
            rp_r = Rot(nc, ph, "rp", [64, 2, 512], 2)
            vst_r = Rot(nc, ph, "vst", [128, 4, 129], 2)
            for j_, t_ in enumerate(vst_r.t):
                kb.op('pool', lambda e, t_=t_: e.memset(t_[:], 1.0), writes=[("vst", j_)])
            junk = sbp("junk", [128, 1024])
            cnt = {"ev": 0}

            def evac(out_ap, in_ap, reads, writes):
                cnt["ev"] += 1
                if cnt["ev"] % 2:
                    kb.op('dve', lambda e: e.tensor_copy(out=out_ap, in_=in_ap), reads=reads, writes=writes)
                else:
                    kb.op('act', lambda e: e.copy(out=out_ap, in_=in_ap), reads=reads, writes=writes)

            def proj_pass(src, blocks, dstT, own):
                for (s0, n) in blocks:
                    mi = 1 if (not own and s0 < CTX) else 0
                    xmT, xmk = xmT_r.next()
                    for i in range(n // 128):
                        xt, xk = xt_r.next()
                        kb.dma('sp', xt[:], src[s0 + i * 128: s0 + (i + 1) * 128, :], writes=[xk])
                        stt, sk = st_r.next()
                        rms_rstd(xt[:], xk, stt, sk, junk, D)
                        xn, nk = xn_r.next()
                        kb.op('dve', lambda e: e.tensor_scalar(out=xn[:], in0=xt[:], scalar1=stt[:, 3:4], scalar2=None, op0=ALU.mult),
                              reads=[xk, sk], writes=[nk])
                        pt, ptk = pst_r.next()
                        for k in range(8):
                            kb.op('pe', lambda e, k=k: e.transpose(out=pt[:, k, :], in_=xn[:, k * 128:(k + 1) * 128], identity=idt[:]),
                                  reads=[nk, "idt"], writes=[ptk], inc=(k == 7))
                        for k in range(8):
                            if k % 2 == 0:
                                kb.op('dve', lambda e, k=k, i=i: e.tensor_scalar(out=xmT[:, k, i * 128:(i + 1) * 128], in0=pt[:, k, :],
                                                                                  scalar1=G1[:, k, mi:mi + 1], scalar2=modfm[:, 0, k, mi:mi + 1],
                                                                                  op0=ALU.mult, op1=ALU.add),
                                      reads=[ptk, "G1", "modfm"], writes=[xmk])
                            else:
                                kb.op('act', lambda e, k=k, i=i: e.activation(out=xmT[:, k, i * 128:(i + 1) * 128], in_=pt[:, k, :], func=AF.Identity,
                                                                               scale=G1[:, k, mi:mi + 1], bias=modfm[:, 0, k, mi:mi + 1]),
                                      reads=[ptk, "G1", "modfm"], writes=[xmk])

                    def proj_cols(c0, m):
                        ps, pk = psp_r.next()
                        for k in range(8):
                            kb.op('pe', lambda e, k=k: e.matmul(ps[:m, :n], lhsT=WB[:, k, c0:c0 + m], rhs=xmT[:, k, :n], start=(k == 0), stop=(k == 7)),
                                  reads=[("WB", k), xmk], writes=[pk], inc=(k == 7))
                        return ps, pk

                    for cc in range(14):
                        ps, pk = proj_cols(cc * 128, 128)
                        sg, sgk = stg_r.next()
                        evac(sg[:, :n], ps[:, :n], [pk], [sgk])
                        kb.dma('sp', dstT[cc * 128:(cc + 1) * 128, s0:s0 + n], sg[:, :n], reads=[sgk])
                    if not own and 'kv' not in SKIP:
                        ps, pk = proj_cols(1792, 128)
                        ckv, ck = tmp_r.next()
                        evac(ckv[:, :n], ps[:, :n], [pk], [ck])
                        sq, sqk = tmp_r.next()
                        kb.op('pool', lambda e: e.tensor_tensor(out=sq[:, :n], in0=ckv[:, :n], in1=ckv[:, :n], op=ALU.mult), reads=[ck], writes=[sqk])
                        pss, pssk = psp_r.next()
                        kb.op('pe', lambda e: e.matmul(pss[:, :n], lhsT=ones[:], rhs=sq[:, :n], start=True, stop=True), reads=["ones", sqk], writes=[pssk])
                        rs, rsk = rs_r.next()
                        cm_rstd(pss, pssk, rs, rsk, n, 128.0)
                        ckn, cnk = ckn_r.next()
                        kb.op('dve', lambda e: e.scalar_tensor_tensor(out=ckn[:, :n], in0=ckv[:, :n], scalar=kvg_t[:, 0:1], in1=rs[:, :n],
                                                                       op0=ALU.mult, op1=ALU.mult), reads=[ck, rsk, "kvg_t"], writes=[cnk])
                        for h in range(0 if 'kvK' in SKIP else 4):
                            psk, pskk = psp_r.next()
                            kb.op('pe', lambda e, h=h: e.matmul(psk[:, :n], lhsT=wk_t[:, h * 128:(h + 1) * 128], rhs=ckn[:, :n], start=True, stop=True),
                                  reads=["wk_t", cnk], writes=[pskk])
                            sg, sgk = stg_r.next()
                            evac(sg[:, :n], psk[:, :n], [pskk], [sgk])
                            kb.dma('sp', KnT[h, :, s0:s0 + n], sg[:, :n], reads=[sgk])
                        for i in range(0 if 'kvV' in SKIP else n // 128):
                            psv, psvk = psp_r.next()
                            kb.op('pe', lambda e, i=i: e.matmul(psv[:, :], lhsT=ckn[:, i * 128:(i + 1) * 128], rhs=wv_t[:], start=True, stop=True),
                                  reads=["wv_t", cnk], writes=[psvk])
                            vt, vk = vst_r.next()
                            evac(vt[:, :, 0:128], psv[:].rearrange("p (h d) -> p h d", h=4), [psvk], [vk])
                            kb.dma('sp', Vs[s0 + i * 128:s0 + (i + 1) * 128, :], vt[:].rearrange("p h d -> p (h d)"), reads=[vk])
                        if 'kvR' in SKIP:
                            continue
                        psa, pak = proj_cols(1920, 64)
                        sg, sgk = stg_r.next()
                        if s0 < CTX:
                            evac(sg[:64, :n], psa[:64, :n], [pak], [sgk])
                        else:
                            psb, pbk = proj_cols(1984, 64)
                            rp, rpk = rp_r.next()
                            kb.dma('sp', rp[:, 0, :n], ropeC[:, s0 - CTX:s0 - CTX + n], writes=[rpk])
                            kb.dma('sp', rp[:, 1, :n], ropeS[:, s0 - CTX:s0 - CTX + n], writes=[rpk])
                            t1, t1k = tmp_r.next()
                            kb.op('dve', lambda e: e.tensor_tensor(out=t1[:64, :n], in0=psa[:64, :n], in1=rp[:, 0, :n], op=ALU.mult), reads=[pak, rpk], writes=[t1k])
                            t2, t2k = tmp_r.next()
                            kb.op('dve', lambda e: e.tensor_tensor(out=t2[:64, :n], in0=psb[:64, :n], in1=rp[:, 1, :n], op=ALU.mult), reads=[pbk, rpk], writes=[t2k])
                            kb.op('pool', lambda e: e.tensor_tensor(out=sg[:64, :n], in0=t1[:64, :n], in1=t2[:64, :n], op=ALU.add), reads=[t1k, t2k], writes=[sgk])
                        kb.dma('sp', KrT[:, s0:s0 + n], sg[:64, :n], reads=[sgk])
                    elif own and s0 < QT and 'q' not in SKIP:
                        ql = []
                        pss, pssk = psp_r.next()
                        for kc in range(2):
                            ps, pk = proj_cols(2048 + kc * 128, 128)
                            qn_, qnk = ql_r.next()
                            evac(qn_[:, :n], ps[:, :n], [pk], [qnk])
                            sq, sqk = tmp_r.next()
                            kb.op('pool', lambda e: e.tensor_tensor(out=sq[:, :n], in0=qn_[:, :n], in1=qn_[:, :n], op=ALU.mult), reads=[qnk], writes=[sqk])
                            kb.op('dve', lambda e, kc=kc: e.tensor_scalar(out=qn_[:, :n], in0=qn_[:, :n], scalar1=qg_t[:, kc:kc + 1], scalar2=None, op0=ALU.mult),
                                  reads=[qnk, sqk, "qg_t"], writes=[qnk])
                            kb.op('pe', lambda e, kc=kc: e.matmul(pss[:, :n], lhsT=ones[:], rhs=sq[:, :n], start=(kc == 0), stop=(kc == 1)),
                                  reads=["ones", sqk], writes=[pssk], inc=(kc == 1))
                            ql.append((qn_, qnk))
                        rs, rsk = rs_r.next()
                        cm_rstd(pss, pssk, rs, rsk, n, 256.0, ATT_SCALE)
                        rp, rpk = rp_r.next()
                        kb.dma('sp', rp[:, 0, :n], ropeCo[:, s0:s0 + n], writes=[rpk])
                        kb.dma('sp', rp[:, 1, :n], ropeSo[:, s0:s0 + n], writes=[rpk])
                        for h in range(4):
                            def qmm(c0, m):
                                ps, pk = psp_r.next()
                                for kc in range(2):
                                    kb.op('pe', lambda e, kc=kc: e.matmul(ps[:m, :n], lhsT=wq_t[:, kc, h * 256 + c0:h * 256 + c0 + m], rhs=ql[kc][0][:, :n],
                                                                          start=(kc == 0), stop=(kc == 1)),
                                          reads=["wq_t", ql[kc][1]], writes=[pk], inc=(kc == 1))
                                return ps, pk
                            ps, pk = qmm(0, 128)
                            sg, sgk = stg_r.next()
                            kb.op('dve', lambda e: e.tensor_tensor(out=sg[:, :n], in0=ps[:, :n], in1=rs[:, :n], op=ALU.mult), reads=[pk, rsk], writes=[sgk])
                            kb.dma('sp', QnT[h, :, s0:s0 + n], sg[:, :n], reads=[sgk])
                            psa, pak = qmm(128, 64)
                            psb, pbk = qmm(192, 64)
                            t1, t1k = tmp_r.next()
                            kb.op('dve', lambda e: e.tensor_tensor(out=t1[:64, :n], in0=psa[:64, :n], in1=rp[:, 0, :n], op=ALU.mult), reads=[pak, rpk], writes=[t1k])
                            t2, t2k = tmp_r.next()
                            kb.op('dve', lambda e: e.tensor_tensor(out=t2[:64, :n], in0=psb[:64, :n], in1=rp[:, 1, :n], op=ALU.mult), reads=[pbk, rpk], writes=[t2k])
                            kb.op('pool', lambda e: e.tensor_tensor(out=t1[:64, :n], in0=t1[:64, :n], in1=t2[:64, :n], op=ALU.add), reads=[t1k, t2k], writes=[t1k])
                            sg, sgk = stg_r.next()
                            kb.op('dve', lambda e: e.tensor_tensor(out=sg[:64, :n], in0=t1[:64, :n], in1=rs[:64, :n], op=ALU.mult), reads=[t1k, rsk], writes=[sgk])
                            kb.dma('sp', QrT[h, :, s0:s0 + n], sg[:64, :n], reads=[sgk])

            proj_pass(xs, NBLK_FULL, pT, False)
            if 'own' not in SKIP:
                proj_pass(xo, OWN_BLKS, poT, True)
            kb.barrier()

        if '2' in RUN:
            C = dict(rw)
            C["idt"] = idt
            rwkv_stage(nc, kb, st, pT, poT, yT, C, dscr)
        elif DEBUG:
            with ExitStack() as ph:
                t = ph.enter_context(nc.sbuf_tensor("yin", [128, 4, QT], F32))
                kb.dma('sp', t[:], yrw_in.rearrange("(k p) n -> p k n", p=128), writes=["yin"])
                kb.dma('sp', yT[0:512, :].rearrange("(k p) n -> p k n", p=128), t[:], reads=["yin"])
                kb.barrier()

        with ExitStack() as ph, Stage('3') as go:
          if go:
            sbp = lambda name, shape: ph.enter_context(nc.sbuf_tensor(name, shape, F32))
            Kn = sbp("Kn", [128, TS])
            Kr = sbp("Kr", [64, TS])
            Vh = sbp("Vh", [128, 66, 129])
            kb.dma('sp', Kr[:], KrT, writes=["Kr"])
            qn_r = Rot(nc, ph, "qn", [128, 512], 2)
            qr_r = Rot(nc, ph, "qr", [64, 512], 2)
            pT_r = Rot(nc, ph, "pTt", [128, 512], 3)
            pss_r = Rot(nc, ph, "pss", [128, 512], 3, psum=True)
            pso = [ph.enter_context(nc.psum_tensor("pso%d" % i, [128, 512], F32)) for i in range(4)]
            ptr_r = Rot(nc, ph, "ptr", [128, 128], 1, psum=True)
            yv_r = Rot(nc, ph, "yv", [128, 132], 3)
            yt_r = Rot(nc, ph, "ytt", [128, 512], 2)
            for h in range(4):
                for part in range(4):
                    c0 = part * 2112
                    kb.dma('sp', Kn[:, c0:c0 + 2112], KnT[h, :, c0:c0 + 2112], writes=["Kn"])
                kb.dma('sp', Vh[:], Vs[:, h * 129:(h + 1) * 129].rearrange("(t p) d -> p t d", p=128), writes=["Vh"])
                for qb in range(4):
                    qn_, qnk = qn_r.next()
                    qr_, qrk = qr_r.next()
                    kb.dma('sp', qn_[:], QnT[h, :, qb * 512:(qb + 1) * 512], writes=[qnk])
                    kb.dma('sp', qr_[:], QrT[h, :, qb * 512:(qb + 1) * 512], writes=[qrk])
                    pend = None
                    for kt in range(67):
                        if kt < 66:
                            ps, pk = pss_r.next()
                            kb.op('pe', lambda e: e.matmul(ps[:], lhsT=Kn[:, kt * 128:(kt + 1) * 128], rhs=qn_[:], start=True, stop=False),
                                  reads=["Kn", qnk], writes=[pk], inc=False)
                            kb.op('pe', lambda e: e.matmul(ps[:], lhsT=Kr[:, kt * 128:(kt + 1) * 128], rhs=qr_[:], start=False, stop=True),
                                  reads=["Kr", qrk], writes=[pk])
                            pt_, ptk = pT_r.next()
                            kb.op('act', lambda e: e.activation(out=pt_[:], in_=ps[:], func=AF.Exp), reads=[pk], writes=[ptk])
                            cur = (pt_, ptk, kt)
                        else:
                            cur = None
                        if pend is not None:
                            ppt, pptk, pkt = pend
                            for qi in range(4):
                                kb.op('pe', lambda e, qi=qi: e.matmul(pso[qi][:, 0:129], lhsT=ppt[:, qi * 128:(qi + 1) * 128], rhs=Vh[:, pkt, :],
                                                                      start=(pkt == 0), stop=(pkt == 65)),
                                      reads=[pptk, "Vh"], writes=[("pso", qi)], inc=(qi == 3))
                        pend = cur
                    ytt, ytk = yt_r.next()
                    for qi in range(4):
                        yv, yvk = yv_r.next()
                        kb.op('dve', lambda e: e.reciprocal(out=yv[:, 129:130], in_=pso[qi][:, 128:129]), reads=[("pso", qi)], writes=[yvk])
                        kb.op('dve', lambda e: e.tensor_scalar(out=yv[:, 0:128], in0=pso[qi][:, 0:128], scalar1=yv[:, 129:130], scalar2=None, op0=ALU.mult),
                              reads=[("pso", qi), yvk], writes=[yvk])
                        ptr, ptrk = ptr_r.next()
                        kb.op('pe', lambda e: e.transpose(out=ptr[:], in_=yv[:, 0:128], identity=idt[:]), reads=[yvk, "idt"], writes=[ptrk])
                        kb.op('act', lambda e: e.copy(out=ytt[:, qi * 128:(qi + 1) * 128], in_=ptr[:]), reads=[ptrk], writes=[ytk])
                    kb.dma('sp', yT[512 + h * 128:512 + (h + 1) * 128, qb * 512:(qb + 1) * 512], ytt[:], reads=[ytk])
            kb.barrier()

        with ExitStack() as ph, Stage('4') as go:
          if go:
            sbp = lambda name, shape: ph.enter_context(nc.sbuf_tensor(name, shape, F32))
            Wo = sbp("Wo", [128, 8, D])
            kb.dma('sp', Wo[:], w_out.rearrange("(k p) n -> p k n", p=128), writes=["Wo"])
            Wr = sbp("Wr", [128, 8, NE])
            kb.dma('sp', Wr[:], router_w.rearrange("(k p) n -> p k n", p=128), writes=["Wr"])
            rbb = sbp("rbb", [128, NE])
            kb.dma('sp', rbb[:], router_b.partition_broadcast(128), writes=["rbb"])
            junk = sbp("junk4", [128, 1024])
            yt_r = Rot(nc, ph, "yt4", [128, 8, 128], 2)
            xo_r = Rot(nc, ph, "xo4", [128, 1024], 2)
            x1_r = Rot(nc, ph, "x14", [128, 1024], 2)
            xn_r = Rot(nc, ph, "xn4", [128, 1024], 2)
            st_r = Rot(nc, ph, "st4", [128, 4], 3)
            h2_r = Rot(nc, ph, "h24", [128, 8, 128], 2)
            lg_r = Rot(nc, ph, "lg4", [128, 2 * NE], 2)
            mx_r = Rot(nc, ph, "mx4", [128, 16], 2)
            psA = Rot(nc, ph, "psA", [128, 512], 2, psum=True)
            psT = Rot(nc, ph, "psT", [128, 8, 128], 1, psum=True)
            psL = Rot(nc, ph, "psL", [128, NE], 1, psum=True)
            H2T = dscr("H2T", [D, QT])
            for i in range(QT // 128):
                yt, ytk = yt_r.next()
                kb.dma('sp', yt[:], yT[:, i * 128:(i + 1) * 128].rearrange("(k p) n -> p k n", p=128), writes=[ytk])
                xt, xk = xo_r.next()
                kb.dma('sp', xt[:], xo[i * 128:(i + 1) * 128, :], writes=[xk])
                x1, x1k = x1_r.next()
                for hf in range(2):
                    ps, pk = psA.next()
                    for k in range(8):
                        kb.op('pe', lambda e, k=k: e.matmul(ps[:], lhsT=yt[:, k, :], rhs=Wo[:, k, hf * 512:(hf + 1) * 512], start=(k == 0), stop=(k == 7)),
                              reads=[ytk, "Wo"], writes=[pk], inc=(k == 7))
                    kb.op('dve', lambda e: e.tensor_tensor(out=x1[:, hf * 512:(hf + 1) * 512], in0=ps[:], in1=GT[:, 0, hf * 512:(hf + 1) * 512], op=ALU.mult),
                          reads=[pk, "GT"], writes=[x1k])
                kb.op('pool', lambda e: e.tensor_tensor(out=x1[:], in0=x1[:], in1=xt[:], op=ALU.add), reads=[x1k, xk], writes=[x1k])
                kb.dma('sp', X1[i * 128:(i + 1) * 128, :], x1[:], reads=[x1k])
                stt, sk = st_r.next()
                rms_rstd(x1[:], x1k, stt, sk, junk, D)
                xn, nk = xn_r.next()
                kb.op('dve', lambda e: e.tensor_scalar(out=xn[:], in0=x1[:], scalar1=stt[:, 3:4], scalar2=None, op0=ALU.mult), reads=[x1k, sk], writes=[nk])
                pt, ptk = psT.next()
                for k in range(8):
                    kb.op('pe', lambda e, k=k: e.transpose(out=pt[:, k, :], in_=xn[:, k * 128:(k + 1) * 128], identity=idt[:]),
                          reads=[nk, "idt"], writes=[ptk], inc=(k == 7))
                h2, h2k = h2_r.next()
                for k in range(8):
                    if k % 2 == 0:
                        kb.op('dve', lambda e, k=k: e.tensor_scalar(out=h2[:, k, :], in0=pt[:, k, :], scalar1=G2[:, k, 0:1], scalar2=modfm[:, 2, k, 0:1],
                                                                     op0=ALU.mult, op1=ALU.add), reads=[ptk, "G2", "modfm"], writes=[h2k])
                    else:
                        kb.op('act', lambda e, k=k: e.activation(out=h2[:, k, :], in_=pt[:, k, :], func=AF.Identity, scale=G2[:, k, 0:1],
                                                                  bias=modfm[:, 2, k, 0:1]), reads=[ptk, "G2", "modfm"], writes=[h2k])
                kb.dma('sp', H2T[:, i * 128:(i + 1) * 128].rearrange("(k p) n -> p k n", p=128), h2[:], reads=[h2k])
                pl, plk = psL.next()
                for k in range(8):
                    kb.op('pe', lambda e, k=k: e.matmul(pl[:], lhsT=h2[:, k, :], rhs=Wr[:, k, :], start=(k == 0), stop=(k == 7)),
                          reads=[h2k, "Wr"], writes=[plk], inc=(k == 7))
                lg, lgk = lg_r.next()
                mx, mxk = mx_r.next()
                kb.op('dve', lambda e: e.tensor_tensor(out=lg[:, 0:NE], in0=pl[:], in1=rbb[:], op=ALU.add), reads=[plk, "rbb"], writes=[lgk])
                kb.op('dve', lambda e: e.max(out=mx[:, 0:8], in_=lg[:, 0:NE]), reads=[lgk], writes=[mxk])
                kb.op('dve', lambda e: e.tensor_scalar(out=mx[:, 8:9], in0=mx[:, 0:1], scalar1=-1.0, scalar2=None, op0=ALU.mult), reads=[mxk], writes=[mxk])
                kb.op('dve', lambda e: e.tensor_scalar(out=lg[:, NE:2 * NE], in0=lg[:, 0:NE], scalar1=mx[:, 3:4], scalar2=None, op0=ALU.is_ge),
                      reads=[lgk, mxk], writes=[lgk])
                kb.op('act', lambda e: e.activation(out=lg[:, 0:NE], in_=lg[:, 0:NE], func=AF.Exp, bias=mx[:, 8:9], scale=1.0), reads=[lgk, mxk], writes=[lgk])
                kb.op('dve', lambda e: e.tensor_tensor(out=lg[:, 0:NE], in0=lg[:, 0:NE], in1=lg[:, NE:2 * NE], op=ALU.mult), reads=[lgk], writes=[lgk])
                kb.op('dve', lambda e: e.reduce_sum(out=mx[:, 9:10], in_=lg[:, 0:NE], axis=AX.X), reads=[lgk], writes=[mxk])
                kb.op('dve', lambda e: e.reciprocal(out=mx[:, 10:11], in_=mx[:, 9:10]), reads=[mxk], writes=[mxk])
                kb.op('dve', lambda e: e.tensor_scalar(out=lg[:, 0:NE], in0=lg[:, 0:NE], scalar1=mx[:, 10:11], scalar2=None, op0=ALU.mult), reads=[lgk, mxk], writes=[lgk])
                kb.dma('sp', LG[i * 128:(i + 1) * 128, :], lg[:], reads=[lgk])
            kb.barrier()

        with ExitStack() as ph, Stage('5') as go:
          if go:
            sbp = lambda name, shape: ph.enter_context(nc.sbuf_tensor(name, shape, F32))
            HT = 512
            h2T = sbp("h2T", [128, 8, HT])
            acc = sbp("acc", [128, 4, D])
            gts = sbp("gts", [128, 4, NE])
            gT = sbp("gT", [NE, 4, 128])
            b1t = sbp("b1t", [128, NE * 16])
            kb.dma('sp', b1t[:], b1fm, writes=["b1t"])
            b2t = sbp("b2t", [NE, D])
            kb.dma('sp', b2t[:], b2, writes=["b2t"])
            wp_r = Rot(nc, ph, "wp", [128, 8, 512], 3)
            w2_r = Rot(nc, ph, "w2p", [128, 4, D], 3)
            actT = sbp("actT", [128, 8, 512])
            ga_r = Rot(nc, ph, "ga", [128, 512], 3)
            sg_r = Rot(nc, ph, "sgm", [128, 512], 3)
            li_r = Rot(nc, ph, "li", [128, 512], 3)
            psU = Rot(nc, ph, "psU", [128, 512], 4, psum=True)
            psY = Rot(nc, ph, "psY", [128, 512], 3, psum=True)
            psG = Rot(nc, ph, "psG", [NE, 128], 1, psum=True)
            for half in range(QT // HT):
                t0 = half * HT
                kb.dma('sp', h2T[:], H2T[:, t0:t0 + HT].rearrange("(k p) n -> p k n", p=128), writes=["h2T"])
                for i in range(4):
                    kb.dma('sp', gts[:, i, :], LG[t0 + i * 128:t0 + (i + 1) * 128, 0:NE], writes=["gts"])
                for i in range(4):
                    pg, pgk = psG.next()
                    kb.op('pe', lambda e: e.transpose(out=pg[:], in_=gts[:, i, :], identity=idt[:]), reads=["gts", "idt"], writes=[pgk])
                    kb.op('act', lambda e: e.copy(out=gT[:, i, :], in_=pg[:]), reads=[pgk], writes=["gT"])
                for i in range(4):
                    for hf in range(2):
                        ps, pk = psY.next()
                        kb.op('pe', lambda e: e.matmul(ps[:], lhsT=gT[:, i, :], rhs=b2t[:, hf * 512:(hf + 1) * 512], start=True, stop=True),
                              reads=["gT", "b2t"], writes=[pk])
                        kb.op('dve', lambda e: e.tensor_copy(out=acc[:, i, hf * 512:(hf + 1) * 512], in_=ps[:]), reads=[pk], writes=[("acc", i)])
                for ex in range(NE):
                    w2p = []
                    for j2 in range(2):
                        wt, wk = w2_r.next()
                        kb.dma('sp', wt[:], w2[ex, j2 * 512:(j2 + 1) * 512, :].rearrange("(k p) n -> p k n", p=128), writes=[wk])
                        w2p.append((wt, wk))
                    for tb in range(1):
                        for pc in range(4):
                            wt, wk = wp_r.next()
                            kb.dma('sp', wt[:, :, 0:256], w1[ex, :, pc * 256:(pc + 1) * 256].rearrange("(k p) n -> p k n", p=128), writes=[wk])
                            kb.dma('sp', wt[:, :, 256:512], w1[ex, :, D + pc * 256:D + (pc + 1) * 256].rearrange("(k p) n -> p k n", p=128), writes=[wk])
                            for jj in range(2):
                                j = pc * 2 + jj
                                pgl, pglk = psU.next()
                                pli, plik = psU.next()
                                for k in range(8):
                                    kb.op('pe', lambda e, k=k: e.matmul(pgl[:], lhsT=wt[:, k, jj * 128:(jj + 1) * 128], rhs=h2T[:, k, tb * 512:(tb + 1) * 512],
                                                                        start=(k == 0), stop=(k == 7)), reads=[wk, "h2T"], writes=[pglk], inc=(k == 7))
                                for k in range(8):
                                    kb.op('pe', lambda e, k=k: e.matmul(pli[:], lhsT=wt[:, k, 256 + jj * 128:256 + (jj + 1) * 128], rhs=h2T[:, k, tb * 512:(tb + 1) * 512],
                                                                        start=(k == 0), stop=(k == 7)), reads=[wk, "h2T"], writes=[plik], inc=(k == 7))
                                bg = b1t[:, ex * 16 + j:ex * 16 + j + 1]
                                bl = b1t[:, ex * 16 + 8 + j:ex * 16 + 8 + j + 1]
                                ga, gak = ga_r.next()
                                kb.op('dve', lambda e: e.tensor_scalar(out=ga[:], in0=pgl[:], scalar1=bg, scalar2=7.0, op0=ALU.add, op1=ALU.min),
                                      reads=[pglk, "b1t"], writes=[gak])
                                sg, sgk = sg_r.next()
                                kb.op('act', lambda e: e.activation(out=sg[:], in_=ga[:], func=AF.Sigmoid, scale=1.702), reads=[gak], writes=[sgk])
                                li, lik = li_r.next()
                                kb.op('dve', lambda e: e.tensor_scalar(out=li[:], in0=pli[:], scalar1=bl, scalar2=7.0, op0=ALU.add, op1=ALU.min),
                                      reads=[plik, "b1t"], writes=[lik])
                                kb.op('pool', lambda e: e.tensor_scalar(out=li[:], in0=li[:], scalar1=-7.0, scalar2=1.0, op0=ALU.max, op1=ALU.add),
                                      reads=[lik], writes=[lik])
                                kb.op('pool', lambda e: e.tensor_tensor(out=ga[:], in0=ga[:], in1=sg[:], op=ALU.mult), reads=[gak, sgk], writes=[gak])
                                kb.op('pool', lambda e, j=j: e.tensor_tensor(out=actT[:, j, :], in0=ga[:], in1=li[:], op=ALU.mult), reads=[gak, lik], writes=[("actT", j)])
                        for ti in range(4):
                            i = tb * 4 + ti
                            for hf in range(2):
                                ps, pk = psY.next()
                                for j in range(8):
                                    wt2, wk2 = w2p[j // 4]
                                    kb.op('pe', lambda e, j=j: e.matmul(ps[:], lhsT=actT[:, j, ti * 128:(ti + 1) * 128], rhs=wt2[:, j % 4, hf * 512:(hf + 1) * 512],
                                                                        start=(j == 0), stop=(j == 7)), reads=[("actT", j), wk2], writes=[pk], inc=(j == 7))
                                kb.op('dve', lambda e: e.scalar_tensor_tensor(out=acc[:, i, hf * 512:(hf + 1) * 512], in0=ps[:], scalar=gts[:, i, ex:ex + 1],
                                                                               in1=acc[:, i, hf * 512:(hf + 1) * 512], op0=ALU.mult, op1=ALU.add),
                                      reads=[pk, "gts", ("acc", i)], writes=[("acc", i)])
                for i in range(4):
                    kb.dma('sp', FF[t0 + i * 128:t0 + (i + 1) * 128, :], acc[:, i, :], reads=[("acc", i)])
            kb.barrier()

        with ExitStack() as ph, Stage('6') as go:
          if go:
            sbp = lambda name, shape: ph.enter_context(nc.sbuf_tensor(name, shape, F32))
            gfb = sbp("gfb", [128, D])
            kb.dma('sp', gfb[:], gfin.partition_broadcast(128), writes=["gfb"])
            junk = sbp("junk6", [128, 1024])
            x1_r = Rot(nc, ph, "x16", [128, 1024], 2)
            ff_r = Rot(nc, ph, "ff6", [128, 1024], 2)
            st_r = Rot(nc, ph, "st6", [128, 4], 3)
            o_r = Rot(nc, ph, "o6", [128, 1024], 2)
            for i in range(QT // 128):
                x1, x1k = x1_r.next()
                ff, ffk = ff_r.next()
                kb.dma('sp', x1[:], X1[i * 128:(i + 1) * 128, :], writes=[x1k])
                kb.dma('sp', ff[:], FF[i * 128:(i + 1) * 128, :], writes=[ffk])
                kb.op('dve', lambda e: e.tensor_tensor(out=ff[:], in0=ff[:], in1=GT[:, 1, :], op=ALU.mult), reads=[ffk, "GT"], writes=[ffk])
                kb.op('pool', lambda e: e.tensor_tensor(out=x1[:], in0=x1[:], in1=ff[:], op=ALU.add), reads=[ffk, x1k], writes=[x1k])
                stt, sk = st_r.next()
                rms_rstd(x1[:], x1k, stt, sk, junk, D)
                o, ok = o_r.next()
                kb.op('dve', lambda e: e.scalar_tensor_tensor(out=o[:], in0=x1[:], scalar=stt[:, 3:4], in1=gfb[:], op0=ALU.mult, op1=ALU.mult),
                      reads=[x1k, sk, "gfb"], writes=[ok])
                kb.dma('sp', out[i * 128:(i + 1) * 128, :], o[:], reads=[ok])
            kb.barrier()
        print("instructions", kb.nins, "dmas", kb.ndma, "cnt", kb.cnt)
    return nc


def rope_tables(tok):
    fr = (10000.0 ** (-np.arange(16, dtype=np.float32) / 16)).astype(np.float32)
    pos = [(tok // 64).astype(np.float32), (tok % 64).astype(np.float32)]
    C = np.zeros((64, len(tok)), np.float32)
    S = np.zeros((64, len(tok)), np.float32)
    for a in range(2):
        ang = (pos[a][None, :] * fr[:, None]).astype(np.float32)
        for hf in range(2):
            r0 = a * 32 + hf * 16
            C[r0:r0 + 16] = np.cos(ang)
            S[r0:r0 + 16] = np.sin(ang) * (-1.0 if hf == 0 else 1.0)
    return C, S


def make_inputs(inp, core):
    b, q = core // 4, core % 4
    f = lambda a: np.ascontiguousarray(a, dtype=np.float32)
    w = inp["w_in"][0]
    perm = np.arange(64).reshape(2, 2, 16)[:, ::-1, :].reshape(64)
    kr = w[:, 1792 + 384:1792 + 448]
    wcat = np.concatenate([w[:, :1792], w[:, 1792 + 256:1792 + 384], kr, kr[:, perm], w[:, 1792:1792 + 256]], axis=1)
    cv = np.stack([inp["c"][b].reshape(8, 128).T, inp["c_ctx"].reshape(8, 128).T], axis=2).reshape(128, 16)
    xb = inp["x"][b]
    xo = np.zeros((XO_ROWS, D), np.float32)
    xo[:QT] = xb[q * QT:(q + 1) * QT]
    if q > 0:
        xo[QT] = xb[q * QT - 1]
    if q < 3:
        xo[QT + 1] = xb[(q + 1) * QT]
    C, S = rope_tables(np.arange(T))
    wukv = inp["mla_w_ukv"][0].reshape(128, 4, 256)
    wuq = inp["mla_w_uq"][0].reshape(256, 4, 192)
    wuq_p = np.concatenate([wuq[:, :, :128], wuq[:, :, 128:], wuq[:, :, 128:][:, :, perm]], axis=2).reshape(256, 1024)
    w1 = inp["exp_w1"][0]
    w1d = np.concatenate([w1[:, :, 0::2], w1[:, :, 1::2]], axis=2)
    b1 = inp["exp_b1"][0]
    b1d = np.concatenate([b1[:, 0::2], b1[:, 1::2]], axis=1)
    b1fm = b1d.reshape(NE, 16, 128).transpose(2, 0, 1).reshape(128, NE * 16)
    d = {
        "xs": f(np.concatenate([inp["ctx"][b], xb], axis=0)),
        "xo": f(xo),
        "cvec": f(cv),
        "mod_w": f(inp["mod_w"][0]),
        "mod_b": f(inp["mod_b"][0]),
        "g1": f(inp["norm1_g"][0]),
        "g2n": f(inp["norm2_g"][0]),
        "gfin": f(inp["final_norm_g"]),
        "w_in": f(wcat),
        "ident": np.eye(128, dtype=np.float32),
        "ropeC": f(C), "ropeS": f(S),
        "ropeCo": f(C[:, q * QT:(q + 1) * QT]), "ropeSo": f(S[:, q * QT:(q + 1) * QT]),
        "kvg": f(inp["mla_kv_norm"][0].reshape(128, 1)),
        "qg": f(inp["mla_q_norm"][0].reshape(2, 128).T),
        "wukv_k": f(wukv[:, :, :128].reshape(128, 512)),
        "wukv_v": f(wukv[:, :, 128:].reshape(128, 512)),
        "wuq": f(wuq_p),
        "w_out": f(inp["w_out"][0]),
        "router_w": f(inp["router_w"][0]),
        "router_b": f(inp["router_b"][0]),
        "w1": f(w1d) if "5" in RUN else None,
        "b1fm": f(b1fm),
        "w2": f(inp["exp_w2"][0]) if "5" in RUN else None,
        "b2": f(inp["exp_b2"][0]),
    }
    if '2' in RUN:
        rc = rwkv_consts(q)
        rc.update(mu=inp["rwkv_mu"][0], w0=inp["rwkv_w0"][0], a0=inp["rwkv_a0"][0], w2=inp["rwkv_w2"][0], a2=inp["rwkv_a2"][0],
                  k_k=inp["rwkv_k_k"][0], k_a=inp["rwkv_k_a"][0], r_k=inp["rwkv_r_k"][0].reshape(512), ln_w=inp["rwkv_ln_w"][0],
                  ln_b=inp["rwkv_ln_b"][0], g2=inp["rwkv_g2"][0])
        for k_, v_ in rc.items():
            d["rw_" + k_] = f(v_)
    return {k: v for k, v in d.items() if v is not None}


def kernel(**inputs):
    inp = {k: np.asarray(v) for k, v in inputs.items()}
    nc = build()
    in_maps = [make_inputs(inp, c) for c in range(8)]
    if DEBUG:
        for c in range(8):
            if '2' not in RUN:
                in_maps[c]["yrw_in"] = kernel.dbg_yrw[c]
    res = run_bass_kernel_spmd(nc, in_maps, core_ids=list(range(8)))
    outp = np.zeros((2, T, D), np.float32)
    for c in range(8):
        b, q = c // 4, c % 4
        outp[b, q * QT:(q + 1) * QT] = res.results[c]["out"]
    if DEBUG:
        kernel.debug = res.results
    return outp
```

```python
import os
import numpy as np
from contextlib import ExitStack
import concourse.bass as bass
import concourse.mybir as mybir
from concourse.bass_utils import run_bass_kernel_spmd

F32 = mybir.dt.float32
AF = mybir.ActivationFunctionType
ALU = mybir.AluOpType
AX = mybir.AxisListType

D = 1024
T = 8192
CTX = 256
TS = CTX + T
QT = 2048
NBLK_FULL = [(0, 256)] + [(256 + i * 512, 512) for i in range(16)]
WCOLS = 2304
DEBUG = os.environ.get("KDEBUG", "")


class KB:
    NRING = 12

    def __init__(self, nc, stack, same_engine_sync=True):
        self.nc = nc
        self.stack = stack
        self.E = {'pe': nc.tensor, 'dve': nc.vector, 'act': nc.scalar, 'pool': nc.gpsimd, 'sp': nc.sync}
        self.sem = {e: stack.enter_context(nc.semaphore("s_" + e)) for e in ('pe', 'dve', 'act', 'pool')}
        self.cnt = {e: 0 for e in self.sem}
        self.ring = [stack.enter_context(nc.semaphore("d%d" % i)) for i in range(self.NRING)]
        self.ndma = 0
        self.seen = {e: {} for e in self.E}
        self.res = {}
        self.pend = {e: ([], []) for e in self.E}
        self.ses = same_engine_sync
        self.nins = 0
        for s_ in list(self.sem.values()) + self.ring:
            nc.gpsimd.sem_clear(s_)
        nc.all_engine_barrier()

    def _wait(self, eng, sem, val):
        k = sem.name
        if self.seen[eng].get(k, 0) >= val:
            return
        self.E[eng].wait_ge(sem, val)
        self.seen[eng][k] = val

    def _deps(self, eng, reads, writes):
        deps = []
        for r in reads:
            st = self.res.get(r)
            if st and st[0]:
                deps.append(st[0])
        for w in writes:
            st = self.res.get(w)
            if st:
                if st[0]:
                    deps.append(st[0])
                deps.extend(st[1].values())
        own = self.sem.get(eng)
        for (sem, val) in deps:
            if own is not None and sem.name == own.name and (eng == 'pe' or not self.ses):
                continue
            self._wait(eng, sem, val)

    def _record(self, tok, reads, writes):
        for r in reads:
            st = self.res.setdefault(r, [None, {}])
            old = st[1].get(tok[0].name)
            if old is None or old[1] < tok[1]:
                st[1][tok[0].name] = tok
        for w in writes:
            self.res[w] = [tok, {}]

    def op(self, eng, fn, reads=(), writes=(), inc=True):
        self._deps(eng, reads, writes)
        ins = fn(self.E[eng])
        self.nins += 1
        pr, pw = self.pend[eng]
        pr.extend(reads)
        pw.extend(writes)
        if inc:
            self.cnt[eng] += 1
            ins.then_inc(self.sem[eng], 1)
            self._record((self.sem[eng], self.cnt[eng]), pr, pw)
            self.pend[eng] = ([], [])
        return ins

    def dma(self, q, out, in_, reads=(), writes=(), **kw):
        i = self.ndma
        self.ndma += 1
        sem = self.ring[i % self.NRING]
        val = 16 * (i // self.NRING + 1)
        if val > 16:
            self._wait(q, sem, val - 16)
        self._deps(q, reads, writes)
        ins = self.E[q].dma_start(out=out, in_=in_, **kw).then_inc(sem, 16)
        self.nins += 1
        self._record((sem, val), list(reads), list(writes))
        return ins

    def barrier(self, engines=('pe', 'dve', 'act', 'pool', 'sp')):
        for e in engines:
            for o, s in self.sem.items():
                if o != e and self.cnt[o] > 0:
                    self._wait(e, s, self.cnt[o])
            for j, s in enumerate(self.ring):
                n = (self.ndma - 1 - j) // self.NRING + 1 if self.ndma > j else 0
                if n > 0:
                    self._wait(e, s, 16 * n)


class Rot:
    def __init__(self, nc, st, name, shape, n, dtype=F32, psum=False):
        mk = nc.psum_tensor if psum else nc.sbuf_tensor
        self.t = [st.enter_context(mk("%s%d" % (name, i), shape, dtype)) for i in range(n)]
        self.name = name
        self.i = 0

    def next(self):
        j = self.i % len(self.t)
        self.i += 1
        return self.t[j], (self.name, j)


KRW_BLKS = int(os.environ.get('KRW_BLKS', '99'))
KRW_BLK0 = int(os.environ.get('KRW_BLK0', '0'))
KRW_BSTOP = int(os.environ.get('KRW_BSTOP', '99'))
KRW_NOB = int(os.environ.get('KRW_NOB', '0'))
KRW_NOCHAIN = int(os.environ.get('KRW_NOCHAIN', '0'))
KRW_MARK = int(os.environ.get('KRW_MARK', '99'))


class StopRegion(Exception):
    pass


def mark(i):
    if i >= KRW_MARK:
        raise StopRegion()


CDEC = 0.6065306597126334
GN_EPS = 64e-5
FULL_BLKS = [(0, 256, True, True)] + [(256 + i * 512, 512, i == 0, i == 15) for i in range(16)]
OWN_RBLKS = [(i * 512, 512, i == 0, i == 3) for i in range(4)]
NCH_FULL = TS // 64
NCH_OWN = QT // 64


def rwkv_stage(nc, kb, st, pT, poT, yT, C, dscr):
    idt = C["idt"]
    GTs = dscr("GTs", [2, 4, 128, NCH_FULL, 128])
    Nsc = dscr("Nsc", [2, 4, 128, NCH_FULL, 128])
    GTo = dscr("GTo", [2, 4, 128, NCH_OWN, 128])
    Nso = dscr("Nso", [2, 4, 128, NCH_OWN, 128])
    RhTo = dscr("RhTo", [2, 4, 128, NCH_OWN, 128])
    Oho = dscr("Oho", [2, 4, 128, NCH_OWN, 128])
    BONs = dscr("BONs", [4, 128, QT])
    Gsc = dscr("Gsc", [4, 128, QT])
    if os.environ.get('KRW_ALLOC_ONLY'):
        return
    with ExitStack() as ph:
        sbp = lambda name, shape: ph.enter_context(nc.sbuf_tensor(name, shape, F32))
        def ld(name, shape, src, **kw):
            t = sbp(name, shape)
            kb.dma('sp', t[:], src, writes=[name], **kw)
            return t
        mu_t = ld("mu_t", [128, 14], C["mu"].rearrange("(c p) -> p c", p=128), allow_slow_non_contiguous=True)
        om_t = sbp("om_t", [128, 14]); hm_t = sbp("hm_t", [128, 14])
        kb.op('dve', lambda e: e.tensor_scalar(out=om_t[:], in0=mu_t[:], scalar1=-1.0, scalar2=1.0, op0=ALU.mult, op1=ALU.add), reads=["mu_t"], writes=["om_t"])
        kb.op('dve', lambda e: e.tensor_scalar(out=hm_t[:], in0=mu_t[:], scalar1=0.5, scalar2=None, op0=ALU.mult), reads=["mu_t"], writes=["hm_t"])
        w0_t = ld("w0_t", [128, 2, 4], C["w0"].rearrange("d (c p) -> p d c", p=128), allow_slow_non_contiguous=True)
        a0_t = ld("a0_t", [128, 2, 4], C["a0"].rearrange("d (c p) -> p d c", p=128), allow_slow_non_contiguous=True)
        W2A2 = sbp("W2A2", [128, 2, 512])
        kb.dma('sp', W2A2[0:64], C["w2"].rearrange("d l c -> l d c"), writes=["W2A2"])
        kb.dma('sp', W2A2[64:128], C["a2"].rearrange("d l c -> l d c"), writes=["W2A2"])
        kk_t = ld("kk_t", [128, 4], C["k_k"].rearrange("(c p) -> p c", p=128), allow_slow_non_contiguous=True)
        ka_t = ld("ka_t", [128, 4], C["k_a"].rearrange("(c p) -> p c", p=128), allow_slow_non_contiguous=True)
        oka_t = sbp("oka_t", [128, 4])
        kb.op('dve', lambda e: e.tensor_scalar(out=oka_t[:], in0=ka_t[:], scalar1=-1.0, scalar2=1.0, op0=ALU.mult, op1=ALU.add), reads=["ka_t"], writes=["oka_t"])
        rk_t = ld("rk_t", [128, 4], C["r_k"].rearrange("(c p) -> p c", p=128), allow_slow_non_contiguous=True)
        lnw_t = ld("lnw_t", [128, 4], C["ln_w"].rearrange("(c p) -> p c", p=128), allow_slow_non_contiguous=True)
        lnb_t = ld("lnb_t", [128, 4], C["ln_b"].rearrange("(c p) -> p c", p=128), allow_slow_non_contiguous=True)
        g2_t = ld("g2_t", [128, 512], C["g2"])
        MASK4 = ld("MASK4", [128, 2, 512], C["mask4"].rearrange("d p n -> p d n"))
        MASKL = ld("MASKL", [128, 2, 128], C["maskl"].rearrange("d p n -> p d n"))
        BLK = ld("BLK", [128, 128], C["blk"])
        UU = ld("UU", [128, 64], C["uu"])
        SEL = ld("SEL", [128, 64], C["sel"])
        selF = ld("selF", [128, 4], C["selF"])
        selB = ld("selB", [128, 4], C["selB"])
        hal = ld("hal", [128, 2], C["hal"])
        if os.environ.get('KRW_STOP') == '1':
            kb.barrier()
            return
        P_r = Rot(nc, ph, "Pl", [128, 514], 3)
        sh_r = Rot(nc, ph, "shf", [128, 512], 4)
        RS_r = Rot(nc, ph, "RSs", [128, 512], 2)
        KS_r = Rot(nc, ph, "KSs", [128, 512], 2)
        VS_r = Rot(nc, ph, "VSs", [128, 512], 2)
        KK_r = Rot(nc, ph, "KKs", [128, 512], 2)
        X12_r = Rot(nc, ph, "X12", [128, 512], 2)
        TX_r = Rot(nc, ph, "TXs", [128, 512], 2)
        dA_r = Rot(nc, ph, "dA", [128, 512], 14)
        bs_r = Rot(nc, ph, "bsr", [128, 512], 2)
        pl_r = Rot(nc, ph, "plr", [128, 8], 4)
        ex_r = {nm: Rot(nc, ph, "ex" + nm, [128, 8, 2, 64], 2) for nm in ("A", "R", "B", "K", "Bp", "Kp")}
        exV_r = Rot(nc, ph, "exV", [128, 8, 2, 64], 2)
        for r_ in list(ex_r.values()) + [exV_r]:
            for j_, t_ in enumerate(r_.t):
                kb.op('pool', lambda e, t_=t_: e.memset(t_[:], 0.0), writes=[(r_.name, j_)])
        psA = Rot(nc, ph, "psRA", [128, 512], 3, psum=True)
        psB = Rot(nc, ph, "psRB", [128, 512], 5, psum=True)
        AT4_r = Rot(nc, ph, "AT4", [128, 4, 128], 2)
        AB_r = Rot(nc, ph, "ABk", [128, 2, 128], 4)
        XT_r = Rot(nc, ph, "XTk", [128, 128], 3)
        TM_r = Rot(nc, ph, "TMk", [128, 4, 128], 2)
        AU_r = Rot(nc, ph, "AUk", [128, 2, 128], 2)
        stG_r = Rot(nc, ph, "stG", [128, 8, 128], 1)
        stN_r = Rot(nc, ph, "stN", [128, 8, 128], 1)
        stR_r = Rot(nc, ph, "stR", [128, 8, 128], 1)
        stO_r = Rot(nc, ph, "stO", [128, 8, 128], 1)
        print('RWKV region sbuf remaining', nc.sbuf_bytes_remaining, nc.SBUF_PARTITION_SIZE_BYTES)
        cnt = {"ev": 0}

        def evac(out_ap, in_ap, reads, writes):
            cnt["ev"] += 1
            if True:
                kb.op('dve', lambda e: e.tensor_copy(out=out_ap, in_=in_ap), reads=reads, writes=writes)
            else:
                kb.op('act', lambda e: e.copy(out=out_ap, in_=in_ap), reads=reads, writes=writes)

        def load_shift(src, row0, c0, n, lb, rb, own, dst, dk, cc):
            P, pk = P_r.next()
            lo = c0 - (0 if lb else 1)
            hi = c0 + n + (0 if rb else 1)
            doff = 1 if lb else 0
            kb.dma('sp', P[:, doff:doff + (hi - lo)], src[row0:row0 + 128, lo:hi], writes=[pk])
            if lb:
                if own:
                    kb.dma('sp', P[:, 0:1], src[row0:row0 + 128, QT:QT + 1], writes=[pk], allow_slow_non_contiguous=True)
                    kb.op('dve', lambda e: e.tensor_scalar(out=P[:, 0:1], in0=P[:, 0:1], scalar1=hal[:, 0:1], scalar2=None, op0=ALU.mult), reads=[pk, "hal"], writes=[pk])
                else:
                    kb.op('pool', lambda e: e.memset(P[:, 0:1], 0.0), writes=[pk])
            if rb:
                if own:
                    kb.dma('sp', P[:, n + 1:n + 2], src[row0:row0 + 128, QT + 1:QT + 2], writes=[pk], allow_slow_non_contiguous=True)
                    kb.op('dve', lambda e: e.tensor_scalar(out=P[:, n + 1:n + 2], in0=P[:, n + 1:n + 2], scalar1=hal[:, 1:2], scalar2=None, op0=ALU.mult), reads=[pk, "hal"], writes=[pk])
                else:
                    kb.op('pool', lambda e: e.memset(P[:, n + 1:n + 2], 0.0), writes=[pk])
            t, tk = sh_r.next()
            kb.op('pool', lambda e: e.tensor_tensor(out=t[:, :n], in0=P[:, 0:n], in1=P[:, 2:n + 2], op=ALU.add), reads=[pk], writes=[tk])
            u, uk = sh_r.next()
            kb.op('dve', lambda e: e.tensor_scalar(out=u[:, :n], in0=P[:, 1:n + 1], scalar1=om_t[:, cc:cc + 1], scalar2=None, op0=ALU.mult), reads=[pk, "om_t"], writes=[uk])
            kb.op('dve', lambda e: e.scalar_tensor_tensor(out=dst[:, :n], in0=t[:, :n], scalar=hm_t[:, cc:cc + 1], in1=u[:, :n], op0=ALU.mult, op1=ALU.add),
                  reads=[tk, uk, "hm_t"], writes=[dk])

        def c3(ap, n):
            return ap.rearrange("p (c t) -> p c t", t=64)

        def exp_write(eng, dst, dk, nch, n, fn, reads):
            for hh in range(2):
                sl = slice(hh * 64, hh * 64 + 64)
                kb.op(eng, lambda e, hh=hh, sl=sl: fn(e, dst[sl, :nch, hh, :], sl), reads=reads, writes=[dk])

        def region(src, blocks, own, GTd, Nd, RhTd, Ohd, chunk0_of_block):
            for bi, (c0, n, lb, rb) in list(enumerate(blocks))[KRW_BLK0:KRW_BLK0 + KRW_BLKS]:
                nch = n // 64
                ch0 = chunk0_of_block(bi)
                X12, xk = X12_r.next()
                load_shift(src, 1536, c0, n, lb, rb, own, X12, xk, 12)
                TX, txk = TX_r.next()
                kb.op('act', lambda e: e.activation(out=TX[0:64, :n], in_=X12[0:64, :n], func=AF.Tanh), reads=[xk], writes=[txk])
                mark(1)
                if own:
                    XG, xgk = dA_r.next()
                    load_shift(src, 1664, c0, n, lb, rb, own, XG, xgk, 13)
                    SGg, sggk = TX_r.next()
                    kb.op('act', lambda e: e.activation(out=SGg[:, :n], in_=XG[:, :n], func=AF.Sigmoid), reads=[xgk], writes=[sggk])
                for hp in range(4):
                    RS, rsk = RS_r.next(); KS, ksk = KS_r.next(); VS, vsk = VS_r.next(); KK, kkk = KK_r.next()
                    load_shift(src, hp * 128, c0, n, lb, rb, own, RS, rsk, hp)
                    load_shift(src, 512 + hp * 128, c0, n, lb, rb, own, KS, ksk, 4 + hp)
                    load_shift(src, 1024 + hp * 128, c0, n, lb, rb, own, VS, vsk, 8 + hp)
                    mark(2)
                    kkr, kkrk = dA_r.next()
                    kb.op('dve', lambda e: e.tensor_scalar(out=kkr[:, :n], in0=KS[:, :n], scalar1=kk_t[:, hp:hp + 1], scalar2=None, op0=ALU.mult), reads=[ksk, "kk_t"], writes=[kkrk])
                    sq, sqk = dA_r.next()
                    kb.op('pool', lambda e: e.tensor_tensor(out=sq[:, :n], in0=kkr[:, :n], in1=kkr[:, :n], op=ALU.mult), reads=[kkrk], writes=[sqk])
                    pss, pssk = psA.next()
                    kb.op('pe', lambda e: e.matmul(pss[:, :n], lhsT=BLK[:], rhs=sq[:, :n], start=True, stop=True), reads=["BLK", sqk], writes=[pssk])
                    kb.op('dve', lambda e: e.tensor_scalar(out=sq[:, :n], in0=pss[:, :n], scalar1=1e-24, scalar2=None, op0=ALU.max), reads=[pssk], writes=[sqk])
                    kb.op('act', lambda e: e.activation(out=sq[:, :n], in_=sq[:, :n], func=AF.Sqrt), reads=[sqk], writes=[sqk])
                    kb.op('dve', lambda e: e.reciprocal(out=sq[:, :n], in_=sq[:, :n]), reads=[sqk], writes=[sqk])
                    kb.op('pool', lambda e: e.tensor_tensor(out=KK[:, :n], in0=kkr[:, :n], in1=sq[:, :n], op=ALU.mult), reads=[kkrk, sqk], writes=[kkk])
                    mark(3)
                    Vd, vdk = exV_r.next()
                    exp_write('pool', Vd, vdk, nch, n, lambda e, o, sl: e.tensor_copy(out=o, in_=c3(VS[sl, :n], n)), [vsk])
                    mark(4)
                    if own:
                        bsum, bsk = bs_r.next()
                    for d in range(2):
                        psw, pswk = psA.next()
                        kb.op('pe', lambda e: e.matmul(psw[:, :n], lhsT=W2A2[0:64, d, hp * 128:(hp + 1) * 128], rhs=TX[0:64, :n], start=True, stop=True),
                              reads=["W2A2", txk], writes=[pswk])
                        SGM, sgk = dA_r.next()
                        kb.op('act', lambda e: e.activation(out=SGM[:, :n], in_=psw[:, :n], func=AF.Sigmoid, bias=w0_t[:, d, hp:hp + 1], scale=1.0), reads=[pswk, "w0_t"], writes=[sgk])
                        psa, psak = psA.next()
                        kb.op('pe', lambda e: e.matmul(psa[:, :n], lhsT=W2A2[64:128, d, hp * 128:(hp + 1) * 128], rhs=X12[64:128, :n], start=True, stop=True),
                              reads=["W2A2", xk], writes=[psak])
                        AA, aak = dA_r.next()
                        kb.op('act', lambda e: e.activation(out=AA[:, :n], in_=psa[:, :n], func=AF.Sigmoid, bias=a0_t[:, d, hp:hp + 1], scale=1.0), reads=[psak, "a0_t"], writes=[aak])
                        mark(5)
                        CIN, cik = dA_r.next()
                        T1, t1k = sh_r.next()
                        src_, srck_ = SGM, sgk
                        for si, s_ in enumerate((1, 2, 4, 8, 16, 32)):
                            dst_, dstk_ = (T1, t1k) if si % 2 == 0 else (CIN, cik)
                            kb.op('pool', lambda e, s_=s_, src_=src_, dst_=dst_: e.tensor_tensor(out=c3(dst_[:, :n], n)[:, :, s_:], in0=c3(src_[:, :n], n)[:, :, s_:],
                                                                                                in1=c3(src_[:, :n], n)[:, :, :64 - s_], op=ALU.add),
                                  reads=[srck_], writes=[dstk_])
                            kb.op('act', lambda e, s_=s_, src_=src_, dst_=dst_: e.copy(out=c3(dst_[:, :n], n)[:, :, :s_], in_=c3(src_[:, :n], n)[:, :, :s_]),
                                  reads=[srck_], writes=[dstk_])
                            src_, srck_ = dst_, dstk_
                        mark(6)
                        CEX, cek = dA_r.next()
                        kb.op('pool', lambda e: e.tensor_tensor(out=CEX[:, :n], in0=CIN[:, :n], in1=SGM[:, :n], op=ALU.subtract), reads=[cik, sgk], writes=[cek])
                        totb = c3(CIN[:, :n], n)[:, :, 63:64].to_broadcast([128, nch, 64])
                        Dm, dmk = dA_r.next()
                        kb.op('dve', lambda e: e.tensor_tensor(out=c3(Dm[:, :n], n), in0=totb, in1=c3(CIN[:, :n], n), op=ALU.subtract), reads=[cik], writes=[dmk])
                        DX, dxk = dA_r.next()
                        kb.op('dve', lambda e: e.tensor_tensor(out=c3(DX[:, :n], n), in0=totb, in1=c3(CEX[:, :n], n), op=ALU.subtract), reads=[cik, cek], writes=[dxk])
                        srcs = {0: ((CIN, cik, -CDEC), (CEX, cek, -CDEC), (CIN, cik, CDEC), (Dm, dmk, -CDEC)),
                                1: ((DX, dxk, -CDEC), (Dm, dmk, -CDEC), (DX, dxk, CDEC), (CEX, cek, -CDEC))}[d]
                        E4 = []
                        for (s_, sk_, sc_) in srcs:
                            o_, ok_ = dA_r.next()
                            kb.op('act', lambda e, s_=s_, o_=o_, sc_=sc_: e.activation(out=o_[:, :n], in_=s_[:, :n], func=AF.Exp, scale=sc_), reads=[sk_], writes=[ok_])
                            E4.append((o_, ok_))
                        (PIN, pik), (PEX, pek), (INV, ink), (EEND, eek) = E4
                        PL, plk = pl_r.next()
                        kb.op('act', lambda e: e.activation(out=PL[:, :nch], in_=CIN[:, 63:n:64], func=AF.Exp, scale=-CDEC), reads=[cik], writes=[plk])
                        mark(7)
                        KD, kdk = dA_r.next()
                        kb.op('dve', lambda e: e.tensor_scalar(out=KD[:, :n], in0=AA[:, :n], scalar1=ka_t[:, hp:hp + 1], scalar2=oka_t[:, hp:hp + 1], op0=ALU.mult, op1=ALU.add),
                              reads=[aak, "ka_t", "oka_t"], writes=[kdk])
                        kb.op('pool', lambda e: e.tensor_tensor(out=KD[:, :n], in0=KD[:, :n], in1=KS[:, :n], op=ALU.mult), reads=[kdk, ksk], writes=[kdk])
                        Bv, bvk = dA_r.next()
                        kb.op('pool', lambda e: e.tensor_tensor(out=Bv[:, :n], in0=KK[:, :n], in1=AA[:, :n], op=ALU.mult), reads=[kkk, aak], writes=[bvk])
                        if own:
                            if d == 0:
                                kb.op('pool', lambda e: e.tensor_tensor(out=bsum[:, :n], in0=RS[:, :n], in1=KD[:, :n], op=ALU.mult), reads=[rsk, kdk], writes=[bsk])
                            else:
                                t_, tk_ = sh_r.next()
                                kb.op('pool', lambda e: e.tensor_tensor(out=t_[:, :n], in0=RS[:, :n], in1=KD[:, :n], op=ALU.mult), reads=[rsk, kdk], writes=[tk_])
                                kb.op('pool', lambda e: e.tensor_tensor(out=bsum[:, :n], in0=bsum[:, :n], in1=t_[:, :n], op=ALU.add), reads=[bsk, tk_], writes=[bsk])
                        mark(8)
                        ex = {nm: ex_r[nm].next() for nm in ex_r}
                        exp_write('dve', ex["A"][0], ex["A"][1], nch, n,
                                  lambda e, o, sl: e.scalar_tensor_tensor(out=o, in0=c3(KK[sl, :n], n), scalar=-1.0, in1=c3(PEX[sl, :n], n), op0=ALU.mult, op1=ALU.mult), [kkk, pek])
                        exp_write('pool', ex["R"][0], ex["R"][1], nch, n, lambda e, o, sl: e.tensor_tensor(out=o, in0=c3(RS[sl, :n], n), in1=c3(PIN[sl, :n], n), op=ALU.mult), [rsk, pik])
                        exp_write('dve', ex["B"][0], ex["B"][1], nch, n, lambda e, o, sl: e.tensor_tensor(out=o, in0=c3(Bv[sl, :n], n), in1=c3(INV[sl, :n], n), op=ALU.mult), [bvk, ink])
                        exp_write('pool', ex["K"][0], ex["K"][1], nch, n, lambda e, o, sl: e.tensor_tensor(out=o, in0=c3(KD[sl, :n], n), in1=c3(INV[sl, :n], n), op=ALU.mult), [kdk, ink])
                        exp_write('dve', ex["Bp"][0], ex["Bp"][1], nch, n, lambda e, o, sl: e.tensor_tensor(out=o, in0=c3(Bv[sl, :n], n), in1=c3(EEND[sl, :n], n), op=ALU.mult), [bvk, eek])
                        exp_write('pool', ex["Kp"][0], ex["Kp"][1], nch, n, lambda e, o, sl: e.tensor_tensor(out=o, in0=c3(KD[sl, :n], n), in1=c3(EEND[sl, :n], n), op=ALU.mult), [kdk, eek])
                        mark(9)
                        stG, stGk = stG_r.next(); stN, stNk = stN_r.next()
                        if own:
                            stR, stRk = stR_r.next(); stO, stOk = stO_r.next()
                        for ci in range(0 if KRW_NOB else nch):
                            f2 = lambda t_: t_[:, ci].rearrange("p a b -> p (a b)")
                            Ad, Rd, Bd, Kd, Bpd, Kpd, Vdd = f2(ex["A"][0]), f2(ex["R"][0]), f2(ex["B"][0]), f2(ex["K"][0]), f2(ex["Bp"][0]), f2(ex["Kp"][0]), f2(Vd)
                            exk = [ex[nm][1] for nm in ("A", "R", "B", "K")]
                            ps1, ps1k = psB.next()
                            for qi, (l_, r_) in enumerate(((Bd, Ad), (Kd, Ad), (Bd, Rd), (Kd, Rd))):
                                kb.op('pe', lambda e, qi=qi, l_=l_, r_=r_: e.matmul(ps1[:, qi * 128:(qi + 1) * 128], lhsT=l_, rhs=r_, start=True, stop=True),
                                      reads=exk, writes=[ps1k], inc=(qi == 3))
                            AT4, atk = AT4_r.next()
                            kb.op('dve', lambda e: e.tensor_tensor(out=AT4[:].rearrange("p a b -> p (a b)"), in0=ps1[:], in1=MASK4[:, d, :], op=ALU.mult), reads=[ps1k, "MASK4"], writes=[atk])
                            if KRW_BSTOP <= 1:
                                continue
                            ps2, ps2k = psB.next()
                            kb.op('pe', lambda e: e.matmul(ps2[:, 0:128], lhsT=Ad, rhs=Bd, start=True, stop=True), reads=exk, writes=[ps2k])
                            AB, abk = AB_r.next()
                            kb.op('dve', lambda e: e.tensor_tensor(out=AB[:, 0, :], in0=ps2[:, 0:128], in1=MASKL[:, d, :], op=ALU.mult), reads=[ps2k, "MASKL"], writes=[abk])
                            kb.op('pool', lambda e: e.tensor_copy(out=AB[:, 1, :], in_=AT4[:, 0, :]), reads=[atk], writes=[abk])
                            XT, xtk = XT_r.next()
                            kb.op('pool', lambda e: e.tensor_tensor(out=XT[:], in0=AT4[:, 0, :], in1=idt[:], op=ALU.add), reads=[atk, "idt"], writes=[xtk])
                            if KRW_BSTOP <= 2:
                                continue
                            for it in range(5):
                                psk_, pskk_ = psB.next()
                                kb.op('pe', lambda e: e.matmul(psk_[:, 0:128], lhsT=AB[:, 1, :], rhs=AB[:, 0, :], start=True, stop=True), reads=[abk], writes=[pskk_], inc=False)
                                kb.op('pe', lambda e: e.matmul(psk_[:, 128:256], lhsT=AB[:, 0, :], rhs=AB[:, 1, :], start=True, stop=True), reads=[abk], writes=[pskk_])
                                AB2, ab2k = AB_r.next()
                                evac(AB2[:].rearrange("p a b -> p (a b)"), psk_[:, 0:256], [pskk_], [ab2k])
                                psx, psxk = psB.next()
                                kb.op('pe', lambda e: e.matmul(psx[:, 0:128], lhsT=AB2[:, 0, :], rhs=XT[:], start=True, stop=True), reads=[ab2k, xtk], writes=[psxk])
                                XT2, xt2k = XT_r.next()
                                kb.op('dve', lambda e: e.tensor_tensor(out=XT2[:], in0=psx[:, 0:128], in1=XT[:], op=ALU.add), reads=[psxk, xtk], writes=[xt2k])
                                AB, abk, XT, xtk = AB2, ab2k, XT2, xt2k
                            WT, wtk = XT, xtk
                            if KRW_BSTOP <= 3:
                                continue
                            pst, pstk = psB.next()
                            exk2 = [ex["A"][1], ex["Bp"][1], ex["Kp"][1], vdk]
                            for qi, s_ in enumerate((Ad, Bpd, Kpd, Vdd)):
                                kb.op('pe', lambda e, qi=qi, s_=s_: e.matmul(pst[:, qi * 128:(qi + 1) * 128], lhsT=s_, rhs=idt[:], start=True, stop=True), reads=exk2 + ["idt"], writes=[pstk], inc=(qi == 3))
                            if os.environ.get('KRW_X') == 'noevac':
                                continue
                            TM, tmk = TM_r.next()
                            AU, auk = AU_r.next()
                            Vtm, vtk = XT_r.next()
                            evac(TM[:, 0, :], pst[:, 0:128], [pstk], [tmk])
                            if os.environ.get('KRW_X') == 'split':
                                evac(TM[:, 2, :], pst[:, 128:256], [pstk], [tmk])
                                evac(TM[:, 3, :], pst[:, 256:384], [pstk], [tmk])
                            else:
                                evac(TM[:, 2:4, :].rearrange("p a b -> p (a b)"), pst[:, 128:384], [pstk], [tmk])
                            evac(Vtm[:], pst[:, 384:512], [pstk], [vtk])
                            if KRW_BSTOP <= 4:
                                continue
                            psx_, psxk_ = psB.next()
                            kb.op('pe', lambda e: e.matmul(psx_[:, 0:128], lhsT=AT4[:, 1, :], rhs=Vtm[:], start=True, stop=True), reads=[atk, vtk], writes=[psxk_])
                            evac(TM[:, 1, :], psx_[:, 0:128], [psxk_], [tmk])
                            psau, psauk = psB.next()
                            kb.op('pe', lambda e: e.matmul(psau[:, 0:256], lhsT=WT[:], rhs=TM[:, 0:2, :].rearrange("p a b -> p (a b)"), start=True, stop=True), reads=[wtk, tmk], writes=[psauk])
                            evac(AU[:].rearrange("p a b -> p (a b)"), psau[:, 0:256], [psauk], [auk])
                            if KRW_BSTOP <= 5:
                                continue
                            psg, psgk = psB.next()
                            kb.op('pe', lambda e: e.matmul(psg[:, 0:128], lhsT=AU[:, 0, :], rhs=TM[:, 2, :], start=True, stop=True), reads=[auk, tmk], writes=[psgk])
                            kb.op('dve', lambda e: e.scalar_tensor_tensor(out=stG[:, ci, :], in0=idt[:], scalar=PL[:, ci:ci + 1], in1=psg[:, 0:128], op0=ALU.mult, op1=ALU.add),
                                  reads=[psgk, plk, "idt"], writes=[stGk])
                            if KRW_BSTOP <= 6:
                                continue
                            psn, psnk = psB.next()
                            kb.op('pe', lambda e: e.matmul(psn[:, 0:128], lhsT=TM[:, 2, :], rhs=AU[:, 1, :], start=True, stop=False), reads=[auk, tmk], writes=[psnk], inc=False)
                            kb.op('pe', lambda e: e.matmul(psn[:, 0:128], lhsT=TM[:, 3, :], rhs=Vtm[:], start=False, stop=True), reads=[tmk, vtk], writes=[psnk])
                            evac(stN[:, ci, :], psn[:, 0:128], [psnk], [stNk])
                            if KRW_BSTOP <= 7:
                                continue
                            if own:
                                psr, psrk = psB.next()
                                kb.op('pe', lambda e: e.matmul(psr[:, 0:128], lhsT=AU[:, 0, :], rhs=AT4[:, 2, :], start=True, stop=True), reads=[auk, atk], writes=[psrk])
                                kb.op('dve', lambda e: e.tensor_tensor(out=stR[:, ci, :], in0=psr[:, 0:128], in1=Rd, op=ALU.add), reads=[psrk, ex["R"][1]], writes=[stRk])
                                if KRW_BSTOP <= 8:
                                    continue
                                pso, psok = psB.next()
                                kb.op('pe', lambda e: e.matmul(pso[:, 0:128], lhsT=AT4[:, 2, :], rhs=AU[:, 1, :], start=True, stop=False), reads=[auk, atk], writes=[psok], inc=False)
                                kb.op('pe', lambda e: e.matmul(pso[:, 0:128], lhsT=AT4[:, 3, :], rhs=Vtm[:], start=False, stop=True), reads=[atk, vtk], writes=[psok])
                                evac(stO[:, ci, :], pso[:, 0:128], [psok], [stOk])
                        if os.environ.get('KRW_NOST'):
                            continue
                        kb.dma('sp', GTd[d, hp, :, ch0:ch0 + nch, :], stG[:, :nch, :], reads=[stGk])
                        kb.dma('sp', Nd[d, hp, :, ch0:ch0 + nch, :], stN[:, :nch, :], reads=[stNk])
                        if own:
                            kb.dma('sp', RhTd[d, hp, :, ch0:ch0 + nch, :], stR[:, :nch, :], reads=[stRk])
                            kb.dma('sp', Ohd[d, hp, :, ch0:ch0 + nch, :], stO[:, :nch, :], reads=[stOk])
                    if own:
                        kb.op('dve', lambda e: e.tensor_scalar(out=bsum[:, :n], in0=bsum[:, :n], scalar1=rk_t[:, hp:hp + 1], scalar2=None, op0=ALU.mult), reads=[bsk, "rk_t"], writes=[bsk])
                        psb_, psbk_ = psA.next()
                        kb.op('pe', lambda e: e.matmul(psb_[:, :n], lhsT=BLK[:], rhs=bsum[:, :n], start=True, stop=True), reads=["BLK", bsk], writes=[psbk_])
                        bo, bok = sh_r.next()
                        kb.op('dve', lambda e: e.tensor_tensor(out=bo[:, :n], in0=psb_[:, :n], in1=VS[:, :n], op=ALU.mult), reads=[psbk_, vsk], writes=[bok])
                        kb.dma('sp', BONs[hp, :, c0:c0 + n], bo[:, :n], reads=[bok])
                        psg_, psgk_ = psA.next()
                        kb.op('pe', lambda e: e.matmul(psg_[:, :n], lhsT=g2_t[:, hp * 128:(hp + 1) * 128], rhs=SGg[:, :n], start=True, stop=True), reads=["g2_t", sggk], writes=[psgk_])
                        go, gok = sh_r.next()
                        evac(go[:, :n], psg_[:, :n], [psgk_], [gok])
                        kb.dma('sp', Gsc[hp, :, c0:c0 + n], go[:, :n], reads=[gok])

        KREG = os.environ.get('KRW_REG', 'both')
        if KRW_MARK < 99:
            try:
                region(pT, FULL_BLKS, False, GTs, Nsc, None, None, lambda bi: 0)
            except StopRegion:
                pass
            kb.barrier()
            return
        if KREG in ('both', 'full'):
            region(pT, FULL_BLKS, False, GTs, Nsc, None, None, lambda bi: 0 if bi == 0 else 4 + (bi - 1) * 8)
        if KREG in ('both', 'own'):
            region(poT, OWN_RBLKS, True, GTo, Nso, RhTo, Oho, lambda bi: bi * 8)
        kb.barrier()

    if KRW_NOCHAIN:
        return
    with ExitStack() as ph:
        sbp = lambda name, shape: ph.enter_context(nc.sbuf_tensor(name, shape, F32))
        idt_ = idt
        selF = sbp("selF2", [128, 4]); kb.dma('sp', selF[:], C["selF"], writes=["selF2"])
        selB = sbp("selB2", [128, 4]); kb.dma('sp', selB[:], C["selB"], writes=["selB2"])
        SEL = sbp("SEL2", [128, 64]); kb.dma('sp', SEL[:], C["sel"], writes=["SEL2"])
        lnw_t = sbp("lnw2", [128, 4]); kb.dma('sp', lnw_t[:], C["ln_w"].rearrange("(c p) -> p c", p=128), writes=["lnw2"], allow_slow_non_contiguous=True)
        lnb_t = sbp("lnb2", [128, 4]); kb.dma('sp', lnb_t[:], C["ln_b"].rearrange("(c p) -> p c", p=128), writes=["lnb2"], allow_slow_non_contiguous=True)
        chains = [(d, hp) for d in range(2) for hp in range(4)]
        S = {}
        for (d, hp) in chains:
            S[(d, hp)] = [sbp("S%d%d_%d" % (d, hp, i), [128, 128]) for i in range(2)]
            kb.op('pool', lambda e: e.memset(S[(d, hp)][0][:], 0.0), writes=[("S", d, hp, 0)])
        CAND = {(d, hp): sbp("CA%d%d" % (d, hp), [128, 4, 128]) for (d, hp) in chains}
        ph1 = ExitStack()
        gl_r = {ch: Rot(nc, ph1, "gl%d%d" % ch, [128, 8, 128], 1) for ch in chains}
        nl_r = {ch: Rot(nc, ph1, "nl%d%d" % ch, [128, 8, 128], 1) for ch in chains}
        psC = Rot(nc, ph, "psC", [128, 512], 8, psum=True)
        cur = {ch: 0 for ch in chains}
        def groups(d):
            if d == 0:
                g = [list(range(0, 4))] + [list(range(4 + i * 8, 12 + i * 8)) for i in range(12)]
            else:
                g = [list(range(3, -1, -1))] + [list(range(4 + i * 8 + 7, 4 + i * 8 - 1, -1)) for i in range(15, 3, -1)]
            return g
        G = {0: groups(0), 1: groups(1)}
        ngroups = len(G[0])
        for gi in range(ngroups):
            loaded = {}
            for ch in chains:
                d, hp = ch
                g = G[d][gi]
                lo = min(g)
                gl, glk = gl_r[ch].next(); nl, nlk = nl_r[ch].next()
                kb.dma('sp', gl[:, :len(g), :], GTs[d, hp, :, lo:lo + len(g), :], writes=[glk])
                kb.dma('sp', nl[:, :len(g), :], Nsc[d, hp, :, lo:lo + len(g), :], writes=[nlk])
                loaded[ch] = (gl, glk, nl, nlk, lo)
            for step in range(len(G[0][gi])):
                for ch in chains:
                    d, hp = ch
                    gl, glk, nl, nlk, lo = loaded[ch]
                    c = G[d][gi][step] - lo
                    i0 = cur[ch]; i1 = 1 - i0
                    ps, pk = psC.next()
                    kb.op('pe', lambda e: e.matmul(ps[:, 0:128], lhsT=gl[:, c, :], rhs=S[ch][i0][:], start=True, stop=True), reads=[glk, ("S", d, hp, i0)], writes=[pk])
                    kb.op('dve', lambda e: e.tensor_tensor(out=S[ch][i1][:], in0=ps[:, 0:128], in1=nl[:, c, :], op=ALU.add), reads=[pk, nlk], writes=[("S", d, hp, i1)])
                    cur[ch] = i1
            if gi in (0, 4, 8, 12):
                ci_ = {0: 0, 4: 1, 8: 2, 12: 3}[gi]
                for ch in chains:
                    d, hp = ch
                    kb.op('dve', lambda e: e.tensor_copy(out=CAND[ch][:, ci_, :], in_=S[ch][cur[ch]][:]), reads=[("S", d, hp, cur[ch])], writes=[("CAND", d, hp)])
        for ch in chains:
            d, hp = ch
            sel = selF if d == 0 else selB
            seln = "selF2" if d == 0 else "selB2"
            i0 = cur[ch]
            kb.op('dve', lambda e: e.tensor_scalar(out=S[ch][i0][:], in0=CAND[ch][:, 0, :], scalar1=sel[:, 0:1], scalar2=None, op0=ALU.mult),
                  reads=[("CAND", d, hp), seln], writes=[("S", d, hp, i0)])
            for i in range(1, 4):
                kb.op('dve', lambda e: e.scalar_tensor_tensor(out=S[ch][i0][:], in0=CAND[ch][:, i, :], scalar=sel[:, i:i + 1], in1=S[ch][i0][:], op0=ALU.mult, op1=ALU.add),
                      reads=[("CAND", d, hp), seln, ("S", d, hp, i0)], writes=[("S", d, hp, i0)])
        kb.barrier()
        ph1.close()
        OD = {ch: sbp("OD%d%d" % ch, [128, NCH_OWN, 64]) for ch in chains}
        gl_r = {ch: Rot(nc, ph, "g2l%d%d" % ch, [128, 4, 128], 1) for ch in chains}
        nl_r = {ch: Rot(nc, ph, "n2l%d%d" % ch, [128, 4, 128], 1) for ch in chains}
        rl_r = {ch: Rot(nc, ph, "rl%d%d" % ch, [128, 4, 128], 1) for ch in chains}
        ol_r = {ch: Rot(nc, ph, "ol%d%d" % ch, [128, 4, 128], 1) for ch in chains}
        tmp_r = Rot(nc, ph, "ctmp", [128, 128], 4)
        for gi in range(8):
            loaded = {}
            for ch in chains:
                d, hp = ch
                g = list(range(gi * 4, gi * 4 + 4)) if d == 0 else list(range(31 - gi * 4, 27 - gi * 4, -1))
                lo = min(g)
                gl, glk = gl_r[ch].next(); nl, nlk = nl_r[ch].next(); rl, rlk = rl_r[ch].next(); ol, olk = ol_r[ch].next()
                for (t_, k_, src_) in ((gl, glk, GTo), (nl, nlk, Nso), (rl, rlk, RhTo), (ol, olk, Oho)):
                    kb.dma('sp', t_[:], src_[d, hp, :, lo:lo + 4, :], writes=[k_])
                loaded[ch] = (gl, glk, nl, nlk, rl, rlk, ol, olk, lo, g)
            for step in range(4):
                for ch in chains:
                    d, hp = ch
                    gl, glk, nl, nlk, rl, rlk, ol, olk, lo, g = loaded[ch]
                    cg = g[step]; c = cg - lo
                    i0 = cur[ch]; i1 = 1 - i0
                    pso, psok = psC.next()
                    kb.op('pe', lambda e: e.matmul(pso[:, 0:128], lhsT=rl[:, c, :], rhs=S[ch][i0][:], start=True, stop=True), reads=[rlk, ("S", d, hp, i0)], writes=[psok], inc=False)
                    kb.op('pe', lambda e: e.matmul(pso[:, 128:256], lhsT=gl[:, c, :], rhs=S[ch][i0][:], start=True, stop=True), reads=[glk, ("S", d, hp, i0)], writes=[psok])
                    kb.op('dve', lambda e: e.tensor_tensor(out=S[ch][i1][:], in0=pso[:, 128:256], in1=nl[:, c, :], op=ALU.add), reads=[psok, nlk], writes=[("S", d, hp, i1)])
                    tt, ttk = tmp_r.next()
                    kb.op('dve', lambda e: e.tensor_tensor(out=tt[:], in0=pso[:, 0:128], in1=ol[:, c, :], op=ALU.add), reads=[psok, olk], writes=[ttk])
                    kb.op('pool', lambda e: e.tensor_tensor(out=OD[ch][:, cg, :], in0=tt[:, 0:64], in1=tt[:, 64:128], op=ALU.add), reads=[ttk], writes=[("OD", d, hp)])
                    cur[ch] = i1
        ye_r = Rot(nc, ph, "yexp", [128, 8, 2, 64], 2)
        for j_, t_ in enumerate(ye_r.t):
            kb.op('pool', lambda e, t_=t_: e.memset(t_[:], 0.0), writes=[("yexp", j_)])
        os_r = Rot(nc, ph, "osum", [128, 8, 64], 2)
        sq_r = Rot(nc, ph, "osq", [128, 8, 64], 2)
        stt_r = Rot(nc, ph, "ostt", [128, 4, 8], 2)
        yn_r = Rot(nc, ph, "ynr", [128, 512], 2)
        bg_r = Rot(nc, ph, "bgr", [128, 2, 512], 2)
        for hp in range(4):
            for bi in range(4):
                OS, osk = os_r.next()
                kb.op('pool', lambda e: e.tensor_tensor(out=OS[:], in0=OD[(0, hp)][:, bi * 8:(bi + 1) * 8, :], in1=OD[(1, hp)][:, bi * 8:(bi + 1) * 8, :], op=ALU.add),
                      reads=[("OD", 0, hp), ("OD", 1, hp)], writes=[osk])
                stt, sk = stt_r.next()
                kb.op('dve', lambda e: e.reduce_sum(out=stt[:, 0, :], in_=OS[:], axis=AX.X), reads=[osk], writes=[sk])
                SQ, sqk = sq_r.next()
                kb.op('pool', lambda e: e.tensor_tensor(out=SQ[:], in0=OS[:], in1=OS[:], op=ALU.mult), reads=[osk], writes=[sqk])
                kb.op('dve', lambda e: e.reduce_sum(out=stt[:, 1, :], in_=SQ[:], axis=AX.X), reads=[sqk], writes=[sk])
                kb.op('dve', lambda e: e.tensor_scalar(out=stt[:, 0, :], in0=stt[:, 0, :], scalar1=1.0 / 64, scalar2=None, op0=ALU.mult), reads=[sk], writes=[sk])
                kb.op('dve', lambda e: e.tensor_tensor(out=stt[:, 2, :], in0=stt[:, 0, :], in1=stt[:, 0, :], op=ALU.mult), reads=[sk], writes=[sk])
                kb.op('dve', lambda e: e.scalar_tensor_tensor(out=stt[:, 3, :], in0=stt[:, 1, :], scalar=1.0 / 64, in1=stt[:, 2, :], op0=ALU.mult, op1=ALU.subtract), reads=[sk], writes=[sk])
                kb.op('dve', lambda e: e.tensor_scalar(out=stt[:, 3, :], in0=stt[:, 3, :], scalar1=GN_EPS, scalar2=None, op0=ALU.add), reads=[sk], writes=[sk])
                kb.op('act', lambda e: e.activation(out=stt[:, 3, :], in_=stt[:, 3, :], func=AF.Sqrt), reads=[sk], writes=[sk])
                kb.op('dve', lambda e: e.reciprocal(out=stt[:, 3, :], in_=stt[:, 3, :]), reads=[sk], writes=[sk])
                kb.op('dve', lambda e: e.tensor_tensor(out=OS[:], in0=OS[:], in1=stt[:, 0, :].unsqueeze(2).to_broadcast([128, 8, 64]), op=ALU.subtract), reads=[osk, sk], writes=[osk])
                ye, yek = ye_r.next()
                for hh in range(2):
                    sl = slice(hh * 64, hh * 64 + 64)
                    kb.op('dve', lambda e, hh=hh, sl=sl: e.tensor_tensor(out=ye[sl, :, hh, :], in0=OS[sl], in1=stt[sl, 3, :].unsqueeze(2).to_broadcast([64, 8, 64]), op=ALU.mult),
                          reads=[osk, sk], writes=[yek])
                ps, pk = psC.next()
                for ci in range(8):
                    kb.op('pe', lambda e, ci=ci: e.matmul(ps[:, ci * 64:(ci + 1) * 64], lhsT=ye[:, ci].rearrange("p a b -> p (a b)"), rhs=SEL[:], start=True, stop=True),
                          reads=[yek, "SEL2"], writes=[pk], inc=(ci == 7))
                bg, bgk = bg_r.next()
                kb.dma('sp', bg[:, 0, :], BONs[hp, :, bi * 512:(bi + 1) * 512], writes=[bgk])
                kb.dma('sp', bg[:, 1, :], Gsc[hp, :, bi * 512:(bi + 1) * 512], writes=[bgk])
                yn, ynk = yn_r.next()
                kb.op('dve', lambda e: e.tensor_scalar(out=yn[:], in0=ps[:], scalar1=lnw_t[:, hp:hp + 1], scalar2=lnb_t[:, hp:hp + 1], op0=ALU.mult, op1=ALU.add),
                      reads=[pk, "lnw2", "lnb2"], writes=[ynk])
                kb.op('pool', lambda e: e.tensor_tensor(out=yn[:], in0=yn[:], in1=bg[:, 0, :], op=ALU.add), reads=[ynk, bgk], writes=[ynk])
                kb.op('pool', lambda e: e.tensor_tensor(out=yn[:], in0=yn[:], in1=bg[:, 1, :], op=ALU.mult), reads=[ynk, bgk], writes=[ynk])
                kb.dma('sp', yT[hp * 128:(hp + 1) * 128, bi * 512:(bi + 1) * 512], yn[:], reads=[ynk])
        kb.barrier()


def rwkv_consts(q):
    tt = np.arange(64)
    lowS = (tt[:, None] < tt[None, :]).astype(np.float32)
    lowI = (tt[:, None] <= tt[None, :]).astype(np.float32)
    def bd(m):
        z = np.zeros((128, 128), np.float32)
        z[:64, :64] = m
        z[64:, 64:] = m
        return z
    mask4 = np.zeros((2, 128, 512), np.float32)
    maskl = np.zeros((2, 128, 128), np.float32)
    for d in range(2):
        S_, I_ = (lowS, lowI) if d == 0 else (lowS.T, lowI.T)
        mask4[d] = np.concatenate([bd(S_), bd(S_), bd(I_), bd(I_)], axis=1)
        maskl[d] = bd(S_.T)
    blk = bd(np.ones((64, 64), np.float32))
    uu = np.concatenate([(tt[:, None] <= tt[None, :]).astype(np.float32)] * 2, axis=0)
    sel = np.concatenate([np.eye(64, dtype=np.float32)] * 2, axis=0)
    selF = np.zeros((128, 4), np.float32); selF[:, q] = 1.0
    selB = np.zeros((128, 4), np.float32); selB[:, 3 - q] = 1.0
    hal = np.zeros((128, 2), np.float32)
    hal[:, 0] = 1.0 if q > 0 else 0.0
    hal[:, 1] = 1.0 if q < 3 else 0.0
    return dict(mask4=mask4, maskl=maskl, blk=blk, uu=uu, sel=sel, selF=selF, selB=selB, hal=hal)


OWN_BLKS = [(0, 512), (512, 512), (1024, 512), (1536, 512), (2048, 128)]
XO_ROWS = QT + 128
NE = 32
ATT_SCALE = 192.0 ** -0.5


RUN = os.environ.get("KSTAGES", "123456")
SKIP = os.environ.get("KSKIP", "").split(",")


class Stage:
    def __init__(self, s):
        self.s = s

    def __enter__(self):
        return self.s in RUN

    def __exit__(self, *a):
        return False


def build():
    nc = bass.Bass("TRN2", target_bir_lowering=False)
    dt = nc.dram_tensor

    def din(name, shape):
        return dt(name, shape, F32, kind="ExternalInput").ap()

    def dscr(name, shape):
        if DEBUG and name in DEBUG.split(","):
            return dt("dbg_" + name, shape, F32, kind="ExternalOutput").ap()
        return dt(name, shape, F32).ap()

    xs = din("xs", [TS, D])
    xo = din("xo", [XO_ROWS, D])
    cvec = din("cvec", [128, 16])
    mod_w = din("mod_w", [D, 6 * D])
    mod_b = din("mod_b", [6 * D])
    g1 = din("g1", [D])
    g2n = din("g2n", [D])
    gfin = din("gfin", [D])
    w_in = din("w_in", [D, WCOLS])
    ident = din("ident", [128, 128])
    ropeC = din("ropeC", [64, T])
    ropeS = din("ropeS", [64, T])
    ropeCo = din("ropeCo", [64, QT])
    ropeSo = din("ropeSo", [64, QT])
    kvg = din("kvg", [128, 1])
    qg = din("qg", [128, 2])
    wukv_k = din("wukv_k", [128, 512])
    wukv_v = din("wukv_v", [128, 512])
    wuq = din("wuq", [256, 1024])
    w_out = din("w_out", [D, D])
    router_w = din("router_w", [D, NE])
    router_b = din("router_b", [NE])
    w1 = din("w1", [NE, D, 2 * D]) if "5" in RUN else None
    b1fm = din("b1fm", [128, NE * 16])
    w2 = din("w2", [NE, D, D]) if "5" in RUN else None
    b2 = din("b2", [NE, D])
    yrw_in = din("yrw_in", [512, QT]) if (DEBUG and '2' not in RUN) else None
    rw = {}
    if '2' in RUN:
        for nm, shp in (("mu", [1792]), ("w0", [2, 512]), ("a0", [2, 512]), ("w2", [2, 64, 512]), ("a2", [2, 64, 512]), ("k_k", [512]), ("k_a", [512]),
                        ("r_k", [512]), ("ln_w", [512]), ("ln_b", [512]), ("g2", [128, 512]), ("mask4", [2, 128, 512]), ("maskl", [2, 128, 128]),
                        ("blk", [128, 128]), ("uu", [128, 64]), ("sel", [128, 64]), ("selF", [128, 4]), ("selB", [128, 4]), ("hal", [128, 2])):
            rw[nm] = din("rw_" + nm, shp)
    out = dt("out", [QT, D], F32, kind="ExternalOutput").ap()

    pT = dscr("pT", [1792, TS])
    poT = dscr("poT", [1792, XO_ROWS])
    KnT = dscr("KnT", [4, 128, TS])
    KrT = dscr("KrT", [64, TS])
    Vs = dscr("Vs", [TS, 4 * 129])
    QnT = dscr("QnT", [4, 128, QT])
    QrT = dscr("QrT", [4, 64, QT])
    yT = dscr("yT", [D, QT])
    X1 = dscr("X1", [QT, D])
    LG = dscr("LG", [QT, 2 * NE])
    FF = dscr("FF", [QT, D])

    with ExitStack() as st:
        kb = KB(nc, st)
        sb = lambda name, shape: st.enter_context(nc.sbuf_tensor(name, shape, F32))
        idt = sb("idt", [128, 128])
        kb.dma('sp', idt[:], ident, writes=["idt"])
        ones = sb("ones", [128, 128])
        kb.op('pool', lambda e: e.memset(ones[:], 1.0), writes=["ones"])
        cv = sb("cv", [128, 16])
        sc = sb("sc", [128, 16])
        kb.dma('sp', cv[:], cvec, writes=["cv"])
        kb.op('act', lambda e: e.activation(out=sc[:], in_=cv[:], func=AF.Silu), reads=["cv"], writes=["sc"])
        modfm = sb("modfm", [128, 4, 8, 2])
        mbfm = sb("mbfm", [128, 6, 8])
        kb.dma('sp', mbfm[:], mod_b.rearrange("(v k p) -> p v k", p=128, k=8), writes=["mbfm"], allow_slow_non_contiguous=True)
        g1t = sb("g1t", [128, 8])
        kb.dma('sp', g1t[:], g1.rearrange("(k p) -> p k", p=128), writes=["g1t"], allow_slow_non_contiguous=True)
        g2t = sb("g2t", [128, 8])
        kb.dma('sp', g2t[:], g2n.rearrange("(k p) -> p k", p=128), writes=["g2t"], allow_slow_non_contiguous=True)
        GT = sb("GT", [128, 2, 1024])
        with ExitStack() as ph:
            wv = Rot(nc, ph, "wv", [128, 8, 1024], 2)
            psm = Rot(nc, ph, "psm", [128, 16], 2, psum=True)
            psg = Rot(nc, ph, "psg", [128, 512], 2, psum=True)
            SCB = ph.enter_context(nc.sbuf_tensor("SCB", [128, 8, 128], F32))
            mbb = ph.enter_context(nc.sbuf_tensor("mbb", [128, 2, 1024], F32))
            for k in range(8):
                kb.op('dve', lambda e, k=k: e.tensor_copy(out=SCB[:, k, :], in_=sc[:, 2 * k:2 * k + 1].to_broadcast([128, 128])),
                      reads=["sc"], writes=["SCB"])
            for gi, v in enumerate((2, 5)):
                kb.dma('sp', mbb[:, gi, :], mod_b[v * 1024:(v + 1) * 1024].partition_broadcast(128), writes=["mbb"])
            for vi, v in enumerate((0, 1, 3, 4)):
                wt, wk = wv.next()
                kb.dma('sp', wt[:], mod_w[:, v * 1024:(v + 1) * 1024].rearrange("(k p) n -> p k n", p=128), writes=[wk])
                ps, pk = psm.next()
                for j in range(8):
                    for k in range(8):
                        kb.op('pe', lambda e, j=j, k=k: e.matmul(ps[:, 2 * j:2 * j + 2], lhsT=wt[:, k, j * 128:(j + 1) * 128],
                                                                 rhs=sc[:, 2 * k:2 * k + 2], start=(k == 0), stop=(k == 7)),
                              reads=[wk, "sc"], writes=[pk], inc=(j == 7 and k == 7))
                kb.op('dve', lambda e, vi=vi, v=v: e.tensor_tensor(out=modfm[:, vi], in0=ps[:].rearrange("p (j c) -> p j c", c=2),
                                                                   in1=mbfm[:, v, :].unsqueeze(2).to_broadcast([128, 8, 2]), op=ALU.add),
                      reads=[pk, "mbfm"], writes=["modfm"])
            for gi, v in enumerate((2, 5)):
                wt, wk = wv.next()
                kb.dma('sp', wt[:], mod_w[:, v * 1024:(v + 1) * 1024].rearrange("(k p) n -> p k n", p=128), writes=[wk])
                for hf in range(2):
                    ps, pk = psg.next()
                    for k in range(8):
                        kb.op('pe', lambda e, k=k, hf=hf: e.matmul(ps[:], lhsT=SCB[:, k, :], rhs=wt[:, k, hf * 512:(hf + 1) * 512],
                                                                   start=(k == 0), stop=(k == 7)),
                              reads=[wk, "SCB"], writes=[pk], inc=(k == 7))
                    kb.op('dve', lambda e, gi=gi, hf=hf: e.tensor_tensor(out=GT[:, gi, hf * 512:(hf + 1) * 512], in0=ps[:],
                                                                         in1=mbb[:, gi, hf * 512:(hf + 1) * 512], op=ALU.add),
                          reads=[pk, "mbb"], writes=["GT"])
            kb.barrier()
        G1 = sb("G1", [128, 8, 2])
        G2 = sb("G2", [128, 8, 2])
        for Gt, gt_, mrow, nm in ((G1, g1t, 1, "G1"), (G2, g2t, 3, "G2")):
            kb.op('dve', lambda e: e.tensor_scalar(out=Gt[:], in0=modfm[:, mrow], scalar1=1.0, scalar2=None, op0=ALU.add), reads=["modfm"], writes=[nm])
            kb.op('dve', lambda e: e.tensor_tensor(out=Gt[:], in0=Gt[:], in1=gt_[:].unsqueeze(2).to_broadcast([128, 8, 2]), op=ALU.mult),
                  reads=[nm, "g1t", "g2t"], writes=[nm])

        def rms_rstd(xt, xk, stt, sk, junk, n_feat):
            kb.op('pool', lambda e: e.memset(stt[:], 0.0), writes=[sk])
            kb.op('act', lambda e: e.activation(out=junk[:], in_=xt, func=AF.Square, accum_out=stt[:, 0:1]), reads=[xk], writes=["junk", sk])
            kb.op('dve', lambda e: e.tensor_scalar(out=stt[:, 1:2], in0=stt[:, 0:1], scalar1=1.0 / n_feat, scalar2=1e-6, op0=ALU.mult, op1=ALU.add),
                  reads=[sk], writes=[sk])
            kb.op('act', lambda e: e.activation(out=stt[:, 2:3], in_=stt[:, 1:2], func=AF.Sqrt), reads=[sk], writes=[sk])
            kb.op('dve', lambda e: e.reciprocal(out=stt[:, 3:4], in_=stt[:, 2:3]), reads=[sk], writes=[sk])

        def cm_rstd(ps_ss, pk, dst, dk, n, n_feat, extra_scale=1.0):
            kb.op('dve', lambda e: e.tensor_scalar(out=dst[:, :n], in0=ps_ss[:, :n], scalar1=1.0 / n_feat, scalar2=1e-6, op0=ALU.mult, op1=ALU.add),
                  reads=[pk], writes=[dk])
            kb.op('act', lambda e: e.activation(out=dst[:, :n], in_=dst[:, :n], func=AF.Sqrt), reads=[dk], writes=[dk])
            kb.op('dve', lambda e: e.reciprocal(out=dst[:, :n], in_=dst[:, :n]), reads=[dk], writes=[dk])
            if extra_scale != 1.0:
                kb.op('dve', lambda e: e.tensor_scalar(out=dst[:, :n], in0=dst[:, :n], scalar1=extra_scale, scalar2=None, op0=ALU.mult), reads=[dk], writes=[dk])

        with ExitStack() as ph, Stage('1') as go:
          if go:
            sbp = lambda name, shape: ph.enter_context(nc.sbuf_tensor(name, shape, F32))
            WB = sbp("WB", [128, 8, WCOLS])
            for k in range(8):
                kb.dma('sp', WB[:, k, :], w_in[k * 128:(k + 1) * 128, :], writes=[("WB", k)])
            wk_t = sbp("wk_t", [128, 512]); kb.dma('sp', wk_t[:], wukv_k, writes=["wk_t"])
            wv_t = sbp("wv_t", [128, 512]); kb.dma('sp', wv_t[:], wukv_v, writes=["wv_t"])
            wq_t = sbp("wq_t", [128, 2, 1024]); kb.dma('sp', wq_t[:], wuq.rearrange("(k p) n -> p k n", p=128), writes=["wq_t"])
            kvg_t = sbp("kvg_t", [128, 1]); kb.dma('sp', kvg_t[:], kvg, writes=["kvg_t"])
            qg_t = sbp("qg_t", [128, 2]); kb.dma('sp', qg_t[:], qg, writes=["qg_t"])
            xt_r = Rot(nc, ph, "xt", [128, 1024], 2)
            xn_r = Rot(nc, ph, "xn", [128, 1024], 1)
            st_r = Rot(nc, ph, "stat", [128, 4], 4)
            xmT_r = Rot(nc, ph, "xmT", [128, 8, 512], 2)
            pst_r = Rot(nc, ph, "pst", [128, 8, 128], 1, psum=True)
            psp_r = Rot(nc, ph, "psp", [128, 512], 6, psum=True)
            stg_r = Rot(nc, ph, "stg", [128, 512], 3)
            tmp_r = Rot(nc, ph, "tmp", [128, 512], 3)
            ql_r = Rot(nc, ph, "qlr", [128, 512], 2)
            rs_r = Rot(nc, ph, "rsr", [128, 512], 1)
            ckn_r = Rot(nc, ph, "cknr", [128, 512], 1)
            rp_r = Rot(nc, ph, "rp", [64, 2, 512], 2)
            vst_r = Rot(nc, ph, "vst", [128, 4, 129], 2)
            for j_, t_ in enumerate(vst_r.t):
                kb.op('pool', lambda e, t_=t_: e.memset(t_[:], 1.0), writes=[("vst", j_)])
            junk = sbp("junk", [128, 1024])
            cnt = {"ev": 0}

            def evac(out_ap, in_ap, reads, writes):
                cnt["ev"] += 1
                if cnt["ev"] % 2:
                    kb.op('dve', lambda e: e.tensor_copy(out=out_ap, in_=in_ap), reads=reads, writes=writes)
                else:
                    kb.op('act', lambda e: e.copy(out=out_ap, in_=in_ap), reads=reads, writes=writes)

            def proj_pass(src, blocks, dstT, own):
                for (s0, n) in blocks:
                    mi = 1 if (not own and s0 < CTX) else 0
                    xmT, xmk = xmT_r.next()
                    for i in range(n // 128):
                        xt, xk = xt_r.next()
                        kb.dma('sp', xt[:], src[s0 + i * 128: s0 + (i + 1) * 128, :], writes=[xk])
                        stt, sk = st_r.next()
                        rms_rstd(xt[:], xk, stt, sk, junk, D)
                        xn, nk = xn_r.next()
                        kb.op('dve', lambda e: e.tensor_scalar(out=xn[:], in0=xt[:], scalar1=stt[:, 3:4], scalar2=None, op0=ALU.mult),
                              reads=[xk, sk], writes=[nk])
                        pt, ptk = pst_r.next()
                        for k in range(8):
                            kb.op('pe', lambda e, k=k: e.transpose(out=pt[:, k, :], in_=xn[:, k * 128:(k + 1) * 128], identity=idt[:]),
                                  reads=[nk, "idt"], writes=[ptk], inc=(k == 7))
                        for k in range(8):
                            if k % 2 == 0:
                                kb.op('dve', lambda e, k=k, i=i: e.tensor_scalar(out=xmT[:, k, i * 128:(i + 1) * 128], in0=pt[:, k, :],
                                                                                  scalar1=G1[:, k, mi:mi + 1], scalar2=modfm[:, 0, k, mi:mi + 1],
                                                                                  op0=ALU.mult, op1=ALU.add),
                                      reads=[ptk, "G1", "modfm"], writes=[xmk])
                            else:
                                kb.op('act', lambda e, k=k, i=i: e.activation(out=xmT[:, k, i * 128:(i + 1) * 128], in_=pt[:, k, :], func=AF.Identity,
                                                                               scale=G1[:, k, mi:mi + 1], bias=modfm[:, 0, k, mi:mi + 1]),
                                      reads=[ptk, "G1", "modfm"], writes=[xmk])

                    def proj_cols(c0, m):
                        ps, pk = psp_r.next()
                        for k in range(8):
                            kb.op('pe', lambda e, k=k: e.matmul(ps[:m, :n], lhsT=WB[:, k, c0:c0 + m], rhs=xmT[:, k, :n], start=(k == 0), stop=(k == 7)),
                                  reads=[("WB", k), xmk], writes=[pk], inc=(k == 7))
                        return ps, pk

                    for cc in range(14):
                        ps, pk = proj_cols(cc * 128, 128)
                        sg, sgk = stg_r.next()
                        evac(sg[:, :n], ps[:, :n], [pk], [sgk])
                        kb.dma('sp', dstT[cc * 128:(cc + 1) * 128, s0:s0 + n], sg[:, :n], reads=[sgk])
                    if not own and 'kv' not in SKIP:
                        ps, pk = proj_cols(1792, 128)
                        ckv, ck = tmp_r.next()
                        evac(ckv[:, :n], ps[:, :n], [pk], [ck])
                        sq, sqk = tmp_r.next()
                        kb.op('pool', lambda e: e.tensor_tensor(out=sq[:, :n], in0=ckv[:, :n], in1=ckv[:, :n], op=ALU.mult), reads=[ck], writes=[sqk])
                        pss, pssk = psp_r.next()
                        kb.op('pe', lambda e: e.matmul(pss[:, :n], lhsT=ones[:], rhs=sq[:, :n], start=True, stop=True), reads=["ones", sqk], writes=[pssk])
                        rs, rsk = rs_r.next()
                        cm_rstd(pss, pssk, rs, rsk, n, 128.0)
                        ckn, cnk = ckn_r.next()
                        kb.op('dve', lambda e: e.scalar_tensor_tensor(out=ckn[:, :n], in0=ckv[:, :n], scalar=kvg_t[:, 0:1], in1=rs[:, :n],
                                                                       op0=ALU.mult, op1=ALU.mult), reads=[ck, rsk, "kvg_t"], writes=[cnk])
                        for h in range(0 if 'kvK' in SKIP else 4):
                            psk, pskk = psp_r.next()
                            kb.op('pe', lambda e, h=h: e.matmul(psk[:, :n], lhsT=wk_t[:, h * 128:(h + 1) * 128], rhs=ckn[:, :n], start=True, stop=True),
                                  reads=["wk_t", cnk], writes=[pskk])
                            sg, sgk = stg_r.next()
                            evac(sg[:, :n], psk[:, :n], [pskk], [sgk])
                            kb.dma('sp', KnT[h, :, s0:s0 + n], sg[:, :n], reads=[sgk])
                        for i in range(0 if 'kvV' in SKIP else n // 128):
                            psv, psvk = psp_r.next()
                            kb.op('pe', lambda e, i=i: e.matmul(psv[:, :], lhsT=ckn[:, i * 128:(i + 1) * 128], rhs=wv_t[:], start=True, stop=True),
                                  reads=["wv_t", cnk], writes=[psvk])
                            vt, vk = vst_r.next()
                            evac(vt[:, :, 0:128], psv[:].rearrange("p (h d) -> p h d", h=4), [psvk], [vk])
                            kb.dma('sp', Vs[s0 + i * 128:s0 + (i + 1) * 128, :], vt[:].rearrange("p h d -> p (h d)"), reads=[vk])
                        if 'kvR' in SKIP:
                            continue
                        psa, pak = proj_cols(1920, 64)
                        sg, sgk = stg_r.next()
                        if s0 < CTX:
                            evac(sg[:64, :n], psa[:64, :n], [pak], [sgk])
                        else:
                            psb, pbk = proj_cols(1984, 64)
                            rp, rpk = rp_r.next()
                            kb.dma('sp', rp[:, 0, :n], ropeC[:, s0 - CTX:s0 - CTX + n], writes=[rpk])
                            kb.dma('sp', rp[:, 1, :n], ropeS[:, s0 - CTX:s0 - CTX + n], writes=[rpk])
                            t1, t1k = tmp_r.next()
                            kb.op('dve', lambda e: e.tensor_tensor(out=t1[:64, :n], in0=psa[:64, :n], in1=rp[:, 0, :n], op=ALU.mult), reads=[pak, rpk], writes=[t1k])
                            t2, t2k = tmp_r.next()
                            kb.op('dve', lambda e: e.tensor_tensor(out=t2[:64, :n], in0=psb[:64, :n], in1=rp[:, 1, :n], op=ALU.mult), reads=[pbk, rpk], writes=[t2k])
                            kb.op('pool', lambda e: e.tensor_tensor(out=sg[:64, :n], in0=t1[:64, :n], in1=t2[:64, :n], op=ALU.add), reads=[t1k, t2k], writes=[sgk])
                        kb.dma('sp', KrT[:, s0:s0 + n], sg[:64, :n], reads=[sgk])
                    elif own and s0 < QT and 'q' not in SKIP:
                        ql = []
                        pss, pssk = psp_r.next()
                        for kc in range(2):
                            ps, pk = proj_cols(2048 + kc * 128, 128)
                            qn_, qnk = ql_r.next()
                            evac(qn_[:, :n], ps[:, :n], [pk], [qnk])
                            sq, sqk = tmp_r.next()
                            kb.op('pool', lambda e: e.tensor_tensor(out=sq[:, :n], in0=qn_[:, :n], in1=qn_[:, :n], op=ALU.mult), reads=[qnk], writes=[sqk])
                            kb.op('dve', lambda e, kc=kc: e.tensor_scalar(out=qn_[:, :n], in0=qn_[:, :n], scalar1=qg_t[:, kc:kc + 1], scalar2=None, op0=ALU.mult),
                                  reads=[qnk, sqk, "qg_t"], writes=[qnk])
                            kb.op('pe', lambda e, kc=kc: e.matmul(pss[:, :n], lhsT=ones[:], rhs=sq[:, :n], start=(kc == 0), stop=(kc == 1)),
                                  reads=["ones", sqk], writes=[pssk], inc=(kc == 1))
                            ql.append((qn_, qnk))
                        rs, rsk = rs_r.next()
                        cm_rstd(pss, pssk, rs, rsk, n, 256.0, ATT_SCALE)
                        rp, rpk = rp_r.next()
                        kb.dma('sp', rp[:, 0, :n], ropeCo[:, s0:s0 + n], writes=[rpk])
                        kb.dma('sp', rp[:, 1, :n], ropeSo[:, s0:s0 + n], writes=[rpk])
                        for h in range(4):
                            def qmm(c0, m):
                                ps, pk = psp_r.next()
                                for kc in range(2):
                                    kb.op('pe', lambda e, kc=kc: e.matmul(ps[:m, :n], lhsT=wq_t[:, kc, h * 256 + c0:h * 256 + c0 + m], rhs=ql[kc][0][:, :n],
                                                                          start=(kc == 0), stop=(kc == 1)),
                                          reads=["wq_t", ql[kc][1]], writes=[pk], inc=(kc == 1))
                                return ps, pk
                            ps, pk = qmm(0, 128)
                            sg, sgk = stg_r.next()
                            kb.op('dve', lambda e: e.tensor_tensor(out=sg[:, :n], in0=ps[:, :n], in1=rs[:, :n], op=ALU.mult), reads=[pk, rsk], writes=[sgk])
                            kb.dma('sp', QnT[h, :, s0:s0 + n], sg[:, :n], reads=[sgk])
                            psa, pak = qmm(128, 64)
                            psb, pbk = qmm(192, 64)
                            t1, t1k = tmp_r.next()
                            kb.op('dve', lambda e: e.tensor_tensor(out=t1[:64, :n], in0=psa[:64, :n], in1=rp[:, 0, :n], op=ALU.mult), reads=[pak, rpk], writes=[t1k])
                            t2, t2k = tmp_r.next()
                            kb.op('dve', lambda e: e.tensor_tensor(out=t2[:64, :n], in0=psb[:64, :n], in1=rp[:, 1, :n], op=ALU.mult), reads=[pbk, rpk], writes=[t2k])
                            kb.op('pool', lambda e: e.tensor_tensor(out=t1[:64, :n], in0=t1[:64, :n], in1=t2[:64, :n], op=ALU.add), reads=[t1k, t2k], writes=[t1k])
                            sg, sgk = stg_r.next()
                            kb.op('dve', lambda e: e.tensor_tensor(out=sg[:64, :n], in0=t1[:64, :n], in1=rs[:64, :n], op=ALU.mult), reads=[t1k, rsk], writes=[sgk])
                            kb.dma('sp', QrT[h, :, s0:s0 + n], sg[:64, :n], reads=[sgk])

            proj_pass(xs, NBLK_FULL, pT, False)
            if 'own' not in SKIP:
                proj_pass(xo, OWN_BLKS, poT, True)
            kb.barrier()

        if '2' in RUN:
            C = dict(rw)
            C["idt"] = idt
            rwkv_stage(nc, kb, st, pT, poT, yT, C, dscr)
        elif DEBUG:
            with ExitStack() as ph:
                t = ph.enter_context(nc.sbuf_tensor("yin", [128, 4, QT], F32))
                kb.dma('sp', t[:], yrw_in.rearrange("(k p) n -> p k n", p=128), writes=["yin"])
                kb.dma('sp', yT[0:512, :].rearrange("(k p) n -> p k n", p=128), t[:], reads=["yin"])
                kb.barrier()

        with ExitStack() as ph, Stage('3') as go:
          if go:
            sbp = lambda name, shape: ph.enter_context(nc.sbuf_tensor(name, shape, F32))
            Kn = sbp("Kn", [128, TS])
            Kr = sbp("Kr", [64, TS])
            Vh = sbp("Vh", [128, 66, 129])
            kb.dma('sp', Kr[:], KrT, writes=["Kr"])
            qn_r = Rot(nc, ph, "qn", [128, 512], 2)
            qr_r = Rot(nc, ph, "qr", [64, 512], 2)
            pT_r = Rot(nc, ph, "pTt", [128, 512], 3)
            pss_r = Rot(nc, ph, "pss", [128, 512], 3, psum=True)
            pso = [ph.enter_context(nc.psum_tensor("pso%d" % i, [128, 512], F32)) for i in range(4)]
            ptr_r = Rot(nc, ph, "ptr", [128, 128], 1, psum=True)
            yv_r = Rot(nc, ph, "yv", [128, 132], 3)
            yt_r = Rot(nc, ph, "ytt", [128, 512], 2)
            for h in range(4):
                for part in range(4):
                    c0 = part * 2112
                    kb.dma('sp', Kn[:, c0:c0 + 2112], KnT[h, :, c0:c0 + 2112], writes=["Kn"])
                kb.dma('sp', Vh[:], Vs[:, h * 129:(h + 1) * 129].rearrange("(t p) d -> p t d", p=128), writes=["Vh"])
                for qb in range(4):
                    qn_, qnk = qn_r.next()
                    qr_, qrk = qr_r.next()
                    kb.dma('sp', qn_[:], QnT[h, :, qb * 512:(qb + 1) * 512], writes=[qnk])
                    kb.dma('sp', qr_[:], QrT[h, :, qb * 512:(qb + 1) * 512], writes=[qrk])
                    pend = None
                    for kt in range(67):
                        if kt < 66:
                            ps, pk = pss_r.next()
                            kb.op('pe', lambda e: e.matmul(ps[:], lhsT=Kn[:, kt * 128:(kt + 1) * 128], rhs=qn_[:], start=True, stop=False),
                                  reads=["Kn", qnk], writes=[pk], inc=False)
                            kb.op('pe', lambda e: e.matmul(ps[:], lhsT=Kr[:, kt * 128:(kt + 1) * 128], rhs=qr_[:], start=False, stop=True),
                                  reads=["Kr", qrk], writes=[pk])
                            pt_, ptk = pT_r.next()
                            kb.op('act', lambda e: e.activation(out=pt_[:], in_=ps[:], func=AF.Exp), reads=[pk], writes=[ptk])
                            cur = (pt_, ptk, kt)
                        else:
                            cur = None
                        if pend is not None:
                            ppt, pptk, pkt = pend
                            for qi in range(4):
                                kb.op('pe', lambda e, qi=qi: e.matmul(pso[qi][:, 0:129], lhsT=ppt[:, qi * 128:(qi + 1) * 128], rhs=Vh[:, pkt, :],
                                                                      start=(pkt == 0), stop=(pkt == 65)),
                                      reads=[pptk, "Vh"], writes=[("pso", qi)], inc=(qi == 3))
                        pend = cur
                    ytt, ytk = yt_r.next()
                    for qi in range(4):
                        yv, yvk = yv_r.next()
                        kb.op('dve', lambda e: e.reciprocal(out=yv[:, 129:130], in_=pso[qi][:, 128:129]), reads=[("pso", qi)], writes=[yvk])
                        kb.op('dve', lambda e: e.tensor_scalar(out=yv[:, 0:128], in0=pso[qi][:, 0:128], scalar1=yv[:, 129:130], scalar2=None, op0=ALU.mult),
                              reads=[("pso", qi), yvk], writes=[yvk])
                        ptr, ptrk = ptr_r.next()
                        kb.op('pe', lambda e: e.transpose(out=ptr[:], in_=yv[:, 0:128], identity=idt[:]), reads=[yvk, "idt"], writes=[ptrk])
                        kb.op('act', lambda e: e.copy(out=ytt[:, qi * 128:(qi + 1) * 128], in_=ptr[:]), reads=[ptrk], writes=[ytk])
                    kb.dma('sp', yT[512 + h * 128:512 + (h + 1) * 128, qb * 512:(qb + 1) * 512], ytt[:], reads=[ytk])
            kb.barrier()

        with ExitStack() as ph, Stage('4') as go:
          if go:
            sbp = lambda name, shape: ph.enter_context(nc.sbuf_tensor(name, shape, F32))
            Wo = sbp("Wo", [128, 8, D])
            kb.dma('sp', Wo[:], w_out.rearrange("(k p) n -> p k n", p=128), writes=["Wo"])
            Wr = sbp("Wr", [128, 8, NE])
            kb.dma('sp', Wr[:], router_w.rearrange("(k p) n -> p k n", p=128), writes=["Wr"])
            rbb = sbp("rbb", [128, NE])
            kb.dma('sp', rbb[:], router_b.partition_broadcast(128), writes=["rbb"])
            junk = sbp("junk4", [128, 1024])
            yt_r = Rot(nc, ph, "yt4", [128, 8, 128], 2)
            xo_r = Rot(nc, ph, "xo4", [128, 1024], 2)
            x1_r = Rot(nc, ph, "x14", [128, 1024], 2)
            xn_r = Rot(nc, ph, "xn4", [128, 1024], 2)
            st_r = Rot(nc, ph, "st4", [128, 4], 3)
            h2_r = Rot(nc, ph, "h24", [128, 8, 128], 2)
            lg_r = Rot(nc, ph, "lg4", [128, 2 * NE], 2)
            mx_r = Rot(nc, ph, "mx4", [128, 16], 2)
            psA = Rot(nc, ph, "psA", [128, 512], 2, psum=True)
            psT = Rot(nc, ph, "psT", [128, 8, 128], 1, psum=True)
            psL = Rot(nc, ph, "psL", [128, NE], 1, psum=True)
            H2T = dscr("H2T", [D, QT])
            for i in range(QT // 128):
                yt, ytk = yt_r.next()
                kb.dma('sp', yt[:], yT[:, i * 128:(i + 1) * 128].rearrange("(k p) n -> p k n", p=128), writes=[ytk])
                xt, xk = xo_r.next()
                kb.dma('sp', xt[:], xo[i * 128:(i + 1) * 128, :], writes=[xk])
                x1, x1k = x1_r.next()
                for hf in range(2):
                    ps, pk = psA.next()
                    for k in range(8):
                        kb.op('pe', lambda e, k=k: e.matmul(ps[:], lhsT=yt[:, k, :], rhs=Wo[:, k, hf * 512:(hf + 1) * 512], start=(k == 0), stop=(k == 7)),
                              reads=[ytk, "Wo"], writes=[pk], inc=(k == 7))
                    kb.op('dve', lambda e: e.tensor_tensor(out=x1[:, hf * 512:(hf + 1) * 512], in0=ps[:], in1=GT[:, 0, hf * 512:(hf + 1) * 512], op=ALU.mult),
                          reads=[pk, "GT"], writes=[x1k])
                kb.op('pool', lambda e: e.tensor_tensor(out=x1[:], in0=x1[:], in1=xt[:], op=ALU.add), reads=[x1k, xk], writes=[x1k])
                kb.dma('sp', X1[i * 128:(i + 1) * 128, :], x1[:], reads=[x1k])
                stt, sk = st_r.next()
                rms_rstd(x1[:], x1k, stt, sk, junk, D)
                xn, nk = xn_r.next()
                kb.op('dve', lambda e: e.tensor_scalar(out=xn[:], in0=x1[:], scalar1=stt[:, 3:4], scalar2=None, op0=ALU.mult), reads=[x1k, sk], writes=[nk])
                pt, ptk = psT.next()
                for k in range(8):
                    kb.op('pe', lambda e, k=k: e.transpose(out=pt[:, k, :], in_=xn[:, k * 128:(k + 1) * 128], identity=idt[:]),
                          reads=[nk, "idt"], writes=[ptk], inc=(k == 7))
                h2, h2k = h2_r.next()
                for k in range(8):
                    if k % 2 == 0:
                        kb.op('dve', lambda e, k=k: e.tensor_scalar(out=h2[:, k, :], in0=pt[:, k, :], scalar1=G2[:, k, 0:1], scalar2=modfm[:, 2, k, 0:1],
                                                                     op0=ALU.mult, op1=ALU.add), reads=[ptk, "G2", "modfm"], writes=[h2k])
                    else:
                        kb.op('act', lambda e, k=k: e.activation(out=h2[:, k, :], in_=pt[:, k, :], func=AF.Identity, scale=G2[:, k, 0:1],
                                                                  bias=modfm[:, 2, k, 0:1]), reads=[ptk, "G2", "modfm"], writes=[h2k])
                kb.dma('sp', H2T[:, i * 128:(i + 1) * 128].rearrange("(k p) n -> p k n", p=128), h2[:], reads=[h2k])
                pl, plk = psL.next()
                for k in range(8):
                    kb.op('pe', lambda e, k=k: e.matmul(pl[:], lhsT=h2[:, k, :], rhs=Wr[:, k, :], start=(k == 0), stop=(k == 7)),
                          reads=[h2k, "Wr"], writes=[plk], inc=(k == 7))
                lg, lgk = lg_r.next()
                mx, mxk = mx_r.next()
                kb.op('dve', lambda e: e.tensor_tensor(out=lg[:, 0:NE], in0=pl[:], in1=rbb[:], op=ALU.add), reads=[plk, "rbb"], writes=[lgk])
                kb.op('dve', lambda e: e.max(out=mx[:, 0:8], in_=lg[:, 0:NE]), reads=[lgk], writes=[mxk])
                kb.op('dve', lambda e: e.tensor_scalar(out=mx[:, 8:9], in0=mx[:, 0:1], scalar1=-1.0, scalar2=None, op0=ALU.mult), reads=[mxk], writes=[mxk])
                kb.op('dve', lambda e: e.tensor_scalar(out=lg[:, NE:2 * NE], in0=lg[:, 0:NE], scalar1=mx[:, 3:4], scalar2=None, op0=ALU.is_ge),
                      reads=[lgk, mxk], writes=[lgk])
                kb.op('act', lambda e: e.activation(out=lg[:, 0:NE], in_=lg[:, 0:NE], func=AF.Exp, bias=mx[:, 8:9], scale=1.0), reads=[lgk, mxk], writes=[lgk])
                kb.op('dve', lambda e: e.tensor_tensor(out=lg[:, 0:NE], in0=lg[:, 0:NE], in1=lg[:, NE:2 * NE], op=ALU.mult), reads=[lgk], writes=[lgk])
                kb.op('dve', lambda e: e.reduce_sum(out=mx[:, 9:10], in_=lg[:, 0:NE], axis=AX.X), reads=[lgk], writes=[mxk])
                kb.op('dve', lambda e: e.reciprocal(out=mx[:, 10:11], in_=mx[:, 9:10]), reads=[mxk], writes=[mxk])
                kb.op('dve', lambda e: e.tensor_scalar(out=lg[:, 0:NE], in0=lg[:, 0:NE], scalar1=mx[:, 10:11], scalar2=None, op0=ALU.mult), reads=[lgk, mxk], writes=[lgk])
                kb.dma('sp', LG[i * 128:(i + 1) * 128, :], lg[:], reads=[lgk])
            kb.barrier()

        with ExitStack() as ph, Stage('5') as go:
          if go:
            sbp = lambda name, shape: ph.enter_context(nc.sbuf_tensor(name, shape, F32))
            HT = 512
            h2T = sbp("h2T", [128, 8, HT])
            acc = sbp("acc", [128, 4, D])
            gts = sbp("gts", [128, 4, NE])
            gT = sbp("gT", [NE, 4, 128])
            b1t = sbp("b1t", [128, NE * 16])
            kb.dma('sp', b1t[:], b1fm, writes=["b1t"])
            b2t = sbp("b2t", [NE, D])
            kb.dma('sp', b2t[:], b2, writes=["b2t"])
            BF16 = mybir.dt.bfloat16
            ph.enter_context(nc.allow_low_precision("bf16 matmul operands with fp32 PSUM accumulation"))
            wp_r = Rot(nc, ph, "wp", [128, 8, 512], 2)
            w2_r = Rot(nc, ph, "w2p", [128, 4, D], 2)
            wpb_r = Rot(nc, ph, "wpb", [128, 8, 512], 2, dtype=BF16)
            w2b_r = Rot(nc, ph, "w2b", [128, 4, D], 2, dtype=BF16)
            h2Tb = ph.enter_context(nc.sbuf_tensor("h2Tb", [128, 8, HT], BF16))
            actT = ph.enter_context(nc.sbuf_tensor("actT", [128, 8, 512], BF16))
            ga_r = Rot(nc, ph, "ga", [128, 512], 3)
            sg_r = Rot(nc, ph, "sgm", [128, 512], 3)
            li_r = Rot(nc, ph, "li", [128, 512], 3)
            psU = Rot(nc, ph, "psU", [128, 512], 4, psum=True)
            psY = Rot(nc, ph, "psY", [128, 512], 3, psum=True)
            psG = Rot(nc, ph, "psG", [NE, 128], 1, psum=True)
            for half in range(QT // HT):
                t0 = half * HT
                kb.dma('sp', h2T[:], H2T[:, t0:t0 + HT].rearrange("(k p) n -> p k n", p=128), writes=["h2T"])
                kb.op('act', lambda e: e.copy(out=h2Tb[:].rearrange("p k n -> p (k n)"), in_=h2T[:].rearrange("p k n -> p (k n)")), reads=["h2T"], writes=["h2Tb"])
                for i in range(4):
                    kb.dma('sp', gts[:, i, :], LG[t0 + i * 128:t0 + (i + 1) * 128, 0:NE], writes=["gts"])
                for i in range(4):
                    pg, pgk = psG.next()
                    kb.op('pe', lambda e: e.transpose(out=pg[:], in_=gts[:, i, :], identity=idt[:]), reads=["gts", "idt"], writes=[pgk])
                    kb.op('act', lambda e: e.copy(out=gT[:, i, :], in_=pg[:]), reads=[pgk], writes=["gT"])
                for i in range(4):
                    for hf in range(2):
                        ps, pk = psY.next()
                        kb.op('pe', lambda e: e.matmul(ps[:], lhsT=gT[:, i, :], rhs=b2t[:, hf * 512:(hf + 1) * 512], start=True, stop=True),
                              reads=["gT", "b2t"], writes=[pk])
                        kb.op('dve', lambda e: e.tensor_copy(out=acc[:, i, hf * 512:(hf + 1) * 512], in_=ps[:]), reads=[pk], writes=[("acc", i)])
                for ex in range(NE):
                    w2p = []
                    for j2 in range(2):
                        wt, wk = w2_r.next()
                        kb.dma('sp', wt[:], w2[ex, j2 * 512:(j2 + 1) * 512, :].rearrange("(k p) n -> p k n", p=128), writes=[wk])
                        wtb, wbk = w2b_r.next()
                        kb.op('act', lambda e: e.copy(out=wtb[:].rearrange("p k n -> p (k n)"), in_=wt[:].rearrange("p k n -> p (k n)")), reads=[wk], writes=[wbk])
                        w2p.append((wtb, wbk))
                    for tb in range(1):
                        for pc in range(4):
                            wt, wk = wp_r.next()
                            kb.dma('sp', wt[:, :, 0:256], w1[ex, :, pc * 256:(pc + 1) * 256].rearrange("(k p) n -> p k n", p=128), writes=[wk])
                            kb.dma('sp', wt[:, :, 256:512], w1[ex, :, D + pc * 256:D + (pc + 1) * 256].rearrange("(k p) n -> p k n", p=128), writes=[wk])
                            wtf, wkf = wt, wk
                            wt, wk = wpb_r.next()
                            kb.op('act', lambda e: e.copy(out=wt[:].rearrange("p k n -> p (k n)"), in_=wtf[:].rearrange("p k n -> p (k n)")), reads=[wkf], writes=[wk])
                            for jj in range(2):
                                j = pc * 2 + jj
                                pgl, pglk = psU.next()
                                pli, plik = psU.next()
                                for k in range(8):
                                    kb.op('pe', lambda e, k=k: e.matmul(pgl[:], lhsT=wt[:, k, jj * 128:(jj + 1) * 128], rhs=h2Tb[:, k, tb * 512:(tb + 1) * 512],
                                                                        start=(k == 0), stop=(k == 7)), reads=[wk, "h2Tb"], writes=[pglk], inc=(k == 7))
                                for k in range(8):
                                    kb.op('pe', lambda e, k=k: e.matmul(pli[:], lhsT=wt[:, k, 256 + jj * 128:256 + (jj + 1) * 128], rhs=h2Tb[:, k, tb * 512:(tb + 1) * 512],
                                                                        start=(k == 0), stop=(k == 7)), reads=[wk, "h2Tb"], writes=[plik], inc=(k == 7))
                                bg = b1t[:, ex * 16 + j:ex * 16 + j + 1]
                                bl = b1t[:, ex * 16 + 8 + j:ex * 16 + 8 + j + 1]
                                ga, gak = ga_r.next()
                                kb.op('dve', lambda e: e.tensor_scalar(out=ga[:], in0=pgl[:], scalar1=bg, scalar2=7.0, op0=ALU.add, op1=ALU.min),
                                      reads=[pglk, "b1t"], writes=[gak])
                                sg, sgk = sg_r.next()
                                kb.op('act', lambda e: e.activation(out=sg[:], in_=ga[:], func=AF.Sigmoid, scale=1.702), reads=[gak], writes=[sgk])
                                li, lik = li_r.next()
                                kb.op('dve', lambda e: e.tensor_scalar(out=li[:], in0=pli[:], scalar1=bl, scalar2=7.0, op0=ALU.add, op1=ALU.min),
                                      reads=[plik, "b1t"], writes=[lik])
                                kb.op('pool', lambda e: e.tensor_scalar(out=li[:], in0=li[:], scalar1=-7.0, scalar2=1.0, op0=ALU.max, op1=ALU.add),
                                      reads=[lik], writes=[lik])
                                kb.op('pool', lambda e: e.tensor_tensor(out=ga[:], in0=ga[:], in1=sg[:], op=ALU.mult), reads=[gak, sgk], writes=[gak])
                                kb.op('pool', lambda e, j=j: e.tensor_tensor(out=actT[:, j, :], in0=ga[:], in1=li[:], op=ALU.mult), reads=[gak, lik], writes=[("actT", j)])
                        for ti in range(4):
                            i = tb * 4 + ti
                            for hf in range(2):
                                ps, pk = psY.next()
                                for j in range(8):
                                    wt2, wk2 = w2p[j // 4]
                                    kb.op('pe', lambda e, j=j: e.matmul(ps[:], lhsT=actT[:, j, ti * 128:(ti + 1) * 128], rhs=wt2[:, j % 4, hf * 512:(hf + 1) * 512],
                                                                        start=(j == 0), stop=(j == 7)), reads=[("actT", j), wk2], writes=[pk], inc=(j == 7))
                                kb.op('dve', lambda e: e.scalar_tensor_tensor(out=acc[:, i, hf * 512:(hf + 1) * 512], in0=ps[:], scalar=gts[:, i, ex:ex + 1],
                                                                               in1=acc[:, i, hf * 512:(hf + 1) * 512], op0=ALU.mult, op1=ALU.add),
                                      reads=[pk, "gts", ("acc", i)], writes=[("acc", i)])
                for i in range(4):
                    kb.dma('sp', FF[t0 + i * 128:t0 + (i + 1) * 128, :], acc[:, i, :], reads=[("acc", i)])
            kb.barrier()

        with ExitStack() as ph, Stage('6') as go:
          if go:
            sbp = lambda name, shape: ph.enter_context(nc.sbuf_tensor(name, shape, F32))
            gfb = sbp("gfb", [128, D])
            kb.dma('sp', gfb[:], gfin.partition_broadcast(128), writes=["gfb"])
            junk = sbp("junk6", [128, 1024])
            x1_r = Rot(nc, ph, "x16", [128, 1024], 2)
            ff_r = Rot(nc, ph, "ff6", [128, 1024], 2)
            st_r = Rot(nc, ph, "st6", [128, 4], 3)
            o_r = Rot(nc, ph, "o6", [128, 1024], 2)
            for i in range(QT // 128):
                x1, x1k = x1_r.next()
                ff, ffk = ff_r.next()
                kb.dma('sp', x1[:], X1[i * 128:(i + 1) * 128, :], writes=[x1k])
                kb.dma('sp', ff[:], FF[i * 128:(i + 1) * 128, :], writes=[ffk])
                kb.op('dve', lambda e: e.tensor_tensor(out=ff[:], in0=ff[:], in1=GT[:, 1, :], op=ALU.mult), reads=[ffk, "GT"], writes=[ffk])
                kb.op('pool', lambda e: e.tensor_tensor(out=x1[:], in0=x1[:], in1=ff[:], op=ALU.add), reads=[ffk, x1k], writes=[x1k])
                stt, sk = st_r.next()
                rms_rstd(x1[:], x1k, stt, sk, junk, D)
                o, ok = o_r.next()
                kb.op('dve', lambda e: e.scalar_tensor_tensor(out=o[:], in0=x1[:], scalar=stt[:, 3:4], in1=gfb[:], op0=ALU.mult, op1=ALU.mult),
                      reads=[x1k, sk, "gfb"], writes=[ok])
                kb.dma('sp', out[i * 128:(i + 1) * 128, :], o[:], reads=[ok])
            kb.barrier()
        print("instructions", kb.nins, "dmas", kb.ndma, "cnt", kb.cnt)
    return nc


def rope_tables(tok):
    fr = (10000.0 ** (-np.arange(16, dtype=np.float32) / 16)).astype(np.float32)
    pos = [(tok // 64).astype(np.float32), (tok % 64).astype(np.float32)]
    C = np.zeros((64, len(tok)), np.float32)
    S = np.zeros((64, len(tok)), np.float32)
    for a in range(2):
        ang = (pos[a][None, :] * fr[:, None]).astype(np.float32)
        for hf in range(2):
            r0 = a * 32 + hf * 16
            C[r0:r0 + 16] = np.cos(ang)
            S[r0:r0 + 16] = np.sin(ang) * (-1.0 if hf == 0 else 1.0)
    return C, S


def make_inputs(inp, core):
    b, q = core // 4, core % 4
    f = lambda a: np.ascontiguousarray(a, dtype=np.float32)
    w = inp["w_in"][0]
    perm = np.arange(64).reshape(2, 2, 16)[:, ::-1, :].reshape(64)
    kr = w[:, 1792 + 384:1792 + 448]
    wcat = np.concatenate([w[:, :1792], w[:, 1792 + 256:1792 + 384], kr, kr[:, perm], w[:, 1792:1792 + 256]], axis=1)
    cv = np.stack([inp["c"][b].reshape(8, 128).T, inp["c_ctx"].reshape(8, 128).T], axis=2).reshape(128, 16)
    xb = inp["x"][b]
    xo = np.zeros((XO_ROWS, D), np.float32)
    xo[:QT] = xb[q * QT:(q + 1) * QT]
    if q > 0:
        xo[QT] = xb[q * QT - 1]
    if q < 3:
        xo[QT + 1] = xb[(q + 1) * QT]
    C, S = rope_tables(np.arange(T))
    wukv = inp["mla_w_ukv"][0].reshape(128, 4, 256)
    wuq = inp["mla_w_uq"][0].reshape(256, 4, 192)
    wuq_p = np.concatenate([wuq[:, :, :128], wuq[:, :, 128:], wuq[:, :, 128:][:, :, perm]], axis=2).reshape(256, 1024)
    w1 = inp["exp_w1"][0]
    w1d = np.concatenate([w1[:, :, 0::2], w1[:, :, 1::2]], axis=2)
    b1 = inp["exp_b1"][0]
    b1d = np.concatenate([b1[:, 0::2], b1[:, 1::2]], axis=1)
    b1fm = b1d.reshape(NE, 16, 128).transpose(2, 0, 1).reshape(128, NE * 16)
    d = {
        "xs": f(np.concatenate([inp["ctx"][b], xb], axis=0)),
        "xo": f(xo),
        "cvec": f(cv),
        "mod_w": f(inp["mod_w"][0]),
        "mod_b": f(inp["mod_b"][0]),
        "g1": f(inp["norm1_g"][0]),
        "g2n": f(inp["norm2_g"][0]),
        "gfin": f(inp["final_norm_g"]),
        "w_in": f(wcat),
        "ident": np.eye(128, dtype=np.float32),
        "ropeC": f(C), "ropeS": f(S),
        "ropeCo": f(C[:, q * QT:(q + 1) * QT]), "ropeSo": f(S[:, q * QT:(q + 1) * QT]),
        "kvg": f(inp["mla_kv_norm"][0].reshape(128, 1)),
        "qg": f(inp["mla_q_norm"][0].reshape(2, 128).T),
        "wukv_k": f(wukv[:, :, :128].reshape(128, 512)),
        "wukv_v": f(wukv[:, :, 128:].reshape(128, 512)),
        "wuq": f(wuq_p),
        "w_out": f(inp["w_out"][0]),
        "router_w": f(inp["router_w"][0]),
        "router_b": f(inp["router_b"][0]),
        "w1": f(w1d) if "5" in RUN else None,
        "b1fm": f(b1fm),
        "w2": f(inp["exp_w2"][0]) if "5" in RUN else None,
        "b2": f(inp["exp_b2"][0]),
    }
    if '2' in RUN:
        rc = rwkv_consts(q)
        rc.update(mu=inp["rwkv_mu"][0], w0=inp["rwkv_w0"][0], a0=inp["rwkv_a0"][0], w2=inp["rwkv_w2"][0], a2=inp["rwkv_a2"][0],
                  k_k=inp["rwkv_k_k"][0], k_a=inp["rwkv_k_a"][0], r_k=inp["rwkv_r_k"][0].reshape(512), ln_w=inp["rwkv_ln_w"][0],
                  ln_b=inp["rwkv_ln_b"][0], g2=inp["rwkv_g2"][0])
        for k_, v_ in rc.items():
            d["rw_" + k_] = f(v_)
    return {k: v for k, v in d.items() if v is not None}


def kernel(**inputs):
    inp = {k: np.asarray(v) for k, v in inputs.items()}
    nc = build()
    in_maps = [make_inputs(inp, c) for c in range(8)]
    if DEBUG:
        for c in range(8):
            if '2' not in RUN:
                in_maps[c]["yrw_in"] = kernel.dbg_yrw[c]
    res = run_bass_kernel_spmd(nc, in_maps, core_ids=list(range(8)))
    outp = np.zeros((2, T, D), np.float32)
    for c in range(8):
        b, q = c // 4, c % 4
        outp[b, q * QT:(q + 1) * QT] = res.results[c]["out"]
    if DEBUG:
        kernel.debug = res.results
    return outp
```

```python
import os
import numpy as np
from contextlib import ExitStack
import concourse.bass as bass
import concourse.mybir as mybir
from concourse.bass_utils import run_bass_kernel_spmd

F32 = mybir.dt.float32
AF = mybir.ActivationFunctionType
ALU = mybir.AluOpType
AX = mybir.AxisListType

D = 1024
T = 8192
CTX = 256
TS = CTX + T
QT = 2048
NBLK_FULL = [(0, 256)] + [(256 + i * 512, 512) for i in range(16)]
WCOLS = 2304
DEBUG = os.environ.get("KDEBUG", "")


class KB:
    NRING = 12

    def __init__(self, nc, stack, same_engine_sync=True):
        self.nc = nc
        self.stack = stack
        self.E = {'pe': nc.tensor, 'dve': nc.vector, 'act': nc.scalar, 'pool': nc.gpsimd, 'sp': nc.sync}
        self.sem = {e: stack.enter_context(nc.semaphore("s_" + e)) for e in ('pe', 'dve', 'act', 'pool')}
        self.cnt = {e: 0 for e in self.sem}
        self.ring = [stack.enter_context(nc.semaphore("d%d" % i)) for i in range(self.NRING)]
        self.ndma = 0
        self.seen = {e: {} for e in self.E}
        self.res = {}
        self.pend = {e: ([], []) for e in self.E}
        self.ses = same_engine_sync
        self.nins = 0
        for s_ in list(self.sem.values()) + self.ring:
            nc.gpsimd.sem_clear(s_)
        nc.all_engine_barrier()

    def _wait(self, eng, sem, val):
        k = sem.name
        if self.seen[eng].get(k, 0) >= val:
            return
        self.E[eng].wait_ge(sem, val)
        self.seen[eng][k] = val

    def _deps(self, eng, reads, writes):
        deps = []
        for r in reads:
            st = self.res.get(r)
            if st and st[0]:
                deps.append(st[0])
        for w in writes:
            st = self.res.get(w)
            if st:
                if st[0]:
                    deps.append(st[0])
                deps.extend(st[1].values())
        own = self.sem.get(eng)
        for (sem, val) in deps:
            if own is not None and sem.name == own.name and (eng == 'pe' or not self.ses):
                continue
            self._wait(eng, sem, val)

    def _record(self, tok, reads, writes):
        for r in reads:
            st = self.res.setdefault(r, [None, {}])
            old = st[1].get(tok[0].name)
            if old is None or old[1] < tok[1]:
                st[1][tok[0].name] = tok
        for w in writes:
            self.res[w] = [tok, {}]

    def op(self, eng, fn, reads=(), writes=(), inc=True):
        self._deps(eng, reads, writes)
        ins = fn(self.E[eng])
        self.nins += 1
        pr, pw = self.pend[eng]
        pr.extend(reads)
        pw.extend(writes)
        if inc:
            self.cnt[eng] += 1
            ins.then_inc(self.sem[eng], 1)
            self._record((self.sem[eng], self.cnt[eng]), pr, pw)
            self.pend[eng] = ([], [])
        return ins

    def dma(self, q, out, in_, reads=(), writes=(), **kw):
        i = self.ndma
        self.ndma += 1
        sem = self.ring[i % self.NRING]
        val = 16 * (i // self.NRING + 1)
        if val > 16:
            self._wait(q, sem, val - 16)
        self._deps(q, reads, writes)
        ins = self.E[q].dma_start(out=out, in_=in_, **kw).then_inc(sem, 16)
        self.nins += 1
        self._record((sem, val), list(reads), list(writes))
        return ins

    def barrier(self, engines=('pe', 'dve', 'act', 'pool', 'sp')):
        for e in engines:
            for o, s in self.sem.items():
                if o != e and self.cnt[o] > 0:
                    self._wait(e, s, self.cnt[o])
            for j, s in enumerate(self.ring):
                n = (self.ndma - 1 - j) // self.NRING + 1 if self.ndma > j else 0
                if n > 0:
                    self._wait(e, s, 16 * n)


class Rot:
    def __init__(self, nc, st, name, shape, n, dtype=F32, psum=False):
        mk = nc.psum_tensor if psum else nc.sbuf_tensor
        self.t = [st.enter_context(mk("%s%d" % (name, i), shape, dtype)) for i in range(n)]
        self.name = name
        self.i = 0

    def next(self):
        j = self.i % len(self.t)
        self.i += 1
        return self.t[j], (self.name, j)


KRW_BLKS = int(os.environ.get('KRW_BLKS', '99'))
KRW_BLK0 = int(os.environ.get('KRW_BLK0', '0'))
KRW_BSTOP = int(os.environ.get('KRW_BSTOP', '99'))
KRW_NOB = int(os.environ.get('KRW_NOB', '0'))
KRW_NOCHAIN = int(os.environ.get('KRW_NOCHAIN', '0'))
KRW_MARK = int(os.environ.get('KRW_MARK', '99'))


class StopRegion(Exception):
    pass


def mark(i):
    if i >= KRW_MARK:
        raise StopRegion()


CDEC = 0.6065306597126334
GN_EPS = 64e-5
FULL_BLKS = [(0, 256, True, True)] + [(256 + i * 512, 512, i == 0, i == 15) for i in range(16)]
OWN_RBLKS = [(i * 512, 512, i == 0, i == 3) for i in range(4)]
NCH_FULL = TS // 64
NCH_OWN = QT // 64


def rwkv_stage(nc, kb, st, pT, poT, yT, C, dscr):
    idt = C["idt"]
    GTs = dscr("GTs", [2, 4, 128, NCH_FULL, 128])
    Nsc = dscr("Nsc", [2, 4, 128, NCH_FULL, 128])
    GTo = dscr("GTo", [2, 4, 128, NCH_OWN, 128])
    Nso = dscr("Nso", [2, 4, 128, NCH_OWN, 128])
    RhTo = dscr("RhTo", [2, 4, 128, NCH_OWN, 128])
    Oho = dscr("Oho", [2, 4, 128, NCH_OWN, 128])
    BONs = dscr("BONs", [4, 128, QT])
    Gsc = dscr("Gsc", [4, 128, QT])
    if os.environ.get('KRW_ALLOC_ONLY'):
        return
    with ExitStack() as ph:
        sbp = lambda name, shape: ph.enter_context(nc.sbuf_tensor(name, shape, F32))
        def ld(name, shape, src, **kw):
            t = sbp(name, shape)
            kb.dma('sp', t[:], src, writes=[name], **kw)
            return t
        mu_t = ld("mu_t", [128, 14], C["mu"].rearrange("(c p) -> p c", p=128), allow_slow_non_contiguous=True)
        om_t = sbp("om_t", [128, 14]); hm_t = sbp("hm_t", [128, 14])
        kb.op('dve', lambda e: e.tensor_scalar(out=om_t[:], in0=mu_t[:], scalar1=-1.0, scalar2=1.0, op0=ALU.mult, op1=ALU.add), reads=["mu_t"], writes=["om_t"])
        kb.op('dve', lambda e: e.tensor_scalar(out=hm_t[:], in0=mu_t[:], scalar1=0.5, scalar2=None, op0=ALU.mult), reads=["mu_t"], writes=["hm_t"])
        w0_t = ld("w0_t", [128, 2, 4], C["w0"].rearrange("d (c p) -> p d c", p=128), allow_slow_non_contiguous=True)
        a0_t = ld("a0_t", [128, 2, 4], C["a0"].rearrange("d (c p) -> p d c", p=128), allow_slow_non_contiguous=True)
        W2A2 = sbp("W2A2", [128, 2, 512])
        kb.dma('sp', W2A2[0:64], C["w2"].rearrange("d l c -> l d c"), writes=["W2A2"])
        kb.dma('sp', W2A2[64:128], C["a2"].rearrange("d l c -> l d c"), writes=["W2A2"])
        kk_t = ld("kk_t", [128, 4], C["k_k"].rearrange("(c p) -> p c", p=128), allow_slow_non_contiguous=True)
        ka_t = ld("ka_t", [128, 4], C["k_a"].rearrange("(c p) -> p c", p=128), allow_slow_non_contiguous=True)
        oka_t = sbp("oka_t", [128, 4])
        kb.op('dve', lambda e: e.tensor_scalar(out=oka_t[:], in0=ka_t[:], scalar1=-1.0, scalar2=1.0, op0=ALU.mult, op1=ALU.add), reads=["ka_t"], writes=["oka_t"])
        rk_t = ld("rk_t", [128, 4], C["r_k"].rearrange("(c p) -> p c", p=128), allow_slow_non_contiguous=True)
        lnw_t = ld("lnw_t", [128, 4], C["ln_w"].rearrange("(c p) -> p c", p=128), allow_slow_non_contiguous=True)
        lnb_t = ld("lnb_t", [128, 4], C["ln_b"].rearrange("(c p) -> p c", p=128), allow_slow_non_contiguous=True)
        g2_t = ld("g2_t", [128, 512], C["g2"])
        MASK4 = ld("MASK4", [128, 2, 512], C["mask4"].rearrange("d p n -> p d n"))
        MASKL = ld("MASKL", [128, 2, 128], C["maskl"].rearrange("d p n -> p d n"))
        BLK = ld("BLK", [128, 128], C["blk"])
        UU = ld("UU", [128, 64], C["uu"])
        SEL = ld("SEL", [128, 64], C["sel"])
        selF = ld("selF", [128, 4], C["selF"])
        selB = ld("selB", [128, 4], C["selB"])
        hal = ld("hal", [128, 2], C["hal"])
        if os.environ.get('KRW_STOP') == '1':
            kb.barrier()
            return
        P_r = Rot(nc, ph, "Pl", [128, 514], 3)
        sh_r = Rot(nc, ph, "shf", [128, 512], 4)
        RS_r = Rot(nc, ph, "RSs", [128, 512], 2)
        KS_r = Rot(nc, ph, "KSs", [128, 512], 2)
        VS_r = Rot(nc, ph, "VSs", [128, 512], 2)
        KK_r = Rot(nc, ph, "KKs", [128, 512], 2)
        X12_r = Rot(nc, ph, "X12", [128, 512], 2)
        TX_r = Rot(nc, ph, "TXs", [128, 512], 2)
        dA_r = Rot(nc, ph, "dA", [128, 512], 14)
        bs_r = Rot(nc, ph, "bsr", [128, 512], 2)
        pl_r = Rot(nc, ph, "plr", [128, 8], 4)
        ex_r = {nm: Rot(nc, ph, "ex" + nm, [128, 8, 2, 64], 2) for nm in ("A", "R", "B", "K", "Bp", "Kp")}
        exV_r = Rot(nc, ph, "exV", [128, 8, 2, 64], 2)
        for r_ in list(ex_r.values()) + [exV_r]:
            for j_, t_ in enumerate(r_.t):
                kb.op('pool', lambda e, t_=t_: e.memset(t_[:], 0.0), writes=[(r_.name, j_)])
        psA = Rot(nc, ph, "psRA", [128, 512], 3, psum=True)
        psB = Rot(nc, ph, "psRB", [128, 512], 5, psum=True)
        AT4_r = Rot(nc, ph, "AT4", [128, 4, 128], 2)
        AB_r = Rot(nc, ph, "ABk", [128, 2, 128], 4)
        XT_r = Rot(nc, ph, "XTk", [128, 128], 3)
        TM_r = Rot(nc, ph, "TMk", [128, 4, 128], 2)
        AU_r = Rot(nc, ph, "AUk", [128, 2, 128], 2)
        stG_r = Rot(nc, ph, "stG", [128, 8, 128], 1)
        stN_r = Rot(nc, ph, "stN", [128, 8, 128], 1)
        stR_r = Rot(nc, ph, "stR", [128, 8, 128], 1)
        stO_r = Rot(nc, ph, "stO", [128, 8, 128], 1)
        print('RWKV region sbuf remaining', nc.sbuf_bytes_remaining, nc.SBUF_PARTITION_SIZE_BYTES)
        cnt = {"ev": 0}

        def evac(out_ap, in_ap, reads, writes):
            cnt["ev"] += 1
            if True:
                kb.op('dve', lambda e: e.tensor_copy(out=out_ap, in_=in_ap), reads=reads, writes=writes)
            else:
                kb.op('act', lambda e: e.copy(out=out_ap, in_=in_ap), reads=reads, writes=writes)

        def load_shift(src, row0, c0, n, lb, rb, own, dst, dk, cc):
            P, pk = P_r.next()
            lo = c0 - (0 if lb else 1)
            hi = c0 + n + (0 if rb else 1)
            doff = 1 if lb else 0
            kb.dma('sp', P[:, doff:doff + (hi - lo)], src[row0:row0 + 128, lo:hi], writes=[pk])
            if lb:
                if own:
                    kb.dma('sp', P[:, 0:1], src[row0:row0 + 128, QT:QT + 1], writes=[pk], allow_slow_non_contiguous=True)
                    kb.op('dve', lambda e: e.tensor_scalar(out=P[:, 0:1], in0=P[:, 0:1], scalar1=hal[:, 0:1], scalar2=None, op0=ALU.mult), reads=[pk, "hal"], writes=[pk])
                else:
                    kb.op('pool', lambda e: e.memset(P[:, 0:1], 0.0), writes=[pk])
            if rb:
                if own:
                    kb.dma('sp', P[:, n + 1:n + 2], src[row0:row0 + 128, QT + 1:QT + 2], writes=[pk], allow_slow_non_contiguous=True)
                    kb.op('dve', lambda e: e.tensor_scalar(out=P[:, n + 1:n + 2], in0=P[:, n + 1:n + 2], scalar1=hal[:, 1:2], scalar2=None, op0=ALU.mult), reads=[pk, "hal"], writes=[pk])
                else:
                    kb.op('pool', lambda e: e.memset(P[:, n + 1:n + 2], 0.0), writes=[pk])
            t, tk = sh_r.next()
            kb.op('pool', lambda e: e.tensor_tensor(out=t[:, :n], in0=P[:, 0:n], in1=P[:, 2:n + 2], op=ALU.add), reads=[pk], writes=[tk])
            u, uk = sh_r.next()
            kb.op('dve', lambda e: e.tensor_scalar(out=u[:, :n], in0=P[:, 1:n + 1], scalar1=om_t[:, cc:cc + 1], scalar2=None, op0=ALU.mult), reads=[pk, "om_t"], writes=[uk])
            kb.op('dve', lambda e: e.scalar_tensor_tensor(out=dst[:, :n], in0=t[:, :n], scalar=hm_t[:, cc:cc + 1], in1=u[:, :n], op0=ALU.mult, op1=ALU.add),
                  reads=[tk, uk, "hm_t"], writes=[dk])

        def c3(ap, n):
            return ap.rearrange("p (c t) -> p c t", t=64)

        def exp_write(eng, dst, dk, nch, n, fn, reads):
            for hh in range(2):
                sl = slice(hh * 64, hh * 64 + 64)
                kb.op(eng, lambda e, hh=hh, sl=sl: fn(e, dst[sl, :nch, hh, :], sl), reads=reads, writes=[dk])

        def region(src, blocks, own, GTd, Nd, RhTd, Ohd, chunk0_of_block):
            for bi, (c0, n, lb, rb) in list(enumerate(blocks))[KRW_BLK0:KRW_BLK0 + KRW_BLKS]:
                nch = n // 64
                ch0 = chunk0_of_block(bi)
                X12, xk = X12_r.next()
                load_shift(src, 1536, c0, n, lb, rb, own, X12, xk, 12)
                TX, txk = TX_r.next()
                kb.op('act', lambda e: e.activation(out=TX[0:64, :n], in_=X12[0:64, :n], func=AF.Tanh), reads=[xk], writes=[txk])
                mark(1)
                if own:
                    XG, xgk = dA_r.next()
                    load_shift(src, 1664, c0, n, lb, rb, own, XG, xgk, 13)
                    SGg, sggk = TX_r.next()
                    kb.op('act', lambda e: e.activation(out=SGg[:, :n], in_=XG[:, :n], func=AF.Sigmoid), reads=[xgk], writes=[sggk])
                for hp in range(4):
                    RS, rsk = RS_r.next(); KS, ksk = KS_r.next(); VS, vsk = VS_r.next(); KK, kkk = KK_r.next()
                    load_shift(src, hp * 128, c0, n, lb, rb, own, RS, rsk, hp)
                    load_shift(src, 512 + hp * 128, c0, n, lb, rb, own, KS, ksk, 4 + hp)
                    load_shift(src, 1024 + hp * 128, c0, n, lb, rb, own, VS, vsk, 8 + hp)
                    mark(2)
                    kkr, kkrk = dA_r.next()
                    kb.op('dve', lambda e: e.tensor_scalar(out=kkr[:, :n], in0=KS[:, :n], scalar1=kk_t[:, hp:hp + 1], scalar2=None, op0=ALU.mult), reads=[ksk, "kk_t"], writes=[kkrk])
                    sq, sqk = dA_r.next()
                    kb.op('pool', lambda e: e.tensor_tensor(out=sq[:, :n], in0=kkr[:, :n], in1=kkr[:, :n], op=ALU.mult), reads=[kkrk], writes=[sqk])
                    pss, pssk = psA.next()
                    kb.op('pe', lambda e: e.matmul(pss[:, :n], lhsT=BLK[:], rhs=sq[:, :n], start=True, stop=True), reads=["BLK", sqk], writes=[pssk])
                    kb.op('dve', lambda e: e.tensor_scalar(out=sq[:, :n], in0=pss[:, :n], scalar1=1e-24, scalar2=None, op0=ALU.max), reads=[pssk], writes=[sqk])
                    kb.op('act', lambda e: e.activation(out=sq[:, :n], in_=sq[:, :n], func=AF.Sqrt), reads=[sqk], writes=[sqk])
                    kb.op('dve', lambda e: e.reciprocal(out=sq[:, :n], in_=sq[:, :n]), reads=[sqk], writes=[sqk])
                    kb.op('pool', lambda e: e.tensor_tensor(out=KK[:, :n], in0=kkr[:, :n], in1=sq[:, :n], op=ALU.mult), reads=[kkrk, sqk], writes=[kkk])
                    mark(3)
                    Vd, vdk = exV_r.next()
                    exp_write('pool', Vd, vdk, nch, n, lambda e, o, sl: e.tensor_copy(out=o, in_=c3(VS[sl, :n], n)), [vsk])
                    mark(4)
                    if own:
                        bsum, bsk = bs_r.next()
                    for d in range(2):
                        psw, pswk = psA.next()
                        kb.op('pe', lambda e: e.matmul(psw[:, :n], lhsT=W2A2[0:64, d, hp * 128:(hp + 1) * 128], rhs=TX[0:64, :n], start=True, stop=True),
                              reads=["W2A2", txk], writes=[pswk])
                        SGM, sgk = dA_r.next()
                        kb.op('act', lambda e: e.activation(out=SGM[:, :n], in_=psw[:, :n], func=AF.Sigmoid, bias=w0_t[:, d, hp:hp + 1], scale=1.0), reads=[pswk, "w0_t"], writes=[sgk])
                        psa, psak = psA.next()
                        kb.op('pe', lambda e: e.matmul(psa[:, :n], lhsT=W2A2[64:128, d, hp * 128:(hp + 1) * 128], rhs=X12[64:128, :n], start=True, stop=True),
                              reads=["W2A2", xk], writes=[psak])
                        AA, aak = dA_r.next()
                        kb.op('act', lambda e: e.activation(out=AA[:, :n], in_=psa[:, :n], func=AF.Sigmoid, bias=a0_t[:, d, hp:hp + 1], scale=1.0), reads=[psak, "a0_t"], writes=[aak])
                        mark(5)
                        CIN, cik = dA_r.next()
                        T1, t1k = sh_r.next()
                        src_, srck_ = SGM, sgk
                        for si, s_ in enumerate((1, 2, 4, 8, 16, 32)):
                            dst_, dstk_ = (T1, t1k) if si % 2 == 0 else (CIN, cik)
                            kb.op('pool', lambda e, s_=s_, src_=src_, dst_=dst_: e.tensor_tensor(out=c3(dst_[:, :n], n)[:, :, s_:], in0=c3(src_[:, :n], n)[:, :, s_:],
                                                                                                in1=c3(src_[:, :n], n)[:, :, :64 - s_], op=ALU.add),
                                  reads=[srck_], writes=[dstk_])
                            kb.op('act', lambda e, s_=s_, src_=src_, dst_=dst_: e.copy(out=c3(dst_[:, :n], n)[:, :, :s_], in_=c3(src_[:, :n], n)[:, :, :s_]),
                                  reads=[srck_], writes=[dstk_])
                            src_, srck_ = dst_, dstk_
                        mark(6)
                        CEX, cek = dA_r.next()
                        kb.op('pool', lambda e: e.tensor_tensor(out=CEX[:, :n], in0=CIN[:, :n], in1=SGM[:, :n], op=ALU.subtract), reads=[cik, sgk], writes=[cek])
                        totb = c3(CIN[:, :n], n)[:, :, 63:64].to_broadcast([128, nch, 64])
                        Dm, dmk = dA_r.next()
                        kb.op('dve', lambda e: e.tensor_tensor(out=c3(Dm[:, :n], n), in0=totb, in1=c3(CIN[:, :n], n), op=ALU.subtract), reads=[cik], writes=[dmk])
                        DX, dxk = dA_r.next()
                        kb.op('dve', lambda e: e.tensor_tensor(out=c3(DX[:, :n], n), in0=totb, in1=c3(CEX[:, :n], n), op=ALU.subtract), reads=[cik, cek], writes=[dxk])
                        srcs = {0: ((CIN, cik, -CDEC), (CEX, cek, -CDEC), (CIN, cik, CDEC), (Dm, dmk, -CDEC)),
                                1: ((DX, dxk, -CDEC), (Dm, dmk, -CDEC), (DX, dxk, CDEC), (CEX, cek, -CDEC))}[d]
                        E4 = []
                        for (s_, sk_, sc_) in srcs:
                            o_, ok_ = dA_r.next()
                            kb.op('act', lambda e, s_=s_, o_=o_, sc_=sc_: e.activation(out=o_[:, :n], in_=s_[:, :n], func=AF.Exp, scale=sc_), reads=[sk_], writes=[ok_])
                            E4.append((o_, ok_))
                        (PIN, pik), (PEX, pek), (INV, ink), (EEND, eek) = E4
                        PL, plk = pl_r.next()
                        kb.op('act', lambda e: e.activation(out=PL[:, :nch], in_=CIN[:, 63:n:64], func=AF.Exp, scale=-CDEC), reads=[cik], writes=[plk])
                        mark(7)
                        KD, kdk = dA_r.next()
                        kb.op('dve', lambda e: e.tensor_scalar(out=KD[:, :n], in0=AA[:, :n], scalar1=ka_t[:, hp:hp + 1], scalar2=oka_t[:, hp:hp + 1], op0=ALU.mult, op1=ALU.add),
                              reads=[aak, "ka_t", "oka_t"], writes=[kdk])
                        kb.op('pool', lambda e: e.tensor_tensor(out=KD[:, :n], in0=KD[:, :n], in1=KS[:, :n], op=ALU.mult), reads=[kdk, ksk], writes=[kdk])
                        Bv, bvk = dA_r.next()
                        kb.op('pool', lambda e: e.tensor_tensor(out=Bv[:, :n], in0=KK[:, :n], in1=AA[:, :n], op=ALU.mult), reads=[kkk, aak], writes=[bvk])
                        if own:
                            if d == 0:
                                kb.op('pool', lambda e: e.tensor_tensor(out=bsum[:, :n], in0=RS[:, :n], in1=KD[:, :n], op=ALU.mult), reads=[rsk, kdk], writes=[bsk])
                            else:
                                t_, tk_ = sh_r.next()
                                kb.op('pool', lambda e: e.tensor_tensor(out=t_[:, :n], in0=RS[:, :n], in1=KD[:, :n], op=ALU.mult), reads=[rsk, kdk], writes=[tk_])
                                kb.op('pool', lambda e: e.tensor_tensor(out=bsum[:, :n], in0=bsum[:, :n], in1=t_[:, :n], op=ALU.add), reads=[bsk, tk_], writes=[bsk])
                        mark(8)
                        ex = {nm: ex_r[nm].next() for nm in ex_r}
                        exp_write('dve', ex["A"][0], ex["A"][1], nch, n,
                                  lambda e, o, sl: e.scalar_tensor_tensor(out=o, in0=c3(KK[sl, :n], n), scalar=-1.0, in1=c3(PEX[sl, :n], n), op0=ALU.mult, op1=ALU.mult), [kkk, pek])
                        exp_write('pool', ex["R"][0], ex["R"][1], nch, n, lambda e, o, sl: e.tensor_tensor(out=o, in0=c3(RS[sl, :n], n), in1=c3(PIN[sl, :n], n), op=ALU.mult), [rsk, pik])
                        exp_write('dve', ex["B"][0], ex["B"][1], nch, n, lambda e, o, sl: e.tensor_tensor(out=o, in0=c3(Bv[sl, :n], n), in1=c3(INV[sl, :n], n), op=ALU.mult), [bvk, ink])
                        exp_write('pool', ex["K"][0], ex["K"][1], nch, n, lambda e, o, sl: e.tensor_tensor(out=o, in0=c3(KD[sl, :n], n), in1=c3(INV[sl, :n], n), op=ALU.mult), [kdk, ink])
                        exp_write('dve', ex["Bp"][0], ex["Bp"][1], nch, n, lambda e, o, sl: e.tensor_tensor(out=o, in0=c3(Bv[sl, :n], n), in1=c3(EEND[sl, :n], n), op=ALU.mult), [bvk, eek])
                        exp_write('pool', ex["Kp"][0], ex["Kp"][1], nch, n, lambda e, o, sl: e.tensor_tensor(out=o, in0=c3(KD[sl, :n], n), in1=c3(EEND[sl, :n], n), op=ALU.mult), [kdk, eek])
                        mark(9)
                        stG, stGk = stG_r.next(); stN, stNk = stN_r.next()
                        if own:
                            stR, stRk = stR_r.next(); stO, stOk = stO_r.next()
                        for ci in range(0 if KRW_NOB else nch):
                            f2 = lambda t_: t_[:, ci].rearrange("p a b -> p (a b)")
                            Ad, Rd, Bd, Kd, Bpd, Kpd, Vdd = f2(ex["A"][0]), f2(ex["R"][0]), f2(ex["B"][0]), f2(ex["K"][0]), f2(ex["Bp"][0]), f2(ex["Kp"][0]), f2(Vd)
                            exk = [ex[nm][1] for nm in ("A", "R", "B", "K")]
                            ps1, ps1k = psB.next()
                            for qi, (l_, r_) in enumerate(((Bd, Ad), (Kd, Ad), (Bd, Rd), (Kd, Rd))):
                                kb.op('pe', lambda e, qi=qi, l_=l_, r_=r_: e.matmul(ps1[:, qi * 128:(qi + 1) * 128], lhsT=l_, rhs=r_, start=True, stop=True),
                                      reads=exk, writes=[ps1k], inc=(qi == 3))
                            AT4, atk = AT4_r.next()
                            kb.op('dve', lambda e: e.tensor_tensor(out=AT4[:].rearrange("p a b -> p (a b)"), in0=ps1[:], in1=MASK4[:, d, :], op=ALU.mult), reads=[ps1k, "MASK4"], writes=[atk])
                            if KRW_BSTOP <= 1:
                                continue
                            ps2, ps2k = psB.next()
                            kb.op('pe', lambda e: e.matmul(ps2[:, 0:128], lhsT=Ad, rhs=Bd, start=True, stop=True), reads=exk, writes=[ps2k])
                            AB, abk = AB_r.next()
                            kb.op('dve', lambda e: e.tensor_tensor(out=AB[:, 0, :], in0=ps2[:, 0:128], in1=MASKL[:, d, :], op=ALU.mult), reads=[ps2k, "MASKL"], writes=[abk])
                            kb.op('pool', lambda e: e.tensor_copy(out=AB[:, 1, :], in_=AT4[:, 0, :]), reads=[atk], writes=[abk])
                            XT, xtk = XT_r.next()
                            kb.op('pool', lambda e: e.tensor_tensor(out=XT[:], in0=AT4[:, 0, :], in1=idt[:], op=ALU.add), reads=[atk, "idt"], writes=[xtk])
                            if KRW_BSTOP <= 2:
                                continue
                            for it in range(5):
                                psk_, pskk_ = psB.next()
                                kb.op('pe', lambda e: e.matmul(psk_[:, 0:128], lhsT=AB[:, 1, :], rhs=AB[:, 0, :], start=True, stop=True), reads=[abk], writes=[pskk_], inc=False)
                                kb.op('pe', lambda e: e.matmul(psk_[:, 128:256], lhsT=AB[:, 0, :], rhs=AB[:, 1, :], start=True, stop=True), reads=[abk], writes=[pskk_])
                                AB2, ab2k = AB_r.next()
                                evac(AB2[:].rearrange("p a b -> p (a b)"), psk_[:, 0:256], [pskk_], [ab2k])
                                psx, psxk = psB.next()
                                kb.op('pe', lambda e: e.matmul(psx[:, 0:128], lhsT=AB2[:, 0, :], rhs=XT[:], start=True, stop=True), reads=[ab2k, xtk], writes=[psxk])
                                XT2, xt2k = XT_r.next()
                                kb.op('dve', lambda e: e.tensor_tensor(out=XT2[:], in0=psx[:, 0:128], in1=XT[:], op=ALU.add), reads=[psxk, xtk], writes=[xt2k])
                                AB, abk, XT, xtk = AB2, ab2k, XT2, xt2k
                            WT, wtk = XT, xtk
                            if KRW_BSTOP <= 3:
                                continue
                            pst, pstk = psB.next()
                            exk2 = [ex["A"][1], ex["Bp"][1], ex["Kp"][1], vdk]
                            for qi, s_ in enumerate((Ad, Bpd, Kpd, Vdd)):
                                kb.op('pe', lambda e, qi=qi, s_=s_: e.matmul(pst[:, qi * 128:(qi + 1) * 128], lhsT=s_, rhs=idt[:], start=True, stop=True), reads=exk2 + ["idt"], writes=[pstk], inc=(qi == 3))
                            if os.environ.get('KRW_X') == 'noevac':
                                continue
                            TM, tmk = TM_r.next()
                            AU, auk = AU_r.next()
                            Vtm, vtk = XT_r.next()
                            evac(TM[:, 0, :], pst[:, 0:128], [pstk], [tmk])
                            if os.environ.get('KRW_X') == 'split':
                                evac(TM[:, 2, :], pst[:, 128:256], [pstk], [tmk])
                                evac(TM[:, 3, :], pst[:, 256:384], [pstk], [tmk])
                            else:
                                evac(TM[:, 2:4, :].rearrange("p a b -> p (a b)"), pst[:, 128:384], [pstk], [tmk])
                            evac(Vtm[:], pst[:, 384:512], [pstk], [vtk])
                            if KRW_BSTOP <= 4:
                                continue
                            psx_, psxk_ = psB.next()
                            kb.op('pe', lambda e: e.matmul(psx_[:, 0:128], lhsT=AT4[:, 1, :], rhs=Vtm[:], start=True, stop=True), reads=[atk, vtk], writes=[psxk_])
                            evac(TM[:, 1, :], psx_[:, 0:128], [psxk_], [tmk])
                            psau, psauk = psB.next()
                            kb.op('pe', lambda e: e.matmul(psau[:, 0:256], lhsT=WT[:], rhs=TM[:, 0:2, :].rearrange("p a b -> p (a b)"), start=True, stop=True), reads=[wtk, tmk], writes=[psauk])
                            evac(AU[:].rearrange("p a b -> p (a b)"), psau[:, 0:256], [psauk], [auk])
                            if KRW_BSTOP <= 5:
                                continue
                            psg, psgk = psB.next()
                            kb.op('pe', lambda e: e.matmul(psg[:, 0:128], lhsT=AU[:, 0, :], rhs=TM[:, 2, :], start=True, stop=True), reads=[auk, tmk], writes=[psgk])
                            kb.op('dve', lambda e: e.scalar_tensor_tensor(out=stG[:, ci, :], in0=idt[:], scalar=PL[:, ci:ci + 1], in1=psg[:, 0:128], op0=ALU.mult, op1=ALU.add),
                                  reads=[psgk, plk, "idt"], writes=[stGk])
                            if KRW_BSTOP <= 6:
                                continue
                            psn, psnk = psB.next()
                            kb.op('pe', lambda e: e.matmul(psn[:, 0:128], lhsT=TM[:, 2, :], rhs=AU[:, 1, :], start=True, stop=False), reads=[auk, tmk], writes=[psnk], inc=False)
                            kb.op('pe', lambda e: e.matmul(psn[:, 0:128], lhsT=TM[:, 3, :], rhs=Vtm[:], start=False, stop=True), reads=[tmk, vtk], writes=[psnk])
                            evac(stN[:, ci, :], psn[:, 0:128], [psnk], [stNk])
                            if KRW_BSTOP <= 7:
                                continue
                            if own:
                                psr, psrk = psB.next()
                                kb.op('pe', lambda e: e.matmul(psr[:, 0:128], lhsT=AU[:, 0, :], rhs=AT4[:, 2, :], start=True, stop=True), reads=[auk, atk], writes=[psrk])
                                kb.op('dve', lambda e: e.tensor_tensor(out=stR[:, ci, :], in0=psr[:, 0:128], in1=Rd, op=ALU.add), reads=[psrk, ex["R"][1]], writes=[stRk])
                                if KRW_BSTOP <= 8:
                                    continue
                                pso, psok = psB.next()
                                kb.op('pe', lambda e: e.matmul(pso[:, 0:128], lhsT=AT4[:, 2, :], rhs=AU[:, 1, :], start=True, stop=False), reads=[auk, atk], writes=[psok], inc=False)
                                kb.op('pe', lambda e: e.matmul(pso[:, 0:128], lhsT=AT4[:, 3, :], rhs=Vtm[:], start=False, stop=True), reads=[atk, vtk], writes=[psok])
                                evac(stO[:, ci, :], pso[:, 0:128], [psok], [stOk])
                        if os.environ.get('KRW_NOST'):
                            continue
                        kb.dma('sp', GTd[d, hp, :, ch0:ch0 + nch, :], stG[:, :nch, :], reads=[stGk])
                        kb.dma('sp', Nd[d, hp, :, ch0:ch0 + nch, :], stN[:, :nch, :], reads=[stNk])
                        if own:
                            kb.dma('sp', RhTd[d, hp, :, ch0:ch0 + nch, :], stR[:, :nch, :], reads=[stRk])
                            kb.dma('sp', Ohd[d, hp, :, ch0:ch0 + nch, :], stO[:, :nch, :], reads=[stOk])
                    if own:
                        kb.op('dve', lambda e: e.tensor_scalar(out=bsum[:, :n], in0=bsum[:, :n], scalar1=rk_t[:, hp:hp + 1], scalar2=None, op0=ALU.mult), reads=[bsk, "rk_t"], writes=[bsk])
                        psb_, psbk_ = psA.next()
                        kb.op('pe', lambda e: e.matmul(psb_[:, :n], lhsT=BLK[:], rhs=bsum[:, :n], start=True, stop=True), reads=["BLK", bsk], writes=[psbk_])
                        bo, bok = sh_r.next()
                        kb.op('dve', lambda e: e.tensor_tensor(out=bo[:, :n], in0=psb_[:, :n], in1=VS[:, :n], op=ALU.mult), reads=[psbk_, vsk], writes=[bok])
                        kb.dma('sp', BONs[hp, :, c0:c0 + n], bo[:, :n], reads=[bok])
                        psg_, psgk_ = psA.next()
                        kb.op('pe', lambda e: e.matmul(psg_[:, :n], lhsT=g2_t[:, hp * 128:(hp + 1) * 128], rhs=SGg[:, :n], start=True, stop=True), reads=["g2_t", sggk], writes=[psgk_])
                        go, gok = sh_r.next()
                        evac(go[:, :n], psg_[:, :n], [psgk_], [gok])
                        kb.dma('sp', Gsc[hp, :, c0:c0 + n], go[:, :n], reads=[gok])

        KREG = os.environ.get('KRW_REG', 'both')
        if KRW_MARK < 99:
            try:
                region(pT, FULL_BLKS, False, GTs, Nsc, None, None, lambda bi: 0)
            except StopRegion:
                pass
            kb.barrier()
            return
        if KREG in ('both', 'full'):
            region(pT, FULL_BLKS, False, GTs, Nsc, None, None, lambda bi: 0 if bi == 0 else 4 + (bi - 1) * 8)
        if KREG in ('both', 'own'):
            region(poT, OWN_RBLKS, True, GTo, Nso, RhTo, Oho, lambda bi: bi * 8)
        kb.barrier()

    if KRW_NOCHAIN:
        return
    with ExitStack() as ph:
        sbp = lambda name, shape: ph.enter_context(nc.sbuf_tensor(name, shape, F32))
        idt_ = idt
        selF = sbp("selF2", [128, 4]); kb.dma('sp', selF[:], C["selF"], writes=["selF2"])
        selB = sbp("selB2", [128, 4]); kb.dma('sp', selB[:], C["selB"], writes=["selB2"])
        SEL = sbp("SEL2", [128, 64]); kb.dma('sp', SEL[:], C["sel"], writes=["SEL2"])
        lnw_t = sbp("lnw2", [128, 4]); kb.dma('sp', lnw_t[:], C["ln_w"].rearrange("(c p) -> p c", p=128), writes=["lnw2"], allow_slow_non_contiguous=True)
        lnb_t = sbp("lnb2", [128, 4]); kb.dma('sp', lnb_t[:], C["ln_b"].rearrange("(c p) -> p c", p=128), writes=["lnb2"], allow_slow_non_contiguous=True)
        chains = [(d, hp) for d in range(2) for hp in range(4)]
        S = {}
        for (d, hp) in chains:
            S[(d, hp)] = [sbp("S%d%d_%d" % (d, hp, i), [128, 128]) for i in range(2)]
            kb.op('pool', lambda e: e.memset(S[(d, hp)][0][:], 0.0), writes=[("S", d, hp, 0)])
        CAND = {(d, hp): sbp("CA%d%d" % (d, hp), [128, 4, 128]) for (d, hp) in chains}
        ph1 = ExitStack()
        gl_r = {ch: Rot(nc, ph1, "gl%d%d" % ch, [128, 8, 128], 1) for ch in chains}
        nl_r = {ch: Rot(nc, ph1, "nl%d%d" % ch, [128, 8, 128], 1) for ch in chains}
        psC = Rot(nc, ph, "psC", [128, 512], 8, psum=True)
        cur = {ch: 0 for ch in chains}
        def groups(d):
            if d == 0:
                g = [list(range(0, 4))] + [list(range(4 + i * 8, 12 + i * 8)) for i in range(12)]
            else:
                g = [list(range(3, -1, -1))] + [list(range(4 + i * 8 + 7, 4 + i * 8 - 1, -1)) for i in range(15, 3, -1)]
            return g
        G = {0: groups(0), 1: groups(1)}
        ngroups = len(G[0])
        for gi in range(ngroups):
            loaded = {}
            for ch in chains:
                d, hp = ch
                g = G[d][gi]
                lo = min(g)
                gl, glk = gl_r[ch].next(); nl, nlk = nl_r[ch].next()
                kb.dma('sp', gl[:, :len(g), :], GTs[d, hp, :, lo:lo + len(g), :], writes=[glk])
                kb.dma('sp', nl[:, :len(g), :], Nsc[d, hp, :, lo:lo + len(g), :], writes=[nlk])
                loaded[ch] = (gl, glk, nl, nlk, lo)
            for step in range(len(G[0][gi])):
                for ch in chains:
                    d, hp = ch
                    gl, glk, nl, nlk, lo = loaded[ch]
                    c = G[d][gi][step] - lo
                    i0 = cur[ch]; i1 = 1 - i0
                    ps, pk = psC.next()
                    kb.op('pe', lambda e: e.matmul(ps[:, 0:128], lhsT=gl[:, c, :], rhs=S[ch][i0][:], start=True, stop=True), reads=[glk, ("S", d, hp, i0)], writes=[pk])
                    kb.op('dve', lambda e: e.tensor_tensor(out=S[ch][i1][:], in0=ps[:, 0:128], in1=nl[:, c, :], op=ALU.add), reads=[pk, nlk], writes=[("S", d, hp, i1)])
                    cur[ch] = i1
            if gi in (0, 4, 8, 12):
                ci_ = {0: 0, 4: 1, 8: 2, 12: 3}[gi]
                for ch in chains:
                    d, hp = ch
                    kb.op('dve', lambda e: e.tensor_copy(out=CAND[ch][:, ci_, :], in_=S[ch][cur[ch]][:]), reads=[("S", d, hp, cur[ch])], writes=[("CAND", d, hp)])
        for ch in chains:
            d, hp = ch
            sel = selF if d == 0 else selB
            seln = "selF2" if d == 0 else "selB2"
            i0 = cur[ch]
            kb.op('dve', lambda e: e.tensor_scalar(out=S[ch][i0][:], in0=CAND[ch][:, 0, :], scalar1=sel[:, 0:1], scalar2=None, op0=ALU.mult),
                  reads=[("CAND", d, hp), seln], writes=[("S", d, hp, i0)])
            for i in range(1, 4):
                kb.op('dve', lambda e: e.scalar_tensor_tensor(out=S[ch][i0][:], in0=CAND[ch][:, i, :], scalar=sel[:, i:i + 1], in1=S[ch][i0][:], op0=ALU.mult, op1=ALU.add),
                      reads=[("CAND", d, hp), seln, ("S", d, hp, i0)], writes=[("S", d, hp, i0)])
        kb.barrier()
        ph1.close()
        OD = {ch: sbp("OD%d%d" % ch, [128, NCH_OWN, 64]) for ch in chains}
        gl_r = {ch: Rot(nc, ph, "g2l%d%d" % ch, [128, 4, 128], 1) for ch in chains}
        nl_r = {ch: Rot(nc, ph, "n2l%d%d" % ch, [128, 4, 128], 1) for ch in chains}
        rl_r = {ch: Rot(nc, ph, "rl%d%d" % ch, [128, 4, 128], 1) for ch in chains}
        ol_r = {ch: Rot(nc, ph, "ol%d%d" % ch, [128, 4, 128], 1) for ch in chains}
        tmp_r = Rot(nc, ph, "ctmp", [128, 128], 4)
        for gi in range(8):
            loaded = {}
            for ch in chains:
                d, hp = ch
                g = list(range(gi * 4, gi * 4 + 4)) if d == 0 else list(range(31 - gi * 4, 27 - gi * 4, -1))
                lo = min(g)
                gl, glk = gl_r[ch].next(); nl, nlk = nl_r[ch].next(); rl, rlk = rl_r[ch].next(); ol, olk = ol_r[ch].next()
                for (t_, k_, src_) in ((gl, glk, GTo), (nl, nlk, Nso), (rl, rlk, RhTo), (ol, olk, Oho)):
                    kb.dma('sp', t_[:], src_[d, hp, :, lo:lo + 4, :], writes=[k_])
                loaded[ch] = (gl, glk, nl, nlk, rl, rlk, ol, olk, lo, g)
            for step in range(4):
                for ch in chains:
                    d, hp = ch
                    gl, glk, nl, nlk, rl, rlk, ol, olk, lo, g = loaded[ch]
                    cg = g[step]; c = cg - lo
                    i0 = cur[ch]; i1 = 1 - i0
                    pso, psok = psC.next()
                    kb.op('pe', lambda e: e.matmul(pso[:, 0:128], lhsT=rl[:, c, :], rhs=S[ch][i0][:], start=True, stop=True), reads=[rlk, ("S", d, hp, i0)], writes=[psok], inc=False)
                    kb.op('pe', lambda e: e.matmul(pso[:, 128:256], lhsT=gl[:, c, :], rhs=S[ch][i0][:], start=True, stop=True), reads=[glk, ("S", d, hp, i0)], writes=[psok])
                    kb.op('dve', lambda e: e.tensor_tensor(out=S[ch][i1][:], in0=pso[:, 128:256], in1=nl[:, c, :], op=ALU.add), reads=[psok, nlk], writes=[("S", d, hp, i1)])
                    tt, ttk = tmp_r.next()
                    kb.op('dve', lambda e: e.tensor_tensor(out=tt[:], in0=pso[:, 0:128], in1=ol[:, c, :], op=ALU.add), reads=[psok, olk], writes=[ttk])
                    kb.op('pool', lambda e: e.tensor_tensor(out=OD[ch][:, cg, :], in0=tt[:, 0:64], in1=tt[:, 64:128], op=ALU.add), reads=[ttk], writes=[("OD", d, hp)])
                    cur[ch] = i1
        ye_r = Rot(nc, ph, "yexp", [128, 8, 2, 64], 2)
        for j_, t_ in enumerate(ye_r.t):
            kb.op('pool', lambda e, t_=t_: e.memset(t_[:], 0.0), writes=[("yexp", j_)])
        os_r = Rot(nc, ph, "osum", [128, 8, 64], 2)
        sq_r = Rot(nc, ph, "osq", [128, 8, 64], 2)
        stt_r = Rot(nc, ph, "ostt", [128, 4, 8], 2)
        yn_r = Rot(nc, ph, "ynr", [128, 512], 2)
        bg_r = Rot(nc, ph, "bgr", [128, 2, 512], 2)
        for hp in range(4):
            for bi in range(4):
                OS, osk = os_r.next()
                kb.op('pool', lambda e: e.tensor_tensor(out=OS[:], in0=OD[(0, hp)][:, bi * 8:(bi + 1) * 8, :], in1=OD[(1, hp)][:, bi * 8:(bi + 1) * 8, :], op=ALU.add),
                      reads=[("OD", 0, hp), ("OD", 1, hp)], writes=[osk])
                stt, sk = stt_r.next()
                kb.op('dve', lambda e: e.reduce_sum(out=stt[:, 0, :], in_=OS[:], axis=AX.X), reads=[osk], writes=[sk])
                SQ, sqk = sq_r.next()
                kb.op('pool', lambda e: e.tensor_tensor(out=SQ[:], in0=OS[:], in1=OS[:], op=ALU.mult), reads=[osk], writes=[sqk])
                kb.op('dve', lambda e: e.reduce_sum(out=stt[:, 1, :], in_=SQ[:], axis=AX.X), reads=[sqk], writes=[sk])
                kb.op('dve', lambda e: e.tensor_scalar(out=stt[:, 0, :], in0=stt[:, 0, :], scalar1=1.0 / 64, scalar2=None, op0=ALU.mult), reads=[sk], writes=[sk])
                kb.op('dve', lambda e: e.tensor_tensor(out=stt[:, 2, :], in0=stt[:, 0, :], in1=stt[:, 0, :], op=ALU.mult), reads=[sk], writes=[sk])
                kb.op('dve', lambda e: e.scalar_tensor_tensor(out=stt[:, 3, :], in0=stt[:, 1, :], scalar=1.0 / 64, in1=stt[:, 2, :], op0=ALU.mult, op1=ALU.subtract), reads=[sk], writes=[sk])
                kb.op('dve', lambda e: e.tensor_scalar(out=stt[:, 3, :], in0=stt[:, 3, :], scalar1=GN_EPS, scalar2=None, op0=ALU.add), reads=[sk], writes=[sk])
                kb.op('act', lambda e: e.activation(out=stt[:, 3, :], in_=stt[:, 3, :], func=AF.Sqrt), reads=[sk], writes=[sk])
                kb.op('dve', lambda e: e.reciprocal(out=stt[:, 3, :], in_=stt[:, 3, :]), reads=[sk], writes=[sk])
                kb.op('dve', lambda e: e.tensor_tensor(out=OS[:], in0=OS[:], in1=stt[:, 0, :].unsqueeze(2).to_broadcast([128, 8, 64]), op=ALU.subtract), reads=[osk, sk], writes=[osk])
                ye, yek = ye_r.next()
                for hh in range(2):
                    sl = slice(hh * 64, hh * 64 + 64)
                    kb.op('dve', lambda e, hh=hh, sl=sl: e.tensor_tensor(out=ye[sl, :, hh, :], in0=OS[sl], in1=stt[sl, 3, :].unsqueeze(2).to_broadcast([64, 8, 64]), op=ALU.mult),
                          reads=[osk, sk], writes=[yek])
                ps, pk = psC.next()
                for ci in range(8):
                    kb.op('pe', lambda e, ci=ci: e.matmul(ps[:, ci * 64:(ci + 1) * 64], lhsT=ye[:, ci].rearrange("p a b -> p (a b)"), rhs=SEL[:], start=True, stop=True),
                          reads=[yek, "SEL2"], writes=[pk], inc=(ci == 7))
                bg, bgk = bg_r.next()
                kb.dma('sp', bg[:, 0, :], BONs[hp, :, bi * 512:(bi + 1) * 512], writes=[bgk])
                kb.dma('sp', bg[:, 1, :], Gsc[hp, :, bi * 512:(bi + 1) * 512], writes=[bgk])
                yn, ynk = yn_r.next()
                kb.op('dve', lambda e: e.tensor_scalar(out=yn[:], in0=ps[:], scalar1=lnw_t[:, hp:hp + 1], scalar2=lnb_t[:, hp:hp + 1], op0=ALU.mult, op1=ALU.add),
                      reads=[pk, "lnw2", "lnb2"], writes=[ynk])
                kb.op('pool', lambda e: e.tensor_tensor(out=yn[:], in0=yn[:], in1=bg[:, 0, :], op=ALU.add), reads=[ynk, bgk], writes=[ynk])
                kb.op('pool', lambda e: e.tensor_tensor(out=yn[:], in0=yn[:], in1=bg[:, 1, :], op=ALU.mult), reads=[ynk, bgk], writes=[ynk])
                kb.dma('sp', yT[hp * 128:(hp + 1) * 128, bi * 512:(bi + 1) * 512], yn[:], reads=[ynk])
        kb.barrier()


def rwkv_consts(q):
    tt = np.arange(64)
    lowS = (tt[:, None] < tt[None, :]).astype(np.float32)
    lowI = (tt[:, None] <= tt[None, :]).astype(np.float32)
    def bd(m):
        z = np.zeros((128, 128), np.float32)
        z[:64, :64] = m
        z[64:, 64:] = m
        return z
    mask4 = np.zeros((2, 128, 512), np.float32)
    maskl = np.zeros((2, 128, 128), np.float32)
    for d in range(2):
        S_, I_ = (lowS, lowI) if d == 0 else (lowS.T, lowI.T)
        mask4[d] = np.concatenate([bd(S_), bd(S_), bd(I_), bd(I_)], axis=1)
        maskl[d] = bd(S_.T)
    blk = bd(np.ones((64, 64), np.float32))
    uu = np.concatenate([(tt[:, None] <= tt[None, :]).astype(np.float32)] * 2, axis=0)
    sel = np.concatenate([np.eye(64, dtype=np.float32)] * 2, axis=0)
    selF = np.zeros((128, 4), np.float32); selF[:, q] = 1.0
    selB = np.zeros((128, 4), np.float32); selB[:, 3 - q] = 1.0
    hal = np.zeros((128, 2), np.float32)
    hal[:, 0] = 1.0 if q > 0 else 0.0
    hal[:, 1] = 1.0 if q < 3 else 0.0
    return dict(mask4=mask4, maskl=maskl, blk=blk, uu=uu, sel=sel, selF=selF, selB=selB, hal=hal)


OWN_BLKS = [(0, 512), (512, 512), (1024, 512), (1536, 512), (2048, 128)]
XO_ROWS = QT + 128
NE = 32
ATT_SCALE = 192.0 ** -0.5


RUN = os.environ.get("KSTAGES", "123456")
SKIP = os.environ.get("KSKIP", "").split(",")


class Stage:
    def __init__(self, s):
        self.s = s

    def __enter__(self):
        return self.s in RUN

    def __exit__(self, *a):
        return False


def build():
    nc = bass.Bass("TRN2", target_bir_lowering=False)
    dt = nc.dram_tensor

    def din(name, shape):
        return dt(name, shape, F32, kind="ExternalInput").ap()

    def dscr(name, shape):
        if DEBUG and name in DEBUG.split(","):
            return dt("dbg_" + name, shape, F32, kind="ExternalOutput").ap()
        return dt(name, shape, F32).ap()

    xs = din("xs", [TS, D])
    xo = din("xo", [XO_ROWS, D])
    cvec = din("cvec", [128, 16])
    mod_w = din("mod_w", [D, 6 * D])
    mod_b = din("mod_b", [6 * D])
    g1 = din("g1", [D])
    g2n = din("g2n", [D])
    gfin = din("gfin", [D])
    w_in = din("w_in", [D, WCOLS])
    ident = din("ident", [128, 128])
    ropeC = din("ropeC", [64, T])
    ropeS = din("ropeS", [64, T])
    ropeCo = din("ropeCo", [64, QT])
    ropeSo = din("ropeSo", [64, QT])
    kvg = din("kvg", [128, 1])
    qg = din("qg", [128, 2])
    wukv_k = din("wukv_k", [128, 512])
    wukv_v = din("wukv_v", [128, 512])
    wuq = din("wuq", [256, 1024])
    w_out = din("w_out", [D, D])
    router_w = din("router_w", [D, NE])
    router_b = din("router_b", [NE])
    w1 = din("w1", [NE, D, 2 * D]) if "5" in RUN else None
    b1fm = din("b1fm", [128, NE * 16])
    w2 = din("w2", [NE, D, D]) if "5" in RUN else None
    b2 = din("b2", [NE, D])
    yrw_in = din("yrw_in", [512, QT]) if (DEBUG and '2' not in RUN) else None
    rw = {}
    if '2' in RUN:
        for nm, shp in (("mu", [1792]), ("w0", [2, 512]), ("a0", [2, 512]), ("w2", [2, 64, 512]), ("a2", [2, 64, 512]), ("k_k", [512]), ("k_a", [512]),
                        ("r_k", [512]), ("ln_w", [512]), ("ln_b", [512]), ("g2", [128, 512]), ("mask4", [2, 128, 512]), ("maskl", [2, 128, 128]),
                        ("blk", [128, 128]), ("uu", [128, 64]), ("sel", [128, 64]), ("selF", [128, 4]), ("selB", [128, 4]), ("hal", [128, 2])):
            rw[nm] = din("rw_" + nm, shp)
    out = dt("out", [QT, D], F32, kind="ExternalOutput").ap()

    pT = dscr("pT", [1792, TS])
    poT = dscr("poT", [1792, XO_ROWS])
    KnT = dscr("KnT", [4, 128, TS])
    KrT = dscr("KrT", [64, TS])
    Vs = dscr("Vs", [TS, 4 * 129])
    QnT = dscr("QnT", [4, 128, QT])
    QrT = dscr("QrT", [4, 64, QT])
    yT = dscr("yT", [D, QT])
    X1 = dscr("X1", [QT, D])
    LG = dscr("LG", [QT, 2 * NE])
    FF = dscr("FF", [QT, D])

    with ExitStack() as st:
        kb = KB(nc, st)
        sb = lambda name, shape: st.enter_context(nc.sbuf_tensor(name, shape, F32))
        idt = sb("idt", [128, 128])
        kb.dma('sp', idt[:], ident, writes=["idt"])
        ones = sb("ones", [128, 128])
        kb.op('pool', lambda e: e.memset(ones[:], 1.0), writes=["ones"])
        cv = sb("cv", [128, 16])
        sc = sb("sc", [128, 16])
        kb.dma('sp', cv[:], cvec, writes=["cv"])
        kb.op('act', lambda e: e.activation(out=sc[:], in_=cv[:], func=AF.Silu), reads=["cv"], writes=["sc"])
        modfm = sb("modfm", [128, 4, 8, 2])
        mbfm = sb("mbfm", [128, 6, 8])
        kb.dma('sp', mbfm[:], mod_b.rearrange("(v k p) -> p v k", p=128, k=8), writes=["mbfm"], allow_slow_non_contiguous=True)
        g1t = sb("g1t", [128, 8])
        kb.dma('sp', g1t[:], g1.rearrange("(k p) -> p k", p=128), writes=["g1t"], allow_slow_non_contiguous=True)
        g2t = sb("g2t", [128, 8])
        kb.dma('sp', g2t[:], g2n.rearrange("(k p) -> p k", p=128), writes=["g2t"], allow_slow_non_contiguous=True)
        GT = sb("GT", [128, 2, 1024])
        with ExitStack() as ph:
            wv = Rot(nc, ph, "wv", [128, 8, 1024], 2)
            psm = Rot(nc, ph, "psm", [128, 16], 2, psum=True)
            psg = Rot(nc, ph, "psg", [128, 512], 2, psum=True)
            SCB = ph.enter_context(nc.sbuf_tensor("SCB", [128, 8, 128], F32))
            mbb = ph.enter_context(nc.sbuf_tensor("mbb", [128, 2, 1024], F32))
            for k in range(8):
                kb.op('dve', lambda e, k=k: e.tensor_copy(out=SCB[:, k, :], in_=sc[:, 2 * k:2 * k + 1].to_broadcast([128, 128])),
                      reads=["sc"], writes=["SCB"])
            for gi, v in enumerate((2, 5)):
                kb.dma('sp', mbb[:, gi, :], mod_b[v * 1024:(v + 1) * 1024].partition_broadcast(128), writes=["mbb"])
            for vi, v in enumerate((0, 1, 3, 4)):
                wt, wk = wv.next()
                kb.dma('sp', wt[:], mod_w[:, v * 1024:(v + 1) * 1024].rearrange("(k p) n -> p k n", p=128), writes=[wk])
                ps, pk = psm.next()
                for j in range(8):
                    for k in range(8):
                        kb.op('pe', lambda e, j=j, k=k: e.matmul(ps[:, 2 * j:2 * j + 2], lhsT=wt[:, k, j * 128:(j + 1) * 128],
                                                                 rhs=sc[:, 2 * k:2 * k + 2], start=(k == 0), stop=(k == 7)),
                              reads=[wk, "sc"], writes=[pk], inc=(j == 7 and k == 7))
                kb.op('dve', lambda e, vi=vi, v=v: e.tensor_tensor(out=modfm[:, vi], in0=ps[:].rearrange("p (j c) -> p j c", c=2),
                                                                   in1=mbfm[:, v, :].unsqueeze(2).to_broadcast([128, 8, 2]), op=ALU.add),
                      reads=[pk, "mbfm"], writes=["modfm"])
            for gi, v in enumerate((2, 5)):
                wt, wk = wv.next()
                kb.dma('sp', wt[:], mod_w[:, v * 1024:(v + 1) * 1024].rearrange("(k p) n -> p k n", p=128), writes=[wk])
                for hf in range(2):
                    ps, pk = psg.next()
                    for k in range(8):
                        kb.op('pe', lambda e, k=k, hf=hf: e.matmul(ps[:], lhsT=SCB[:, k, :], rhs=wt[:, k, hf * 512:(hf + 1) * 512],
                                                                   start=(k == 0), stop=(k == 7)),
                              reads=[wk, "SCB"], writes=[pk], inc=(k == 7))
                    kb.op('dve', lambda e, gi=gi, hf=hf: e.tensor_tensor(out=GT[:, gi, hf * 512:(hf + 1) * 512], in0=ps[:],
                                                                         in1=mbb[:, gi, hf * 512:(hf + 1) * 512], op=ALU.add),
                          reads=[pk, "mbb"], writes=["GT"])
            kb.barrier()
        G1 = sb("G1", [128, 8, 2])
        G2 = sb("G2", [128, 8, 2])
        for Gt, gt_, mrow, nm in ((G1, g1t, 1, "G1"), (G2, g2t, 3, "G2")):
            kb.op('dve', lambda e: e.tensor_scalar(out=Gt[:], in0=modfm[:, mrow], scalar1=1.0, scalar2=None, op0=ALU.add), reads=["modfm"], writes=[nm])
            kb.op('dve', lambda e: e.tensor_tensor(out=Gt[:], in0=Gt[:], in1=gt_[:].unsqueeze(2).to_broadcast([128, 8, 2]), op=ALU.mult),
                  reads=[nm, "g1t", "g2t"], writes=[nm])

        def rms_rstd(xt, xk, stt, sk, junk, n_feat):
            kb.op('pool', lambda e: e.memset(stt[:], 0.0), writes=[sk])
            kb.op('act', lambda e: e.activation(out=junk[:], in_=xt, func=AF.Square, accum_out=stt[:, 0:1]), reads=[xk], writes=["junk", sk])
            kb.op('dve', lambda e: e.tensor_scalar(out=stt[:, 1:2], in0=stt[:, 0:1], scalar1=1.0 / n_feat, scalar2=1e-6, op0=ALU.mult, op1=ALU.add),
                  reads=[sk], writes=[sk])
            kb.op('act', lambda e: e.activation(out=stt[:, 2:3], in_=stt[:, 1:2], func=AF.Sqrt), reads=[sk], writes=[sk])
            kb.op('dve', lambda e: e.reciprocal(out=stt[:, 3:4], in_=stt[:, 2:3]), reads=[sk], writes=[sk])

        def cm_rstd(ps_ss, pk, dst, dk, n, n_feat, extra_scale=1.0):
            kb.op('dve', lambda e: e.tensor_scalar(out=dst[:, :n], in0=ps_ss[:, :n], scalar1=1.0 / n_feat, scalar2=1e-6, op0=ALU.mult, op1=ALU.add),
                  reads=[pk], writes=[dk])
            kb.op('act', lambda e: e.activation(out=dst[:, :n], in_=dst[:, :n], func=AF.Sqrt), reads=[dk], writes=[dk])
            kb.op('dve', lambda e: e.reciprocal(out=dst[:, :n], in_=dst[:, :n]), reads=[dk], writes=[dk])
            if extra_scale != 1.0:
                kb.op('dve', lambda e: e.tensor_scalar(out=dst[:, :n], in0=dst[:, :n], scalar1=extra_scale, scalar2=None, op0=ALU.mult), reads=[dk], writes=[dk])

        with ExitStack() as ph, Stage('1') as go:
          if go:
            sbp = lambda name, shape: ph.enter_context(nc.sbuf_tensor(name, shape, F32))
            WB = sbp("WB", [128, 8, WCOLS])
            for k in range(8):
                kb.dma('sp', WB[:, k, :], w_in[k * 128:(k + 1) * 128, :], writes=[("WB", k)])
            wk_t = sbp("wk_t", [128, 512]); kb.dma('sp', wk_t[:], wukv_k, writes=["wk_t"])
            wv_t = sbp("wv_t", [128, 512]); kb.dma('sp', wv_t[:], wukv_v, writes=["wv_t"])
            wq_t = sbp("wq_t", [128, 2, 1024]); kb.dma('sp', wq_t[:], wuq.rearrange("(k p) n -> p k n", p=128), writes=["wq_t"])
            kvg_t = sbp("kvg_t", [128, 1]); kb.dma('sp', kvg_t[:], kvg, writes=["kvg_t"])
            qg_t = sbp("qg_t", [128, 2]); kb.dma('sp', qg_t[:], qg, writes=["qg_t"])
            xt_r = Rot(nc, ph, "xt", [128, 1024], 2)
            xn_r = Rot(nc, ph, "xn", [128, 1024], 1)
            st_r = Rot(nc, ph, "stat", [128, 4], 4)
            xmT_r = Rot(nc, ph, "xmT", [128, 8, 512], 2)
            pst_r = Rot(nc, ph, "pst", [128, 8, 128], 1, psum=True)
            psp_r = Rot(nc, ph, "psp", [128, 512], 6, psum=True)
            stg_r = Rot(nc, ph, "stg", [128, 512], 3)
            tmp_r = Rot(nc, ph, "tmp", [128, 512], 3)
            ql_r = Rot(nc, ph, "qlr", [128, 512], 2)
            rs_r = Rot(nc, ph, "rsr", [128, 512], 1)
            ckn_r = Rot(nc, ph, "cknr", [128, 512], 1)
            rp_r = Rot(nc, ph, "rp", [64, 2, 512], 2)
            vst_r = Rot(nc, ph, "vst", [128, 4, 129], 2)
            for j_, t_ in enumerate(vst_r.t):
                kb.op('pool', lambda e, t_=t_: e.memset(t_[:], 1.0), writes=[("vst", j_)])
            junk = sbp("junk", [128, 1024])
            cnt = {"ev": 0}

            def evac(out_ap, in_ap, reads, writes):
                cnt["ev"] += 1
                if cnt["ev"] % 2:
                    kb.op('dve', lambda e: e.tensor_copy(out=out_ap, in_=in_ap), reads=reads, writes=writes)
                else:
                    kb.op('act', lambda e: e.copy(out=out_ap, in_=in_ap), reads=reads, writes=writes)

            def proj_pass(src, blocks, dstT, own):
                for (s0, n) in blocks:
                    mi = 1 if (not own and s0 < CTX) else 0
                    xmT, xmk = xmT_r.next()
                    for i in range(n // 128):
                        xt, xk = xt_r.next()
                        kb.dma('sp', xt[:], src[s0 + i * 128: s0 + (i + 1) * 128, :], writes=[xk])
                        stt, sk = st_r.next()
                        rms_rstd(xt[:], xk, stt, sk, junk, D)
                        xn, nk = xn_r.next()
                        kb.op('dve', lambda e: e.tensor_scalar(out=xn[:], in0=xt[:], scalar1=stt[:, 3:4], scalar2=None, op0=ALU.mult),
                              reads=[xk, sk], writes=[nk])
                        pt, ptk = pst_r.next()
                        for k in range(8):
                            kb.op('pe', lambda e, k=k: e.transpose(out=pt[:, k, :], in_=xn[:, k * 128:(k + 1) * 128], identity=idt[:]),
                                  reads=[nk, "idt"], writes=[ptk], inc=(k == 7))
                        for k in range(8):
                            if k % 2 == 0:
                                kb.op('dve', lambda e, k=k, i=i: e.tensor_scalar(out=xmT[:, k, i * 128:(i + 1) * 128], in0=pt[:, k, :],
                                                                                  scalar1=G1[:, k, mi:mi + 1], scalar2=modfm[:, 0, k, mi:mi + 1],
                                                                                  op0=ALU.mult, op1=ALU.add),
                                      reads=[ptk, "G1", "modfm"], writes=[xmk])
                            else:
                                kb.op('act', lambda e, k=k, i=i: e.activation(out=xmT[:, k, i * 128:(i + 1) * 128], in_=pt[:, k, :], func=AF.Identity,
                                                                               scale=G1[:, k, mi:mi + 1], bias=modfm[:, 0, k, mi:mi + 1]),
                                      reads=[ptk, "G1", "modfm"], writes=[xmk])

                    def proj_cols(c0, m):
                        ps, pk = psp_r.next()
                        for k in range(8):
                            kb.op('pe', lambda e, k=k: e.matmul(ps[:m, :n], lhsT=WB[:, k, c0:c0 + m], rhs=xmT[:, k, :n], start=(k == 0), stop=(k == 7)),
                                  reads=[("WB", k), xmk], writes=[pk], inc=(k == 7))
                        return ps, pk

                    for cc in range(14):
                        ps, pk = proj_cols(cc * 128, 128)
                        sg, sgk = stg_r.next()
                        evac(sg[:, :n], ps[:, :n], [pk], [sgk])
                        kb.dma('sp', dstT[cc * 128:(cc + 1) * 128, s0:s0 + n], sg[:, :n], reads=[sgk])
                    if not own and 'kv' not in SKIP:
                        ps, pk = proj_cols(1792, 128)
                        ckv, ck = tmp_r.next()
                        evac(ckv[:, :n], ps[:, :n], [pk], [ck])
                        sq, sqk = tmp_r.next()
                        kb.op('pool', lambda e: e.tensor_tensor(out=sq[:, :n], in0=ckv[:, :n], in1=ckv[:, :n], op=ALU.mult), reads=[ck], writes=[sqk])
                        pss, pssk = psp_r.next()
                        kb.op('pe', lambda e: e.matmul(pss[:, :n], lhsT=ones[:], rhs=sq[:, :n], start=True, stop=True), reads=["ones", sqk], writes=[pssk])
                        rs, rsk = rs_r.next()
                        cm_rstd(pss, pssk, rs, rsk, n, 128.0)
                        ckn, cnk = ckn_r.next()
                        kb.op('dve', lambda e: e.scalar_tensor_tensor(out=ckn[:, :n], in0=ckv[:, :n], scalar=kvg_t[:, 0:1], in1=rs[:, :n],
                                                                       op0=ALU.mult, op1=ALU.mult), reads=[ck, rsk, "kvg_t"], writes=[cnk])
                        for h in range(0 if 'kvK' in SKIP else 4):
                            psk, pskk = psp_r.next()
                            kb.op('pe', lambda e, h=h: e.matmul(psk[:, :n], lhsT=wk_t[:, h * 128:(h + 1) * 128], rhs=ckn[:, :n], start=True, stop=True),
                                  reads=["wk_t", cnk], writes=[pskk])
                            sg, sgk = stg_r.next()
                            evac(sg[:, :n], psk[:, :n], [pskk], [sgk])
                            kb.dma('sp', KnT[h, :, s0:s0 + n], sg[:, :n], reads=[sgk])
                        for i in range(0 if 'kvV' in SKIP else n // 128):
                            psv, psvk = psp_r.next()
                            kb.op('pe', lambda e, i=i: e.matmul(psv[:, :], lhsT=ckn[:, i * 128:(i + 1) * 128], rhs=wv_t[:], start=True, stop=True),
                                  reads=["wv_t", cnk], writes=[psvk])
                            vt, vk = vst_r.next()
                            evac(vt[:, :, 0:128], psv[:].rearrange("p (h d) -> p h d", h=4), [psvk], [vk])
                            kb.dma('sp', Vs[s0 + i * 128:s0 + (i + 1) * 128, :], vt[:].rearrange("p h d -> p (h d)"), reads=[vk])
                        if 'kvR' in SKIP:
                            continue
                        psa, pak = proj_cols(1920, 64)
                        sg, sgk = stg_r.next()
                        if s0 < CTX:
                            evac(sg[:64, :n], psa[:64, :n], [pak], [sgk])
                        else:
                            psb, pbk = proj_cols(1984, 64)
                            rp, rpk = rp_r.next()
                            kb.dma('sp', rp[:, 0, :n], ropeC[:, s0 - CTX:s0 - CTX + n], writes=[rpk])
                            kb.dma('sp', rp[:, 1, :n], ropeS[:, s0 - CTX:s0 - CTX + n], writes=[rpk])
                            t1, t1k = tmp_r.next()
                            kb.op('dve', lambda e: e.tensor_tensor(out=t1[:64, :n], in0=psa[:64, :n], in1=rp[:, 0, :n], op=ALU.mult), reads=[pak, rpk], writes=[t1k])
                            t2, t2k = tmp_r.next()
                            kb.op('dve', lambda e: e.tensor_tensor(out=t2[:64, :n], in0=psb[:64, :n], in1=rp[:, 1, :n], op=ALU.mult), reads=[pbk, rpk], writes=[t2k])
                            kb.op('pool', lambda e: e.tensor_tensor(out=sg[:64, :n], in0=t1[:64, :n], in1=t2[:64, :n], op=ALU.add), reads=[t1k, t2k], writes=[sgk])
                        kb.dma('sp', KrT[:, s0:s0 + n], sg[:64, :n], reads=[sgk])
                    elif own and s0 < QT and 'q' not in SKIP:
                        ql = []
                        pss, pssk = psp_r.next()
                        for kc in range(2):
                            ps, pk = proj_cols(2048 + kc * 128, 128)
                            qn_, qnk = ql_r.next()
                            evac(qn_[:, :n], ps[:, :n], [pk], [qnk])
                            sq, sqk = tmp_r.next()
                            kb.op('pool', lambda e: e.tensor_tensor(out=sq[:, :n], in0=qn_[:, :n], in1=qn_[:, :n], op=ALU.mult), reads=[qnk], writes=[sqk])
                            kb.op('dve', lambda e, kc=kc: e.tensor_scalar(out=qn_[:, :n], in0=qn_[:, :n], scalar1=qg_t[:, kc:kc + 1], scalar2=None, op0=ALU.mult),
                                  reads=[qnk, sqk, "qg_t"], writes=[qnk])
                            kb.op('pe', lambda e, kc=kc: e.matmul(pss[:, :n], lhsT=ones[:], rhs=sq[:, :n], start=(kc == 0), stop=(kc == 1)),
                                  reads=["ones", sqk], writes=[pssk], inc=(kc == 1))
                            ql.append((qn_, qnk))
                        rs, rsk = rs_r.next()
                        cm_rstd(pss, pssk, rs, rsk, n, 256.0, ATT_SCALE)
                        rp, rpk = rp_r.next()
                        kb.dma('sp', rp[:, 0, :n], ropeCo[:, s0:s0 + n], writes=[rpk])
                        kb.dma('sp', rp[:, 1, :n], ropeSo[:, s0:s0 + n], writes=[rpk])
                        for h in range(4):
                            def qmm(c0, m):
                                ps, pk = psp_r.next()
                                for kc in range(2):
                                    kb.op('pe', lambda e, kc=kc: e.matmul(ps[:m, :n], lhsT=wq_t[:, kc, h * 256 + c0:h * 256 + c0 + m], rhs=ql[kc][0][:, :n],
                                                                          start=(kc == 0), stop=(kc == 1)),
                                          reads=["wq_t", ql[kc][1]], writes=[pk], inc=(kc == 1))
                                return ps, pk
                            ps, pk = qmm(0, 128)
                            sg, sgk = stg_r.next()
                            kb.op('dve', lambda e: e.tensor_tensor(out=sg[:, :n], in0=ps[:, :n], in1=rs[:, :n], op=ALU.mult), reads=[pk, rsk], writes=[sgk])
                            kb.dma('sp', QnT[h, :, s0:s0 + n], sg[:, :n], reads=[sgk])
                            psa, pak = qmm(128, 64)
                            psb, pbk = qmm(192, 64)
                            t1, t1k = tmp_r.next()
                            kb.op('dve', lambda e: e.tensor_tensor(out=t1[:64, :n], in0=psa[:64, :n], in1=rp[:, 0, :n], op=ALU.mult), reads=[pak, rpk], writes=[t1k])
                            t2, t2k = tmp_r.next()
                            kb.op('dve', lambda e: e.tensor_tensor(out=t2[:64, :n], in0=psb[:64, :n], in1=rp[:, 1, :n], op=ALU.mult), reads=[pbk, rpk], writes=[t2k])
                            kb.op('pool', lambda e: e.tensor_tensor(out=t1[:64, :n], in0=t1[:64, :n], in1=t2[:64, :n], op=ALU.add), reads=[t1k, t2k], writes=[t1k])
                            sg, sgk = stg_r.next()
                            kb.op('dve', lambda e: e.tensor_tensor(out=sg[:64, :n], in0=t1[:64, :n], in1=rs[:64, :n], op=ALU.mult), reads=[t1k, rsk], writes=[sgk])
                            kb.dma('sp', QrT[h, :, s0:s0 + n], sg[:64, :n], reads=[sgk])

            proj_pass(xs, NBLK_FULL, pT, False)
            if 'own' not in SKIP:
                proj_pass(xo, OWN_BLKS, poT, True)
            kb.barrier()

        if '2' in RUN:
            C = dict(rw)
            C["idt"] = idt
            rwkv_stage(nc, kb, st, pT, poT, yT, C, dscr)
        elif DEBUG:
            with ExitStack() as ph:
                t = ph.enter_context(nc.sbuf_tensor("yin", [128, 4, QT], F32))
                kb.dma('sp', t[:], yrw_in.rearrange("(k p) n -> p k n", p=128), writes=["yin"])
                kb.dma('sp', yT[0:512, :].rearrange("(k p) n -> p k n", p=128), t[:], reads=["yin"])
                kb.barrier()

        with ExitStack() as ph, Stage('3') as go:
          if go:
            sbp = lambda name, shape: ph.enter_context(nc.sbuf_tensor(name, shape, F32))
            Kn = sbp("Kn", [128, TS])
            Kr = sbp("Kr", [64, TS])
            Vh = sbp("Vh", [128, 66, 129])
            kb.dma('sp', Kr[:], KrT, writes=["Kr"])
            qn_r = Rot(nc, ph, "qn", [128, 512], 2)
            qr_r = Rot(nc, ph, "qr", [64, 512], 2)
            pT_r = Rot(nc, ph, "pTt", [128, 512], 3)
            pss_r = Rot(nc, ph, "pss", [128, 512], 3, psum=True)
            pso = [ph.enter_context(nc.psum_tensor("pso%d" % i, [128, 512], F32)) for i in range(4)]
            ptr_r = Rot(nc, ph, "ptr", [128, 128], 1, psum=True)
            yv_r = Rot(nc, ph, "yv", [128, 132], 3)
            yt_r = Rot(nc, ph, "ytt", [128, 512], 2)
            for h in range(4):
                for part in range(4):
                    c0 = part * 2112
                    kb.dma('sp', Kn[:, c0:c0 + 2112], KnT[h, :, c0:c0 + 2112], writes=["Kn"])
                kb.dma('sp', Vh[:], Vs[:, h * 129:(h + 1) * 129].rearrange("(t p) d -> p t d", p=128), writes=["Vh"])
                for qb in range(4):
                    qn_, qnk = qn_r.next()
                    qr_, qrk = qr_r.next()
                    kb.dma('sp', qn_[:], QnT[h, :, qb * 512:(qb + 1) * 512], writes=[qnk])
                    kb.dma('sp', qr_[:], QrT[h, :, qb * 512:(qb + 1) * 512], writes=[qrk])
                    pend = None
                    for kt in range(67):
                        if kt < 66:
                            ps, pk = pss_r.next()
                            kb.op('pe', lambda e: e.matmul(ps[:], lhsT=Kn[:, kt * 128:(kt + 1) * 128], rhs=qn_[:], start=True, stop=False),
                                  reads=["Kn", qnk], writes=[pk], inc=False)
                            kb.op('pe', lambda e: e.matmul(ps[:], lhsT=Kr[:, kt * 128:(kt + 1) * 128], rhs=qr_[:], start=False, stop=True),
                                  reads=["Kr", qrk], writes=[pk])
                            pt_, ptk = pT_r.next()
                            kb.op('act', lambda e: e.activation(out=pt_[:], in_=ps[:], func=AF.Exp), reads=[pk], writes=[ptk])
                            cur = (pt_, ptk, kt)
                        else:
                            cur = None
                        if pend is not None:
                            ppt, pptk, pkt = pend
                            for qi in range(4):
                                kb.op('pe', lambda e, qi=qi: e.matmul(pso[qi][:, 0:129], lhsT=ppt[:, qi * 128:(qi + 1) * 128], rhs=Vh[:, pkt, :],
                                                                      start=(pkt == 0), stop=(pkt == 65)),
                                      reads=[pptk, "Vh"], writes=[("pso", qi)], inc=(qi == 3))
                        pend = cur
                    ytt, ytk = yt_r.next()
                    for qi in range(4):
                        yv, yvk = yv_r.next()
                        kb.op('dve', lambda e: e.reciprocal(out=yv[:, 129:130], in_=pso[qi][:, 128:129]), reads=[("pso", qi)], writes=[yvk])
                        kb.op('dve', lambda e: e.tensor_scalar(out=yv[:, 0:128], in0=pso[qi][:, 0:128], scalar1=yv[:, 129:130], scalar2=None, op0=ALU.mult),
                              reads=[("pso", qi), yvk], writes=[yvk])
                        ptr, ptrk = ptr_r.next()
                        kb.op('pe', lambda e: e.transpose(out=ptr[:], in_=yv[:, 0:128], identity=idt[:]), reads=[yvk, "idt"], writes=[ptrk])
                        kb.op('act', lambda e: e.copy(out=ytt[:, qi * 128:(qi + 1) * 128], in_=ptr[:]), reads=[ptrk], writes=[ytk])
                    kb.dma('sp', yT[512 + h * 128:512 + (h + 1) * 128, qb * 512:(qb + 1) * 512], ytt[:], reads=[ytk])
            kb.barrier()

        with ExitStack() as ph, Stage('4') as go:
          if go:
            sbp = lambda name, shape: ph.enter_context(nc.sbuf_tensor(name, shape, F32))
            Wo = sbp("Wo", [128, 8, D])
            kb.dma('sp', Wo[:], w_out.rearrange("(k p) n -> p k n", p=128), writes=["Wo"])
            Wr = sbp("Wr", [128, 8, NE])
            kb.dma('sp', Wr[:], router_w.rearrange("(k p) n -> p k n", p=128), writes=["Wr"])
            rbb = sbp("rbb", [128, NE])
            kb.dma('sp', rbb[:], router_b.partition_broadcast(128), writes=["rbb"])
            junk = sbp("junk4", [128, 1024])
            yt_r = Rot(nc, ph, "yt4", [128, 8, 128], 2)
            xo_r = Rot(nc, ph, "xo4", [128, 1024], 2)
            x1_r = Rot(nc, ph, "x14", [128, 1024], 2)
            xn_r = Rot(nc, ph, "xn4", [128, 1024], 2)
            st_r = Rot(nc, ph, "st4", [128, 4], 3)
            h2_r = Rot(nc, ph, "h24", [128, 8, 128], 2)
            lg_r = Rot(nc, ph, "lg4", [128, 2 * NE], 2)
            mx_r = Rot(nc, ph, "mx4", [128, 16], 2)
            psA = Rot(nc, ph, "psA", [128, 512], 2, psum=True)
            psT = Rot(nc, ph, "psT", [128, 8, 128], 1, psum=True)
            psL = Rot(nc, ph, "psL", [128, NE], 1, psum=True)
            H2T = dscr("H2T", [D, QT])
            for i in range(QT // 128):
                yt, ytk = yt_r.next()
                kb.dma('sp', yt[:], yT[:, i * 128:(i + 1) * 128].rearrange("(k p) n -> p k n", p=128), writes=[ytk])
                xt, xk = xo_r.next()
                kb.dma('sp', xt[:], xo[i * 128:(i + 1) * 128, :], writes=[xk])
                x1, x1k = x1_r.next()
                for hf in range(2):
                    ps, pk = psA.next()
                    for k in range(8):
                        kb.op('pe', lambda e, k=k: e.matmul(ps[:], lhsT=yt[:, k, :], rhs=Wo[:, k, hf * 512:(hf + 1) * 512], start=(k == 0), stop=(k == 7)),
                              reads=[ytk, "Wo"], writes=[pk], inc=(k == 7))
                    kb.op('dve', lambda e: e.tensor_tensor(out=x1[:, hf * 512:(hf + 1) * 512], in0=ps[:], in1=GT[:, 0, hf * 512:(hf + 1) * 512], op=ALU.mult),
                          reads=[pk, "GT"], writes=[x1k])
                kb.op('pool', lambda e: e.tensor_tensor(out=x1[:], in0=x1[:], in1=xt[:], op=ALU.add), reads=[x1k, xk], writes=[x1k])
                kb.dma('sp', X1[i * 128:(i + 1) * 128, :], x1[:], reads=[x1k])
                stt, sk = st_r.next()
                rms_rstd(x1[:], x1k, stt, sk, junk, D)
                xn, nk = xn_r.next()
                kb.op('dve', lambda e: e.tensor_scalar(out=xn[:], in0=x1[:], scalar1=stt[:, 3:4], scalar2=None, op0=ALU.mult), reads=[x1k, sk], writes=[nk])
                pt, ptk = psT.next()
                for k in range(8):
                    kb.op('pe', lambda e, k=k: e.transpose(out=pt[:, k, :], in_=xn[:, k * 128:(k + 1) * 128], identity=idt[:]),
                          reads=[nk, "idt"], writes=[ptk], inc=(k == 7))
                h2, h2k = h2_r.next()
                for k in range(8):
                    if k % 2 == 0:
                        kb.op('dve', lambda e, k=k: e.tensor_scalar(out=h2[:, k, :], in0=pt[:, k, :], scalar1=G2[:, k, 0:1], scalar2=modfm[:, 2, k, 0:1],
                                                                     op0=ALU.mult, op1=ALU.add), reads=[ptk, "G2", "modfm"], writes=[h2k])
                    else:
                        kb.op('act', lambda e, k=k: e.activation(out=h2[:, k, :], in_=pt[:, k, :], func=AF.Identity, scale=G2[:, k, 0:1],
                                                                  bias=modfm[:, 2, k, 0:1]), reads=[ptk, "G2", "modfm"], writes=[h2k])
                kb.dma('sp', H2T[:, i * 128:(i + 1) * 128].rearrange("(k p) n -> p k n", p=128), h2[:], reads=[h2k])
                pl, plk = psL.next()
                for k in range(8):
                    kb.op('pe', lambda e, k=k: e.matmul(pl[:], lhsT=h2[:, k, :], rhs=Wr[:, k, :], start=(k == 0), stop=(k == 7)),
                          reads=[h2k, "Wr"], writes=[plk], inc=(k == 7))
                lg, lgk = lg_r.next()
                mx, mxk = mx_r.next()
                kb.op('dve', lambda e: e.tensor_tensor(out=lg[:, 0:NE], in0=pl[:], in1=rbb[:], op=ALU.add), reads=[plk, "rbb"], writes=[lgk])
                kb.op('dve', lambda e: e.max(out=mx[:, 0:8], in_=lg[:, 0:NE]), reads=[lgk], writes=[mxk])
                kb.op('dve', lambda e: e.tensor_scalar(out=mx[:, 8:9], in0=mx[:, 0:1], scalar1=-1.0, scalar2=None, op0=ALU.mult), reads=[mxk], writes=[mxk])
                kb.op('dve', lambda e: e.tensor_scalar(out=lg[:, NE:2 * NE], in0=lg[:, 0:NE], scalar1=mx[:, 3:4], scalar2=None, op0=ALU.is_ge),
                      reads=[lgk, mxk], writes=[lgk])
                kb.op('act', lambda e: e.activation(out=lg[:, 0:NE], in_=lg[:, 0:NE], func=AF.Exp, bias=mx[:, 8:9], scale=1.0), reads=[lgk, mxk], writes=[lgk])
                kb.op('dve', lambda e: e.tensor_tensor(out=lg[:, 0:NE], in0=lg[:, 0:NE], in1=lg[:, NE:2 * NE], op=ALU.mult), reads=[lgk], writes=[lgk])
                kb.op('dve', lambda e: e.reduce_sum(out=mx[:, 9:10], in_=lg[:, 0:NE], axis=AX.X), reads=[lgk], writes=[mxk])
                kb.op('dve', lambda e: e.reciprocal(out=mx[:, 10:11], in_=mx[:, 9:10]), reads=[mxk], writes=[mxk])
                kb.op('dve', lambda e: e.tensor_scalar(out=lg[:, 0:NE], in0=lg[:, 0:NE], scalar1=mx[:, 10:11], scalar2=None, op0=ALU.mult), reads=[lgk, mxk], writes=[lgk])
                kb.dma('sp', LG[i * 128:(i + 1) * 128, :], lg[:], reads=[lgk])
            kb.barrier()

        with ExitStack() as ph, Stage('5') as go:
          if go:
            sbp = lambda name, shape: ph.enter_context(nc.sbuf_tensor(name, shape, F32))
            HT = 1024
            h2T = sbp("h2T", [128, 8, HT])
            acc = sbp("acc", [128, 8, D])
            gts = sbp("gts", [128, 8, NE])
            gT = sbp("gT", [NE, 8, 128])
            b1t = sbp("b1t", [128, NE * 16])
            kb.dma('sp', b1t[:], b1fm, writes=["b1t"])
            b2t = sbp("b2t", [NE, D])
            kb.dma('sp', b2t[:], b2, writes=["b2t"])
            BF16 = mybir.dt.bfloat16
            ph.enter_context(nc.allow_low_precision("bf16 matmul operands with fp32 PSUM accumulation"))
            wp_r = Rot(nc, ph, "wp", [128, 8, 512], 1)
            w2_r = Rot(nc, ph, "w2p", [128, 4, D], 1)
            wpb_r = Rot(nc, ph, "wpb", [128, 8, 512], 2, dtype=BF16)
            w2b_r = Rot(nc, ph, "w2b", [128, 4, D], 2, dtype=BF16)
            h2Tb = ph.enter_context(nc.sbuf_tensor("h2Tb", [128, 8, HT], BF16))
            actT = ph.enter_context(nc.sbuf_tensor("actT", [128, 16, 512], BF16))
            ga_r = Rot(nc, ph, "ga", [128, 512], 2)
            sg_r = Rot(nc, ph, "sgm", [128, 512], 2)
            li_r = Rot(nc, ph, "li", [128, 512], 2)
            psU = Rot(nc, ph, "psU", [128, 512], 4, psum=True)
            psY = Rot(nc, ph, "psY", [128, 512], 3, psum=True)
            psG = Rot(nc, ph, "psG", [NE, 128], 1, psum=True)
            for half in range(QT // HT):
                t0 = half * HT
                kb.dma('sp', h2T[:], H2T[:, t0:t0 + HT].rearrange("(k p) n -> p k n", p=128), writes=["h2T"])
                kb.op('act', lambda e: e.copy(out=h2Tb[:].rearrange("p k n -> p (k n)"), in_=h2T[:].rearrange("p k n -> p (k n)")), reads=["h2T"], writes=["h2Tb"])
                for i in range(8):
                    kb.dma('sp', gts[:, i, :], LG[t0 + i * 128:t0 + (i + 1) * 128, 0:NE], writes=["gts"])
                for i in range(8):
                    pg, pgk = psG.next()
                    kb.op('pe', lambda e: e.transpose(out=pg[:], in_=gts[:, i, :], identity=idt[:]), reads=["gts", "idt"], writes=[pgk])
                    kb.op('act', lambda e: e.copy(out=gT[:, i, :], in_=pg[:]), reads=[pgk], writes=["gT"])
                for i in range(8):
                    for hf in range(2):
                        ps, pk = psY.next()
                        kb.op('pe', lambda e: e.matmul(ps[:], lhsT=gT[:, i, :], rhs=b2t[:, hf * 512:(hf + 1) * 512], start=True, stop=True),
                              reads=["gT", "b2t"], writes=[pk])
                        kb.op('dve', lambda e: e.tensor_copy(out=acc[:, i, hf * 512:(hf + 1) * 512], in_=ps[:]), reads=[pk], writes=[("acc", i)])
                for ex in range(NE):
                    w2p = []
                    for j2 in range(2):
                        wt, wk = w2_r.next()
                        kb.dma('sp', wt[:], w2[ex, j2 * 512:(j2 + 1) * 512, :].rearrange("(k p) n -> p k n", p=128), writes=[wk])
                        wtb, wbk = w2b_r.next()
                        kb.op('act', lambda e: e.copy(out=wtb[:].rearrange("p k n -> p (k n)"), in_=wt[:].rearrange("p k n -> p (k n)")), reads=[wk], writes=[wbk])
                        w2p.append((wtb, wbk))
                    for pc in range(4):
                        wt, wk = wp_r.next()
                        kb.dma('sp', wt[:, :, 0:256], w1[ex, :, pc * 256:(pc + 1) * 256].rearrange("(k p) n -> p k n", p=128), writes=[wk])
                        kb.dma('sp', wt[:, :, 256:512], w1[ex, :, D + pc * 256:D + (pc + 1) * 256].rearrange("(k p) n -> p k n", p=128), writes=[wk])
                        wtf, wkf = wt, wk
                        wt, wk = wpb_r.next()
                        kb.op('act', lambda e: e.copy(out=wt[:].rearrange("p k n -> p (k n)"), in_=wtf[:].rearrange("p k n -> p (k n)")), reads=[wkf], writes=[wk])
                        for tb in range(2):
                            for jj in range(2):
                                j = pc * 2 + jj
                                pgl, pglk = psU.next()
                                pli, plik = psU.next()
                                for k in range(8):
                                    kb.op('pe', lambda e, k=k: e.matmul(pgl[:], lhsT=wt[:, k, jj * 128:(jj + 1) * 128], rhs=h2Tb[:, k, tb * 512:(tb + 1) * 512],
                                                                        start=(k == 0), stop=(k == 7)), reads=[wk, "h2Tb"], writes=[pglk], inc=(k == 7))
                                for k in range(8):
                                    kb.op('pe', lambda e, k=k: e.matmul(pli[:], lhsT=wt[:, k, 256 + jj * 128:256 + (jj + 1) * 128], rhs=h2Tb[:, k, tb * 512:(tb + 1) * 512],
                                                                        start=(k == 0), stop=(k == 7)), reads=[wk, "h2Tb"], writes=[plik], inc=(k == 7))
                                bg = b1t[:, ex * 16 + j:ex * 16 + j + 1]
                                bl = b1t[:, ex * 16 + 8 + j:ex * 16 + 8 + j + 1]
                                ga, gak = ga_r.next()
                                kb.op('dve', lambda e: e.tensor_scalar(out=ga[:], in0=pgl[:], scalar1=bg, scalar2=7.0, op0=ALU.add, op1=ALU.min),
                                      reads=[pglk, "b1t"], writes=[gak])
                                sg, sgk = sg_r.next()
                                kb.op('act', lambda e: e.activation(out=sg[:], in_=ga[:], func=AF.Sigmoid, scale=1.702), reads=[gak], writes=[sgk])
                                li, lik = li_r.next()
                                kb.op('dve', lambda e: e.tensor_scalar(out=li[:], in0=pli[:], scalar1=bl, scalar2=7.0, op0=ALU.add, op1=ALU.min),
                                      reads=[plik, "b1t"], writes=[lik])
                                kb.op('pool', lambda e: e.tensor_scalar(out=li[:], in0=li[:], scalar1=-7.0, scalar2=1.0, op0=ALU.max, op1=ALU.add),
                                      reads=[lik], writes=[lik])
                                kb.op('pool', lambda e: e.tensor_tensor(out=ga[:], in0=ga[:], in1=sg[:], op=ALU.mult), reads=[gak, sgk], writes=[gak])
                                kb.op('pool', lambda e, j=j: e.tensor_tensor(out=actT[:, tb * 8 + j, :], in0=ga[:], in1=li[:], op=ALU.mult), reads=[gak, lik], writes=[("actT", tb * 8 + j)])
                    for tb in range(2):
                        for ti in range(4):
                            i = tb * 4 + ti
                            for hf in range(2):
                                ps, pk = psY.next()
                                for j in range(8):
                                    wt2, wk2 = w2p[j // 4]
                                    kb.op('pe', lambda e, j=j: e.matmul(ps[:], lhsT=actT[:, tb * 8 + j, ti * 128:(ti + 1) * 128], rhs=wt2[:, j % 4, hf * 512:(hf + 1) * 512],
                                                                        start=(j == 0), stop=(j == 7)), reads=[("actT", tb * 8 + j), wk2], writes=[pk], inc=(j == 7))
                                kb.op('dve', lambda e: e.scalar_tensor_tensor(out=acc[:, i, hf * 512:(hf + 1) * 512], in0=ps[:], scalar=gts[:, i, ex:ex + 1],
                                                                               in1=acc[:, i, hf * 512:(hf + 1) * 512], op0=ALU.mult, op1=ALU.add),
                                      reads=[pk, "gts", ("acc", i)], writes=[("acc", i)])
                for i in range(8):
                    kb.dma('sp', FF[t0 + i * 128:t0 + (i + 1) * 128, :], acc[:, i, :], reads=[("acc", i)])
            kb.barrier()

        with ExitStack() as ph, Stage('6') as go:
          if go:
            sbp = lambda name, shape: ph.enter_context(nc.sbuf_tensor(name, shape, F32))
            gfb = sbp("gfb", [128, D])
            kb.dma('sp', gfb[:], gfin.partition_broadcast(128), writes=["gfb"])
            junk = sbp("junk6", [128, 1024])
            x1_r = Rot(nc, ph, "x16", [128, 1024], 2)
            ff_r = Rot(nc, ph, "ff6", [128, 1024], 2)
            st_r = Rot(nc, ph, "st6", [128, 4], 3)
            o_r = Rot(nc, ph, "o6", [128, 1024], 2)
            for i in range(QT // 128):
                x1, x1k = x1_r.next()
                ff, ffk = ff_r.next()
                kb.dma('sp', x1[:], X1[i * 128:(i + 1) * 128, :], writes=[x1k])
                kb.dma('sp', ff[:], FF[i * 128:(i + 1) * 128, :], writes=[ffk])
                kb.op('dve', lambda e: e.tensor_tensor(out=ff[:], in0=ff[:], in1=GT[:, 1, :], op=ALU.mult), reads=[ffk, "GT"], writes=[ffk])
                kb.op('pool', lambda e: e.tensor_tensor(out=x1[:], in0=x1[:], in1=ff[:], op=ALU.add), reads=[ffk, x1k], writes=[x1k])
                stt, sk = st_r.next()
                rms_rstd(x1[:], x1k, stt, sk, junk, D)
                o, ok = o_r.next()
                kb.op('dve', lambda e: e.scalar_tensor_tensor(out=o[:], in0=x1[:], scalar=stt[:, 3:4], in1=gfb[:], op0=ALU.mult, op1=ALU.mult),
                      reads=[x1k, sk, "gfb"], writes=[ok])
                kb.dma('sp', out[i * 128:(i + 1) * 128, :], o[:], reads=[ok])
            kb.barrier()
        print("instructions", kb.nins, "dmas", kb.ndma, "cnt", kb.cnt)
    return nc


def rope_tables(tok):
    fr = (10000.0 ** (-np.arange(16, dtype=np.float32) / 16)).astype(np.float32)
    pos = [(tok // 64).astype(np.float32), (tok % 64).astype(np.float32)]
    C = np.zeros((64, len(tok)), np.float32)
    S = np.zeros((64, len(tok)), np.float32)
    for a in range(2):
        ang = (pos[a][None, :] * fr[:, None]).astype(np.float32)
        for hf in range(2):
            r0 = a * 32 + hf * 16
            C[r0:r0 + 16] = np.cos(ang)
            S[r0:r0 + 16] = np.sin(ang) * (-1.0 if hf == 0 else 1.0)
    return C, S


def make_inputs(inp, core):
    b, q = core // 4, core % 4
    f = lambda a: np.ascontiguousarray(a, dtype=np.float32)
    w = inp["w_in"][0]
    perm = np.arange(64).reshape(2, 2, 16)[:, ::-1, :].reshape(64)
    kr = w[:, 1792 + 384:1792 + 448]
    wcat = np.concatenate([w[:, :1792], w[:, 1792 + 256:1792 + 384], kr, kr[:, perm], w[:, 1792:1792 + 256]], axis=1)
    cv = np.stack([inp["c"][b].reshape(8, 128).T, inp["c_ctx"].reshape(8, 128).T], axis=2).reshape(128, 16)
    xb = inp["x"][b]
    xo = np.zeros((XO_ROWS, D), np.float32)
    xo[:QT] = xb[q * QT:(q + 1) * QT]
    if q > 0:
        xo[QT] = xb[q * QT - 1]
    if q < 3:
        xo[QT + 1] = xb[(q + 1) * QT]
    C, S = rope_tables(np.arange(T))
    wukv = inp["mla_w_ukv"][0].reshape(128, 4, 256)
    wuq = inp["mla_w_uq"][0].reshape(256, 4, 192)
    wuq_p = np.concatenate([wuq[:, :, :128], wuq[:, :, 128:], wuq[:, :, 128:][:, :, perm]], axis=2).reshape(256, 1024)
    w1 = inp["exp_w1"][0]
    w1d = np.concatenate([w1[:, :, 0::2], w1[:, :, 1::2]], axis=2)
    b1 = inp["exp_b1"][0]
    b1d = np.concatenate([b1[:, 0::2], b1[:, 1::2]], axis=1)
    b1fm = b1d.reshape(NE, 16, 128).transpose(2, 0, 1).reshape(128, NE * 16)
    d = {
        "xs": f(np.concatenate([inp["ctx"][b], xb], axis=0)),
        "xo": f(xo),
        "cvec": f(cv),
        "mod_w": f(inp["mod_w"][0]),
        "mod_b": f(inp["mod_b"][0]),
        "g1": f(inp["norm1_g"][0]),
        "g2n": f(inp["norm2_g"][0]),
        "gfin": f(inp["final_norm_g"]),
        "w_in": f(wcat),
        "ident": np.eye(128, dtype=np.float32),
        "ropeC": f(C), "ropeS": f(S),
        "ropeCo": f(C[:, q * QT:(q + 1) * QT]), "ropeSo": f(S[:, q * QT:(q + 1) * QT]),
        "kvg": f(inp["mla_kv_norm"][0].reshape(128, 1)),
        "qg": f(inp["mla_q_norm"][0].reshape(2, 128).T),
        "wukv_k": f(wukv[:, :, :128].reshape(128, 512)),
        "wukv_v": f(wukv[:, :, 128:].reshape(128, 512)),
        "wuq": f(wuq_p),
        "w_out": f(inp["w_out"][0]),
        "router_w": f(inp["router_w"][0]),
        "router_b": f(inp["router_b"][0]),
        "w1": f(w1d) if "5" in RUN else None,
        "b1fm": f(b1fm),
        "w2": f(inp["exp_w2"][0]) if "5" in RUN else None,
        "b2": f(inp["exp_b2"][0]),
    }
    if '2' in RUN:
        rc = rwkv_consts(q)
        rc.update(mu=inp["rwkv_mu"][0], w0=inp["rwkv_w0"][0], a0=inp["rwkv_a0"][0], w2=inp["rwkv_w2"][0], a2=inp["rwkv_a2"][0],
                  k_k=inp["rwkv_k_k"][0], k_a=inp["rwkv_k_a"][0], r_k=inp["rwkv_r_k"][0].reshape(512), ln_w=inp["rwkv_ln_w"][0],
                  ln_b=inp["rwkv_ln_b"][0], g2=inp["rwkv_g2"][0])
        for k_, v_ in rc.items():
            d["rw_" + k_] = f(v_)
    return {k: v for k, v in d.items() if v is not None}


def kernel(**inputs):
    inp = {k: np.asarray(v) for k, v in inputs.items()}
    nc = build()
    in_maps = [make_inputs(inp, c) for c in range(8)]
    if DEBUG:
        for c in range(8):
            if '2' not in RUN:
                in_maps[c]["yrw_in"] = kernel.dbg_yrw[c]
    res = run_bass_kernel_spmd(nc, in_maps, core_ids=list(range(8)))
    outp = np.zeros((2, T, D), np.float32)
    for c in range(8):
        b, q = c // 4, c % 4
        outp[b, q * QT:(q + 1) * QT] = res.results[c]["out"]
    if DEBUG:
        kernel.debug = res.results
    return outp
```
